# Optimizing a Trainium2 kernel written in Bass

```python
import jax, jax.numpy as jnp
from jax import lax
import numpy as np

D_MODEL = 1024
BATCH = 4
SEQ = 8192
DEPTH = 1

N_META = 16
ROPE_THETA = 10000.0
EPS = 1e-6
BLOCK_Q = 128
MLA_HEADS = 8
MLA_Q_RANK = 256
MLA_KV_RANK = 128
MLA_NOPE_DIM = 64
MLA_ROPE_DIM = 32
MLA_QK_DIM = MLA_NOPE_DIM + MLA_ROPE_DIM
MLA_V_DIM = 64
DSA_HEADS = 8
DSA_HEAD_DIM = 64
IDX_HEADS = 8
IDX_DIM = 64
TOPK_MAX = 256
MIX_WIDTH = MLA_HEADS * MLA_V_DIM + DSA_HEADS * DSA_HEAD_DIM
IN_SIZES = (MLA_Q_RANK, MLA_KV_RANK, MLA_ROPE_DIM,
            DSA_HEADS * DSA_HEAD_DIM, DSA_HEADS * DSA_HEAD_DIM, DSA_HEADS * DSA_HEAD_DIM,
            IDX_HEADS * IDX_DIM, IDX_DIM, IDX_HEADS)
C_IN = sum(IN_SIZES)
D_FF_RAW = -(-8 * D_MODEL // 3)
D_FF = -(-D_FF_RAW // 256) * 256

kernel_name = 'hybrid_mla_dsa_meta_layer'


def rmsnorm(x, g):
    x32 = x.astype(jnp.float32)
    y = x32 * lax.rsqrt(jnp.mean(x32 * x32, axis=-1, keepdims=True) + EPS)
    return (y * g.astype(jnp.float32)).astype(x.dtype)


def rope(x, pos):
    half = x.shape[-1] // 2
    inv = jnp.power(ROPE_THETA, -jnp.arange(half, dtype=jnp.float32) / half)
    ang = pos.astype(jnp.float32)[:, None] * inv[None, :]
    cos = jnp.cos(ang)[:, None, :]
    sin = jnp.sin(ang)[:, None, :]
    x32 = x.astype(jnp.float32)
    x1, x2 = x32[..., :half], x32[..., half:]
    return jnp.concatenate([x1 * cos - x2 * sin, x2 * cos + x1 * sin], axis=-1).astype(x.dtype)


def to_blocks(a):
    b, tp = a.shape[0], a.shape[1]
    return a.reshape((b, tp // BLOCK_Q, BLOCK_Q) + a.shape[2:]).swapaxes(0, 1)


def from_blocks(a):
    a = a.swapaxes(0, 1)
    return a.reshape((a.shape[0], a.shape[1] * a.shape[2]) + a.shape[3:])


def causal_block_attention(q, k, v):
    tp = q.shape[1]
    scale = q.shape[-1] ** -0.5
    kpos = jnp.arange(tp)

    def one_block(args):
        qb, blk = args
        qpos = blk * BLOCK_Q + jnp.arange(BLOCK_Q)
        s = jnp.einsum('bqhd,bkhd->bhqk', qb, k, preferred_element_type=jnp.float32) * scale
        s = jnp.where((kpos[None, :] <= qpos[:, None])[None, None], s, -jnp.inf)
        p = jax.nn.softmax(s, axis=-1).astype(v.dtype)
        return jnp.einsum('bhqk,bkhd->bqhd', p, v)

    out = lax.map(one_block, (to_blocks(q), jnp.arange(tp // BLOCK_Q)))
    return from_blocks(out)


def indexed_sparse_attention(q, k, v, q_idx, k_idx, w_idx, topk):
    tp = q.shape[1]
    scale = q.shape[-1] ** -0.5
    idx_scale = IDX_DIM ** -0.5
    kpos = jnp.arange(tp)

    def one_block(args):
        qb, qib, wib, blk = args
        qpos = blk * BLOCK_Q + jnp.arange(BLOCK_Q)
        causal = kpos[None, :] <= qpos[:, None]
        logits = jnp.einsum('bqhd,bkd->bqhk', qib, k_idx, preferred_element_type=jnp.float32) * idx_scale
        score = jnp.einsum('bqh,bqhk->bqk', wib.astype(jnp.float32), jax.nn.relu(logits))
        score = jnp.where(causal[None], score, -jnp.inf)
        _, sel = lax.top_k(score, topk)
        valid = sel <= qpos[None, :, None]
        k_sel = jax.vmap(lambda kb, ib: kb[ib])(k, sel)
        v_sel = jax.vmap(lambda vb, ib: vb[ib])(v, sel)
        s = jnp.einsum('bqhd,bqkhd->bhqk', qb, k_sel, preferred_element_type=jnp.float32) * scale
        s = jnp.where(valid[:, None], s, -jnp.inf)
        p = jax.nn.softmax(s, axis=-1).astype(v.dtype)
        return jnp.einsum('bhqk,bqkhd->bqhd', p, v_sel)

    out = lax.map(one_block, (to_blocks(q), to_blocks(q_idx), to_blocks(w_idx), jnp.arange(tp // BLOCK_Q)))
    return from_blocks(out)


def hybrid_mixer(u, pos, topk, w_in, q_norm_g, w_uq, kv_norm_g, w_ukv, w_o):
    b, tp, _ = u.shape
    proj = u @ w_in
    splits = [int(v) for v in np.cumsum(IN_SIZES)[:-1]]
    c_q, c_kv, k_r, q_s, k_s, v_s, q_i, k_i, w_i = jnp.split(proj, splits, axis=-1)

    q = (rmsnorm(c_q, q_norm_g) @ w_uq).reshape(b, tp, MLA_HEADS, MLA_QK_DIM)
    q_mla = jnp.concatenate([q[..., :MLA_NOPE_DIM], rope(q[..., MLA_NOPE_DIM:], pos)], axis=-1)
    kv = (rmsnorm(c_kv, kv_norm_g) @ w_ukv).reshape(b, tp, MLA_HEADS, MLA_NOPE_DIM + MLA_V_DIM)
    k_rope = rope(k_r[:, :, None, :], pos)
    k_mla = jnp.concatenate([kv[..., :MLA_NOPE_DIM],
                             jnp.broadcast_to(k_rope, (b, tp, MLA_HEADS, MLA_ROPE_DIM))], axis=-1)
    v_mla = kv[..., MLA_NOPE_DIM:]
    o_mla = causal_block_attention(q_mla, k_mla, v_mla)

    q_s = rope(q_s.reshape(b, tp, DSA_HEADS, DSA_HEAD_DIM), pos)
    k_s = rope(k_s.reshape(b, tp, DSA_HEADS, DSA_HEAD_DIM), pos)
    v_s = v_s.reshape(b, tp, DSA_HEADS, DSA_HEAD_DIM)
    q_i = rope(q_i.reshape(b, tp, IDX_HEADS, IDX_DIM), pos)
    k_i = rope(k_i[:, :, None, :], pos)[:, :, 0, :]
    w_i = w_i * (IDX_HEADS ** -0.5)
    o_dsa = indexed_sparse_attention(q_s, k_s, v_s, q_i, k_i, w_i, topk)

    o = jnp.concatenate([o_mla.reshape(b, tp, MLA_HEADS * MLA_V_DIM),
                         o_dsa.reshape(b, tp, DSA_HEADS * DSA_HEAD_DIM)], axis=-1)
    return o @ w_o


def swiglu(u, w_gate, w_up, w_down):
    return (jax.nn.silu(u @ w_gate) * (u @ w_up)) @ w_down


def setup_inputs(seed: int = 0) -> dict:
    key = jax.random.key(seed)
    ks = jax.random.split(key, 16)
    f32 = jnp.float32

    def w(k, shape, fan_in):
        return jax.random.normal(k, shape, f32) * (fan_in ** -0.5)

    def g(k, shape):
        return 1.0 + 0.02 * jax.random.normal(k, shape, f32)

    return {
        'x': jax.random.normal(ks[0], (BATCH, SEQ, D_MODEL), f32),
        'meta_tokens': jax.random.normal(ks[1], (N_META, D_MODEL), f32),
        'attn_norm_g': g(ks[2], (DEPTH, D_MODEL)),
        'w_in': w(ks[3], (DEPTH, D_MODEL, C_IN), D_MODEL),
        'mla_q_norm_g': g(ks[4], (DEPTH, MLA_Q_RANK)),
        'w_uq': w(ks[5], (DEPTH, MLA_Q_RANK, MLA_HEADS * MLA_QK_DIM), MLA_Q_RANK),
        'mla_kv_norm_g': g(ks[6], (DEPTH, MLA_KV_RANK)),
        'w_ukv': w(ks[7], (DEPTH, MLA_KV_RANK, MLA_HEADS * (MLA_NOPE_DIM + MLA_V_DIM)), MLA_KV_RANK),
        'w_o': w(ks[8], (DEPTH, MIX_WIDTH, D_MODEL), MIX_WIDTH),
        'ffn_norm_g': g(ks[9], (DEPTH, D_MODEL)),
        'w_gate': w(ks[10], (DEPTH, D_MODEL, D_FF), D_MODEL),
        'w_up': w(ks[11], (DEPTH, D_MODEL, D_FF), D_MODEL),
        'w_down': w(ks[12], (DEPTH, D_FF, D_MODEL), D_FF),
        'final_norm_g': g(ks[13], (D_MODEL,)),
    }


def reference(x, meta_tokens, attn_norm_g, w_in, mla_q_norm_g, w_uq, mla_kv_norm_g, w_ukv, w_o,
              ffn_norm_g, w_gate, w_up, w_down, final_norm_g):
    b, seq, _ = x.shape
    topk = min(TOPK_MAX, seq // 4)
    meta = jnp.broadcast_to(meta_tokens[None].astype(x.dtype), (b, N_META, D_MODEL))
    h = jnp.concatenate([meta, x], axis=1)
    t = h.shape[1]
    tp = -(-t // BLOCK_Q) * BLOCK_Q
    h = jnp.pad(h, ((0, 0), (0, tp - t), (0, 0)))
    pos = jnp.arange(tp, dtype=jnp.int32)
    for l in range(DEPTH):
        h = h + hybrid_mixer(rmsnorm(h, attn_norm_g[l]), pos, topk, w_in[l], mla_q_norm_g[l], w_uq[l],
                             mla_kv_norm_g[l], w_ukv[l], w_o[l])
        h = h + swiglu(rmsnorm(h, ffn_norm_g[l]), w_gate[l], w_up[l], w_down[l])
    h = rmsnorm(h, final_norm_g)
    return h[:, N_META:t]
```

```python
import numpy as np
import ml_dtypes
from contextlib import ExitStack
import concourse.bass as bass
import concourse.mybir as mybir
from concourse.bass_utils import run_bass_kernel_spmd

F32 = mybir.dt.float32
BF16 = mybir.dt.bfloat16
AF = mybir.ActivationFunctionType
ALU = mybir.AluOpType
AX = mybir.AxisListType

D = 1024
NQB = 32
NKB = 65
EPS = 1e-6
DFF = 2816
NFC = DFF // 128
TOPK = 256
NEG = -1.0e30
LAST_INPUTS = []
import os
CUT = float(os.environ.get('KCUT', '99'))


class Sched:
    ENG = ('pe', 'act', 'dve', 'pool', 'sp')

    def __init__(self):
        self.q = {e: [] for e in self.ENG}
        self.cnt = {}

    def add(self, eng, fn, deps=(), sig=False, dma=None):
        tok = None
        inc = None
        if dma is not None:
            self.cnt[dma] = self.cnt.get(dma, 0) + 16
            inc = (dma, 16)
            tok = (dma, self.cnt[dma])
        elif sig:
            name = 'c_' + eng
            self.cnt[name] = self.cnt.get(name, 0) + 1
            inc = (name, 1)
            tok = (name, self.cnt[name])
        dl = []
        for d in deps:
            if d is None:
                continue
            if isinstance(d, list):
                dl.extend(x for x in d if x is not None)
            else:
                dl.append(d)
        self.q[eng].append((fn, dl, inc))
        return tok

    def final_wait(self, eng='sp'):
        deps = [(n, v) for n, v in self.cnt.items()]
        self.q[eng].append((lambda e: e.nop(), deps, None))

    def run(self, nc, name, semstack=None):
        with ExitStack() as es:
            sems = {n: (semstack or es).enter_context(nc.semaphore(name + '_' + n)) for n in self.cnt}
            block = es.enter_context(nc.Block())

            def mk(engname):
                def body(eng):
                    waited = {}
                    for fn, deps, inc in self.q[engname]:
                        for (s, v) in deps:
                            if engname == 'pe' and s == 'c_pe':
                                continue
                            if waited.get(s, 0) < v:
                                eng.wait_ge(sems[s], v)
                                waited[s] = v
                        inst = fn(eng)
                        if inc is not None:
                            inst.then_inc(sems[inc[0]], inc[1])
                return body
            block.tensor(mk('pe'))
            block.scalar(mk('act'))
            block.vector(mk('dve'))
            block.gpsimd(mk('pool'))
            block.sync(mk('sp'))


def _bc(ap, shape_mid):
    return ap


def build(nkb=NKB, nqb=NQB, debug=False, phases='ABCD'):
    nc = bass.Bass("TRN2", target_bir_lowering=False)
    T = nkb * 128

    _din = {}

    def din(name, shape, dt=F32):
        if name not in _din:
            _din[name] = nc.dram_tensor(name, list(shape), dt, kind="ExternalInput").ap()
        return _din[name]

    def dscr(name, shape, dt):
        return nc.dram_tensor(name, list(shape), dt).ap()

    out = nc.dram_tensor("out", [NQB * 128, D], F32, kind="ExternalOutput").ap()

    s_ksT = dscr("s_ksT", [NKB, 128, 512], BF16)
    s_vds = dscr("s_vds", [NKB, 128, 520], BF16)
    s_kiT = dscr("s_kiT", [NKB, 64, 128], BF16)
    s_h1 = dscr("s_h1", [NQB * 128, D], F32)
    s_omla = dscr("s_omla", [NQB, 128, 512], BF16)
    s_odsa = dscr("s_odsa", [NQB, 128, 512], BF16)
    dbg = {}
    if debug:
        def dout(name, shape, dt=F32):
            t = nc.dram_tensor(name, list(shape), dt, kind="ExternalOutput").ap()
            dbg[name] = t
            return t
        d_knT = dout("d_knT", [128, 4 * T], BF16)
        d_krT = dout("d_krT", [32, T], BF16)
        d_vm = dout("d_vm", [128, nkb * 520], BF16)
        d_ksT = dout("d_ksT", [NKB, 128, 512], BF16)
        d_vds = dout("d_vds", [NKB, 128, 520], BF16)
        d_kiT = dout("d_kiT", [NKB, 64, 128], BF16)
        d_omla = dout("d_omla", [128, nqb * 512], BF16)
        d_h1 = dout("d_h1", [NQB * 128, D], F32)

    with ExitStack() as top:
        def sb(name, shape, dt, es=top):
            return es.enter_context(nc.sbuf_tensor(name, list(shape), dt))

        def ps(name, shape, dt, es=top):
            return es.enter_context(nc.psum_tensor(name, list(shape), dt))

        identF = sb("identF", [128, 128], F32)
        identB = sb("identB", [128, 128], BF16)
        epsT = sb("epsT", [128, 1], F32)
        abst = ExitStack()
        knT = sb("knT", [128, 4, T], BF16, abst)
        krT = sb("krT", [32, T], BF16, abst)
        vm = sb("vm", [128, nkb, 8, 65], BF16, abst)

        if 'A' in phases:
            with ExitStack() as pa:
                S = Sched()
                xk = din("xk", [nkb * 128, D])
                wk_in = din("wk_in", [D, 1248])
                g_attn = din("g_attn", [128, 8])
                w_ukv_k = din("w_ukv_k", [128, 512])
                w_ukv_v = din("w_ukv_v", [128, 512])
                g_kv = din("g_kv", [128, 1])
                ropeK = din("ropeK", [nkb, 128, 192])
                ident_f = din("ident_f", [128, 128])
                ident_b = din("ident_b", [128, 128], BF16)
                vcol0 = din("vcol0", [128, 8], BF16)
                Wk = sb("Wk", [128, 8, 1248], BF16, pa)
                WukK = sb("WukK", [128, 512], BF16, pa)
                WukV = sb("WukV", [128, 512], BF16, pa)
                gA = sb("gA", [128, 8], F32, pa)
                gKV = sb("gKV", [128, 1], F32, pa)
                stg = [sb("stgA0", [128, 1248], F32, pa)] * 2
                xs = [sb(f"xsA{i}", [128, D], F32, pa) for i in range(2)]
                tab = [sb(f"tabA{i}", [128, 192], F32, pa) for i in range(2)]
                tabs = [sb(f"tabsA{i}", [128, 192], F32, pa) for i in range(2)]
                junk = sb("junkA", [128, D], BF16, pa)
                ss = [sb(f"ssA{i}", [128, 4], F32, pa) for i in range(2)]
                ss2 = [sb(f"ss2A{i}", [128, 4], F32, pa) for i in range(2)]
                xT = [sb(f"xTA{i}", [128, 8, 128], BF16, pa) for i in range(2)]
                ra = sb("ra", [128, 512], F32, pa)
                rb = sb("rb", [128, 512], F32, pa)
                ksr = sb("ksr", [128, 512], BF16, pa)
                ri_a = sb("ri_a", [128, 64], F32, pa)
                ri_b = sb("ri_b", [128, 64], F32, pa)
                kir = sb("kir", [128, 64], BF16, pa)
                rr_a = sb("rr_a", [128, 32], F32, pa)
                rr_b = sb("rr_b", [128, 32], F32, pa)
                krr = sb("krr", [128, 32], BF16, pa)
                ckv = sb("ckv", [128, 128], F32, pa)
                junk2 = sb("junk2A", [128, 128], BF16, pa)
                ckvn = sb("ckvn", [128, 128], BF16, pa)
                ckvnT = sb("ckvnT", [128, 128], BF16, pa)
                vst = [sb(f"vstA{i}", [128, 8, 65], BF16, pa) for i in range(2)]
                kst = [sb(f"kstA{i}", [128, 512], BF16, pa) for i in range(2)]
                kit = [sb(f"kitA{i}", [64, 128], BF16, pa) for i in range(2)]
                pT = [ps(f"pTA{i}", [128, 512], F32, pa) for i in range(2)]
                pP = [ps(f"pPA{i}", [128, 512], F32, pa) for i in range(3)]
                pS = ps("pSA", [128, 1024], BF16, pa)
                pK = ps("pKA", [128, 512], F32, pa)
                pV = ps("pVA", [128, 512], F32, pa)

                S.add('sp', lambda e: e.dma_start(out=identF[:, :], in_=ident_f[:, :]), dma='cst')
                S.add('sp', lambda e: e.dma_start(out=identB[:, :], in_=ident_b[:, :]), dma='cst')
                S.add('sp', lambda e: e.dma_start(out=gA[:, :], in_=g_attn[:, :]), dma='cst')
                S.add('sp', lambda e: e.dma_start(out=gKV[:, :], in_=g_kv[:, :]), dma='cst')
                t_eps = S.add('dve', lambda e: e.memset(epsT[:, :], EPS), sig=True)
                t_vm1 = S.add('pool', lambda e: e.memset(vm[:, :, :, 64:65], 1.0), sig=True)
                t_vst1 = [None, S.add('pool', lambda e: e.memset(vst[1][:, :, 64:65], 1.0), sig=True)]
                S.add('sp', lambda e: e.dma_start(out=vst[0][:, :, 64:65], in_=vcol0[:, :].unsqueeze(2), allow_slow_non_contiguous=True), dma='cst')
                t_cst = S.add('sp', lambda e: e.dma_start(out=vm[:, 0, :, 64:65], in_=vcol0[:, :].unsqueeze(2), allow_slow_non_contiguous=True), deps=[t_vm1], dma='cst')
                t_id = t_cst
                t_vc0 = t_cst
                t_vst1[0] = t_cst
                stg_free = [None, None]
                t_w = []
                for c in range(8):
                    s = 0
                    td = S.add('sp', lambda e, c=c, s=s: e.dma_start(out=stg[s][:, :], in_=wk_in[c * 128:(c + 1) * 128, :]),
                               deps=[stg_free[s]], dma=f'stg{s}')
                    eng = 'dve' if s == 0 else 'pool'
                    stg_free[s] = S.add(eng, lambda e, c=c, s=s: e.tensor_scalar(
                        out=Wk[:, c, :], in0=stg[s][:, :], scalar1=gA[:, c:c + 1], scalar2=None, op0=ALU.mult),
                        deps=[td, t_cst], sig=True)
                    t_w.append(stg_free[s])
                for (dst, src, s) in ((WukK, w_ukv_k, 0), (WukV, w_ukv_v, 0)):
                    td = S.add('sp', lambda e, s=s, src=src: e.dma_start(out=stg[s][:, 0:512], in_=src[:, :]),
                               deps=[stg_free[s]], dma=f'stg{s}')
                    eng = 'dve' if s == 0 else 'pool'
                    stg_free[s] = S.add(eng, lambda e, s=s, dst=dst: e.tensor_scalar(
                        out=dst[:, :], in0=stg[s][:, 0:512], scalar1=gKV[:, 0:1], scalar2=None, op0=ALU.mult),
                        deps=[td, t_cst], sig=True)
                    t_w.append(stg_free[s])

                xs_free = [None, None]
                tab_free = [None, None]
                xT_free = [None, None]
                pT_free = [None, None]
                pP_free = [[None], [None], [None]]
                pS_free = [None]
                pK_free = None
                pV_free = None
                vst_free = [None, None]
                kst_free = [None, None]
                kit_free = [None, None]
                ss_free = [None, None]
                tmp_free = {}
                ckvnT_free = None
                for k in range(nkb):
                    s = k % 2
                    t_x = S.add('sp', lambda e, k=k, s=s: e.dma_start(out=xs[s][:, :], in_=xk[k * 128:(k + 1) * 128, :]),
                                deps=[xs_free[s], tab_free[s]], dma=f'ld{s}')
                    t_tab = S.add('sp', lambda e, k=k, s=s: e.dma_start(out=tab[s][:, :], in_=ropeK[k, :, :]),
                                  deps=[tab_free[s]], dma=f'ld{s}')
                    t_x = t_tab
                    t_ss = S.add('act', lambda e, s=s: e.activation(out=junk[:, :], in_=xs[s][:, :], func=AF.Square,
                                                                   accum_out=ss[s][:, 0:1]),
                                 deps=[t_x, ss_free[s]], sig=True)
                    t_sd = S.add('act', lambda e, s=s: e.activation(out=ss[s][:, 1:2], in_=ss[s][:, 0:1], func=AF.Sqrt,
                                                                   scale=1.0 / D, bias=epsT[:, 0:1]),
                                 deps=[t_ss, t_eps], sig=True)
                    t_rstd = S.add('dve', lambda e, s=s: e.reciprocal(out=ss[s][:, 2:3], in_=ss[s][:, 1:2]),
                                   deps=[t_sd], sig=True)
                    rstd = ss[s][:, 2:3]
                    if CUT <= 1:
                        continue
                    t_tr = []
                    for hlf in range(2):
                        for j in range(4):
                            c = hlf * 4 + j
                            tt = S.add('pe', lambda e, s=s, c=c, hlf=hlf, j=j: e.transpose(
                                out=pT[hlf][:, j * 128:(j + 1) * 128], in_=xs[s][:, c * 128:(c + 1) * 128],
                                identity=identF[:, :]),
                                deps=[t_x, t_id, pT_free[hlf]], sig=(j == 3))
                        t_tr.append(tt)
                    te0 = S.add('act', lambda e, s=s: e.activation(out=xT[s][:, 0:4, :], in_=pT[0][:, :], func=AF.Copy),
                                deps=[t_tr[0], xT_free[s]], sig=True)
                    te1 = S.add('dve', lambda e, s=s: e.tensor_copy(out=xT[s][:, 4:8, :], in_=pT[1][:, :]),
                                deps=[t_tr[1], xT_free[s]], sig=True)
                    pT_free = [te0, te1]
                    xs_free[s] = [te0, te1, t_ss]
                    if CUT <= 2:
                        continue
                    t_mm = []
                    for bnk, (c0, c1) in enumerate(((0, 512), (512, 1024), (1024, 1248))):
                        for c in range(8):
                            tt = S.add('pe', lambda e, s=s, c=c, bnk=bnk, c0=c0, c1=c1: e.matmul(
                                pP[bnk][:, 0:c1 - c0], lhsT=xT[s][:, c, :], rhs=Wk[:, c, c0:c1],
                                start=(c == 0), stop=(c == 7)),
                                deps=[te0, te1, pP_free[bnk], t_w], sig=(c == 7))
                        t_mm.append(tt)
                    xT_free[s] = t_mm[2]
                    if CUT <= 3:
                        continue
                    t_tabs = S.add('dve', lambda e, s=s: e.tensor_scalar(
                        out=tabs[s][:, :], in0=tab[s][:, :], scalar1=ss[s][:, 2:3], scalar2=None, op0=ALU.mult),
                        deps=[t_tab, t_rstd, tab_free[s]], sig=True)
                    t_a = S.add('dve', lambda e, s=s: e.tensor_tensor(
                        out=ra[:, :].rearrange("p (h d) -> p h d", h=8), in0=pP[0][:, :].rearrange("p (h d) -> p h d", h=8),
                        in1=bcast(tabs[s][:, 0:64], 8), op=ALU.mult),
                        deps=[t_mm[0], t_tabs, tmp_free.get('ra')], sig=True)
                    t_b1 = S.add('dve', lambda e, s=s: e.tensor_tensor(
                        out=rb[:, :].rearrange("p (h d) -> p h d", h=8)[:, :, 0:32],
                        in0=pP[0][:, :].rearrange("p (h d) -> p h d", h=8)[:, :, 32:64],
                        in1=bcast(tabs[s][:, 64:96], 8), op=ALU.mult),
                        deps=[t_mm[0], t_tabs, tmp_free.get('ra')], sig=True)
                    t_b2 = S.add('dve', lambda e, s=s: e.tensor_tensor(
                        out=rb[:, :].rearrange("p (h d) -> p h d", h=8)[:, :, 32:64],
                        in0=pP[0][:, :].rearrange("p (h d) -> p h d", h=8)[:, :, 0:32],
                        in1=bcast(tabs[s][:, 96:128], 8), op=ALU.mult),
                        deps=[t_mm[0], t_tabs, tmp_free.get('ra')], sig=True)
                    pP_free[0] = [t_a, t_b1, t_b2]
                    t_ksr = S.add('pool', lambda e: e.tensor_tensor(out=ksr[:, :], in0=ra[:, :], in1=rb[:, :], op=ALU.add),
                                  deps=[t_a, t_b1, t_b2, tmp_free.get('ksr')], sig=True)
                    tmp_free['ra'] = t_ksr
                    if CUT <= 4:
                        continue
                    t_v = S.add('act', lambda e, s=s: e.activation(
                        out=vst[s][:, :, 0:64], in_=pP[1][:, :].rearrange("p (h d) -> p h d", h=8), func=AF.Copy,
                        scale=ss[s][:, 2:3]),
                        deps=[t_mm[1], t_rstd, vst_free[s], t_vst1[s]], sig=True)
                    pP_free[1] = [t_v]
                    t_vd = S.add('sp', lambda e, s=s, k=k: e.dma_start(
                        out=s_vds[k, :, :], in_=vst[s][:, :, :].rearrange("p h d -> p (h d)")),
                        deps=[t_v], dma=f'st{s}')
                    if CUT <= 5:
                        continue
                    t_ckv = S.add('dve', lambda e, s=s: e.tensor_scalar(out=ckv[:, :], in0=pP[2][:, 0:128], scalar1=ss[s][:, 2:3],
                                                                       scalar2=None, op0=ALU.mult),
                                  deps=[t_mm[2], t_rstd, tmp_free.get('ckv')], sig=True)
                    t_ss2 = S.add('act', lambda e, s=s: e.activation(out=junk2[:, :], in_=ckv[:, :], func=AF.Square,
                                                                    accum_out=ss2[s][:, 0:1]),
                                  deps=[t_ckv, tmp_free.get('ss2%d' % s)], sig=True)
                    t_sd2 = S.add('act', lambda e, s=s: e.activation(out=ss2[s][:, 1:2], in_=ss2[s][:, 0:1], func=AF.Sqrt,
                                                                    scale=1.0 / 128, bias=epsT[:, 0:1]),
                                  deps=[t_ss2], sig=True)
                    t_r2 = S.add('dve', lambda e, s=s: e.reciprocal(out=ss2[s][:, 2:3], in_=ss2[s][:, 1:2]),
                                 deps=[t_sd2], sig=True)
                    t_ckvn = S.add('dve', lambda e, s=s: e.tensor_scalar(
                        out=ckvn[:, :], in0=ckv[:, :], scalar1=ss2[s][:, 2:3], scalar2=None, op0=ALU.mult),
                        deps=[t_r2, t_ckv, tmp_free.get('ckvn')], sig=True)
                    tmp_free['ckv'] = [t_ckvn, t_ss2]
                    tmp_free['ss2%d' % s] = t_ckvn
                    if CUT <= 6:
                        continue
                    t_ra = S.add('dve', lambda e, s=s: e.tensor_tensor(
                        out=rr_a[:, :], in0=pP[2][:, 128:160], in1=tabs[s][:, 128:160], op=ALU.mult),
                        deps=[t_mm[2], t_tabs, tmp_free.get('rr')], sig=True)
                    t_rb1 = S.add('dve', lambda e, s=s: e.tensor_tensor(
                        out=rr_b[:, 0:16], in0=pP[2][:, 144:160], in1=tabs[s][:, 160:176], op=ALU.mult),
                        deps=[t_mm[2], t_tabs, tmp_free.get('rr')], sig=True)
                    t_rb2 = S.add('dve', lambda e, s=s: e.tensor_tensor(
                        out=rr_b[:, 16:32], in0=pP[2][:, 128:144], in1=tabs[s][:, 176:192], op=ALU.mult),
                        deps=[t_mm[2], t_tabs, tmp_free.get('rr')], sig=True)
                    t_krr = S.add('pool', lambda e: e.tensor_tensor(out=krr[:, :], in0=rr_a[:, :], in1=rr_b[:, :], op=ALU.add),
                                  deps=[t_ra, t_rb1, t_rb2, tmp_free.get('krr')], sig=True)
                    tmp_free['rr'] = t_krr
                    t_ia = S.add('dve', lambda e, s=s: e.tensor_tensor(
                        out=ri_a[:, :], in0=pP[2][:, 160:224], in1=tabs[s][:, 0:64], op=ALU.mult),
                        deps=[t_mm[2], t_tabs, tmp_free.get('ri')], sig=True)
                    t_ib1 = S.add('dve', lambda e, s=s: e.tensor_tensor(
                        out=ri_b[:, 0:32], in0=pP[2][:, 192:224], in1=tabs[s][:, 64:96], op=ALU.mult),
                        deps=[t_mm[2], t_tabs, tmp_free.get('ri')], sig=True)
                    t_ib2 = S.add('dve', lambda e, s=s: e.tensor_tensor(
                        out=ri_b[:, 32:64], in0=pP[2][:, 160:192], in1=tabs[s][:, 96:128], op=ALU.mult),
                        deps=[t_mm[2], t_tabs, tmp_free.get('ri')], sig=True)
                    t_kir = S.add('pool', lambda e: e.tensor_tensor(out=kir[:, :], in0=ri_a[:, :], in1=ri_b[:, :], op=ALU.add),
                                  deps=[t_ia, t_ib1, t_ib2, tmp_free.get('kir')], sig=True)
                    tmp_free['ri'] = t_kir
                    pP_free[2] = [t_ckv, t_ra, t_rb1, t_rb2, t_ia, t_ib1, t_ib2]
                    tab_free[s] = [t_a, t_b1, t_b2, t_ra, t_rb1, t_rb2, t_ia, t_ib1, t_ib2]
                    ss_free[s] = [t_tabs, t_v, t_ckv]
                    if CUT <= 7:
                        continue
                    for a in range(4):
                        t_t1 = S.add('pe', lambda e, a=a: e.transpose(
                            out=pS[:, a * 128:(a + 1) * 128], in_=ksr[:, a * 128:(a + 1) * 128], identity=identB[:, :]),
                            deps=[t_ksr, pS_free], sig=(a == 3))
                    if CUT <= 7.1:
                        continue
                    t_t2 = S.add('pe', lambda e: e.transpose(out=pS[0:64, 512:640], in_=kir[:, :], identity=identB[:, :]),
                                 deps=[t_kir, pS_free], sig=True)
                    if CUT <= 7.2:
                        continue
                    t_t3 = S.add('pe', lambda e: e.transpose(out=pS[0:32, 640:768], in_=krr[:, :], identity=identB[:, :]),
                                 deps=[t_krr, pS_free], sig=True)
                    if CUT <= 7.3:
                        continue
                    t_t4 = S.add('pe', lambda e: e.transpose(out=pS[:, 768:896], in_=ckvn[:, :], identity=identB[:, :]),
                                 deps=[t_ckvn, pS_free], sig=True)
                    tmp_free['ksr'] = t_t1
                    tmp_free['kir'] = t_t2
                    tmp_free['krr'] = t_t3
                    tmp_free['ckvn'] = t_t4
                    if CUT <= 8:
                        continue
                    t_e1 = S.add('dve', lambda e, s=s: e.tensor_copy(out=kst[s][:, :], in_=pS[:, 0:512]),
                                 deps=[t_t4, kst_free[s]], sig=True)
                    if CUT <= 8.1:
                        continue
                    t_d1 = S.add('sp', lambda e, s=s, k=k: e.dma_start(out=s_ksT[k, :, :], in_=kst[s][:, :]),
                                 deps=[t_e1], dma=f'st{s}')
                    if CUT <= 8.2:
                        continue
                    t_e2 = S.add('dve', lambda e, s=s: e.tensor_copy(out=kit[s][:, :], in_=pS[0:64, 512:640]),
                                 deps=[t_t4, kit_free[s]], sig=True)
                    if CUT <= 8.3:
                        continue
                    t_d2 = S.add('sp', lambda e, s=s, k=k: e.dma_start(out=s_kiT[k, :, :], in_=kit[s][:, :]),
                                 deps=[t_e2], dma=f'st{s}')
                    kit_free[s] = t_d2
                    kst_free[s] = t_d2
                    vst_free[s] = t_d2
                    if k == 0:
                        vst_free[s] = S.add('pool', lambda e, s=s: e.memset(vst[s][:, :, 64:65], 1.0), deps=[t_d2], sig=True)
                    if CUT <= 8.4:
                        continue
                    t_e3 = S.add('dve', lambda e, k=k: e.tensor_copy(out=krT[:, k * 128:(k + 1) * 128], in_=pS[0:32, 640:768]),
                                 deps=[t_t4], sig=True)
                    if CUT <= 8.5:
                        continue
                    t_e4 = S.add('dve', lambda e: e.tensor_copy(out=ckvnT[:, :], in_=pS[:, 768:896]),
                                 deps=[t_t4, ckvnT_free], sig=True)
                    pS_free = [t_e1, t_e2, t_e3, t_e4]
                    if CUT <= 9:
                        continue
                    for a in range(4):
                        t_k = S.add('pe', lambda e, a=a: e.matmul(
                            pK[:, a * 128:(a + 1) * 128], lhsT=WukK[:, a * 128:(a + 1) * 128], rhs=ckvnT[:, :],
                            start=True, stop=True), deps=[t_e4, pK_free, t_w], sig=(a == 3))
                    t_vv = S.add('pe', lambda e: e.matmul(pV[:, :], lhsT=ckvnT[:, :], rhs=WukV[:, :], start=True, stop=True),
                                 deps=[t_e4, pV_free, t_w], sig=True)
                    ckvnT_free = t_vv
                    pK_free = S.add('act', lambda e, k=k: e.activation(
                        out=knT[:, :, k * 128:(k + 1) * 128], in_=pK[:, :].rearrange("p (a t) -> p a t", a=4), func=AF.Copy),
                        deps=[t_k], sig=True)
                    pV_free = S.add('dve', lambda e, k=k: e.tensor_copy(
                        out=vm[:, k, :, 0:64], in_=pV[:, :].rearrange("p (h d) -> p h d", h=8)),
                        deps=[t_vv, t_vm1, t_vc0], sig=True)
                last = [pK_free, pV_free, kst_free[0], kst_free[1], kit_free[0], kit_free[1], vst_free[0], vst_free[1]]
                if debug and CUT > 50:
                    S.add('sp', lambda e: e.dma_start(out=d_knT[:, :], in_=knT[:, :, :].rearrange("p a t -> p (a t)")),
                          deps=last, dma='dbg')
                    S.add('sp', lambda e: e.dma_start(out=d_krT[:, :], in_=krT[:, :]), deps=last, dma='dbg')
                    t_dbg = S.add('sp', lambda e: e.dma_start(out=d_vm[:, :], in_=vm[:, :, :, :].rearrange("p k h d -> p (k h d)")),
                                  deps=last, dma='dbg')
                    last = last + [t_dbg]
                S.final_wait()
                S.run(nc, "A", top)
                if debug and CUT > 50:
                    S2_ = Sched()
                    t1 = S2_.add('sp', lambda e: e.dma_start(out=d_ksT[0:nkb, :, :], in_=s_ksT[0:nkb, :, :]), dma='d')
                    t1 = S2_.add('sp', lambda e: e.dma_start(out=d_vds[0:nkb, :, :], in_=s_vds[0:nkb, :, :]), dma='d')
                    t1 = S2_.add('sp', lambda e: e.dma_start(out=d_kiT[0:nkb, :, :], in_=s_kiT[0:nkb, :, :]), dma='d')
                    S2_.add('sp', lambda e: e.nop(), deps=[t1])
                    S2_.run(nc, "Ad", top)
        if 'B' in phases:
            with ExitStack() as pb_:
                S = Sched()
                xq = din("xq", [nqb * 128, D])
                wq_in = din("wq_in", [D, 1288])
                g_attn = din("g_attn", [128, 8])
                w_uq_n = din("w_uq_n", [256, 512])
                w_uq_r = din("w_uq_r", [256, 256])
                g_q = din("g_q", [128, 2])
                ropeQ = din("ropeQ", [nqb, 128, 192])
                mk_mult = din("mk_mult", [2, 128, 128], BF16)
                SC = 96.0 ** -0.5
                Wcq = sb("Wcq", [128, 8, 256], BF16, pb_)
                WuqN = sb("WuqN", [128, 2, 512], BF16, pb_)
                WuqR = sb("WuqR", [128, 2, 256], BF16, pb_)
                gA = sb("gAB", [128, 8], F32, pb_)
                gQ = sb("gQB", [128, 2], F32, pb_)
                mkm = sb("mkmB", [128, 2, 128], BF16, pb_)
                stg = [sb(f"stgB{i}", [128, 512], F32, pb_) for i in range(2)]
                xs = sb("xsB", [128, D], F32, pb_)
                xb = sb("xbB", [128, D], BF16, pb_)
                tab = sb("tabB", [128, 192], F32, pb_)
                tabq = sb("tabqB", [128, 64], F32, pb_)
                junk = sb("junkB", [128, D], BF16, pb_)
                ss = sb("ssB", [128, 8], F32, pb_)
                xT = sb("xTB", [128, 8, 128], BF16, pb_)
                cq = sb("cqB", [128, 256], F32, pb_)
                cqn = sb("cqnB", [128, 256], BF16, pb_)
                cqnT = sb("cqnTB", [128, 2, 128], BF16, pb_)
                qbd = sb("qbdB", [128, 4, 256], BF16, pb_)
                qra = sb("qraB", [128, 256], F32, pb_)
                qrb = sb("qrbB", [128, 256], F32, pb_)
                qr = sb("qrB", [128, 256], BF16, pb_)
                qrT = sb("qrTB", [32, 1024], BF16, pb_)
                PT = [sb(f"PTB{i}", [128, 1024], BF16, pb_) for i in range(2)]
                rden = sb("rdenB", [128, 8], F32, pb_)
                ost = [sb(f"ostB{i}", [128, 512], BF16, pb_) for i in range(2)]
                pT = ps("pTB", [128, 1024], BF16, pb_)
                pQ = ps("pQB", [128, 512], F32, pb_)
                pST = [ps(f"pSTB{i}", [128, 512], F32, pb_) for i in range(4)]
                pO = [ps(f"pOB{i}", [128, 4, 65], F32, pb_) for i in range(2)]

                S.add('sp', lambda e: e.dma_start(out=gA[:, :], in_=g_attn[:, :]), dma='cst')
                S.add('sp', lambda e: e.dma_start(out=gQ[:, :], in_=g_q[:, :]), dma='cst')
                t_cst = S.add('sp', lambda e: e.dma_start(out=mkm[:, :, :], in_=mk_mult.rearrange("m k q -> k m q")), dma='cst')
                t_z = S.add('pool', lambda e: e.memset(qbd[:, :, :], 0.0), sig=True)
                stg_free = [None, None]
                t_w = []
                jobs = [(Wcq[:, c, :], wq_in[c * 128:(c + 1) * 128, 0:256], gA[:, c:c + 1], 256) for c in range(8)]
                jobs += [(WuqN[:, c, :], w_uq_n[c * 128:(c + 1) * 128, :], gQ[:, c:c + 1], 512) for c in range(2)]
                jobs += [(WuqR[:, c, :], w_uq_r[c * 128:(c + 1) * 128, :], gQ[:, c:c + 1], 256) for c in range(2)]
                for j, (dst, src, gs, n) in enumerate(jobs):
                    s = j % 2
                    td = S.add('sp', lambda e, s=s, src=src, n=n: e.dma_start(out=stg[s][:, 0:n], in_=src),
                               deps=[stg_free[s]], dma=f'stg{s}')
                    stg_free[s] = S.add('dve' if s == 0 else 'pool', lambda e, s=s, dst=dst, gs=gs, n=n: e.tensor_scalar(
                        out=dst, in0=stg[s][:, 0:n], scalar1=gs, scalar2=None, op0=ALU.mult), deps=[td, t_cst], sig=True)
                    t_w.append(stg_free[s])

                prev = []
                pst_free = [None] * 4
                PT_free = [None, None]
                ost_free = [None, None]
                t_onorm = None
                for i in range(nqb):
                    nk = min(2 * i + 3, nkb)
                    t_x = S.add('sp', lambda e, i=i: e.dma_start(out=xs[:, :], in_=xq[i * 128:(i + 1) * 128, :]), deps=prev, dma='ld')
                    t_x = S.add('sp', lambda e, i=i: e.dma_start(out=tab[:, :], in_=ropeQ[i, :, :]), deps=prev, dma='ld')
                    t_ss = S.add('act', lambda e: e.activation(out=junk[:, :], in_=xs[:, :], func=AF.Square, accum_out=ss[:, 0:1]),
                                 deps=[t_x] + prev, sig=True)
                    t_sd = S.add('act', lambda e: e.activation(out=ss[:, 1:2], in_=ss[:, 0:1], func=AF.Sqrt, scale=1.0 / D, bias=epsT[:, 0:1]),
                                 deps=[t_ss], sig=True)
                    t_rstd = S.add('dve', lambda e: e.reciprocal(out=ss[:, 2:3], in_=ss[:, 1:2]), deps=[t_sd], sig=True)
                    t_xb = S.add('dve', lambda e: e.tensor_copy(out=xb[:, :], in_=xs[:, :]), deps=[t_x] + prev, sig=True)
                    t_tq = S.add('dve', lambda e: e.tensor_scalar(out=tabq[:, :], in0=tab[:, 128:192], scalar1=SC, scalar2=None, op0=ALU.mult),
                                 deps=[t_x] + prev, sig=True)
                    for c in range(8):
                        t_tr = S.add('pe', lambda e, c=c: e.transpose(out=pT[:, c * 128:(c + 1) * 128], in_=xb[:, c * 128:(c + 1) * 128],
                                                                      identity=identB[:, :]), deps=[t_xb] + prev, sig=(c == 7))
                    t_xT = S.add('dve', lambda e: e.tensor_copy(out=xT[:, :, :], in_=pT[:, :].rearrange("p (c t) -> p c t", c=8)),
                                 deps=[t_tr], sig=True)
                    for c in range(8):
                        t_mm = S.add('pe', lambda e, c=c: e.matmul(pQ[:, 0:256], lhsT=xT[:, c, :], rhs=Wcq[:, c, :], start=(c == 0), stop=(c == 7)),
                                     deps=[t_xT, t_w] + prev, sig=(c == 7))
                    t_cq = S.add('dve', lambda e: e.tensor_scalar(out=cq[:, :], in0=pQ[:, 0:256], scalar1=ss[:, 2:3], scalar2=None, op0=ALU.mult),
                                 deps=[t_mm, t_rstd], sig=True)
                    t_ss2 = S.add('act', lambda e: e.activation(out=junk[:, 0:256], in_=cq[:, :], func=AF.Square, accum_out=ss[:, 4:5]),
                                  deps=[t_cq], sig=True)
                    t_sd2 = S.add('act', lambda e: e.activation(out=ss[:, 5:6], in_=ss[:, 4:5], func=AF.Sqrt, scale=1.0 / 256, bias=epsT[:, 0:1]),
                                  deps=[t_ss2], sig=True)
                    t_r2 = S.add('dve', lambda e: e.reciprocal(out=ss[:, 6:7], in_=ss[:, 5:6]), deps=[t_sd2], sig=True)
                    t_cqn = S.add('dve', lambda e: e.tensor_scalar(out=cqn[:, :], in0=cq[:, :], scalar1=ss[:, 6:7], scalar2=None, op0=ALU.mult),
                                  deps=[t_r2], sig=True)
                    for c in range(2):
                        t_tr = S.add('pe', lambda e, c=c: e.transpose(out=pT[:, c * 128:(c + 1) * 128], in_=cqn[:, c * 128:(c + 1) * 128],
                                                                      identity=identB[:, :]), deps=[t_cqn, t_xT], sig=(c == 1))
                    t_cT = S.add('dve', lambda e: e.tensor_copy(out=cqnT[:, :, :], in_=pT[:, 0:256].rearrange("p (c t) -> p c t", c=2)),
                                 deps=[t_tr], sig=True)
                    for a in range(4):
                        for c in range(2):
                            t_mm = S.add('pe', lambda e, a=a, c=c: e.matmul(pQ[:, a * 128:(a + 1) * 128], lhsT=WuqN[:, c, a * 128:(a + 1) * 128],
                                                                            rhs=cqnT[:, c, :], start=(c == 0), stop=(c == 1)),
                                         deps=[t_cT, t_cq], sig=(a == 3 and c == 1))
                    pQv = pQ[:, :].rearrange("p (a t) -> p a t", a=4)
                    t_q1 = S.add('dve', lambda e: e.tensor_scalar(out=qbd[0:64, :, 0:128], in0=pQ[:, :].rearrange("p (a t) -> p a t", a=4)[0:64, :, :],
                                                                  scalar1=SC, scalar2=None, op0=ALU.mult), deps=[t_mm, t_z] + prev, sig=True)
                    t_q2 = S.add('dve', lambda e: e.tensor_scalar(out=qbd[64:128, :, 128:256], in0=pQ[:, :].rearrange("p (a t) -> p a t", a=4)[64:128, :, :],
                                                                  scalar1=SC, scalar2=None, op0=ALU.mult), deps=[t_mm, t_z] + prev, sig=True)
                    for c in range(2):
                        t_mm = S.add('pe', lambda e, c=c: e.matmul(pQ[:, 0:256], lhsT=cqnT[:, c, :], rhs=WuqR[:, c, :], start=(c == 0), stop=(c == 1)),
                                     deps=[t_q1, t_q2], sig=(c == 1))
                    v3 = lambda ap: ap.rearrange("p (h d) -> p h d", h=8)
                    t_a = S.add('dve', lambda e: e.tensor_tensor(out=v3(qra[:, :]), in0=v3(pQ[:, 0:256]), in1=bcast(tabq[:, 0:32], 8), op=ALU.mult),
                                deps=[t_mm, t_tq] + prev, sig=True)
                    t_b1 = S.add('dve', lambda e: e.tensor_tensor(out=v3(qrb[:, :])[:, :, 0:16], in0=v3(pQ[:, 0:256])[:, :, 16:32],
                                                                  in1=bcast(tabq[:, 32:48], 8), op=ALU.mult), deps=[t_mm, t_tq] + prev, sig=True)
                    t_b2 = S.add('dve', lambda e: e.tensor_tensor(out=v3(qrb[:, :])[:, :, 16:32], in0=v3(pQ[:, 0:256])[:, :, 0:16],
                                                                  in1=bcast(tabq[:, 48:64], 8), op=ALU.mult), deps=[t_mm, t_tq] + prev, sig=True)
                    t_qr = S.add('pool', lambda e: e.tensor_tensor(out=qr[:, :], in0=qra[:, :], in1=qrb[:, :], op=ALU.add),
                                 deps=[t_a, t_b1, t_b2] + prev, sig=True)
                    for h in range(8):
                        t_tr = S.add('pe', lambda e, h=h: e.transpose(out=pT[0:32, h * 128:(h + 1) * 128], in_=qr[:, h * 32:(h + 1) * 32],
                                                                      identity=identB[:, :]), deps=[t_qr, t_cT], sig=(h == 7))
                    t_qrT = S.add('dve', lambda e: e.tensor_copy(out=qrT[:, :], in_=pT[0:32, :]), deps=[t_tr] + prev, sig=True)
                    qready = [t_q1, t_q2, t_qrT]
                    prev_q = [t_b2, t_qrT, t_ss2, t_cqn]
                    t_exp = {}
                    t_pv = None

                    def emit_qk(kb):
                        for g in range(2):
                            bank = pST[(kb % 2) * 2 + g]
                            for j in range(2):
                                a = g * 2 + j
                                S.add('pe', lambda e, bank=bank, j=j, a=a, kb=kb: e.matmul(
                                    bank[:, j * 256:(j + 1) * 256], lhsT=knT[:, a, kb * 128:(kb + 1) * 128], rhs=qbd[:, a, :],
                                    start=(j == 0), stop=False, skip_group_check=True),
                                    deps=qready + [pst_free[(kb % 2) * 2 + g]])
                            t = S.add('pe', lambda e, bank=bank, g=g, kb=kb: e.matmul(
                                bank[:, :], lhsT=krT[0:32, kb * 128:(kb + 1) * 128], rhs=qrT[0:32, g * 512:(g + 1) * 512],
                                start=False, stop=True, skip_group_check=True), deps=qready, sig=True)
                            t_qk[(kb, g)] = t
                    t_qk = {}

                    def emit_exp(kb):
                        for g in range(2):
                            bi = (kb % 2) * 2 + g
                            t = S.add('act', lambda e, bi=bi, kb=kb, g=g: e.activation(
                                out=PT[kb % 2][:, g * 512:(g + 1) * 512], in_=pST[bi][:, :], func=AF.Exp),
                                deps=[t_qk[(kb, g)], PT_free[kb % 2]], sig=True)
                            pst_free[bi] = t
                            mi = kb - (2 * i + 1)
                            if mi >= 0:
                                t = S.add('pool', lambda e, kb=kb, g=g, mi=mi: e.tensor_tensor(
                                    out=PT[kb % 2][:, g * 512:(g + 1) * 512].rearrange("p (h q) -> p h q", h=4),
                                    in0=PT[kb % 2][:, g * 512:(g + 1) * 512].rearrange("p (h q) -> p h q", h=4),
                                    in1=bcast(mkm[:, mi, :], 4), op=ALU.mult), deps=[t, t_cst], sig=True)
                            t_exp[(kb, g)] = t

                    def emit_pv(kb):
                        nonlocal t_pv
                        for h in range(8):
                            t_pv = S.add('pe', lambda e, h=h, kb=kb: e.matmul(
                                pO[h // 4][:, h % 4, :], lhsT=PT[kb % 2][:, h * 128:(h + 1) * 128], rhs=vm[:, kb, h, :],
                                start=(kb == 0 and h % 4 == 0), stop=(kb == nk - 1), skip_group_check=True),
                                deps=[t_exp[(kb, h // 4)], t_onorm], sig=(h == 7))
                        PT_free[kb % 2] = t_pv

                    emit_qk(0)
                    for kb in range(nk):
                        if kb + 1 < nk:
                            emit_qk(kb + 1)
                        emit_exp(kb)
                        emit_pv(kb)
                    so = i % 2
                    t_rd = S.add('dve', lambda e: e.reciprocal(out=rden[:, 0:4], in_=pO[0][:, :, 64]), deps=[t_pv], sig=True)
                    t_rd2 = S.add('dve', lambda e: e.reciprocal(out=rden[:, 4:8], in_=pO[1][:, :, 64]), deps=[t_pv], sig=True)
                    for g in range(2):
                        t_onorm = S.add('dve', lambda e, g=g, so=so: e.tensor_tensor(
                            out=ost[so][:, g * 256:(g + 1) * 256].rearrange("p (h d) -> p h d", h=4), in0=pO[g][:, :, 0:64],
                            in1=rden[:, g * 4:(g + 1) * 4].unsqueeze(2).broadcast_to([128, 4, 64]), op=ALU.mult),
                            deps=[t_rd, t_rd2, ost_free[so]], sig=True)
                    ost_free[so] = S.add('sp', lambda e, i=i, so=so: e.dma_start(out=s_omla[i, :, :], in_=ost[so][:, :]),
                                         deps=[t_onorm], dma=f'ost{so}')
                    prev = [t_pv, t_onorm] + prev_q
                if debug:
                    S.final_wait()
                    S.add('sp', lambda e: e.dma_start(out=d_omla.rearrange("p (i f) -> i p f", i=nqb), in_=s_omla[0:nqb, :, :]), dma='dbg')
                S.final_wait()
                S.run(nc, "B", top)
        abst.close()
        if 'C' in phases:
            with ExitStack() as pc_:
                S = Sched()
                xq = din("xq", [nqb * 128, D])
                wq_in = din("wq_in", [D, 1288])
                g_attn = din("g_attn", [128, 8])
                ropeQ = din("ropeQ", [nqb, 128, 192])
                mk_add = din("mk_add", [3, 128, 128])
                NIT = 18
                Wq2 = sb("Wq2C", [128, 8, 1032], BF16, pc_)
                gA = sb("gAC", [128, 8], F32, pc_)
                mka = sb("mkaC", [128, 3, 128], F32, pc_)
                stg = [sb(f"stgC{i}", [128, 1032], F32, pc_) for i in range(2)]
                ksT = sb("ksTC", [128, nkb, 512], BF16, pc_)
                kiT2 = sb("kiT2C", [128, nkb, 128], BF16, pc_)
                Isc = sb("IscC", [128, nkb * 128], F32, pc_)
                Msk = sb("MskC", [128, nkb * 128], BF16, pc_)
                xs = sb("xsC", [128, D], F32, pc_)
                xb = sb("xbC", [128, D], BF16, pc_)
                tab = sb("tabC", [128, 192], F32, pc_)
                tabs = sb("tabsC", [128, 128], F32, pc_)
                junk = sb("junkC", [128, D], BF16, pc_)
                ss = sb("ssC", [128, 8], F32, pc_)
                xT = sb("xTC", [128, 8, 128], BF16, pc_)
                ra = sb("raC", [128, 512], F32, pc_)
                rb = sb("rbC", [128, 512], F32, pc_)
                qs = sb("qsC", [128, 512], BF16, pc_)
                qi = sb("qiC", [128, 512], BF16, pc_)
                wv = sb("wvC", [128, 8], F32, pc_)
                qbd = sb("qbdC", [128, 4, 256], BF16, pc_)
                qiT = sb("qiTC", [128, 4, 128], BF16, pc_)
                tmpR = [sb(f"tmpRC{i}", [128, 512], F32, pc_) for i in range(2)]
                bs = sb("bsC", [128, 16], F32, pc_)
                PT = [sb(f"PTC{i}", [128, 1024], BF16, pc_) for i in range(2)]
                vb = [sb(f"vbC{i}", [128, 8, 65], BF16, pc_) for i in range(3)]
                rden = sb("rdenC", [128, 8], F32, pc_)
                ost = [sb(f"ostC{i}", [128, 512], BF16, pc_) for i in range(2)]
                pT = [ps(f"pTC{i}", [128, 1024], BF16, pc_) for i in range(2)]
                pST = [ps(f"pSTC{i}", [128, 512], F32, pc_) for i in range(4)]
                pO = [ps(f"pOC{i}", [128, 4, 65], F32, pc_) for i in range(2)]

                S.add('sp', lambda e: e.dma_start(out=gA[:, :], in_=g_attn[:, :]), dma='cst')
                S.add('sp', lambda e: e.dma_start(out=mka[:, :, :], in_=mk_add.rearrange("m q k -> q m k")), dma='cst')
                for k0 in range(0, nkb, 4):
                    k1 = min(nkb, k0 + 4)
                    S.add('sp', lambda e, k0=k0, k1=k1: e.dma_start(out=ksT[:, k0:k1, :], in_=s_ksT[k0:k1, :, :].rearrange("k p f -> p k f")), dma='cst')
                for k0 in range(0, nkb, 8):
                    k1 = min(nkb, k0 + 8)
                    S.add('sp', lambda e, k0=k0, k1=k1: e.dma_start(out=kiT2[0:64, k0:k1, :], in_=s_kiT[k0:k1, :, :].rearrange("k p t -> p k t")), dma='cst')
                    t_cst = S.add('sp', lambda e, k0=k0, k1=k1: e.dma_start(out=kiT2[64:128, k0:k1, :], in_=s_kiT[k0:k1, :, :].rearrange("k p t -> p k t")), dma='cst')
                t_z = S.add('pool', lambda e: e.memset(qbd[:, :, :], 0.0), sig=True)
                stg_free = [None, None]
                t_w = []
                for c in range(8):
                    s = c % 2
                    td = S.add('sp', lambda e, s=s, c=c: e.dma_start(out=stg[s][:, :], in_=wq_in[c * 128:(c + 1) * 128, 256:1288]),
                               deps=[stg_free[s]], dma=f'stg{s}')
                    stg_free[s] = S.add(('dve', 'pool')[s], lambda e, s=s, c=c: e.tensor_scalar(
                        out=Wq2[:, c, :], in0=stg[s][:, :], scalar1=gA[:, c:c + 1], scalar2=None, op0=ALU.mult), deps=[td, t_cst], sig=True)
                    t_w.append(stg_free[s])
                v3 = lambda ap: ap.rearrange("p (h d) -> p h d", h=8)
                prev = []
                pst_free = [None] * 4
                PT_free = [None, None]
                pT_free = [None, None]
                vb_free = [None] * 3
                ost_free = [None, None]
                t_onorm = None
                vcount = 0
                for i in range(nqb):
                    nk = min(2 * i + 3, nkb)
                    W = nk * 128
                    S.add('sp', lambda e, i=i: e.dma_start(out=xs[:, :], in_=xq[i * 128:(i + 1) * 128, :]), deps=prev, dma='ld')
                    t_x = S.add('sp', lambda e, i=i: e.dma_start(out=tab[:, :], in_=ropeQ[i, :, :]), deps=prev, dma='ld')
                    t_ss = S.add('act', lambda e: e.activation(out=junk[:, :], in_=xs[:, :], func=AF.Square, accum_out=ss[:, 0:1]), deps=[t_x] + prev, sig=True)
                    t_sd = S.add('act', lambda e: e.activation(out=ss[:, 1:2], in_=ss[:, 0:1], func=AF.Sqrt, scale=1.0 / D, bias=epsT[:, 0:1]), deps=[t_ss], sig=True)
                    t_rstd = S.add('dve', lambda e: e.reciprocal(out=ss[:, 2:3], in_=ss[:, 1:2]), deps=[t_sd], sig=True)
                    t_r8 = S.add('dve', lambda e: e.tensor_scalar(out=ss[:, 3:4], in0=ss[:, 2:3], scalar1=0.125, scalar2=None, op0=ALU.mult), deps=[t_rstd], sig=True)
                    t_tabs = S.add('dve', lambda e: e.tensor_scalar(out=tabs[:, :], in0=tab[:, 0:128], scalar1=ss[:, 3:4], scalar2=None, op0=ALU.mult),
                                   deps=[t_r8, t_x] + prev, sig=True)
                    t_xb = S.add('dve', lambda e: e.tensor_copy(out=xb[:, :], in_=xs[:, :]), deps=[t_x] + prev, sig=True)
                    for c in range(8):
                        t_tr = S.add('pe', lambda e, c=c: e.transpose(out=pT[0][:, c * 128:(c + 1) * 128], in_=xb[:, c * 128:(c + 1) * 128],
                                                                      identity=identB[:, :]), deps=[t_xb] + prev, sig=(c == 7))
                    t_xT = S.add('dve', lambda e: e.tensor_copy(out=xT[:, :, :], in_=pT[0][:, :].rearrange("p (c t) -> p c t", c=8)), deps=[t_tr], sig=True)
                    t_mms = []
                    for bnk, (c0, c1) in enumerate(((0, 512), (512, 1024), (1024, 1032))):
                        for c in range(8):
                            t_mm = S.add('pe', lambda e, c=c, bnk=bnk, c0=c0, c1=c1: e.matmul(pST[bnk][:, 0:c1 - c0], lhsT=xT[:, c, :], rhs=Wq2[:, c, c0:c1],
                                                                                             start=(c == 0), stop=(c == 7)), deps=[t_xT, t_w] + prev, sig=(c == 7))
                        t_mms.append(t_mm)
                    outs = []
                    for bnk, dst in ((0, qs), (1, qi)):
                        t_a = S.add('dve', lambda e, bnk=bnk: e.tensor_tensor(out=v3(ra[:, :]), in0=v3(pST[bnk][:, :]), in1=bcast(tabs[:, 0:64], 8), op=ALU.mult),
                                    deps=[t_mms[bnk], t_tabs] + outs + prev, sig=True)
                        t_b1 = S.add('dve', lambda e, bnk=bnk: e.tensor_tensor(out=v3(rb[:, :])[:, :, 0:32], in0=v3(pST[bnk][:, :])[:, :, 32:64],
                                                                              in1=bcast(tabs[:, 64:96], 8), op=ALU.mult), deps=[t_mms[bnk], t_tabs] + outs + prev, sig=True)
                        t_b2 = S.add('dve', lambda e, bnk=bnk: e.tensor_tensor(out=v3(rb[:, :])[:, :, 32:64], in0=v3(pST[bnk][:, :])[:, :, 0:32],
                                                                              in1=bcast(tabs[:, 96:128], 8), op=ALU.mult), deps=[t_mms[bnk], t_tabs] + outs + prev, sig=True)
                        t_q = S.add('pool', lambda e, dst=dst: e.tensor_tensor(out=dst[:, :], in0=ra[:, :], in1=rb[:, :], op=ALU.add),
                                    deps=[t_a, t_b1, t_b2] + prev, sig=True)
                        outs = [t_q]
                        if bnk == 0:
                            t_qs = t_q
                        else:
                            t_qi = t_q
                    t_wv = S.add('dve', lambda e: e.tensor_scalar(out=wv[:, :], in0=pST[2][:, 0:8], scalar1=ss[:, 2:3], scalar2=8.0 ** -0.5,
                                                                  op0=ALU.mult, op1=ALU.mult), deps=[t_mms[2], t_rstd] + prev, sig=True)
                    pst_q = [t_b2, t_wv]
                    for a in range(4):
                        t_tr = S.add('pe', lambda e, a=a: e.transpose(out=pT[0][:, a * 128:(a + 1) * 128], in_=qs[:, a * 128:(a + 1) * 128],
                                                                      identity=identB[:, :]), deps=[t_qs, t_xT], sig=(a == 3))
                    for a in range(4):
                        t_tr2 = S.add('pe', lambda e, a=a: e.transpose(out=pT[1][:, a * 128:(a + 1) * 128], in_=qi[:, a * 128:(a + 1) * 128],
                                                                       identity=identB[:, :]), deps=[t_qi, pT_free[1]] + prev, sig=(a == 3))
                    pTv = lambda t: t[:, 0:512].rearrange("p (a t) -> p a t", a=4)
                    t_q1 = S.add('dve', lambda e: e.tensor_copy(out=qbd[0:64, :, 0:128], in_=pTv(pT[0])[0:64, :, :]), deps=[t_tr, t_z] + prev, sig=True)
                    t_q2 = S.add('dve', lambda e: e.tensor_copy(out=qbd[64:128, :, 128:256], in_=pTv(pT[0])[64:128, :, :]), deps=[t_tr, t_z] + prev, sig=True)
                    t_qiT = S.add('dve', lambda e: e.tensor_copy(out=qiT[:, :, :], in_=pTv(pT[1])), deps=[t_tr2] + prev, sig=True)
                    pT_free = [t_q2, t_qiT]
                    t_acc = None
                    tmp_free = [None, None]
                    nch = (W + 511) // 512
                    cnt_ = 0
                    for ch in range(nch):
                        c0 = ch * 512
                        n = min(512, W - c0)
                        for h in range(8):
                            hp = (h % 2) * 64
                            bnk = h % 4
                            t_lg = S.add('pe', lambda e, hp=hp, h=h, bnk=bnk, c0=c0, n=n: e.matmul(
                                pST[bnk][:, 0:n], lhsT=qiT[hp:hp + 64, h // 2, :],
                                rhs=kiT2[hp:hp + 64, :, :].rearrange("p k t -> p (k t)")[:, c0:c0 + n], start=True, stop=True),
                                deps=[t_qiT, t_cst, pst_free[bnk]] + pst_q, sig=True)
                            if h == 0:
                                t_acc = S.add('dve', lambda e, bnk=bnk, c0=c0, n=n: e.tensor_scalar(
                                    out=Isc[:, c0:c0 + n], in0=pST[bnk][:, 0:n], scalar1=0.0, scalar2=wv[:, 0:1], op0=ALU.max, op1=ALU.mult),
                                    deps=[t_lg, t_wv, t_acc] + prev, sig=True)
                                pst_free[bnk] = t_acc
                            else:
                                sl = cnt_ % 2
                                cnt_ += 1
                                t_r = S.add('dve', lambda e, bnk=bnk, sl=sl, n=n, h=h: e.tensor_scalar(
                                    out=tmpR[sl][:, 0:n], in0=pST[bnk][:, 0:n], scalar1=0.0, scalar2=wv[:, h:h + 1], op0=ALU.max, op1=ALU.mult),
                                    deps=[t_lg, t_wv, tmp_free[sl]], sig=True)
                                pst_free[bnk] = t_r
                                t_acc = S.add('pool', lambda e, sl=sl, c0=c0, n=n: e.tensor_tensor(
                                    out=Isc[:, c0:c0 + n], in0=Isc[:, c0:c0 + n], in1=tmpR[sl][:, 0:n], op=ALU.add), deps=[t_r, t_acc], sig=True)
                                tmp_free[sl] = t_acc
                    t_mx = S.add('dve', lambda e, W=W: e.tensor_reduce(out=bs[:, 1:2], in_=Isc[:, 0:W], axis=AX.X, op=ALU.max), deps=[t_acc] + prev, sig=True)
                    t_mn = S.add('dve', lambda e, W=W: e.tensor_reduce(out=bs[:, 0:1], in_=Isc[:, 0:W], axis=AX.X, op=ALU.min), deps=[t_acc] + prev, sig=True)
                    t_b = S.add('dve', lambda e: e.tensor_scalar(out=bs[:, 1:2], in0=bs[:, 1:2], scalar1=1.0, scalar2=None, op0=ALU.add), deps=[t_mx], sig=True)
                    t_b = S.add('dve', lambda e: e.tensor_scalar(out=bs[:, 0:1], in0=bs[:, 0:1], scalar1=-1.0, scalar2=None, op0=ALU.add), deps=[t_mn, t_b], sig=True)
                    for (mi, kb) in ((0, 0), (1, nk - 2), (2, nk - 1)):
                        t_b = S.add('dve', lambda e, mi=mi, kb=kb: e.tensor_tensor(out=Isc[:, kb * 128:(kb + 1) * 128], in0=Isc[:, kb * 128:(kb + 1) * 128],
                                                                                  in1=mka[:, mi, :], op=ALU.add), deps=[t_b, t_mn, t_mx, t_cst], sig=True)
                    for it in range(NIT):
                        t_b = S.add('dve', lambda e: e.tensor_scalar(out=bs[:, 2:3], in0=bs[:, 0:1], scalar1=bs[:, 1:2], scalar2=0.5, op0=ALU.add, op1=ALU.mult),
                                    deps=[t_b], sig=True)
                        t_b = S.add('dve', lambda e, W=W: e.tensor_scalar(out=Msk[:, 0:W], in0=Isc[:, 0:W], scalar1=bs[:, 2:3], scalar2=0.0,
                                                                        op0=ALU.is_ge, op1=ALU.add, accum_out=bs[:, 3:4]), deps=[t_b] + prev, sig=True)
                        t_b = S.add('dve', lambda e: e.tensor_scalar(out=bs[:, 4:5], in0=bs[:, 3:4], scalar1=float(TOPK) - 0.5, scalar2=None, op0=ALU.is_ge),
                                    deps=[t_b], sig=True)
                        t_b = S.add('dve', lambda e: e.tensor_tensor(out=bs[:, 5:6], in0=bs[:, 2:3], in1=bs[:, 0:1], op=ALU.subtract), deps=[t_b], sig=True)
                        t_b = S.add('dve', lambda e: e.tensor_tensor(out=bs[:, 6:7], in0=bs[:, 1:2], in1=bs[:, 2:3], op=ALU.subtract), deps=[t_b], sig=True)
                        t_b = S.add('dve', lambda e: e.scalar_tensor_tensor(out=bs[:, 0:1], in0=bs[:, 5:6], scalar=bs[:, 4:5], in1=bs[:, 0:1],
                                                                            op0=ALU.mult, op1=ALU.add), deps=[t_b], sig=True)
                        t_b = S.add('dve', lambda e: e.scalar_tensor_tensor(out=bs[:, 1:2], in0=bs[:, 6:7], scalar=bs[:, 4:5], in1=bs[:, 2:3],
                                                                            op0=ALU.mult, op1=ALU.add), deps=[t_b], sig=True)
                    t_msk = S.add('dve', lambda e, W=W: e.tensor_scalar(out=Msk[:, 0:W], in0=Isc[:, 0:W], scalar1=bs[:, 0:1], scalar2=None, op0=ALU.is_ge),
                                  deps=[t_b], sig=True)
                    qready = [t_q1, t_q2, t_msk]
                    t_qk = {}
                    t_exp = {}
                    t_v = {}
                    t_pv = None

                    def emit_qk(kb):
                        for g in range(2):
                            bi = (kb % 2) * 2 + g
                            for j in range(2):
                                a = g * 2 + j
                                t = S.add('pe', lambda e, bi=bi, j=j, a=a, kb=kb: e.matmul(
                                    pST[bi][:, j * 256:(j + 1) * 256], lhsT=ksT[:, kb, a * 128:(a + 1) * 128], rhs=qbd[:, a, :],
                                    start=(j == 0), stop=(j == 1), skip_group_check=True), deps=qready + [pst_free[bi], t_cst], sig=(j == 1))
                            t_qk[(kb, g)] = t
                        t_qk[(kb, 'm')] = S.add('pe', lambda e, kb=kb: e.transpose(out=pT[kb % 2][:, 0:128], in_=Msk[:, kb * 128:(kb + 1) * 128],
                                                                                  identity=identB[:, :]), deps=[t_msk, pT_free[kb % 2]], sig=True)

                    def emit_v(kb):
                        nonlocal vcount
                        sl = vcount % 3
                        vcount += 1
                        t_v[kb] = (S.add('sp', lambda e, kb=kb, sl=sl: e.dma_start(out=vb[sl][:, :, :].rearrange("p h d -> p (h d)"), in_=s_vds[kb, :, :]),
                                         deps=[vb_free[sl]], dma=f'vb{sl}'), sl)

                    def emit_exp(kb):
                        for g in range(2):
                            bi = (kb % 2) * 2 + g
                            t = S.add('act', lambda e, bi=bi, kb=kb, g=g: e.activation(
                                out=PT[kb % 2][:, g * 512:(g + 1) * 512], in_=pST[bi][:, :], func=AF.Exp),
                                deps=[t_qk[(kb, g)], PT_free[kb % 2]], sig=True)
                            pst_free[bi] = t
                            t_exp[(kb, g)] = t
                        t = S.add('dve', lambda e, kb=kb: e.tensor_tensor(
                            out=PT[kb % 2][:, :].rearrange("p (h q) -> p h q", h=8), in0=PT[kb % 2][:, :].rearrange("p (h q) -> p h q", h=8),
                            in1=pT[kb % 2][:, 0:128].unsqueeze(1).broadcast_to([128, 8, 128]), op=ALU.mult),
                            deps=[t_exp[(kb, 0)], t_exp[(kb, 1)], t_qk[(kb, 'm')]], sig=True)
                        pT_free[kb % 2] = t
                        t_exp[kb] = t

                    def emit_pv(kb):
                        nonlocal t_pv
                        tv, sl = t_v[kb]
                        for h in range(8):
                            t_pv = S.add('pe', lambda e, h=h, kb=kb, sl=sl: e.matmul(
                                pO[h // 4][:, h % 4, :], lhsT=PT[kb % 2][:, h * 128:(h + 1) * 128], rhs=vb[sl][:, h, :],
                                start=(kb == 0 and h % 4 == 0), stop=(kb == nk - 1), skip_group_check=True),
                                deps=[t_exp[kb], tv, t_onorm], sig=(h == 7))
                        PT_free[kb % 2] = t_pv
                        vb_free[sl] = t_pv

                    emit_v(0)
                    emit_qk(0)
                    for kb in range(nk):
                        if kb + 1 < nk:
                            emit_v(kb + 1)
                            emit_qk(kb + 1)
                        emit_exp(kb)
                        emit_pv(kb)
                    so = i % 2
                    t_rd = S.add('dve', lambda e: e.reciprocal(out=rden[:, 0:4], in_=pO[0][:, :, 64]), deps=[t_pv], sig=True)
                    t_rd2 = S.add('dve', lambda e: e.reciprocal(out=rden[:, 4:8], in_=pO[1][:, :, 64]), deps=[t_pv], sig=True)
                    for g in range(2):
                        t_onorm = S.add('dve', lambda e, g=g, so=so: e.tensor_tensor(
                            out=ost[so][:, g * 256:(g + 1) * 256].rearrange("p (h d) -> p h d", h=4), in0=pO[g][:, :, 0:64],
                            in1=rden[:, g * 4:(g + 1) * 4].unsqueeze(2).broadcast_to([128, 4, 64]), op=ALU.mult),
                            deps=[t_rd, t_rd2, ost_free[so]], sig=True)
                    ost_free[so] = S.add('sp', lambda e, i=i, so=so: e.dma_start(out=s_odsa[i, :, :], in_=ost[so][:, :]), deps=[t_onorm], dma=f'ost{so}')
                    prev = [t_pv, t_onorm, t_msk, t_qiT, t_q2]
                S.final_wait()
                S.run(nc, "C", top)
        if 'D' in phases:
            with ExitStack() as pd_:
                S = Sched()
                xq = din("xq", [nqb * 128, D])
                w_o = din("w_o", [D, D])
                g_ffn = din("g_ffn", [128, 8])
                w_gate = din("w_gate", [D, DFF])
                w_up = din("w_up", [D, DFF])
                w_down = din("w_down", [DFF, D])
                g_fin = din("g_fin", [128, D])
                Wo = sb("WoD", [128, 8, D], BF16, pd_)
                Wg = sb("WgD", [128, 8, DFF], BF16, pd_)
                Wu = sb("WuD", [128, 8, DFF], BF16, pd_)
                Wd = sb("WdD", [128, NFC, D], BF16, pd_)
                gF = sb("gFD", [128, 8], F32, pd_)
                gfin = sb("gfinD", [128, D], F32, pd_)
                stg = [sb(f"stgD{i}", [128, 1024], F32, pd_) for i in range(2)]
                xs = sb("xsD", [128, D], F32, pd_)
                ob = sb("obD", [128, D], BF16, pd_)
                oT = sb("oTD", [128, 8, 128], BF16, pd_)
                h1 = sb("h1D", [128, D], F32, pd_)
                ub = sb("ubD", [128, D], BF16, pd_)
                uT = sb("uTD", [128, 8, 128], BF16, pd_)
                junk = sb("junkD", [128, D], BF16, pd_)
                ss = sb("ssD", [128, 8], F32, pd_)
                sil = [sb(f"silD{i}", [128, 512], F32, pd_) for i in range(2)]
                actT = sb("actTD", [128, NFC, 128], BF16, pd_)
                h2 = sb("h2D", [128, D], F32, pd_)
                ot = sb("otD", [128, D], F32, pd_)
                pT = ps("pTD", [128, 1024], BF16, pd_)
                pA = [ps(f"pAD{i}", [128, 512], F32, pd_) for i in range(2)]
                pG = [ps(f"pGD{i}", [128, 512], F32, pd_) for i in range(2)]
                pU = [ps(f"pUD{i}", [128, 512], F32, pd_) for i in range(2)]
                S.add('sp', lambda e: e.dma_start(out=gF[:, :], in_=g_ffn[:, :]), dma='cst')
                t_cst = S.add('sp', lambda e: e.dma_start(out=gfin[:, :], in_=g_fin[:, :]), dma='cst')
                stg_free = [None, None]
                t_w = []
                jobs = []
                for c in range(8):
                    jobs.append((Wo[:, c, :], w_o[c * 128:(c + 1) * 128, :], None, D))
                    for (Wt, wsrc) in ((Wg, w_gate), (Wu, w_up)):
                        for c0 in range(0, DFF, 1024):
                            n = min(1024, DFF - c0)
                            jobs.append((Wt[:, c, c0:c0 + n], wsrc[c * 128:(c + 1) * 128, c0:c0 + n], gF[:, c:c + 1], n))
                for f in range(NFC):
                    jobs.append((Wd[:, f, :], w_down[f * 128:(f + 1) * 128, :], None, D))
                for j, (dst, src, gs, n) in enumerate(jobs):
                    s = j % 2
                    td = S.add('sp', lambda e, s=s, src=src, n=n: e.dma_start(out=stg[s][:, 0:n], in_=src), deps=[stg_free[s]], dma=f'stg{s}')
                    eng = ('dve', 'pool')[s]
                    if gs is None:
                        stg_free[s] = S.add(eng, lambda e, s=s, dst=dst, n=n: e.tensor_copy(out=dst, in_=stg[s][:, 0:n]), deps=[td], sig=True)
                    else:
                        stg_free[s] = S.add(eng, lambda e, s=s, dst=dst, gs=gs, n=n: e.tensor_scalar(
                            out=dst, in0=stg[s][:, 0:n], scalar1=gs, scalar2=None, op0=ALU.mult), deps=[td, t_cst], sig=True)
                    t_w.append(stg_free[s])
                prev = []
                pG_free = [None, None]
                pU_free = [None, None]
                sil_free = [None, None]
                for i in range(nqb):
                    S.add('sp', lambda e, i=i: e.dma_start(out=xs[:, :], in_=xq[i * 128:(i + 1) * 128, :]), deps=prev, dma='ld')
                    S.add('sp', lambda e, i=i: e.dma_start(out=ob[:, 0:512], in_=s_omla[i, :, :]), deps=prev, dma='ld')
                    t_x = S.add('sp', lambda e, i=i: e.dma_start(out=ob[:, 512:1024], in_=s_odsa[i, :, :]), deps=prev, dma='ld')
                    for c in range(8):
                        t_tr = S.add('pe', lambda e, c=c: e.transpose(out=pT[:, c * 128:(c + 1) * 128], in_=ob[:, c * 128:(c + 1) * 128],
                                                                      identity=identB[:, :]), deps=[t_x] + prev, sig=(c == 7))
                    t_oT = S.add('dve', lambda e: e.tensor_copy(out=oT[:, :, :], in_=pT[:, :].rearrange("p (c t) -> p c t", c=8)), deps=[t_tr] + prev, sig=True)
                    for hf in range(2):
                        for c in range(8):
                            t_mm = S.add('pe', lambda e, c=c, hf=hf: e.matmul(pA[hf][:, :], lhsT=oT[:, c, :], rhs=Wo[:, c, hf * 512:(hf + 1) * 512],
                                                                              start=(c == 0), stop=(c == 7)), deps=[t_oT, t_w] + prev, sig=(c == 7))
                    for hf in range(2):
                        t_h1 = S.add('dve', lambda e, hf=hf: e.tensor_tensor(out=h1[:, hf * 512:(hf + 1) * 512], in0=pA[hf][:, :],
                                                                             in1=xs[:, hf * 512:(hf + 1) * 512], op=ALU.add), deps=[t_mm, t_x] + prev, sig=True)
                    t_ss = S.add('act', lambda e: e.activation(out=junk[:, :], in_=h1[:, :], func=AF.Square, accum_out=ss[:, 0:1]), deps=[t_h1] + prev, sig=True)
                    t_sd = S.add('act', lambda e: e.activation(out=ss[:, 1:2], in_=ss[:, 0:1], func=AF.Sqrt, scale=1.0 / D, bias=epsT[:, 0:1]), deps=[t_ss], sig=True)
                    t_r = S.add('dve', lambda e: e.reciprocal(out=ss[:, 2:3], in_=ss[:, 1:2]), deps=[t_sd], sig=True)
                    t_ub = S.add('dve', lambda e: e.tensor_scalar(out=ub[:, :], in0=h1[:, :], scalar1=ss[:, 2:3], scalar2=None, op0=ALU.mult), deps=[t_r] + prev, sig=True)
                    for c in range(8):
                        t_tr = S.add('pe', lambda e, c=c: e.transpose(out=pT[:, c * 128:(c + 1) * 128], in_=ub[:, c * 128:(c + 1) * 128],
                                                                      identity=identB[:, :]), deps=[t_ub, t_oT], sig=(c == 7))
                    t_uT = S.add('dve', lambda e: e.tensor_copy(out=uT[:, :, :], in_=pT[:, :].rearrange("p (c t) -> p c t", c=8)), deps=[t_tr] + prev, sig=True)
                    t_act = None
                    ngrp = (NFC + 3) // 4
                    for gi in range(ngrp):
                        sl = gi % 2
                        nf = min(4, NFC - gi * 4)
                        for (pX, Wt, fr) in ((pG, Wg, pG_free), (pU, Wu, pU_free)):
                            for j in range(nf):
                                f = gi * 4 + j
                                for c in range(8):
                                    t_mm = S.add('pe', lambda e, pX=pX, Wt=Wt, sl=sl, j=j, f=f, c=c: e.matmul(
                                        pX[sl][:, j * 128:(j + 1) * 128], lhsT=Wt[:, c, f * 128:(f + 1) * 128], rhs=uT[:, c, :],
                                        start=(c == 0), stop=(c == 7)), deps=[t_uT, t_w, fr[sl]], sig=(c == 7 and j == nf - 1))
                            if pX is pG:
                                t_g = t_mm
                            else:
                                t_u = t_mm
                        t_sil = S.add('act', lambda e, sl=sl, nf=nf: e.activation(out=sil[sl][:, 0:nf * 128], in_=pG[sl][:, 0:nf * 128], func=AF.Silu),
                                      deps=[t_g, sil_free[sl]], sig=True)
                        pG_free[sl] = t_sil
                        t_act = S.add('dve', lambda e, sl=sl, nf=nf, gi=gi: e.tensor_tensor(
                            out=actT[:, gi * 4:gi * 4 + nf, :], in0=pU[sl][:, 0:nf * 128].rearrange("p (f t) -> p f t", f=nf),
                            in1=sil[sl][:, 0:nf * 128].rearrange("p (f t) -> p f t", f=nf), op=ALU.mult), deps=[t_sil, t_u] + prev, sig=True)
                        pU_free[sl] = t_act
                        sil_free[sl] = t_act
                    for hf in range(2):
                        for f in range(NFC):
                            t_mm = S.add('pe', lambda e, f=f, hf=hf: e.matmul(pA[hf][:, :], lhsT=actT[:, f, :], rhs=Wd[:, f, hf * 512:(hf + 1) * 512],
                                                                              start=(f == 0), stop=(f == NFC - 1)), deps=[t_act, t_h1], sig=(f == NFC - 1))
                    for hf in range(2):
                        t_h2 = S.add('dve', lambda e, hf=hf: e.tensor_tensor(out=h2[:, hf * 512:(hf + 1) * 512], in0=pA[hf][:, :],
                                                                             in1=h1[:, hf * 512:(hf + 1) * 512], op=ALU.add), deps=[t_mm] + prev, sig=True)
                    t_ss = S.add('act', lambda e: e.activation(out=junk[:, :], in_=h2[:, :], func=AF.Square, accum_out=ss[:, 4:5]), deps=[t_h2], sig=True)
                    t_sd = S.add('act', lambda e: e.activation(out=ss[:, 5:6], in_=ss[:, 4:5], func=AF.Sqrt, scale=1.0 / D, bias=epsT[:, 0:1]), deps=[t_ss], sig=True)
                    t_r = S.add('dve', lambda e: e.reciprocal(out=ss[:, 6:7], in_=ss[:, 5:6]), deps=[t_sd], sig=True)
                    t_o = S.add('dve', lambda e: e.scalar_tensor_tensor(out=ot[:, :], in0=h2[:, :], scalar=ss[:, 6:7], in1=gfin[:, :], op0=ALU.mult, op1=ALU.mult),
                                deps=[t_r, t_cst] + prev, sig=True)
                    t_st = S.add('sp', lambda e, i=i: e.dma_start(out=out[i * 128:(i + 1) * 128, :], in_=ot[:, :]), deps=[t_o], dma='st')
                    prev = [t_st, t_o, t_mm, t_h2, t_ss]
                S.final_wait()
                S.run(nc, "D", top)
    global LAST_INPUTS
    LAST_INPUTS = list(_din.keys())
    return nc, dbg


def bcast(ap, h):
    n = ap.shape[-1]
    return ap.unsqueeze(1).broadcast_to([ap.shape[0], h, n])


BF = ml_dtypes.bfloat16
def rope_tab(pos):
    pos = pos.astype(np.float32)
    out = np.zeros((pos.shape[0], 192), np.float32)
    for (half, off) in ((32, 0), (16, 128)):
        inv = np.power(np.float32(10000.0), -np.arange(half, dtype=np.float32) / np.float32(half)).astype(np.float32)
        ang = (pos[:, None] * inv[None, :]).astype(np.float32)
        c = np.cos(ang).astype(np.float32); s = np.sin(ang).astype(np.float32)
        d = 2 * half
        out[:, off:off + d] = np.concatenate([c, c], 1)
        out[:, off + d:off + 2 * d] = np.concatenate([-s, s], 1)
    return out
def core_inputs(inp, core):
    f32 = np.float32
    b, par = core // 2, core % 2
    x = np.asarray(inp['x'][b], f32)
    xk = np.zeros((65 * 128, 1024), f32); xk[0:16] = inp['meta_tokens']; xk[128:] = x
    xq = np.ascontiguousarray(x.reshape(64, 128, 1024)[par::2].reshape(32 * 128, 1024))
    w_in = np.asarray(inp['w_in'][0], f32)
    c_q, c_kv, k_r, q_s, k_s, v_s, q_i, k_i, w_i = np.split(w_in, np.cumsum([256,128,32,512,512,512,512,64,8])[:-1], axis=1)
    def pc(g, n):
        return np.ascontiguousarray(np.asarray(g, f32).reshape(n, 128).T)
    w_uq = np.asarray(inp['w_uq'][0], f32).reshape(256, 8, 96)
    w_ukv = np.asarray(inp['w_ukv'][0], f32).reshape(128, 8, 128)
    posK = np.concatenate([np.arange(128), 16 + np.arange(64 * 128)])
    ropeK = rope_tab(posK).reshape(65, 128, 192)
    g_idx = 2 * np.arange(32) + par
    posQ = (16 + 128 * g_idx[:, None] + np.arange(128)[None, :]).reshape(-1)
    ropeQ = rope_tab(posQ).reshape(32, 128, 192)
    tri = (np.arange(128)[:, None] <= np.arange(128)[None, :]).astype(f32)
    ones = np.ones((128, 128), f32); zeros = np.zeros((128, 128), f32)
    mk_mult = np.stack([tri, zeros] if par == 0 else [ones, tri]).astype(BF)
    NEG = np.float32(-1e30)
    meta_add = np.zeros((128, 128), f32); meta_add[:, 16:] = NEG
    triA = np.where(tri.T > 0, 0, NEG).astype(f32)
    allneg = np.full((128, 128), NEG, f32)
    mk_add = np.stack([meta_add, triA, allneg] if par == 0 else [meta_add, zeros, triA]).astype(f32)
    vcol0 = np.zeros((128, 8), f32); vcol0[0:16] = 1
    return {
        'xk': xk, 'xq': xq,
        'wk_in': np.ascontiguousarray(np.concatenate([k_s, v_s, c_kv, k_r, k_i], 1)),
        'wq_in': np.ascontiguousarray(np.concatenate([c_q, q_s, q_i, w_i], 1)),
        'g_attn': pc(inp['attn_norm_g'][0], 8),
        'w_uq_n': np.ascontiguousarray(w_uq[:, :, :64].reshape(256, 512)),
        'w_uq_r': np.ascontiguousarray(w_uq[:, :, 64:].reshape(256, 256)),
        'g_q': pc(inp['mla_q_norm_g'][0], 2),
        'w_ukv_k': np.ascontiguousarray(w_ukv[:, :, :64].reshape(128, 512)),
        'w_ukv_v': np.ascontiguousarray(w_ukv[:, :, 64:].reshape(128, 512)),
        'g_kv': pc(inp['mla_kv_norm_g'][0], 1),
        'w_o': np.asarray(inp['w_o'][0], f32),
        'g_ffn': pc(inp['ffn_norm_g'][0], 8),
        'w_gate': np.asarray(inp['w_gate'][0], f32), 'w_up': np.asarray(inp['w_up'][0], f32),
        'w_down': np.asarray(inp['w_down'][0], f32),
        'g_fin': np.ascontiguousarray(np.broadcast_to(np.asarray(inp['final_norm_g'], f32)[None, :], (128, 1024))),
        'ropeK': ropeK, 'ropeQ': ropeQ,
        'ident_f': np.eye(128, dtype=f32), 'ident_b': np.eye(128, dtype=f32).astype(BF),
        'vcol0': vcol0.astype(BF), 'mk_mult': mk_mult, 'mk_add': mk_add,
    }


def kernel(**inputs):
    inp = {k: np.asarray(v) for k, v in inputs.items()}
    nc, _ = build()
    names = set(LAST_INPUTS)
    in_maps = []
    for core in range(8):
        ci = core_inputs(inp, core)
        in_maps.append({k: np.ascontiguousarray(v) for k, v in ci.items() if k in names})
    res = run_bass_kernel_spmd(nc, in_maps, core_ids=list(range(8)))
    out = np.zeros((4, 64, 128, 1024), np.float32)
    for core in range(8):
        b, par = core // 2, core % 2
        out[b, par::2] = np.asarray(res.results[core]["out"], np.float32).reshape(32, 128, 1024)
    return out.reshape(4, 8192, 1024)
```

```python
import numpy as np
import ml_dtypes
from contextlib import ExitStack
import concourse.bass as bass
import concourse.mybir as mybir
from concourse.bass_utils import run_bass_kernel_spmd

F32 = mybir.dt.float32
BF16 = mybir.dt.bfloat16
AF = mybir.ActivationFunctionType
ALU = mybir.AluOpType
AX = mybir.AxisListType

D = 1024
NQB = 32
NKB = 65
EPS = 1e-6
DFF = 2816
NFC = DFF // 128
TOPK = 256
NEG = -1.0e30
LAST_INPUTS = []
import os
CUT = float(os.environ.get('KCUT', '99'))


class Sched:
    ENG = ('pe', 'act', 'dve', 'pool', 'sp')

    def __init__(self):
        self.q = {e: [] for e in self.ENG}
        self.cnt = {}

    def add(self, eng, fn, deps=(), sig=False, dma=None):
        tok = None
        inc = None
        if dma is not None:
            self.cnt[dma] = self.cnt.get(dma, 0) + 16
            inc = (dma, 16)
            tok = (dma, self.cnt[dma])
        elif sig:
            name = 'c_' + eng
            self.cnt[name] = self.cnt.get(name, 0) + 1
            inc = (name, 1)
            tok = (name, self.cnt[name])
        dl = []
        for d in deps:
            if d is None:
                continue
            if isinstance(d, list):
                dl.extend(x for x in d if x is not None)
            else:
                dl.append(d)
        self.q[eng].append((fn, dl, inc))
        return tok

    def final_wait(self, eng='sp'):
        deps = [(n, v) for n, v in self.cnt.items()]
        self.q[eng].append((lambda e: e.nop(), deps, None))

    def run(self, nc, name, semstack=None):
        with ExitStack() as es:
            sems = {n: (semstack or es).enter_context(nc.semaphore(name + '_' + n)) for n in self.cnt}
            block = es.enter_context(nc.Block())

            def mk(engname):
                def body(eng):
                    waited = {}
                    for fn, deps, inc in self.q[engname]:
                        for (s, v) in deps:
                            if engname == 'pe' and s == 'c_pe':
                                continue
                            if waited.get(s, 0) < v:
                                eng.wait_ge(sems[s], v)
                                waited[s] = v
                        inst = fn(eng)
                        if inc is not None:
                            inst.then_inc(sems[inc[0]], inc[1])
                return body
            block.tensor(mk('pe'))
            block.scalar(mk('act'))
            block.vector(mk('dve'))
            block.gpsimd(mk('pool'))
            block.sync(mk('sp'))


def _bc(ap, shape_mid):
    return ap


def build(nkb=NKB, nqb=NQB, debug=False, phases='ABCD'):
    nc = bass.Bass("TRN2", target_bir_lowering=False)
    T = nkb * 128

    _din = {}

    def din(name, shape, dt=F32):
        if name not in _din:
            _din[name] = nc.dram_tensor(name, list(shape), dt, kind="ExternalInput").ap()
        return _din[name]

    def dscr(name, shape, dt):
        return nc.dram_tensor(name, list(shape), dt).ap()

    out = nc.dram_tensor("out", [NQB * 128, D], F32, kind="ExternalOutput").ap()

    s_ksT = dscr("s_ksT", [NKB, 128, 512], BF16)
    s_vds = dscr("s_vds", [NKB, 128, 520], BF16)
    s_kiT = dscr("s_kiT", [NKB, 64, 128], BF16)
    s_h1 = dscr("s_h1", [NQB * 128, D], F32)
    s_omla = dscr("s_omla", [NQB, 128, 512], BF16)
    s_odsa = dscr("s_odsa", [NQB, 128, 512], BF16)
    dbg = {}
    if debug:
        def dout(name, shape, dt=F32):
            t = nc.dram_tensor(name, list(shape), dt, kind="ExternalOutput").ap()
            dbg[name] = t
            return t
        d_knT = dout("d_knT", [128, 4 * T], BF16)
        d_krT = dout("d_krT", [32, T], BF16)
        d_vm = dout("d_vm", [128, nkb * 520], BF16)
        d_ksT = dout("d_ksT", [NKB, 128, 512], BF16)
        d_vds = dout("d_vds", [NKB, 128, 520], BF16)
        d_kiT = dout("d_kiT", [NKB, 64, 128], BF16)
        d_omla = dout("d_omla", [128, nqb * 512], BF16)
        d_h1 = dout("d_h1", [NQB * 128, D], F32)

    with ExitStack() as top:
        def sb(name, shape, dt, es=top):
            return es.enter_context(nc.sbuf_tensor(name, list(shape), dt))

        def ps(name, shape, dt, es=top):
            return es.enter_context(nc.psum_tensor(name, list(shape), dt))

        identF = sb("identF", [128, 128], F32)
        identB = sb("identB", [128, 128], BF16)
        epsT = sb("epsT", [128, 1], F32)
        abst = ExitStack()
        knT = sb("knT", [128, 4, T], BF16, abst)
        krT = sb("krT", [32, T], BF16, abst)
        vm = sb("vm", [128, nkb, 8, 65], BF16, abst)

        if 'A' in phases:
            with ExitStack() as pa:
                S = Sched()
                xk = din("xk", [nkb * 128, D])
                wk_in = din("wk_in", [D, 1248])
                g_attn = din("g_attn", [128, 8])
                w_ukv_k = din("w_ukv_k", [128, 512])
                w_ukv_v = din("w_ukv_v", [128, 512])
                g_kv = din("g_kv", [128, 1])
                ropeK = din("ropeK", [nkb, 128, 192])
                ident_f = din("ident_f", [128, 128])
                ident_b = din("ident_b", [128, 128], BF16)
                vcol0 = din("vcol0", [128, 8], BF16)
                Wk = sb("Wk", [128, 8, 1248], BF16, pa)
                WukK = sb("WukK", [128, 512], BF16, pa)
                WukV = sb("WukV", [128, 512], BF16, pa)
                gA = sb("gA", [128, 8], F32, pa)
                gKV = sb("gKV", [128, 1], F32, pa)
                stg = [sb("stgA0", [128, 1248], F32, pa)] * 2
                xs = [sb(f"xsA{i}", [128, D], F32, pa) for i in range(2)]
                tab = [sb(f"tabA{i}", [128, 192], F32, pa) for i in range(2)]
                tabs = [sb(f"tabsA{i}", [128, 192], F32, pa) for i in range(2)]
                junk = sb("junkA", [128, D], BF16, pa)
                ss = [sb(f"ssA{i}", [128, 4], F32, pa) for i in range(2)]
                ss2 = [sb(f"ss2A{i}", [128, 4], F32, pa) for i in range(2)]
                xT = [sb(f"xTA{i}", [128, 8, 128], BF16, pa) for i in range(2)]
                ra = sb("ra", [128, 512], F32, pa)
                rb = sb("rb", [128, 512], F32, pa)
                ksr = sb("ksr", [128, 512], BF16, pa)
                ri_a = sb("ri_a", [128, 64], F32, pa)
                ri_b = sb("ri_b", [128, 64], F32, pa)
                kir = sb("kir", [128, 64], BF16, pa)
                rr_a = sb("rr_a", [128, 32], F32, pa)
                rr_b = sb("rr_b", [128, 32], F32, pa)
                krr = sb("krr", [128, 32], BF16, pa)
                ckv = sb("ckv", [128, 128], F32, pa)
                junk2 = sb("junk2A", [128, 128], BF16, pa)
                ckvn = sb("ckvn", [128, 128], BF16, pa)
                ckvnT = sb("ckvnT", [128, 128], BF16, pa)
                vst = [sb(f"vstA{i}", [128, 8, 65], BF16, pa) for i in range(2)]
                kst = [sb(f"kstA{i}", [128, 512], BF16, pa) for i in range(2)]
                kit = [sb(f"kitA{i}", [64, 128], BF16, pa) for i in range(2)]
                pT = [ps(f"pTA{i}", [128, 512], F32, pa) for i in range(2)]
                pP = [ps(f"pPA{i}", [128, 512], F32, pa) for i in range(3)]
                pS = ps("pSA", [128, 1024], BF16, pa)
                pK = ps("pKA", [128, 512], F32, pa)
                pV = ps("pVA", [128, 512], F32, pa)

                S.add('sp', lambda e: e.dma_start(out=identF[:, :], in_=ident_f[:, :]), dma='cst')
                S.add('sp', lambda e: e.dma_start(out=identB[:, :], in_=ident_b[:, :]), dma='cst')
                S.add('sp', lambda e: e.dma_start(out=gA[:, :], in_=g_attn[:, :]), dma='cst')
                S.add('sp', lambda e: e.dma_start(out=gKV[:, :], in_=g_kv[:, :]), dma='cst')
                t_eps = S.add('dve', lambda e: e.memset(epsT[:, :], EPS), sig=True)
                t_vm1 = S.add('pool', lambda e: e.memset(vm[:, :, :, 64:65], 1.0), sig=True)
                t_vst1 = [None, S.add('pool', lambda e: e.memset(vst[1][:, :, 64:65], 1.0), sig=True)]
                S.add('sp', lambda e: e.dma_start(out=vst[0][:, :, 64:65], in_=vcol0[:, :].unsqueeze(2), allow_slow_non_contiguous=True), dma='cst')
                t_cst = S.add('sp', lambda e: e.dma_start(out=vm[:, 0, :, 64:65], in_=vcol0[:, :].unsqueeze(2), allow_slow_non_contiguous=True), deps=[t_vm1], dma='cst')
                t_id = t_cst
                t_vc0 = t_cst
                t_vst1[0] = t_cst
                stg_free = [None, None]
                t_w = []
                for c in range(8):
                    s = 0
                    td = S.add('sp', lambda e, c=c, s=s: e.dma_start(out=stg[s][:, :], in_=wk_in[c * 128:(c + 1) * 128, :]),
                               deps=[stg_free[s]], dma=f'stg{s}')
                    eng = 'dve' if s == 0 else 'pool'
                    stg_free[s] = S.add(eng, lambda e, c=c, s=s: e.tensor_scalar(
                        out=Wk[:, c, :], in0=stg[s][:, :], scalar1=gA[:, c:c + 1], scalar2=None, op0=ALU.mult),
                        deps=[td, t_cst], sig=True)
                    t_w.append(stg_free[s])
                for (dst, src, s) in ((WukK, w_ukv_k, 0), (WukV, w_ukv_v, 0)):
                    td = S.add('sp', lambda e, s=s, src=src: e.dma_start(out=stg[s][:, 0:512], in_=src[:, :]),
                               deps=[stg_free[s]], dma=f'stg{s}')
                    eng = 'dve' if s == 0 else 'pool'
                    stg_free[s] = S.add(eng, lambda e, s=s, dst=dst: e.tensor_scalar(
                        out=dst[:, :], in0=stg[s][:, 0:512], scalar1=gKV[:, 0:1], scalar2=None, op0=ALU.mult),
                        deps=[td, t_cst], sig=True)
                    t_w.append(stg_free[s])

                xs_free = [None, None]
                tab_free = [None, None]
                xT_free = [None, None]
                pT_free = [None, None]
                pP_free = [[None], [None], [None]]
                pS_free = [None]
                pK_free = None
                pV_free = None
                vst_free = [None, None]
                kst_free = [None, None]
                kit_free = [None, None]
                ss_free = [None, None]
                tmp_free = {}
                ckvnT_free = None
                for k in range(nkb):
                    s = k % 2
                    t_x = S.add('sp', lambda e, k=k, s=s: e.dma_start(out=xs[s][:, :], in_=xk[k * 128:(k + 1) * 128, :]),
                                deps=[xs_free[s], tab_free[s]], dma=f'ld{s}')
                    t_tab = S.add('sp', lambda e, k=k, s=s: e.dma_start(out=tab[s][:, :], in_=ropeK[k, :, :]),
                                  deps=[tab_free[s]], dma=f'ld{s}')
                    t_x = t_tab
                    t_ss = S.add('act', lambda e, s=s: e.activation(out=junk[:, :], in_=xs[s][:, :], func=AF.Square,
                                                                   accum_out=ss[s][:, 0:1]),
                                 deps=[t_x, ss_free[s]], sig=True)
                    t_sd = S.add('act', lambda e, s=s: e.activation(out=ss[s][:, 1:2], in_=ss[s][:, 0:1], func=AF.Sqrt,
                                                                   scale=1.0 / D, bias=epsT[:, 0:1]),
                                 deps=[t_ss, t_eps], sig=True)
                    t_rstd = S.add('dve', lambda e, s=s: e.reciprocal(out=ss[s][:, 2:3], in_=ss[s][:, 1:2]),
                                   deps=[t_sd], sig=True)
                    rstd = ss[s][:, 2:3]
                    if CUT <= 1:
                        continue
                    t_tr = []
                    for hlf in range(2):
                        for j in range(4):
                            c = hlf * 4 + j
                            tt = S.add('pe', lambda e, s=s, c=c, hlf=hlf, j=j: e.transpose(
                                out=pT[hlf][:, j * 128:(j + 1) * 128], in_=xs[s][:, c * 128:(c + 1) * 128],
                                identity=identF[:, :]),
                                deps=[t_x, t_id, pT_free[hlf]], sig=(j == 3))
                        t_tr.append(tt)
                    te0 = S.add('act', lambda e, s=s: e.activation(out=xT[s][:, 0:4, :], in_=pT[0][:, :], func=AF.Copy),
                                deps=[t_tr[0], xT_free[s]], sig=True)
                    te1 = S.add('dve', lambda e, s=s: e.tensor_copy(out=xT[s][:, 4:8, :], in_=pT[1][:, :]),
                                deps=[t_tr[1], xT_free[s]], sig=True)
                    pT_free = [te0, te1]
                    xs_free[s] = [te0, te1, t_ss]
                    if CUT <= 2:
                        continue
                    t_mm = []
                    for bnk, (c0, c1) in enumerate(((0, 512), (512, 1024), (1024, 1248))):
                        for c in range(8):
                            tt = S.add('pe', lambda e, s=s, c=c, bnk=bnk, c0=c0, c1=c1: e.matmul(
                                pP[bnk][:, 0:c1 - c0], lhsT=xT[s][:, c, :], rhs=Wk[:, c, c0:c1],
                                start=(c == 0), stop=(c == 7)),
                                deps=[te0, te1, pP_free[bnk], t_w], sig=(c == 7))
                        t_mm.append(tt)
                    xT_free[s] = t_mm[2]
                    if CUT <= 3:
                        continue
                    t_tabs = S.add('dve', lambda e, s=s: e.tensor_scalar(
                        out=tabs[s][:, :], in0=tab[s][:, :], scalar1=ss[s][:, 2:3], scalar2=None, op0=ALU.mult),
                        deps=[t_tab, t_rstd, tab_free[s]], sig=True)
                    t_a = S.add('dve', lambda e, s=s: e.tensor_tensor(
                        out=ra[:, :].rearrange("p (h d) -> p h d", h=8), in0=pP[0][:, :].rearrange("p (h d) -> p h d", h=8),
                        in1=bcast(tabs[s][:, 0:64], 8), op=ALU.mult),
                        deps=[t_mm[0], t_tabs, tmp_free.get('ra')], sig=True)
                    t_b1 = S.add('dve', lambda e, s=s: e.tensor_tensor(
                        out=rb[:, :].rearrange("p (h d) -> p h d", h=8)[:, :, 0:32],
                        in0=pP[0][:, :].rearrange("p (h d) -> p h d", h=8)[:, :, 32:64],
                        in1=bcast(tabs[s][:, 64:96], 8), op=ALU.mult),
                        deps=[t_mm[0], t_tabs, tmp_free.get('ra')], sig=True)
                    t_b2 = S.add('dve', lambda e, s=s: e.tensor_tensor(
                        out=rb[:, :].rearrange("p (h d) -> p h d", h=8)[:, :, 32:64],
                        in0=pP[0][:, :].rearrange("p (h d) -> p h d", h=8)[:, :, 0:32],
                        in1=bcast(tabs[s][:, 96:128], 8), op=ALU.mult),
                        deps=[t_mm[0], t_tabs, tmp_free.get('ra')], sig=True)
                    pP_free[0] = [t_a, t_b1, t_b2]
                    t_ksr = S.add('pool', lambda e: e.tensor_tensor(out=ksr[:, :], in0=ra[:, :], in1=rb[:, :], op=ALU.add),
                                  deps=[t_a, t_b1, t_b2, tmp_free.get('ksr')], sig=True)
                    tmp_free['ra'] = t_ksr
                    if CUT <= 4:
                        continue
                    t_v = S.add('act', lambda e, s=s: e.activation(
                        out=vst[s][:, :, 0:64], in_=pP[1][:, :].rearrange("p (h d) -> p h d", h=8), func=AF.Copy,
                        scale=ss[s][:, 2:3]),
                        deps=[t_mm[1], t_rstd, vst_free[s], t_vst1[s]], sig=True)
                    pP_free[1] = [t_v]
                    t_vd = S.add('sp', lambda e, s=s, k=k: e.dma_start(
                        out=s_vds[k, :, :], in_=vst[s][:, :, :].rearrange("p h d -> p (h d)")),
                        deps=[t_v], dma=f'st{s}')
                    if CUT <= 5:
                        continue
                    t_ckv = S.add('dve', lambda e, s=s: e.tensor_scalar(out=ckv[:, :], in0=pP[2][:, 0:128], scalar1=ss[s][:, 2:3],
                                                                       scalar2=None, op0=ALU.mult),
                                  deps=[t_mm[2], t_rstd, tmp_free.get('ckv')], sig=True)
                    t_ss2 = S.add('act', lambda e, s=s: e.activation(out=junk2[:, :], in_=ckv[:, :], func=AF.Square,
                                                                    accum_out=ss2[s][:, 0:1]),
                                  deps=[t_ckv, tmp_free.get('ss2%d' % s)], sig=True)
                    t_sd2 = S.add('act', lambda e, s=s: e.activation(out=ss2[s][:, 1:2], in_=ss2[s][:, 0:1], func=AF.Sqrt,
                                                                    scale=1.0 / 128, bias=epsT[:, 0:1]),
                                  deps=[t_ss2], sig=True)
                    t_r2 = S.add('dve', lambda e, s=s: e.reciprocal(out=ss2[s][:, 2:3], in_=ss2[s][:, 1:2]),
                                 deps=[t_sd2], sig=True)
                    t_ckvn = S.add('dve', lambda e, s=s: e.tensor_scalar(
                        out=ckvn[:, :], in0=ckv[:, :], scalar1=ss2[s][:, 2:3], scalar2=None, op0=ALU.mult),
                        deps=[t_r2, t_ckv, tmp_free.get('ckvn')], sig=True)
                    tmp_free['ckv'] = [t_ckvn, t_ss2]
                    tmp_free['ss2%d' % s] = t_ckvn
                    if CUT <= 6:
                        continue
                    t_ra = S.add('dve', lambda e, s=s: e.tensor_tensor(
                        out=rr_a[:, :], in0=pP[2][:, 128:160], in1=tabs[s][:, 128:160], op=ALU.mult),
                        deps=[t_mm[2], t_tabs, tmp_free.get('rr')], sig=True)
                    t_rb1 = S.add('dve', lambda e, s=s: e.tensor_tensor(
                        out=rr_b[:, 0:16], in0=pP[2][:, 144:160], in1=tabs[s][:, 160:176], op=ALU.mult),
                        deps=[t_mm[2], t_tabs, tmp_free.get('rr')], sig=True)
                    t_rb2 = S.add('dve', lambda e, s=s: e.tensor_tensor(
                        out=rr_b[:, 16:32], in0=pP[2][:, 128:144], in1=tabs[s][:, 176:192], op=ALU.mult),
                        deps=[t_mm[2], t_tabs, tmp_free.get('rr')], sig=True)
                    t_krr = S.add('pool', lambda e: e.tensor_tensor(out=krr[:, :], in0=rr_a[:, :], in1=rr_b[:, :], op=ALU.add),
                                  deps=[t_ra, t_rb1, t_rb2, tmp_free.get('krr')], sig=True)
                    tmp_free['rr'] = t_krr
                    t_ia = S.add('dve', lambda e, s=s: e.tensor_tensor(
                        out=ri_a[:, :], in0=pP[2][:, 160:224], in1=tabs[s][:, 0:64], op=ALU.mult),
                        deps=[t_mm[2], t_tabs, tmp_free.get('ri')], sig=True)
                    t_ib1 = S.add('dve', lambda e, s=s: e.tensor_tensor(
                        out=ri_b[:, 0:32], in0=pP[2][:, 192:224], in1=tabs[s][:, 64:96], op=ALU.mult),
                        deps=[t_mm[2], t_tabs, tmp_free.get('ri')], sig=True)
                    t_ib2 = S.add('dve', lambda e, s=s: e.tensor_tensor(
                        out=ri_b[:, 32:64], in0=pP[2][:, 160:192], in1=tabs[s][:, 96:128], op=ALU.mult),
                        deps=[t_mm[2], t_tabs, tmp_free.get('ri')], sig=True)
                    t_kir = S.add('pool', lambda e: e.tensor_tensor(out=kir[:, :], in0=ri_a[:, :], in1=ri_b[:, :], op=ALU.add),
                                  deps=[t_ia, t_ib1, t_ib2, tmp_free.get('kir')], sig=True)
                    tmp_free['ri'] = t_kir
                    pP_free[2] = [t_ckv, t_ra, t_rb1, t_rb2, t_ia, t_ib1, t_ib2]
                    tab_free[s] = [t_a, t_b1, t_b2, t_ra, t_rb1, t_rb2, t_ia, t_ib1, t_ib2]
                    ss_free[s] = [t_tabs, t_v, t_ckv]
                    if CUT <= 7:
                        continue
                    for a in range(4):
                        t_t1 = S.add('pe', lambda e, a=a: e.transpose(
                            out=pS[:, a * 128:(a + 1) * 128], in_=ksr[:, a * 128:(a + 1) * 128], identity=identB[:, :]),
                            deps=[t_ksr, pS_free], sig=(a == 3))
                    if CUT <= 7.1:
                        continue
                    t_t2 = S.add('pe', lambda e: e.transpose(out=pS[0:64, 512:640], in_=kir[:, :], identity=identB[:, :]),
                                 deps=[t_kir, pS_free], sig=True)
                    if CUT <= 7.2:
                        continue
                    t_t3 = S.add('pe', lambda e: e.transpose(out=pS[0:32, 640:768], in_=krr[:, :], identity=identB[:, :]),
                                 deps=[t_krr, pS_free], sig=True)
                    if CUT <= 7.3:
                        continue
                    t_t4 = S.add('pe', lambda e: e.transpose(out=pS[:, 768:896], in_=ckvn[:, :], identity=identB[:, :]),
                                 deps=[t_ckvn, pS_free], sig=True)
                    tmp_free['ksr'] = t_t1
                    tmp_free['kir'] = t_t2
                    tmp_free['krr'] = t_t3
                    tmp_free['ckvn'] = t_t4
                    if CUT <= 8:
                        continue
                    t_e1 = S.add('dve', lambda e, s=s: e.tensor_copy(out=kst[s][:, :], in_=pS[:, 0:512]),
                                 deps=[t_t4, kst_free[s]], sig=True)
                    if CUT <= 8.1:
                        continue
                    t_d1 = S.add('sp', lambda e, s=s, k=k: e.dma_start(out=s_ksT[k, :, :], in_=kst[s][:, :]),
                                 deps=[t_e1], dma=f'st{s}')
                    if CUT <= 8.2:
                        continue
                    t_e2 = S.add('dve', lambda e, s=s: e.tensor_copy(out=kit[s][:, :], in_=pS[0:64, 512:640]),
                                 deps=[t_t4, kit_free[s]], sig=True)
                    if CUT <= 8.3:
                        continue
                    t_d2 = S.add('sp', lambda e, s=s, k=k: e.dma_start(out=s_kiT[k, :, :], in_=kit[s][:, :]),
                                 deps=[t_e2], dma=f'st{s}')
                    kit_free[s] = t_d2
                    kst_free[s] = t_d2
                    vst_free[s] = t_d2
                    if k == 0:
                        vst_free[s] = S.add('pool', lambda e, s=s: e.memset(vst[s][:, :, 64:65], 1.0), deps=[t_d2], sig=True)
                    if CUT <= 8.4:
                        continue
                    t_e3 = S.add('dve', lambda e, k=k: e.tensor_copy(out=krT[:, k * 128:(k + 1) * 128], in_=pS[0:32, 640:768]),
                                 deps=[t_t4], sig=True)
                    if CUT <= 8.5:
                        continue
                    t_e4 = S.add('dve', lambda e: e.tensor_copy(out=ckvnT[:, :], in_=pS[:, 768:896]),
                                 deps=[t_t4, ckvnT_free], sig=True)
                    pS_free = [t_e1, t_e2, t_e3, t_e4]
                    if CUT <= 9:
                        continue
                    for a in range(4):
                        t_k = S.add('pe', lambda e, a=a: e.matmul(
                            pK[:, a * 128:(a + 1) * 128], lhsT=WukK[:, a * 128:(a + 1) * 128], rhs=ckvnT[:, :],
                            start=True, stop=True), deps=[t_e4, pK_free, t_w], sig=(a == 3))
                    t_vv = S.add('pe', lambda e: e.matmul(pV[:, :], lhsT=ckvnT[:, :], rhs=WukV[:, :], start=True, stop=True),
                                 deps=[t_e4, pV_free, t_w], sig=True)
                    ckvnT_free = t_vv
                    pK_free = S.add('act', lambda e, k=k: e.activation(
                        out=knT[:, :, k * 128:(k + 1) * 128], in_=pK[:, :].rearrange("p (a t) -> p a t", a=4), func=AF.Copy),
                        deps=[t_k], sig=True)
                    pV_free = S.add('dve', lambda e, k=k: e.tensor_copy(
                        out=vm[:, k, :, 0:64], in_=pV[:, :].rearrange("p (h d) -> p h d", h=8)),
                        deps=[t_vv, t_vm1, t_vc0], sig=True)
                last = [pK_free, pV_free, kst_free[0], kst_free[1], kit_free[0], kit_free[1], vst_free[0], vst_free[1]]
                if debug and CUT > 50:
                    S.add('sp', lambda e: e.dma_start(out=d_knT[:, :], in_=knT[:, :, :].rearrange("p a t -> p (a t)")),
                          deps=last, dma='dbg')
                    S.add('sp', lambda e: e.dma_start(out=d_krT[:, :], in_=krT[:, :]), deps=last, dma='dbg')
                    t_dbg = S.add('sp', lambda e: e.dma_start(out=d_vm[:, :], in_=vm[:, :, :, :].rearrange("p k h d -> p (k h d)")),
                                  deps=last, dma='dbg')
                    last = last + [t_dbg]
                S.final_wait()
                S.run(nc, "A", top)
                if debug and CUT > 50:
                    S2_ = Sched()
                    t1 = S2_.add('sp', lambda e: e.dma_start(out=d_ksT[0:nkb, :, :], in_=s_ksT[0:nkb, :, :]), dma='d')
                    t1 = S2_.add('sp', lambda e: e.dma_start(out=d_vds[0:nkb, :, :], in_=s_vds[0:nkb, :, :]), dma='d')
                    t1 = S2_.add('sp', lambda e: e.dma_start(out=d_kiT[0:nkb, :, :], in_=s_kiT[0:nkb, :, :]), dma='d')
                    S2_.add('sp', lambda e: e.nop(), deps=[t1])
                    S2_.run(nc, "Ad", top)
        if 'B' in phases:
            with ExitStack() as pb_:
                S = Sched()
                xq = din("xq", [nqb * 128, D])
                wq_in = din("wq_in", [D, 1288])
                g_attn = din("g_attn", [128, 8])
                w_uq_n = din("w_uq_n", [256, 512])
                w_uq_r = din("w_uq_r", [256, 256])
                g_q = din("g_q", [128, 2])
                ropeQ = din("ropeQ", [nqb, 128, 192])
                mk_mult = din("mk_mult", [2, 128, 128], BF16)
                SC = 96.0 ** -0.5
                Wcq = sb("Wcq", [128, 8, 256], BF16, pb_)
                WuqN = sb("WuqN", [128, 2, 512], BF16, pb_)
                WuqR = sb("WuqR", [128, 2, 256], BF16, pb_)
                gA = sb("gAB", [128, 8], F32, pb_)
                gQ = sb("gQB", [128, 2], F32, pb_)
                mkm = sb("mkmB", [128, 2, 128], BF16, pb_)
                stg = [sb(f"stgB{i}", [128, 512], F32, pb_) for i in range(2)]
                xs = sb("xsB", [128, D], F32, pb_)
                xb = sb("xbB", [128, D], BF16, pb_)
                tab = sb("tabB", [128, 192], F32, pb_)
                tabq = sb("tabqB", [128, 64], F32, pb_)
                junk = sb("junkB", [128, D], BF16, pb_)
                ss = sb("ssB", [128, 8], F32, pb_)
                xT = sb("xTB", [128, 8, 128], BF16, pb_)
                cq = sb("cqB", [128, 256], F32, pb_)
                cqn = sb("cqnB", [128, 256], BF16, pb_)
                cqnT = sb("cqnTB", [128, 2, 128], BF16, pb_)
                qbd = sb("qbdB", [128, 4, 256], BF16, pb_)
                qra = sb("qraB", [128, 256], F32, pb_)
                qrb = sb("qrbB", [128, 256], F32, pb_)
                qr = sb("qrB", [128, 256], BF16, pb_)
                qrT = sb("qrTB", [32, 1024], BF16, pb_)
                PT = [sb(f"PTB{i}", [128, 1024], BF16, pb_) for i in range(2)]
                rden = sb("rdenB", [128, 8], F32, pb_)
                ost = [sb(f"ostB{i}", [128, 512], BF16, pb_) for i in range(2)]
                pT = ps("pTB", [128, 1024], BF16, pb_)
                pQ = ps("pQB", [128, 512], F32, pb_)
                pST = [ps(f"pSTB{i}", [128, 512], F32, pb_) for i in range(4)]
                pO = [ps(f"pOB{i}", [128, 4, 65], F32, pb_) for i in range(2)]

                S.add('sp', lambda e: e.dma_start(out=gA[:, :], in_=g_attn[:, :]), dma='cst')
                S.add('sp', lambda e: e.dma_start(out=gQ[:, :], in_=g_q[:, :]), dma='cst')
                t_cst = S.add('sp', lambda e: e.dma_start(out=mkm[:, :, :], in_=mk_mult.rearrange("m k q -> k m q")), dma='cst')
                t_z = S.add('pool', lambda e: e.memset(qbd[:, :, :], 0.0), sig=True)
                stg_free = [None, None]
                t_w = []
                jobs = [(Wcq[:, c, :], wq_in[c * 128:(c + 1) * 128, 0:256], gA[:, c:c + 1], 256) for c in range(8)]
                jobs += [(WuqN[:, c, :], w_uq_n[c * 128:(c + 1) * 128, :], gQ[:, c:c + 1], 512) for c in range(2)]
                jobs += [(WuqR[:, c, :], w_uq_r[c * 128:(c + 1) * 128, :], gQ[:, c:c + 1], 256) for c in range(2)]
                for j, (dst, src, gs, n) in enumerate(jobs):
                    s = j % 2
                    td = S.add('sp', lambda e, s=s, src=src, n=n: e.dma_start(out=stg[s][:, 0:n], in_=src),
                               deps=[stg_free[s]], dma=f'stg{s}')
                    stg_free[s] = S.add('dve' if s == 0 else 'pool', lambda e, s=s, dst=dst, gs=gs, n=n: e.tensor_scalar(
                        out=dst, in0=stg[s][:, 0:n], scalar1=gs, scalar2=None, op0=ALU.mult), deps=[td, t_cst], sig=True)
                    t_w.append(stg_free[s])

                prev = []
                pst_free = [None] * 4
                PT_free = [None, None]
                ost_free = [None, None]
                t_onorm = None
                for i in range(nqb):
                    nk = min(2 * i + 3, nkb)
                    t_x = S.add('sp', lambda e, i=i: e.dma_start(out=xs[:, :], in_=xq[i * 128:(i + 1) * 128, :]), deps=prev, dma='ld')
                    t_x = S.add('sp', lambda e, i=i: e.dma_start(out=tab[:, :], in_=ropeQ[i, :, :]), deps=prev, dma='ld')
                    t_ss = S.add('act', lambda e: e.activation(out=junk[:, :], in_=xs[:, :], func=AF.Square, accum_out=ss[:, 0:1]),
                                 deps=[t_x] + prev, sig=True)
                    t_sd = S.add('act', lambda e: e.activation(out=ss[:, 1:2], in_=ss[:, 0:1], func=AF.Sqrt, scale=1.0 / D, bias=epsT[:, 0:1]),
                                 deps=[t_ss], sig=True)
                    t_rstd = S.add('dve', lambda e: e.reciprocal(out=ss[:, 2:3], in_=ss[:, 1:2]), deps=[t_sd], sig=True)
                    t_xb = S.add('dve', lambda e: e.tensor_copy(out=xb[:, :], in_=xs[:, :]), deps=[t_x] + prev, sig=True)
                    t_tq = S.add('dve', lambda e: e.tensor_scalar(out=tabq[:, :], in0=tab[:, 128:192], scalar1=SC, scalar2=None, op0=ALU.mult),
                                 deps=[t_x] + prev, sig=True)
                    for c in range(8):
                        t_tr = S.add('pe', lambda e, c=c: e.transpose(out=pT[:, c * 128:(c + 1) * 128], in_=xb[:, c * 128:(c + 1) * 128],
                                                                      identity=identB[:, :]), deps=[t_xb] + prev, sig=(c == 7))
                    t_xT = S.add('dve', lambda e: e.tensor_copy(out=xT[:, :, :], in_=pT[:, :].rearrange("p (c t) -> p c t", c=8)),
                                 deps=[t_tr], sig=True)
                    for c in range(8):
                        t_mm = S.add('pe', lambda e, c=c: e.matmul(pQ[:, 0:256], lhsT=xT[:, c, :], rhs=Wcq[:, c, :], start=(c == 0), stop=(c == 7)),
                                     deps=[t_xT, t_w] + prev, sig=(c == 7))
                    t_cq = S.add('dve', lambda e: e.tensor_scalar(out=cq[:, :], in0=pQ[:, 0:256], scalar1=ss[:, 2:3], scalar2=None, op0=ALU.mult),
                                 deps=[t_mm, t_rstd], sig=True)
                    t_ss2 = S.add('act', lambda e: e.activation(out=junk[:, 0:256], in_=cq[:, :], func=AF.Square, accum_out=ss[:, 4:5]),
                                  deps=[t_cq], sig=True)
                    t_sd2 = S.add('act', lambda e: e.activation(out=ss[:, 5:6], in_=ss[:, 4:5], func=AF.Sqrt, scale=1.0 / 256, bias=epsT[:, 0:1]),
                                  deps=[t_ss2], sig=True)
                    t_r2 = S.add('dve', lambda e: e.reciprocal(out=ss[:, 6:7], in_=ss[:, 5:6]), deps=[t_sd2], sig=True)
                    t_cqn = S.add('dve', lambda e: e.tensor_scalar(out=cqn[:, :], in0=cq[:, :], scalar1=ss[:, 6:7], scalar2=None, op0=ALU.mult),
                                  deps=[t_r2], sig=True)
                    for c in range(2):
                        t_tr = S.add('pe', lambda e, c=c: e.transpose(out=pT[:, c * 128:(c + 1) * 128], in_=cqn[:, c * 128:(c + 1) * 128],
                                                                      identity=identB[:, :]), deps=[t_cqn, t_xT], sig=(c == 1))
                    t_cT = S.add('dve', lambda e: e.tensor_copy(out=cqnT[:, :, :], in_=pT[:, 0:256].rearrange("p (c t) -> p c t", c=2)),
                                 deps=[t_tr], sig=True)
                    for a in range(4):
                        for c in range(2):
                            t_mm = S.add('pe', lambda e, a=a, c=c: e.matmul(pQ[:, a * 128:(a + 1) * 128], lhsT=WuqN[:, c, a * 128:(a + 1) * 128],
                                                                            rhs=cqnT[:, c, :], start=(c == 0), stop=(c == 1)),
                                         deps=[t_cT, t_cq], sig=(a == 3 and c == 1))
                    pQv = pQ[:, :].rearrange("p (a t) -> p a t", a=4)
                    t_q1 = S.add('dve', lambda e: e.tensor_scalar(out=qbd[0:64, :, 0:128], in0=pQ[:, :].rearrange("p (a t) -> p a t", a=4)[0:64, :, :],
                                                                  scalar1=SC, scalar2=None, op0=ALU.mult), deps=[t_mm, t_z] + prev, sig=True)
                    t_q2 = S.add('dve', lambda e: e.tensor_scalar(out=qbd[64:128, :, 128:256], in0=pQ[:, :].rearrange("p (a t) -> p a t", a=4)[64:128, :, :],
                                                                  scalar1=SC, scalar2=None, op0=ALU.mult), deps=[t_mm, t_z] + prev, sig=True)
                    for c in range(2):
                        t_mm = S.add('pe', lambda e, c=c: e.matmul(pQ[:, 0:256], lhsT=cqnT[:, c, :], rhs=WuqR[:, c, :], start=(c == 0), stop=(c == 1)),
                                     deps=[t_q1, t_q2], sig=(c == 1))
                    v3 = lambda ap: ap.rearrange("p (h d) -> p h d", h=8)
                    t_a = S.add('dve', lambda e: e.tensor_tensor(out=v3(qra[:, :]), in0=v3(pQ[:, 0:256]), in1=bcast(tabq[:, 0:32], 8), op=ALU.mult),
                                deps=[t_mm, t_tq] + prev, sig=True)
                    t_b1 = S.add('dve', lambda e: e.tensor_tensor(out=v3(qrb[:, :])[:, :, 0:16], in0=v3(pQ[:, 0:256])[:, :, 16:32],
                                                                  in1=bcast(tabq[:, 32:48], 8), op=ALU.mult), deps=[t_mm, t_tq] + prev, sig=True)
                    t_b2 = S.add('dve', lambda e: e.tensor_tensor(out=v3(qrb[:, :])[:, :, 16:32], in0=v3(pQ[:, 0:256])[:, :, 0:16],
                                                                  in1=bcast(tabq[:, 48:64], 8), op=ALU.mult), deps=[t_mm, t_tq] + prev, sig=True)
                    t_qr = S.add('pool', lambda e: e.tensor_tensor(out=qr[:, :], in0=qra[:, :], in1=qrb[:, :], op=ALU.add),
                                 deps=[t_a, t_b1, t_b2] + prev, sig=True)
                    for h in range(8):
                        t_tr = S.add('pe', lambda e, h=h: e.transpose(out=pT[0:32, h * 128:(h + 1) * 128], in_=qr[:, h * 32:(h + 1) * 32],
                                                                      identity=identB[:, :]), deps=[t_qr, t_cT], sig=(h == 7))
                    t_qrT = S.add('dve', lambda e: e.tensor_copy(out=qrT[:, :], in_=pT[0:32, :]), deps=[t_tr] + prev, sig=True)
                    qready = [t_q1, t_q2, t_qrT]
                    prev_q = [t_b2, t_qrT, t_ss2, t_cqn]
                    t_exp = {}
                    t_pv = None

                    def emit_qk(kb):
                        for g in range(2):
                            bank = pST[(kb % 2) * 2 + g]
                            for j in range(2):
                                a = g * 2 + j
                                S.add('pe', lambda e, bank=bank, j=j, a=a, kb=kb: e.matmul(
                                    bank[:, j * 256:(j + 1) * 256], lhsT=knT[:, a, kb * 128:(kb + 1) * 128], rhs=qbd[:, a, :],
                                    start=(j == 0), stop=False, skip_group_check=True),
                                    deps=qready + [pst_free[(kb % 2) * 2 + g]])
                            t = S.add('pe', lambda e, bank=bank, g=g, kb=kb: e.matmul(
                                bank[:, :], lhsT=krT[0:32, kb * 128:(kb + 1) * 128], rhs=qrT[0:32, g * 512:(g + 1) * 512],
                                start=False, stop=True, skip_group_check=True), deps=qready, sig=True)
                            t_qk[(kb, g)] = t
                    t_qk = {}

                    def emit_exp(kb):
                        for g in range(2):
                            bi = (kb % 2) * 2 + g
                            t = S.add('act', lambda e, bi=bi, kb=kb, g=g: e.activation(
                                out=PT[kb % 2][:, g * 512:(g + 1) * 512], in_=pST[bi][:, :], func=AF.Exp),
                                deps=[t_qk[(kb, g)], PT_free[kb % 2]], sig=True)
                            pst_free[bi] = t
                            mi = kb - (2 * i + 1)
                            if mi >= 0:
                                t = S.add('pool', lambda e, kb=kb, g=g, mi=mi: e.tensor_tensor(
                                    out=PT[kb % 2][:, g * 512:(g + 1) * 512].rearrange("p (h q) -> p h q", h=4),
                                    in0=PT[kb % 2][:, g * 512:(g + 1) * 512].rearrange("p (h q) -> p h q", h=4),
                                    in1=bcast(mkm[:, mi, :], 4), op=ALU.mult), deps=[t, t_cst], sig=True)
                            t_exp[(kb, g)] = t

                    def emit_pv(kb):
                        nonlocal t_pv
                        for h in range(8):
                            t_pv = S.add('pe', lambda e, h=h, kb=kb: e.matmul(
                                pO[h // 4][:, h % 4, :], lhsT=PT[kb % 2][:, h * 128:(h + 1) * 128], rhs=vm[:, kb, h, :],
                                start=(kb == 0 and h % 4 == 0), stop=(kb == nk - 1), skip_group_check=True),
                                deps=[t_exp[(kb, h // 4)], t_onorm], sig=(h == 7))
                        PT_free[kb % 2] = t_pv

                    emit_qk(0)
                    for kb in range(nk):
                        if kb + 1 < nk:
                            emit_qk(kb + 1)
                        emit_exp(kb)
                        emit_pv(kb)
                    so = i % 2
                    t_rd = S.add('dve', lambda e: e.reciprocal(out=rden[:, 0:4], in_=pO[0][:, :, 64]), deps=[t_pv], sig=True)
                    t_rd2 = S.add('dve', lambda e: e.reciprocal(out=rden[:, 4:8], in_=pO[1][:, :, 64]), deps=[t_pv], sig=True)
                    for g in range(2):
                        t_onorm = S.add('dve', lambda e, g=g, so=so: e.tensor_tensor(
                            out=ost[so][:, g * 256:(g + 1) * 256].rearrange("p (h d) -> p h d", h=4), in0=pO[g][:, :, 0:64],
                            in1=rden[:, g * 4:(g + 1) * 4].unsqueeze(2).broadcast_to([128, 4, 64]), op=ALU.mult),
                            deps=[t_rd, t_rd2, ost_free[so]], sig=True)
                    ost_free[so] = S.add('sp', lambda e, i=i, so=so: e.dma_start(out=s_omla[i, :, :], in_=ost[so][:, :]),
                                         deps=[t_onorm], dma=f'ost{so}')
                    prev = [t_pv, t_onorm] + prev_q
                if debug:
                    S.final_wait()
                    S.add('sp', lambda e: e.dma_start(out=d_omla.rearrange("p (i f) -> i p f", i=nqb), in_=s_omla[0:nqb, :, :]), dma='dbg')
                S.final_wait()
                S.run(nc, "B", top)
        abst.close()
        if 'C' in phases:
            with ExitStack() as pc_:
                S = Sched()
                xq = din("xq", [nqb * 128, D])
                wq_in = din("wq_in", [D, 1288])
                g_attn = din("g_attn", [128, 8])
                ropeQ = din("ropeQ", [nqb, 128, 192])
                mk_add = din("mk_add", [3, 128, 128])
                NIT = 16
                Wq2 = sb("Wq2C", [128, 8, 1032], BF16, pc_)
                gA = sb("gAC", [128, 8], F32, pc_)
                mka = sb("mkaC", [128, 3, 128], F32, pc_)
                stg = [sb(f"stgC{i}", [128, 1032], F32, pc_) for i in range(2)]
                ksT = sb("ksTC", [128, nkb, 512], BF16, pc_)
                kiT2 = sb("kiT2C", [128, nkb, 128], BF16, pc_)
                Isc = sb("IscC", [128, nkb * 128], F32, pc_)
                Msk = sb("MskC", [128, nkb * 128], BF16, pc_)
                xs = sb("xsC", [128, D], F32, pc_)
                xb = sb("xbC", [128, D], BF16, pc_)
                tab = sb("tabC", [128, 192], F32, pc_)
                tabs = sb("tabsC", [128, 128], F32, pc_)
                junk = sb("junkC", [128, D], BF16, pc_)
                ss = sb("ssC", [128, 8], F32, pc_)
                xT = sb("xTC", [128, 8, 128], BF16, pc_)
                ra = sb("raC", [128, 512], F32, pc_)
                rb = sb("rbC", [128, 512], F32, pc_)
                qs = sb("qsC", [128, 512], BF16, pc_)
                qi = sb("qiC", [128, 512], BF16, pc_)
                wv = sb("wvC", [128, 8], F32, pc_)
                qbd = sb("qbdC", [128, 4, 256], BF16, pc_)
                qiT = sb("qiTC", [128, 4, 128], BF16, pc_)
                tmpR = [sb(f"tmpRC{i}", [128, 512], F32, pc_) for i in range(4)]
                pw2 = sb("pw2C", [128, 32], F32, pc_)
                hwt = sb("hwtC", [128, 32], F32, pc_)
                bs = sb("bsC", [128, 16], F32, pc_)
                PT = [sb(f"PTC{i}", [128, 1024], BF16, pc_) for i in range(2)]
                vb = [sb(f"vbC{i}", [128, 8, 65], BF16, pc_) for i in range(3)]
                rden = sb("rdenC", [128, 8], F32, pc_)
                ost = [sb(f"ostC{i}", [128, 512], BF16, pc_) for i in range(2)]
                pT = [ps(f"pTC{i}", [128, 1024], BF16, pc_) for i in range(2)]
                pST = [ps(f"pSTC{i}", [128, 512], F32, pc_) for i in range(4)]
                pO = [ps(f"pOC{i}", [128, 4, 65], F32, pc_) for i in range(2)]

                S.add('sp', lambda e: e.dma_start(out=gA[:, :], in_=g_attn[:, :]), dma='cst')
                S.add('sp', lambda e: e.dma_start(out=mka[:, :, :], in_=mk_add.rearrange("m q k -> q m k")), dma='cst')
                for k0 in range(0, nkb, 4):
                    k1 = min(nkb, k0 + 4)
                    S.add('sp', lambda e, k0=k0, k1=k1: e.dma_start(out=ksT[:, k0:k1, :], in_=s_ksT[k0:k1, :, :].rearrange("k p f -> p k f")), dma='cst')
                for k0 in range(0, nkb, 8):
                    k1 = min(nkb, k0 + 8)
                    S.add('sp', lambda e, k0=k0, k1=k1: e.dma_start(out=kiT2[0:64, k0:k1, :], in_=s_kiT[k0:k1, :, :].rearrange("k p t -> p k t")), dma='cst')
                    t_cst = S.add('sp', lambda e, k0=k0, k1=k1: e.dma_start(out=kiT2[64:128, k0:k1, :], in_=s_kiT[k0:k1, :, :].rearrange("k p t -> p k t")), dma='cst')
                t_z = S.add('pool', lambda e: e.memset(qbd[:, :, :], 0.0), sig=True)
                for it in range(NIT):
                    t_pw2 = S.add('pool', lambda e, it=it: e.memset(pw2[:, it:it + 1], 2.0 ** -(it + 1)), sig=True)
                stg_free = [None, None]
                t_w = []
                for c in range(8):
                    s = c % 2
                    td = S.add('sp', lambda e, s=s, c=c: e.dma_start(out=stg[s][:, :], in_=wq_in[c * 128:(c + 1) * 128, 256:1288]),
                               deps=[stg_free[s]], dma=f'stg{s}')
                    stg_free[s] = S.add(('dve', 'pool')[s], lambda e, s=s, c=c: e.tensor_scalar(
                        out=Wq2[:, c, :], in0=stg[s][:, :], scalar1=gA[:, c:c + 1], scalar2=None, op0=ALU.mult), deps=[td, t_cst], sig=True)
                    t_w.append(stg_free[s])
                v3 = lambda ap: ap.rearrange("p (h d) -> p h d", h=8)
                prev = []
                pst_free = [None] * 4
                PT_free = [None, None]
                pT_free = [None, None]
                vb_free = [None] * 3
                ost_free = [None, None]
                t_onorm = None
                vcount = 0
                for i in range(nqb):
                    nk = min(2 * i + 3, nkb)
                    W = nk * 128
                    S.add('sp', lambda e, i=i: e.dma_start(out=xs[:, :], in_=xq[i * 128:(i + 1) * 128, :]), deps=prev, dma='ld')
                    t_x = S.add('sp', lambda e, i=i: e.dma_start(out=tab[:, :], in_=ropeQ[i, :, :]), deps=prev, dma='ld')
                    t_ss = S.add('act', lambda e: e.activation(out=junk[:, :], in_=xs[:, :], func=AF.Square, accum_out=ss[:, 0:1]), deps=[t_x] + prev, sig=True)
                    t_sd = S.add('act', lambda e: e.activation(out=ss[:, 1:2], in_=ss[:, 0:1], func=AF.Sqrt, scale=1.0 / D, bias=epsT[:, 0:1]), deps=[t_ss], sig=True)
                    t_rstd = S.add('dve', lambda e: e.reciprocal(out=ss[:, 2:3], in_=ss[:, 1:2]), deps=[t_sd], sig=True)
                    t_r8 = S.add('dve', lambda e: e.tensor_scalar(out=ss[:, 3:4], in0=ss[:, 2:3], scalar1=0.125, scalar2=None, op0=ALU.mult), deps=[t_rstd], sig=True)
                    t_tabs = S.add('dve', lambda e: e.tensor_scalar(out=tabs[:, :], in0=tab[:, 0:128], scalar1=ss[:, 3:4], scalar2=None, op0=ALU.mult),
                                   deps=[t_r8, t_x] + prev, sig=True)
                    t_xb = S.add('dve', lambda e: e.tensor_copy(out=xb[:, :], in_=xs[:, :]), deps=[t_x] + prev, sig=True)
                    for c in range(8):
                        t_tr = S.add('pe', lambda e, c=c: e.transpose(out=pT[0][:, c * 128:(c + 1) * 128], in_=xb[:, c * 128:(c + 1) * 128],
                                                                      identity=identB[:, :]), deps=[t_xb] + prev, sig=(c == 7))
                    t_xT = S.add('dve', lambda e: e.tensor_copy(out=xT[:, :, :], in_=pT[0][:, :].rearrange("p (c t) -> p c t", c=8)), deps=[t_tr], sig=True)
                    t_mms = []
                    for bnk, (c0, c1) in enumerate(((0, 512), (512, 1024), (1024, 1032))):
                        for c in range(8):
                            t_mm = S.add('pe', lambda e, c=c, bnk=bnk, c0=c0, c1=c1: e.matmul(pST[bnk][:, 0:c1 - c0], lhsT=xT[:, c, :], rhs=Wq2[:, c, c0:c1],
                                                                                             start=(c == 0), stop=(c == 7)), deps=[t_xT, t_w] + prev, sig=(c == 7))
                        t_mms.append(t_mm)
                    outs = []
                    for bnk, dst in ((0, qs), (1, qi)):
                        t_a = S.add('dve', lambda e, bnk=bnk: e.tensor_tensor(out=v3(ra[:, :]), in0=v3(pST[bnk][:, :]), in1=bcast(tabs[:, 0:64], 8), op=ALU.mult),
                                    deps=[t_mms[bnk], t_tabs] + outs + prev, sig=True)
                        t_b1 = S.add('dve', lambda e, bnk=bnk: e.tensor_tensor(out=v3(rb[:, :])[:, :, 0:32], in0=v3(pST[bnk][:, :])[:, :, 32:64],
                                                                              in1=bcast(tabs[:, 64:96], 8), op=ALU.mult), deps=[t_mms[bnk], t_tabs] + outs + prev, sig=True)
                        t_b2 = S.add('dve', lambda e, bnk=bnk: e.tensor_tensor(out=v3(rb[:, :])[:, :, 32:64], in0=v3(pST[bnk][:, :])[:, :, 0:32],
                                                                              in1=bcast(tabs[:, 96:128], 8), op=ALU.mult), deps=[t_mms[bnk], t_tabs] + outs + prev, sig=True)
                        t_q = S.add('pool', lambda e, dst=dst: e.tensor_tensor(out=dst[:, :], in0=ra[:, :], in1=rb[:, :], op=ALU.add),
                                    deps=[t_a, t_b1, t_b2] + prev, sig=True)
                        outs = [t_q]
                        if bnk == 0:
                            t_qs = t_q
                        else:
                            t_qi = t_q
                    t_wv = S.add('dve', lambda e: e.tensor_scalar(out=wv[:, :], in0=pST[2][:, 0:8], scalar1=ss[:, 2:3], scalar2=8.0 ** -0.5,
                                                                  op0=ALU.mult, op1=ALU.mult), deps=[t_mms[2], t_rstd] + prev, sig=True)
                    pst_q = [t_b2, t_wv]
                    for a in range(4):
                        t_tr = S.add('pe', lambda e, a=a: e.transpose(out=pT[0][:, a * 128:(a + 1) * 128], in_=qs[:, a * 128:(a + 1) * 128],
                                                                      identity=identB[:, :]), deps=[t_qs, t_xT], sig=(a == 3))
                    for a in range(4):
                        t_tr2 = S.add('pe', lambda e, a=a: e.transpose(out=pT[1][:, a * 128:(a + 1) * 128], in_=qi[:, a * 128:(a + 1) * 128],
                                                                       identity=identB[:, :]), deps=[t_qi, pT_free[1]] + prev, sig=(a == 3))
                    pTv = lambda t: t[:, 0:512].rearrange("p (a t) -> p a t", a=4)
                    t_q1 = S.add('dve', lambda e: e.tensor_copy(out=qbd[0:64, :, 0:128], in_=pTv(pT[0])[0:64, :, :]), deps=[t_tr, t_z] + prev, sig=True)
                    t_q2 = S.add('dve', lambda e: e.tensor_copy(out=qbd[64:128, :, 128:256], in_=pTv(pT[0])[64:128, :, :]), deps=[t_tr, t_z] + prev, sig=True)
                    t_qiT = S.add('dve', lambda e: e.tensor_copy(out=qiT[:, :, :], in_=pTv(pT[1])), deps=[t_tr2] + prev, sig=True)
                    pT_free = [t_q2, t_qiT]
                    t_acc = None
                    tmp_free = [None] * 4
                    nch = (W + 511) // 512
                    cnt_ = 0
                    for ch in range(nch):
                        c0 = ch * 512
                        n = min(512, W - c0)
                        for h in range(8):
                            hp = (h % 2) * 64
                            bnk = h % 4
                            t_lg = S.add('pe', lambda e, hp=hp, h=h, bnk=bnk, c0=c0, n=n: e.matmul(
                                pST[bnk][:, 0:n], lhsT=qiT[hp:hp + 64, h // 2, :],
                                rhs=kiT2[hp:hp + 64, :, :].rearrange("p k t -> p (k t)")[:, c0:c0 + n], start=True, stop=True),
                                deps=[t_qiT, t_cst, pst_free[bnk]] + pst_q, sig=True)
                            sl = cnt_ % 4
                            cnt_ += 1
                            t_r = S.add('act', lambda e, bnk=bnk, sl=sl, n=n: e.activation(out=tmpR[sl][:, 0:n], in_=pST[bnk][:, 0:n], func=AF.Relu),
                                        deps=[t_lg, tmp_free[sl]], sig=True)
                            pst_free[bnk] = t_r
                            if h == 0:
                                t_acc = S.add('dve', lambda e, sl=sl, c0=c0, n=n: e.tensor_scalar(
                                    out=Isc[:, c0:c0 + n], in0=tmpR[sl][:, 0:n], scalar1=wv[:, 0:1], scalar2=None, op0=ALU.mult),
                                    deps=[t_r, t_wv, t_acc] + prev, sig=True)
                            else:
                                t_acc = S.add('dve', lambda e, sl=sl, c0=c0, n=n, h=h: e.scalar_tensor_tensor(
                                    out=Isc[:, c0:c0 + n], in0=tmpR[sl][:, 0:n], scalar=wv[:, h:h + 1], in1=Isc[:, c0:c0 + n],
                                    op0=ALU.mult, op1=ALU.add), deps=[t_r, t_wv, t_acc], sig=True)
                            tmp_free[sl] = t_acc
                    t_mx = S.add('dve', lambda e, W=W: e.tensor_reduce(out=bs[:, 1:2], in_=Isc[:, 0:W], axis=AX.X, op=ALU.max), deps=[t_acc] + prev, sig=True)
                    t_mn = S.add('dve', lambda e, W=W: e.tensor_reduce(out=bs[:, 0:1], in_=Isc[:, 0:W], axis=AX.X, op=ALU.min), deps=[t_acc] + prev, sig=True)
                    t_b = S.add('dve', lambda e: e.tensor_scalar(out=bs[:, 1:2], in0=bs[:, 1:2], scalar1=1.0, scalar2=None, op0=ALU.add), deps=[t_mx], sig=True)
                    t_b = S.add('dve', lambda e: e.tensor_scalar(out=bs[:, 0:1], in0=bs[:, 0:1], scalar1=-1.0, scalar2=None, op0=ALU.add), deps=[t_mn, t_b], sig=True)
                    for (mi, kb) in ((0, 0), (1, nk - 2), (2, nk - 1)):
                        t_b = S.add('dve', lambda e, mi=mi, kb=kb: e.tensor_tensor(out=Isc[:, kb * 128:(kb + 1) * 128], in0=Isc[:, kb * 128:(kb + 1) * 128],
                                                                                  in1=mka[:, mi, :], op=ALU.add), deps=[t_b, t_mn, t_mx, t_cst], sig=True)
                    W1 = (nk // 2) * 128
                    W2 = W - W1
                    t_b = S.add('dve', lambda e: e.tensor_tensor(out=bs[:, 7:8], in0=bs[:, 1:2], in1=bs[:, 0:1], op=ALU.subtract), deps=[t_b], sig=True)
                    t_b = S.add('dve', lambda e: e.tensor_scalar(out=hwt[:, 0:NIT], in0=pw2[:, 0:NIT], scalar1=bs[:, 7:8], scalar2=None, op0=ALU.mult),
                                deps=[t_b, t_pw2], sig=True)
                    t_b = S.add('dve', lambda e: e.tensor_tensor(out=bs[:, 2:3], in0=bs[:, 0:1], in1=hwt[:, 0:1], op=ALU.add), deps=[t_b], sig=True)
                    for it in range(NIT):
                        t_c1 = S.add('dve', lambda e, W1=W1: e.tensor_scalar(out=Msk[:, 0:W1], in0=Isc[:, 0:W1], scalar1=bs[:, 2:3], scalar2=0.0,
                                                                          op0=ALU.is_ge, op1=ALU.add, accum_out=bs[:, 3:4]), deps=[t_b] + prev, sig=True)
                        t_c2 = S.add('act', lambda e, W1=W1, W=W: e.activation(out=Msk[:, W1:W], in_=Isc[:, W1:W], func=AF.Sign, scale=-1.0,
                                                                             bias=bs[:, 2:3], accum_out=bs[:, 8:9]), deps=[t_b] + prev, sig=True)
                        t_b = S.add('dve', lambda e: e.scalar_tensor_tensor(out=bs[:, 9:10], in0=bs[:, 8:9], scalar=-0.5, in1=bs[:, 3:4],
                                                                            op0=ALU.mult, op1=ALU.add), deps=[t_c1, t_c2], sig=True)
                        t_b = S.add('dve', lambda e, W2=W2: e.tensor_scalar(out=bs[:, 4:5], in0=bs[:, 9:10], scalar1=float(TOPK) - 0.5 - W2 / 2.0,
                                                                          scalar2=None, op0=ALU.is_ge), deps=[t_b], sig=True)
                        t_b = S.add('dve', lambda e, it=it: e.scalar_tensor_tensor(out=bs[:, 0:1], in0=bs[:, 4:5], scalar=hwt[:, it:it + 1], in1=bs[:, 0:1],
                                                                                   op0=ALU.mult, op1=ALU.add), deps=[t_b], sig=True)
                        if it + 1 < NIT:
                            t_b = S.add('dve', lambda e, it=it: e.tensor_tensor(out=bs[:, 2:3], in0=bs[:, 0:1], in1=hwt[:, it + 1:it + 2], op=ALU.add),
                                        deps=[t_b], sig=True)
                    t_msk = S.add('dve', lambda e, W=W: e.tensor_scalar(out=Msk[:, 0:W], in0=Isc[:, 0:W], scalar1=bs[:, 0:1], scalar2=None, op0=ALU.is_ge),
                                  deps=[t_b], sig=True)
                    qready = [t_q1, t_q2, t_msk]
                    t_qk = {}
                    t_exp = {}
                    t_v = {}
                    t_pv = None

                    def emit_qk(kb):
                        for g in range(2):
                            bi = (kb % 2) * 2 + g
                            for j in range(2):
                                a = g * 2 + j
                                t = S.add('pe', lambda e, bi=bi, j=j, a=a, kb=kb: e.matmul(
                                    pST[bi][:, j * 256:(j + 1) * 256], lhsT=ksT[:, kb, a * 128:(a + 1) * 128], rhs=qbd[:, a, :],
                                    start=(j == 0), stop=(j == 1), skip_group_check=True), deps=qready + [pst_free[bi], t_cst], sig=(j == 1))
                            t_qk[(kb, g)] = t
                        t_qk[(kb, 'm')] = S.add('pe', lambda e, kb=kb: e.transpose(out=pT[kb % 2][:, 0:128], in_=Msk[:, kb * 128:(kb + 1) * 128],
                                                                                  identity=identB[:, :]), deps=[t_msk, pT_free[kb % 2]], sig=True)

                    def emit_v(kb):
                        nonlocal vcount
                        sl = vcount % 3
                        vcount += 1
                        t_v[kb] = (S.add('sp', lambda e, kb=kb, sl=sl: e.dma_start(out=vb[sl][:, :, :].rearrange("p h d -> p (h d)"), in_=s_vds[kb, :, :]),
                                         deps=[vb_free[sl]], dma=f'vb{sl}'), sl)

                    def emit_exp(kb):
                        for g in range(2):
                            bi = (kb % 2) * 2 + g
                            t = S.add('act', lambda e, bi=bi, kb=kb, g=g: e.activation(
                                out=PT[kb % 2][:, g * 512:(g + 1) * 512], in_=pST[bi][:, :], func=AF.Exp),
                                deps=[t_qk[(kb, g)], PT_free[kb % 2]], sig=True)
                            pst_free[bi] = t
                            t_exp[(kb, g)] = t
                        t = S.add('dve', lambda e, kb=kb: e.tensor_tensor(
                            out=PT[kb % 2][:, :].rearrange("p (h q) -> p h q", h=8), in0=PT[kb % 2][:, :].rearrange("p (h q) -> p h q", h=8),
                            in1=pT[kb % 2][:, 0:128].unsqueeze(1).broadcast_to([128, 8, 128]), op=ALU.mult),
                            deps=[t_exp[(kb, 0)], t_exp[(kb, 1)], t_qk[(kb, 'm')]], sig=True)
                        pT_free[kb % 2] = t
                        t_exp[kb] = t

                    def emit_pv(kb):
                        nonlocal t_pv
                        tv, sl = t_v[kb]
                        for h in range(8):
                            t_pv = S.add('pe', lambda e, h=h, kb=kb, sl=sl: e.matmul(
                                pO[h // 4][:, h % 4, :], lhsT=PT[kb % 2][:, h * 128:(h + 1) * 128], rhs=vb[sl][:, h, :],
                                start=(kb == 0 and h % 4 == 0), stop=(kb == nk - 1), skip_group_check=True),
                                deps=[t_exp[kb], tv, t_onorm], sig=(h == 7))
                        PT_free[kb % 2] = t_pv
                        vb_free[sl] = t_pv

                    emit_v(0)
                    emit_qk(0)
                    for kb in range(nk):
                        if kb + 1 < nk:
                            emit_v(kb + 1)
                            emit_qk(kb + 1)
                        emit_exp(kb)
                        emit_pv(kb)
                    so = i % 2
                    t_rd = S.add('dve', lambda e: e.reciprocal(out=rden[:, 0:4], in_=pO[0][:, :, 64]), deps=[t_pv], sig=True)
                    t_rd2 = S.add('dve', lambda e: e.reciprocal(out=rden[:, 4:8], in_=pO[1][:, :, 64]), deps=[t_pv], sig=True)
                    for g in range(2):
                        t_onorm = S.add('dve', lambda e, g=g, so=so: e.tensor_tensor(
                            out=ost[so][:, g * 256:(g + 1) * 256].rearrange("p (h d) -> p h d", h=4), in0=pO[g][:, :, 0:64],
                            in1=rden[:, g * 4:(g + 1) * 4].unsqueeze(2).broadcast_to([128, 4, 64]), op=ALU.mult),
                            deps=[t_rd, t_rd2, ost_free[so]], sig=True)
                    ost_free[so] = S.add('sp', lambda e, i=i, so=so: e.dma_start(out=s_odsa[i, :, :], in_=ost[so][:, :]), deps=[t_onorm], dma=f'ost{so}')
                    prev = [t_pv, t_onorm, t_msk, t_qiT, t_q2]
                S.final_wait()
                S.run(nc, "C", top)
        if 'D' in phases:
            with ExitStack() as pd_:
                S = Sched()
                xq = din("xq", [nqb * 128, D])
                w_o = din("w_o", [D, D])
                g_ffn = din("g_ffn", [128, 8])
                w_gate = din("w_gate", [D, DFF])
                w_up = din("w_up", [D, DFF])
                w_down = din("w_down", [DFF, D])
                g_fin = din("g_fin", [128, D])
                Wo = sb("WoD", [128, 8, D], BF16, pd_)
                Wg = sb("WgD", [128, 8, DFF], BF16, pd_)
                Wu = sb("WuD", [128, 8, DFF], BF16, pd_)
                Wd = sb("WdD", [128, NFC, D], BF16, pd_)
                gF = sb("gFD", [128, 8], F32, pd_)
                gfin = sb("gfinD", [128, D], F32, pd_)
                stg = [sb(f"stgD{i}", [128, 1024], F32, pd_) for i in range(2)]
                xs = sb("xsD", [128, D], F32, pd_)
                ob = sb("obD", [128, D], BF16, pd_)
                oT = sb("oTD", [128, 8, 128], BF16, pd_)
                h1 = sb("h1D", [128, D], F32, pd_)
                ub = sb("ubD", [128, D], BF16, pd_)
                uT = sb("uTD", [128, 8, 128], BF16, pd_)
                junk = sb("junkD", [128, D], BF16, pd_)
                ss = sb("ssD", [128, 8], F32, pd_)
                sil = [sb(f"silD{i}", [128, 512], F32, pd_) for i in range(2)]
                actT = sb("actTD", [128, NFC, 128], BF16, pd_)
                h2 = sb("h2D", [128, D], F32, pd_)
                ot = sb("otD", [128, D], F32, pd_)
                pT = ps("pTD", [128, 1024], BF16, pd_)
                pA = [ps(f"pAD{i}", [128, 512], F32, pd_) for i in range(2)]
                pG = [ps(f"pGD{i}", [128, 512], F32, pd_) for i in range(2)]
                pU = [ps(f"pUD{i}", [128, 512], F32, pd_) for i in range(2)]
                S.add('sp', lambda e: e.dma_start(out=gF[:, :], in_=g_ffn[:, :]), dma='cst')
                t_cst = S.add('sp', lambda e: e.dma_start(out=gfin[:, :], in_=g_fin[:, :]), dma='cst')
                stg_free = [None, None]
                t_w = []
                jobs = []
                for c in range(8):
                    jobs.append((Wo[:, c, :], w_o[c * 128:(c + 1) * 128, :], None, D))
                    for (Wt, wsrc) in ((Wg, w_gate), (Wu, w_up)):
                        for c0 in range(0, DFF, 1024):
                            n = min(1024, DFF - c0)
                            jobs.append((Wt[:, c, c0:c0 + n], wsrc[c * 128:(c + 1) * 128, c0:c0 + n], gF[:, c:c + 1], n))
                for f in range(NFC):
                    jobs.append((Wd[:, f, :], w_down[f * 128:(f + 1) * 128, :], None, D))
                for j, (dst, src, gs, n) in enumerate(jobs):
                    s = j % 2
                    td = S.add('sp', lambda e, s=s, src=src, n=n: e.dma_start(out=stg[s][:, 0:n], in_=src), deps=[stg_free[s]], dma=f'stg{s}')
                    eng = ('dve', 'pool')[s]
                    if gs is None:
                        stg_free[s] = S.add(eng, lambda e, s=s, dst=dst, n=n: e.tensor_copy(out=dst, in_=stg[s][:, 0:n]), deps=[td], sig=True)
                    else:
                        stg_free[s] = S.add(eng, lambda e, s=s, dst=dst, gs=gs, n=n: e.tensor_scalar(
                            out=dst, in0=stg[s][:, 0:n], scalar1=gs, scalar2=None, op0=ALU.mult), deps=[td, t_cst], sig=True)
                    t_w.append(stg_free[s])
                prev = []
                pG_free = [None, None]
                pU_free = [None, None]
                sil_free = [None, None]
                for i in range(nqb):
                    S.add('sp', lambda e, i=i: e.dma_start(out=xs[:, :], in_=xq[i * 128:(i + 1) * 128, :]), deps=prev, dma='ld')
                    S.add('sp', lambda e, i=i: e.dma_start(out=ob[:, 0:512], in_=s_omla[i, :, :]), deps=prev, dma='ld')
                    t_x = S.add('sp', lambda e, i=i: e.dma_start(out=ob[:, 512:1024], in_=s_odsa[i, :, :]), deps=prev, dma='ld')
                    for c in range(8):
                        t_tr = S.add('pe', lambda e, c=c: e.transpose(out=pT[:, c * 128:(c + 1) * 128], in_=ob[:, c * 128:(c + 1) * 128],
                                                                      identity=identB[:, :]), deps=[t_x] + prev, sig=(c == 7))
                    t_oT = S.add('dve', lambda e: e.tensor_copy(out=oT[:, :, :], in_=pT[:, :].rearrange("p (c t) -> p c t", c=8)), deps=[t_tr] + prev, sig=True)
                    for hf in range(2):
                        for c in range(8):
                            t_mm = S.add('pe', lambda e, c=c, hf=hf: e.matmul(pA[hf][:, :], lhsT=oT[:, c, :], rhs=Wo[:, c, hf * 512:(hf + 1) * 512],
                                                                              start=(c == 0), stop=(c == 7)), deps=[t_oT, t_w] + prev, sig=(c == 7))
                    for hf in range(2):
                        t_h1 = S.add('dve', lambda e, hf=hf: e.tensor_tensor(out=h1[:, hf * 512:(hf + 1) * 512], in0=pA[hf][:, :],
                                                                             in1=xs[:, hf * 512:(hf + 1) * 512], op=ALU.add), deps=[t_mm, t_x] + prev, sig=True)
                    t_ss = S.add('act', lambda e: e.activation(out=junk[:, :], in_=h1[:, :], func=AF.Square, accum_out=ss[:, 0:1]), deps=[t_h1] + prev, sig=True)
                    t_sd = S.add('act', lambda e: e.activation(out=ss[:, 1:2], in_=ss[:, 0:1], func=AF.Sqrt, scale=1.0 / D, bias=epsT[:, 0:1]), deps=[t_ss], sig=True)
                    t_r = S.add('dve', lambda e: e.reciprocal(out=ss[:, 2:3], in_=ss[:, 1:2]), deps=[t_sd], sig=True)
                    t_ub = S.add('dve', lambda e: e.tensor_scalar(out=ub[:, :], in0=h1[:, :], scalar1=ss[:, 2:3], scalar2=None, op0=ALU.mult), deps=[t_r] + prev, sig=True)
                    for c in range(8):
                        t_tr = S.add('pe', lambda e, c=c: e.transpose(out=pT[:, c * 128:(c + 1) * 128], in_=ub[:, c * 128:(c + 1) * 128],
                                                                      identity=identB[:, :]), deps=[t_ub, t_oT], sig=(c == 7))
                    t_uT = S.add('dve', lambda e: e.tensor_copy(out=uT[:, :, :], in_=pT[:, :].rearrange("p (c t) -> p c t", c=8)), deps=[t_tr] + prev, sig=True)
                    t_act = None
                    ngrp = (NFC + 3) // 4
                    for gi in range(ngrp):
                        sl = gi % 2
                        nf = min(4, NFC - gi * 4)
                        for (pX, Wt, fr) in ((pG, Wg, pG_free), (pU, Wu, pU_free)):
                            for j in range(nf):
                                f = gi * 4 + j
                                for c in range(8):
                                    t_mm = S.add('pe', lambda e, pX=pX, Wt=Wt, sl=sl, j=j, f=f, c=c: e.matmul(
                                        pX[sl][:, j * 128:(j + 1) * 128], lhsT=Wt[:, c, f * 128:(f + 1) * 128], rhs=uT[:, c, :],
                                        start=(c == 0), stop=(c == 7)), deps=[t_uT, t_w, fr[sl]], sig=(c == 7 and j == nf - 1))
                            if pX is pG:
                                t_g = t_mm
                            else:
                                t_u = t_mm
                        t_sil = S.add('act', lambda e, sl=sl, nf=nf: e.activation(out=sil[sl][:, 0:nf * 128], in_=pG[sl][:, 0:nf * 128], func=AF.Silu),
                                      deps=[t_g, sil_free[sl]], sig=True)
                        pG_free[sl] = t_sil
                        t_act = S.add('dve', lambda e, sl=sl, nf=nf, gi=gi: e.tensor_tensor(
                            out=actT[:, gi * 4:gi * 4 + nf, :], in0=pU[sl][:, 0:nf * 128].rearrange("p (f t) -> p f t", f=nf),
                            in1=sil[sl][:, 0:nf * 128].rearrange("p (f t) -> p f t", f=nf), op=ALU.mult), deps=[t_sil, t_u] + prev, sig=True)
                        pU_free[sl] = t_act
                        sil_free[sl] = t_act
                    for hf in range(2):
                        for f in range(NFC):
                            t_mm = S.add('pe', lambda e, f=f, hf=hf: e.matmul(pA[hf][:, :], lhsT=actT[:, f, :], rhs=Wd[:, f, hf * 512:(hf + 1) * 512],
                                                                              start=(f == 0), stop=(f == NFC - 1)), deps=[t_act, t_h1], sig=(f == NFC - 1))
                    for hf in range(2):
                        t_h2 = S.add('dve', lambda e, hf=hf: e.tensor_tensor(out=h2[:, hf * 512:(hf + 1) * 512], in0=pA[hf][:, :],
                                                                             in1=h1[:, hf * 512:(hf + 1) * 512], op=ALU.add), deps=[t_mm] + prev, sig=True)
                    t_ss = S.add('act', lambda e: e.activation(out=junk[:, :], in_=h2[:, :], func=AF.Square, accum_out=ss[:, 4:5]), deps=[t_h2], sig=True)
                    t_sd = S.add('act', lambda e: e.activation(out=ss[:, 5:6], in_=ss[:, 4:5], func=AF.Sqrt, scale=1.0 / D, bias=epsT[:, 0:1]), deps=[t_ss], sig=True)
                    t_r = S.add('dve', lambda e: e.reciprocal(out=ss[:, 6:7], in_=ss[:, 5:6]), deps=[t_sd], sig=True)
                    t_o = S.add('dve', lambda e: e.scalar_tensor_tensor(out=ot[:, :], in0=h2[:, :], scalar=ss[:, 6:7], in1=gfin[:, :], op0=ALU.mult, op1=ALU.mult),
                                deps=[t_r, t_cst] + prev, sig=True)
                    t_st = S.add('sp', lambda e, i=i: e.dma_start(out=out[i * 128:(i + 1) * 128, :], in_=ot[:, :]), deps=[t_o], dma='st')
                    prev = [t_st, t_o, t_mm, t_h2, t_ss]
                S.final_wait()
                S.run(nc, "D", top)
    global LAST_INPUTS
    LAST_INPUTS = list(_din.keys())
    return nc, dbg


def bcast(ap, h):
    n = ap.shape[-1]
    return ap.unsqueeze(1).broadcast_to([ap.shape[0], h, n])


BF = ml_dtypes.bfloat16
def rope_tab(pos):
    pos = pos.astype(np.float32)
    out = np.zeros((pos.shape[0], 192), np.float32)
    for (half, off) in ((32, 0), (16, 128)):
        inv = np.power(np.float32(10000.0), -np.arange(half, dtype=np.float32) / np.float32(half)).astype(np.float32)
        ang = (pos[:, None] * inv[None, :]).astype(np.float32)
        c = np.cos(ang).astype(np.float32); s = np.sin(ang).astype(np.float32)
        d = 2 * half
        out[:, off:off + d] = np.concatenate([c, c], 1)
        out[:, off + d:off + 2 * d] = np.concatenate([-s, s], 1)
    return out
def core_inputs(inp, core):
    f32 = np.float32
    b, par = core // 2, core % 2
    x = np.asarray(inp['x'][b], f32)
    xk = np.zeros((65 * 128, 1024), f32); xk[0:16] = inp['meta_tokens']; xk[128:] = x
    xq = np.ascontiguousarray(x.reshape(64, 128, 1024)[par::2].reshape(32 * 128, 1024))
    w_in = np.asarray(inp['w_in'][0], f32)
    c_q, c_kv, k_r, q_s, k_s, v_s, q_i, k_i, w_i = np.split(w_in, np.cumsum([256,128,32,512,512,512,512,64,8])[:-1], axis=1)
    def pc(g, n):
        return np.ascontiguousarray(np.asarray(g, f32).reshape(n, 128).T)
    w_uq = np.asarray(inp['w_uq'][0], f32).reshape(256, 8, 96)
    w_ukv = np.asarray(inp['w_ukv'][0], f32).reshape(128, 8, 128)
    posK = np.concatenate([np.arange(128), 16 + np.arange(64 * 128)])
    ropeK = rope_tab(posK).reshape(65, 128, 192)
    g_idx = 2 * np.arange(32) + par
    posQ = (16 + 128 * g_idx[:, None] + np.arange(128)[None, :]).reshape(-1)
    ropeQ = rope_tab(posQ).reshape(32, 128, 192)
    tri = (np.arange(128)[:, None] <= np.arange(128)[None, :]).astype(f32)
    ones = np.ones((128, 128), f32); zeros = np.zeros((128, 128), f32)
    mk_mult = np.stack([tri, zeros] if par == 0 else [ones, tri]).astype(BF)
    NEG = np.float32(-1e30)
    meta_add = np.zeros((128, 128), f32); meta_add[:, 16:] = NEG
    triA = np.where(tri.T > 0, 0, NEG).astype(f32)
    allneg = np.full((128, 128), NEG, f32)
    mk_add = np.stack([meta_add, triA, allneg] if par == 0 else [meta_add, zeros, triA]).astype(f32)
    vcol0 = np.zeros((128, 8), f32); vcol0[0:16] = 1
    return {
        'xk': xk, 'xq': xq,
        'wk_in': np.ascontiguousarray(np.concatenate([k_s, v_s, c_kv, k_r, k_i], 1)),
        'wq_in': np.ascontiguousarray(np.concatenate([c_q, q_s, q_i, w_i], 1)),
        'g_attn': pc(inp['attn_norm_g'][0], 8),
        'w_uq_n': np.ascontiguousarray(w_uq[:, :, :64].reshape(256, 512)),
        'w_uq_r': np.ascontiguousarray(w_uq[:, :, 64:].reshape(256, 256)),
        'g_q': pc(inp['mla_q_norm_g'][0], 2),
        'w_ukv_k': np.ascontiguousarray(w_ukv[:, :, :64].reshape(128, 512)),
        'w_ukv_v': np.ascontiguousarray(w_ukv[:, :, 64:].reshape(128, 512)),
        'g_kv': pc(inp['mla_kv_norm_g'][0], 1),
        'w_o': np.asarray(inp['w_o'][0], f32),
        'g_ffn': pc(inp['ffn_norm_g'][0], 8),
        'w_gate': np.asarray(inp['w_gate'][0], f32), 'w_up': np.asarray(inp['w_up'][0], f32),
        'w_down': np.asarray(inp['w_down'][0], f32),
        'g_fin': np.ascontiguousarray(np.broadcast_to(np.asarray(inp['final_norm_g'], f32)[None, :], (128, 1024))),
        'ropeK': ropeK, 'ropeQ': ropeQ,
        'ident_f': np.eye(128, dtype=f32), 'ident_b': np.eye(128, dtype=f32).astype(BF),
        'vcol0': vcol0.astype(BF), 'mk_mult': mk_mult, 'mk_add': mk_add,
    }


def kernel(**inputs):
    inp = {k: np.asarray(v) for k, v in inputs.items()}
    nc, _ = build()
    names = set(LAST_INPUTS)
    in_maps = []
    for core in range(8):
        ci = core_inputs(inp, core)
        in_maps.append({k: np.ascontiguousarray(v) for k, v in ci.items() if k in names})
    res = run_bass_kernel_spmd(nc, in_maps, core_ids=list(range(8)))
    out = np.zeros((4, 64, 128, 1024), np.float32)
    for core in range(8):
        b, par = core // 2, core % 2
        out[b, par::2] = np.asarray(res.results[core]["out"], np.float32).reshape(32, 128, 1024)
    return out.reshape(4, 8192, 1024)
```

```python
import numpy as np
import ml_dtypes
from contextlib import ExitStack
import concourse.bass as bass
import concourse.mybir as mybir
from concourse.bass_utils import run_bass_kernel_spmd

F32 = mybir.dt.float32
BF16 = mybir.dt.bfloat16
AF = mybir.ActivationFunctionType
ALU = mybir.AluOpType
AX = mybir.AxisListType

D = 1024
NQB = 32
NKB = 65
EPS = 1e-6
DFF = 2816
NFC = DFF // 128
TOPK = 256
NEG = -1.0e30
LAST_INPUTS = []
import os
CUT = float(os.environ.get('KCUT', '99'))


class Sched:
    ENG = ('pe', 'act', 'dve', 'pool', 'sp')

    def __init__(self):
        self.q = {e: [] for e in self.ENG}
        self.cnt = {}

    def add(self, eng, fn, deps=(), sig=False, dma=None):
        tok = None
        inc = None
        if dma is not None:
            self.cnt[dma] = self.cnt.get(dma, 0) + 16
            inc = (dma, 16)
            tok = (dma, self.cnt[dma])
        elif sig:
            name = 'c_' + eng
            self.cnt[name] = self.cnt.get(name, 0) + 1
            inc = (name, 1)
            tok = (name, self.cnt[name])
        dl = []
        for d in deps:
            if d is None:
                continue
            if isinstance(d, list):
                dl.extend(x for x in d if x is not None)
            else:
                dl.append(d)
        self.q[eng].append((fn, dl, inc))
        return tok

    def final_wait(self, eng='sp'):
        deps = [(n, v) for n, v in self.cnt.items()]
        self.q[eng].append((lambda e: e.nop(), deps, None))

    def run(self, nc, name, semstack=None):
        with ExitStack() as es:
            sems = {n: (semstack or es).enter_context(nc.semaphore(name + '_' + n)) for n in self.cnt}
            block = es.enter_context(nc.Block())

            def mk(engname):
                def body(eng):
                    waited = {}
                    for fn, deps, inc in self.q[engname]:
                        for (s, v) in deps:
                            if engname == 'pe' and s == 'c_pe':
                                continue
                            if waited.get(s, 0) < v:
                                eng.wait_ge(sems[s], v)
                                waited[s] = v
                        inst = fn(eng)
                        if inc is not None:
                            inst.then_inc(sems[inc[0]], inc[1])
                return body
            block.tensor(mk('pe'))
            block.scalar(mk('act'))
            block.vector(mk('dve'))
            block.gpsimd(mk('pool'))
            block.sync(mk('sp'))


def _bc(ap, shape_mid):
    return ap


def build(nkb=NKB, nqb=NQB, debug=False, phases='ABCD'):
    nc = bass.Bass("TRN2", target_bir_lowering=False)
    T = nkb * 128

    _din = {}

    def din(name, shape, dt=F32):
        if name not in _din:
            _din[name] = nc.dram_tensor(name, list(shape), dt, kind="ExternalInput").ap()
        return _din[name]

    def dscr(name, shape, dt):
        return nc.dram_tensor(name, list(shape), dt).ap()

    out = nc.dram_tensor("out", [NQB * 128, D], F32, kind="ExternalOutput").ap()

    s_ksT = dscr("s_ksT", [NKB, 128, 512], BF16)
    s_vds = dscr("s_vds", [NKB, 128, 520], BF16)
    s_kiT = dscr("s_kiT", [NKB, 64, 128], BF16)
    s_h1 = dscr("s_h1", [NQB * 128, D], F32)
    s_omla = dscr("s_omla", [NQB, 128, 512], BF16)
    s_odsa = dscr("s_odsa", [NQB, 128, 512], BF16)
    dbg = {}
    if debug:
        def dout(name, shape, dt=F32):
            t = nc.dram_tensor(name, list(shape), dt, kind="ExternalOutput").ap()
            dbg[name] = t
            return t
        d_knT = dout("d_knT", [128, 4 * T], BF16)
        d_krT = dout("d_krT", [32, T], BF16)
        d_vm = dout("d_vm", [128, nkb * 520], BF16)
        d_ksT = dout("d_ksT", [NKB, 128, 512], BF16)
        d_vds = dout("d_vds", [NKB, 128, 520], BF16)
        d_kiT = dout("d_kiT", [NKB, 64, 128], BF16)
        d_omla = dout("d_omla", [128, nqb * 512], BF16)
        d_h1 = dout("d_h1", [NQB * 128, D], F32)

    with ExitStack() as top:
        def sb(name, shape, dt, es=top):
            return es.enter_context(nc.sbuf_tensor(name, list(shape), dt))

        def ps(name, shape, dt, es=top):
            return es.enter_context(nc.psum_tensor(name, list(shape), dt))

        identF = sb("identF", [128, 128], F32)
        identB = sb("identB", [128, 128], BF16)
        epsT = sb("epsT", [128, 1], F32)
        abst = ExitStack()
        knT = sb("knT", [128, 4, T], BF16, abst)
        krT = sb("krT", [32, T], BF16, abst)
        vm = sb("vm", [128, nkb, 8, 65], BF16, abst)

        if 'A' in phases:
            with ExitStack() as pa:
                S = Sched()
                xk = din("xk", [nkb * 128, D])
                wk_in = din("wk_in", [D, 1248])
                g_attn = din("g_attn", [128, 8])
                w_ukv_k = din("w_ukv_k", [128, 512])
                w_ukv_v = din("w_ukv_v", [128, 512])
                g_kv = din("g_kv", [128, 1])
                ropeK = din("ropeK", [nkb, 128, 192])
                ident_f = din("ident_f", [128, 128])
                ident_b = din("ident_b", [128, 128], BF16)
                vcol0 = din("vcol0", [128, 8], BF16)
                Wk = sb("Wk", [128, 8, 1248], BF16, pa)
                WukK = sb("WukK", [128, 512], BF16, pa)
                WukV = sb("WukV", [128, 512], BF16, pa)
                gA = sb("gA", [128, 8], F32, pa)
                gKV = sb("gKV", [128, 1], F32, pa)
                stg = [sb("stgA0", [128, 1248], F32, pa)] * 2
                xs = [sb(f"xsA{i}", [128, D], F32, pa) for i in range(2)]
                tab = [sb(f"tabA{i}", [128, 192], F32, pa) for i in range(2)]
                tabs = [sb(f"tabsA{i}", [128, 192], F32, pa) for i in range(2)]
                junk = sb("junkA", [128, D], BF16, pa)
                ss = [sb(f"ssA{i}", [128, 4], F32, pa) for i in range(2)]
                ss2 = [sb(f"ss2A{i}", [128, 4], F32, pa) for i in range(2)]
                xT = [sb(f"xTA{i}", [128, 8, 128], BF16, pa) for i in range(2)]
                ra = sb("ra", [128, 512], F32, pa)
                rb = sb("rb", [128, 512], F32, pa)
                ksr = sb("ksr", [128, 512], BF16, pa)
                ri_a = sb("ri_a", [128, 64], F32, pa)
                ri_b = sb("ri_b", [128, 64], F32, pa)
                kir = sb("kir", [128, 64], BF16, pa)
                rr_a = sb("rr_a", [128, 32], F32, pa)
                rr_b = sb("rr_b", [128, 32], F32, pa)
                krr = sb("krr", [128, 32], BF16, pa)
                ckv = sb("ckv", [128, 128], F32, pa)
                junk2 = sb("junk2A", [128, 128], BF16, pa)
                ckvn = sb("ckvn", [128, 128], BF16, pa)
                ckvnT = sb("ckvnT", [128, 128], BF16, pa)
                vst = [sb(f"vstA{i}", [128, 8, 65], BF16, pa) for i in range(2)]
                kst = [sb(f"kstA{i}", [128, 512], BF16, pa) for i in range(2)]
                kit = [sb(f"kitA{i}", [64, 128], BF16, pa) for i in range(2)]
                pT = [ps(f"pTA{i}", [128, 512], F32, pa) for i in range(2)]
                pP = [ps(f"pPA{i}", [128, 512], F32, pa) for i in range(3)]
                pS = ps("pSA", [128, 1024], BF16, pa)
                pK = ps("pKA", [128, 512], F32, pa)
                pV = ps("pVA", [128, 512], F32, pa)

                S.add('sp', lambda e: e.dma_start(out=identF[:, :], in_=ident_f[:, :]), dma='cst')
                S.add('sp', lambda e: e.dma_start(out=identB[:, :], in_=ident_b[:, :]), dma='cst')
                S.add('sp', lambda e: e.dma_start(out=gA[:, :], in_=g_attn[:, :]), dma='cst')
                S.add('sp', lambda e: e.dma_start(out=gKV[:, :], in_=g_kv[:, :]), dma='cst')
                t_eps = S.add('dve', lambda e: e.memset(epsT[:, :], EPS), sig=True)
                t_vm1 = S.add('pool', lambda e: e.memset(vm[:, :, :, 64:65], 1.0), sig=True)
                t_vst1 = [None, S.add('pool', lambda e: e.memset(vst[1][:, :, 64:65], 1.0), sig=True)]
                S.add('sp', lambda e: e.dma_start(out=vst[0][:, :, 64:65], in_=vcol0[:, :].unsqueeze(2), allow_slow_non_contiguous=True), dma='cst')
                t_cst = S.add('sp', lambda e: e.dma_start(out=vm[:, 0, :, 64:65], in_=vcol0[:, :].unsqueeze(2), allow_slow_non_contiguous=True), deps=[t_vm1], dma='cst')
                t_id = t_cst
                t_vc0 = t_cst
                t_vst1[0] = t_cst
                stg_free = [None, None]
                t_w = []
                for c in range(8):
                    s = 0
                    td = S.add('sp', lambda e, c=c, s=s: e.dma_start(out=stg[s][:, :], in_=wk_in[c * 128:(c + 1) * 128, :]),
                               deps=[stg_free[s]], dma=f'stg{s}')
                    eng = 'dve' if s == 0 else 'pool'
                    stg_free[s] = S.add(eng, lambda e, c=c, s=s: e.tensor_scalar(
                        out=Wk[:, c, :], in0=stg[s][:, :], scalar1=gA[:, c:c + 1], scalar2=None, op0=ALU.mult),
                        deps=[td, t_cst], sig=True)
                    t_w.append(stg_free[s])
                for (dst, src, s) in ((WukK, w_ukv_k, 0), (WukV, w_ukv_v, 0)):
                    td = S.add('sp', lambda e, s=s, src=src: e.dma_start(out=stg[s][:, 0:512], in_=src[:, :]),
                               deps=[stg_free[s]], dma=f'stg{s}')
                    eng = 'dve' if s == 0 else 'pool'
                    stg_free[s] = S.add(eng, lambda e, s=s, dst=dst: e.tensor_scalar(
                        out=dst[:, :], in0=stg[s][:, 0:512], scalar1=gKV[:, 0:1], scalar2=None, op0=ALU.mult),
                        deps=[td, t_cst], sig=True)
                    t_w.append(stg_free[s])

                xs_free = [None, None]
                tab_free = [None, None]
                xT_free = [None, None]
                pT_free = [None, None]
                pP_free = [[None], [None], [None]]
                pS_free = [None]
                pK_free = None
                pV_free = None
                vst_free = [None, None]
                kst_free = [None, None]
                kit_free = [None, None]
                ss_free = [None, None]
                tmp_free = {}
                ckvnT_free = None
                for k in range(nkb):
                    s = k % 2
                    t_x = S.add('sp', lambda e, k=k, s=s: e.dma_start(out=xs[s][:, :], in_=xk[k * 128:(k + 1) * 128, :]),
                                deps=[xs_free[s], tab_free[s]], dma=f'ld{s}')
                    t_tab = S.add('sp', lambda e, k=k, s=s: e.dma_start(out=tab[s][:, :], in_=ropeK[k, :, :]),
                                  deps=[tab_free[s]], dma=f'ld{s}')
                    t_x = t_tab
                    t_ss = S.add('act', lambda e, s=s: e.activation(out=junk[:, :], in_=xs[s][:, :], func=AF.Square,
                                                                   accum_out=ss[s][:, 0:1]),
                                 deps=[t_x, ss_free[s]], sig=True)
                    t_sd = S.add('act', lambda e, s=s: e.activation(out=ss[s][:, 1:2], in_=ss[s][:, 0:1], func=AF.Sqrt,
                                                                   scale=1.0 / D, bias=epsT[:, 0:1]),
                                 deps=[t_ss, t_eps], sig=True)
                    t_rstd = S.add('dve', lambda e, s=s: e.reciprocal(out=ss[s][:, 2:3], in_=ss[s][:, 1:2]),
                                   deps=[t_sd], sig=True)
                    rstd = ss[s][:, 2:3]
                    if CUT <= 1:
                        continue
                    t_tr = []
                    for hlf in range(2):
                        for j in range(4):
                            c = hlf * 4 + j
                            tt = S.add('pe', lambda e, s=s, c=c, hlf=hlf, j=j: e.transpose(
                                out=pT[hlf][:, j * 128:(j + 1) * 128], in_=xs[s][:, c * 128:(c + 1) * 128],
                                identity=identF[:, :]),
                                deps=[t_x, t_id, pT_free[hlf]], sig=(j == 3))
                        t_tr.append(tt)
                    te0 = S.add('act', lambda e, s=s: e.activation(out=xT[s][:, 0:4, :], in_=pT[0][:, :], func=AF.Copy),
                                deps=[t_tr[0], xT_free[s]], sig=True)
                    te1 = S.add('dve', lambda e, s=s: e.tensor_copy(out=xT[s][:, 4:8, :], in_=pT[1][:, :]),
                                deps=[t_tr[1], xT_free[s]], sig=True)
                    pT_free = [te0, te1]
                    xs_free[s] = [te0, te1, t_ss]
                    if CUT <= 2:
                        continue
                    t_mm = []
                    for bnk, (c0, c1) in enumerate(((0, 512), (512, 1024), (1024, 1248))):
                        for c in range(8):
                            tt = S.add('pe', lambda e, s=s, c=c, bnk=bnk, c0=c0, c1=c1: e.matmul(
                                pP[bnk][:, 0:c1 - c0], lhsT=xT[s][:, c, :], rhs=Wk[:, c, c0:c1],
                                start=(c == 0), stop=(c == 7)),
                                deps=[te0, te1, pP_free[bnk], t_w], sig=(c == 7))
                        t_mm.append(tt)
                    xT_free[s] = t_mm[2]
                    if CUT <= 3:
                        continue
                    t_tabs = S.add('dve', lambda e, s=s: e.tensor_scalar(
                        out=tabs[s][:, :], in0=tab[s][:, :], scalar1=ss[s][:, 2:3], scalar2=None, op0=ALU.mult),
                        deps=[t_tab, t_rstd, tab_free[s]], sig=True)
                    t_a = S.add('dve', lambda e, s=s: e.tensor_tensor(
                        out=ra[:, :].rearrange("p (h d) -> p h d", h=8), in0=pP[0][:, :].rearrange("p (h d) -> p h d", h=8),
                        in1=bcast(tabs[s][:, 0:64], 8), op=ALU.mult),
                        deps=[t_mm[0], t_tabs, tmp_free.get('ra')], sig=True)
                    t_b1 = S.add('dve', lambda e, s=s: e.tensor_tensor(
                        out=rb[:, :].rearrange("p (h d) -> p h d", h=8)[:, :, 0:32],
                        in0=pP[0][:, :].rearrange("p (h d) -> p h d", h=8)[:, :, 32:64],
                        in1=bcast(tabs[s][:, 64:96], 8), op=ALU.mult),
                        deps=[t_mm[0], t_tabs, tmp_free.get('ra')], sig=True)
                    t_b2 = S.add('dve', lambda e, s=s: e.tensor_tensor(
                        out=rb[:, :].rearrange("p (h d) -> p h d", h=8)[:, :, 32:64],
                        in0=pP[0][:, :].rearrange("p (h d) -> p h d", h=8)[:, :, 0:32],
                        in1=bcast(tabs[s][:, 96:128], 8), op=ALU.mult),
                        deps=[t_mm[0], t_tabs, tmp_free.get('ra')], sig=True)
                    pP_free[0] = [t_a, t_b1, t_b2]
                    t_ksr = S.add('pool', lambda e: e.tensor_tensor(out=ksr[:, :], in0=ra[:, :], in1=rb[:, :], op=ALU.add),
                                  deps=[t_a, t_b1, t_b2, tmp_free.get('ksr')], sig=True)
                    tmp_free['ra'] = t_ksr
                    if CUT <= 4:
                        continue
                    t_v = S.add('act', lambda e, s=s: e.activation(
                        out=vst[s][:, :, 0:64], in_=pP[1][:, :].rearrange("p (h d) -> p h d", h=8), func=AF.Copy,
                        scale=ss[s][:, 2:3]),
                        deps=[t_mm[1], t_rstd, vst_free[s], t_vst1[s]], sig=True)
                    pP_free[1] = [t_v]
                    t_vd = S.add('sp', lambda e, s=s, k=k: e.dma_start(
                        out=s_vds[k, :, :], in_=vst[s][:, :, :].rearrange("p h d -> p (h d)")),
                        deps=[t_v], dma=f'st{s}')
                    if CUT <= 5:
                        continue
                    t_ckv = S.add('dve', lambda e, s=s: e.tensor_scalar(out=ckv[:, :], in0=pP[2][:, 0:128], scalar1=ss[s][:, 2:3],
                                                                       scalar2=None, op0=ALU.mult),
                                  deps=[t_mm[2], t_rstd, tmp_free.get('ckv')], sig=True)
                    t_ss2 = S.add('act', lambda e, s=s: e.activation(out=junk2[:, :], in_=ckv[:, :], func=AF.Square,
                                                                    accum_out=ss2[s][:, 0:1]),
                                  deps=[t_ckv, tmp_free.get('ss2%d' % s)], sig=True)
                    t_sd2 = S.add('act', lambda e, s=s: e.activation(out=ss2[s][:, 1:2], in_=ss2[s][:, 0:1], func=AF.Sqrt,
                                                                    scale=1.0 / 128, bias=epsT[:, 0:1]),
                                  deps=[t_ss2], sig=True)
                    t_r2 = S.add('dve', lambda e, s=s: e.reciprocal(out=ss2[s][:, 2:3], in_=ss2[s][:, 1:2]),
                                 deps=[t_sd2], sig=True)
                    t_ckvn = S.add('dve', lambda e, s=s: e.tensor_scalar(
                        out=ckvn[:, :], in0=ckv[:, :], scalar1=ss2[s][:, 2:3], scalar2=None, op0=ALU.mult),
                        deps=[t_r2, t_ckv, tmp_free.get('ckvn')], sig=True)
                    tmp_free['ckv'] = [t_ckvn, t_ss2]
                    tmp_free['ss2%d' % s] = t_ckvn
                    if CUT <= 6:
                        continue
                    t_ra = S.add('dve', lambda e, s=s: e.tensor_tensor(
                        out=rr_a[:, :], in0=pP[2][:, 128:160], in1=tabs[s][:, 128:160], op=ALU.mult),
                        deps=[t_mm[2], t_tabs, tmp_free.get('rr')], sig=True)
                    t_rb1 = S.add('dve', lambda e, s=s: e.tensor_tensor(
                        out=rr_b[:, 0:16], in0=pP[2][:, 144:160], in1=tabs[s][:, 160:176], op=ALU.mult),
                        deps=[t_mm[2], t_tabs, tmp_free.get('rr')], sig=True)
                    t_rb2 = S.add('dve', lambda e, s=s: e.tensor_tensor(
                        out=rr_b[:, 16:32], in0=pP[2][:, 128:144], in1=tabs[s][:, 176:192], op=ALU.mult),
                        deps=[t_mm[2], t_tabs, tmp_free.get('rr')], sig=True)
                    t_krr = S.add('pool', lambda e: e.tensor_tensor(out=krr[:, :], in0=rr_a[:, :], in1=rr_b[:, :], op=ALU.add),
                                  deps=[t_ra, t_rb1, t_rb2, tmp_free.get('krr')], sig=True)
                    tmp_free['rr'] = t_krr
                    t_ia = S.add('dve', lambda e, s=s: e.tensor_tensor(
                        out=ri_a[:, :], in0=pP[2][:, 160:224], in1=tabs[s][:, 0:64], op=ALU.mult),
                        deps=[t_mm[2], t_tabs, tmp_free.get('ri')], sig=True)
                    t_ib1 = S.add('dve', lambda e, s=s: e.tensor_tensor(
                        out=ri_b[:, 0:32], in0=pP[2][:, 192:224], in1=tabs[s][:, 64:96], op=ALU.mult),
                        deps=[t_mm[2], t_tabs, tmp_free.get('ri')], sig=True)
                    t_ib2 = S.add('dve', lambda e, s=s: e.tensor_tensor(
                        out=ri_b[:, 32:64], in0=pP[2][:, 160:192], in1=tabs[s][:, 96:128], op=ALU.mult),
                        deps=[t_mm[2], t_tabs, tmp_free.get('ri')], sig=True)
                    t_kir = S.add('pool', lambda e: e.tensor_tensor(out=kir[:, :], in0=ri_a[:, :], in1=ri_b[:, :], op=ALU.add),
                                  deps=[t_ia, t_ib1, t_ib2, tmp_free.get('kir')], sig=True)
                    tmp_free['ri'] = t_kir
                    pP_free[2] = [t_ckv, t_ra, t_rb1, t_rb2, t_ia, t_ib1, t_ib2]
                    tab_free[s] = [t_a, t_b1, t_b2, t_ra, t_rb1, t_rb2, t_ia, t_ib1, t_ib2]
                    ss_free[s] = [t_tabs, t_v, t_ckv]
                    if CUT <= 7:
                        continue
                    for a in range(4):
                        t_t1 = S.add('pe', lambda e, a=a: e.transpose(
                            out=pS[:, a * 128:(a + 1) * 128], in_=ksr[:, a * 128:(a + 1) * 128], identity=identB[:, :]),
                            deps=[t_ksr, pS_free], sig=(a == 3))
                    if CUT <= 7.1:
                        continue
                    t_t2 = S.add('pe', lambda e: e.transpose(out=pS[0:64, 512:640], in_=kir[:, :], identity=identB[:, :]),
                                 deps=[t_kir, pS_free], sig=True)
                    if CUT <= 7.2:
                        continue
                    t_t3 = S.add('pe', lambda e: e.transpose(out=pS[0:32, 640:768], in_=krr[:, :], identity=identB[:, :]),
                                 deps=[t_krr, pS_free], sig=True)
                    if CUT <= 7.3:
                        continue
                    t_t4 = S.add('pe', lambda e: e.transpose(out=pS[:, 768:896], in_=ckvn[:, :], identity=identB[:, :]),
                                 deps=[t_ckvn, pS_free], sig=True)
                    tmp_free['ksr'] = t_t1
                    tmp_free['kir'] = t_t2
                    tmp_free['krr'] = t_t3
                    tmp_free['ckvn'] = t_t4
                    if CUT <= 8:
                        continue
                    t_e1 = S.add('dve', lambda e, s=s: e.tensor_copy(out=kst[s][:, :], in_=pS[:, 0:512]),
                                 deps=[t_t4, kst_free[s]], sig=True)
                    if CUT <= 8.1:
                        continue
                    t_d1 = S.add('sp', lambda e, s=s, k=k: e.dma_start(out=s_ksT[k, :, :], in_=kst[s][:, :]),
                                 deps=[t_e1], dma=f'st{s}')
                    if CUT <= 8.2:
                        continue
                    t_e2 = S.add('dve', lambda e, s=s: e.tensor_copy(out=kit[s][:, :], in_=pS[0:64, 512:640]),
                                 deps=[t_t4, kit_free[s]], sig=True)
                    if CUT <= 8.3:
                        continue
                    t_d2 = S.add('sp', lambda e, s=s, k=k: e.dma_start(out=s_kiT[k, :, :], in_=kit[s][:, :]),
                                 deps=[t_e2], dma=f'st{s}')
                    kit_free[s] = t_d2
                    kst_free[s] = t_d2
                    vst_free[s] = t_d2
                    if k == 0:
                        vst_free[s] = S.add('pool', lambda e, s=s: e.memset(vst[s][:, :, 64:65], 1.0), deps=[t_d2], sig=True)
                    if CUT <= 8.4:
                        continue
                    t_e3 = S.add('dve', lambda e, k=k: e.tensor_copy(out=krT[:, k * 128:(k + 1) * 128], in_=pS[0:32, 640:768]),
                                 deps=[t_t4], sig=True)
                    if CUT <= 8.5:
                        continue
                    t_e4 = S.add('dve', lambda e: e.tensor_copy(out=ckvnT[:, :], in_=pS[:, 768:896]),
                                 deps=[t_t4, ckvnT_free], sig=True)
                    pS_free = [t_e1, t_e2, t_e3, t_e4]
                    if CUT <= 9:
                        continue
                    for a in range(4):
                        t_k = S.add('pe', lambda e, a=a: e.matmul(
                            pK[:, a * 128:(a + 1) * 128], lhsT=WukK[:, a * 128:(a + 1) * 128], rhs=ckvnT[:, :],
                            start=True, stop=True), deps=[t_e4, pK_free, t_w], sig=(a == 3))
                    t_vv = S.add('pe', lambda e: e.matmul(pV[:, :], lhsT=ckvnT[:, :], rhs=WukV[:, :], start=True, stop=True),
                                 deps=[t_e4, pV_free, t_w], sig=True)
                    ckvnT_free = t_vv
                    pK_free = S.add('act', lambda e, k=k: e.activation(
                        out=knT[:, :, k * 128:(k + 1) * 128], in_=pK[:, :].rearrange("p (a t) -> p a t", a=4), func=AF.Copy),
                        deps=[t_k], sig=True)
                    pV_free = S.add('dve', lambda e, k=k: e.tensor_copy(
                        out=vm[:, k, :, 0:64], in_=pV[:, :].rearrange("p (h d) -> p h d", h=8)),
                        deps=[t_vv, t_vm1, t_vc0], sig=True)
                last = [pK_free, pV_free, kst_free[0], kst_free[1], kit_free[0], kit_free[1], vst_free[0], vst_free[1]]
                if debug and CUT > 50:
                    S.add('sp', lambda e: e.dma_start(out=d_knT[:, :], in_=knT[:, :, :].rearrange("p a t -> p (a t)")),
                          deps=last, dma='dbg')
                    S.add('sp', lambda e: e.dma_start(out=d_krT[:, :], in_=krT[:, :]), deps=last, dma='dbg')
                    t_dbg = S.add('sp', lambda e: e.dma_start(out=d_vm[:, :], in_=vm[:, :, :, :].rearrange("p k h d -> p (k h d)")),
                                  deps=last, dma='dbg')
                    last = last + [t_dbg]
                S.final_wait()
                S.run(nc, "A", top)
                if debug and CUT > 50:
                    S2_ = Sched()
                    t1 = S2_.add('sp', lambda e: e.dma_start(out=d_ksT[0:nkb, :, :], in_=s_ksT[0:nkb, :, :]), dma='d')
                    t1 = S2_.add('sp', lambda e: e.dma_start(out=d_vds[0:nkb, :, :], in_=s_vds[0:nkb, :, :]), dma='d')
                    t1 = S2_.add('sp', lambda e: e.dma_start(out=d_kiT[0:nkb, :, :], in_=s_kiT[0:nkb, :, :]), dma='d')
                    S2_.add('sp', lambda e: e.nop(), deps=[t1])
                    S2_.run(nc, "Ad", top)
        if 'B' in phases:
            with ExitStack() as pb_:
                S = Sched()
                xq = din("xq", [nqb * 128, D])
                wq_in = din("wq_in", [D, 1288])
                g_attn = din("g_attn", [128, 8])
                w_uq_n = din("w_uq_n", [256, 512])
                w_uq_r = din("w_uq_r", [256, 256])
                g_q = din("g_q", [128, 2])
                ropeQ = din("ropeQ", [nqb, 128, 192])
                mk_mult = din("mk_mult", [2, 128, 128], BF16)
                SC = 96.0 ** -0.5
                Wcq = sb("Wcq", [128, 8, 256], BF16, pb_)
                WuqN = sb("WuqN", [128, 2, 512], BF16, pb_)
                WuqR = sb("WuqR", [128, 2, 256], BF16, pb_)
                gA = sb("gAB", [128, 8], F32, pb_)
                gQ = sb("gQB", [128, 2], F32, pb_)
                mkm = sb("mkmB", [128, 2, 128], BF16, pb_)
                stg = [sb(f"stgB{i}", [128, 512], F32, pb_) for i in range(2)]
                xs = sb("xsB", [128, D], F32, pb_)
                xb = sb("xbB", [128, D], BF16, pb_)
                tab = sb("tabB", [128, 192], F32, pb_)
                tabq = sb("tabqB", [128, 64], F32, pb_)
                junk = sb("junkB", [128, D], BF16, pb_)
                ss = sb("ssB", [128, 8], F32, pb_)
                xT = sb("xTB", [128, 8, 128], BF16, pb_)
                cq = sb("cqB", [128, 256], F32, pb_)
                cqn = sb("cqnB", [128, 256], BF16, pb_)
                cqnT = sb("cqnTB", [128, 2, 128], BF16, pb_)
                qbd = sb("qbdB", [128, 4, 256], BF16, pb_)
                qra = sb("qraB", [128, 256], F32, pb_)
                qrb = sb("qrbB", [128, 256], F32, pb_)
                qr = sb("qrB", [128, 256], BF16, pb_)
                qrT = sb("qrTB", [32, 1024], BF16, pb_)
                PT = [sb(f"PTB{i}", [128, 1024], BF16, pb_) for i in range(2)]
                rden = sb("rdenB", [128, 8], F32, pb_)
                ost = [sb(f"ostB{i}", [128, 512], BF16, pb_) for i in range(2)]
                pT = ps("pTB", [128, 1024], BF16, pb_)
                pQ = ps("pQB", [128, 512], F32, pb_)
                pST = [ps(f"pSTB{i}", [128, 512], F32, pb_) for i in range(4)]
                pO = [ps(f"pOB{i}", [128, 4, 65], F32, pb_) for i in range(2)]

                S.add('sp', lambda e: e.dma_start(out=gA[:, :], in_=g_attn[:, :]), dma='cst')
                S.add('sp', lambda e: e.dma_start(out=gQ[:, :], in_=g_q[:, :]), dma='cst')
                t_cst = S.add('sp', lambda e: e.dma_start(out=mkm[:, :, :], in_=mk_mult.rearrange("m k q -> k m q")), dma='cst')
                t_z = S.add('pool', lambda e: e.memset(qbd[:, :, :], 0.0), sig=True)
                stg_free = [None, None]
                t_w = []
                jobs = [(Wcq[:, c, :], wq_in[c * 128:(c + 1) * 128, 0:256], gA[:, c:c + 1], 256) for c in range(8)]
                jobs += [(WuqN[:, c, :], w_uq_n[c * 128:(c + 1) * 128, :], gQ[:, c:c + 1], 512) for c in range(2)]
                jobs += [(WuqR[:, c, :], w_uq_r[c * 128:(c + 1) * 128, :], gQ[:, c:c + 1], 256) for c in range(2)]
                for j, (dst, src, gs, n) in enumerate(jobs):
                    s = j % 2
                    td = S.add('sp', lambda e, s=s, src=src, n=n: e.dma_start(out=stg[s][:, 0:n], in_=src),
                               deps=[stg_free[s]], dma=f'stg{s}')
                    stg_free[s] = S.add('dve' if s == 0 else 'pool', lambda e, s=s, dst=dst, gs=gs, n=n: e.tensor_scalar(
                        out=dst, in0=stg[s][:, 0:n], scalar1=gs, scalar2=None, op0=ALU.mult), deps=[td, t_cst], sig=True)
                    t_w.append(stg_free[s])

                prev = []
                pst_free = [None] * 4
                PT_free = [None, None]
                ost_free = [None, None]
                t_onorm = None
                for i in range(nqb):
                    nk = min(2 * i + 3, nkb)
                    t_x = S.add('sp', lambda e, i=i: e.dma_start(out=xs[:, :], in_=xq[i * 128:(i + 1) * 128, :]), deps=prev, dma='ld')
                    t_x = S.add('sp', lambda e, i=i: e.dma_start(out=tab[:, :], in_=ropeQ[i, :, :]), deps=prev, dma='ld')
                    t_ss = S.add('act', lambda e: e.activation(out=junk[:, :], in_=xs[:, :], func=AF.Square, accum_out=ss[:, 0:1]),
                                 deps=[t_x] + prev, sig=True)
                    t_sd = S.add('act', lambda e: e.activation(out=ss[:, 1:2], in_=ss[:, 0:1], func=AF.Sqrt, scale=1.0 / D, bias=epsT[:, 0:1]),
                                 deps=[t_ss], sig=True)
                    t_rstd = S.add('dve', lambda e: e.reciprocal(out=ss[:, 2:3], in_=ss[:, 1:2]), deps=[t_sd], sig=True)
                    t_xb = S.add('dve', lambda e: e.tensor_copy(out=xb[:, :], in_=xs[:, :]), deps=[t_x] + prev, sig=True)
                    t_tq = S.add('dve', lambda e: e.tensor_scalar(out=tabq[:, :], in0=tab[:, 128:192], scalar1=SC, scalar2=None, op0=ALU.mult),
                                 deps=[t_x] + prev, sig=True)
                    for c in range(8):
                        t_tr = S.add('pe', lambda e, c=c: e.transpose(out=pT[:, c * 128:(c + 1) * 128], in_=xb[:, c * 128:(c + 1) * 128],
                                                                      identity=identB[:, :]), deps=[t_xb] + prev, sig=(c == 7))
                    t_xT = S.add('dve', lambda e: e.tensor_copy(out=xT[:, :, :], in_=pT[:, :].rearrange("p (c t) -> p c t", c=8)),
                                 deps=[t_tr], sig=True)
                    for c in range(8):
                        t_mm = S.add('pe', lambda e, c=c: e.matmul(pQ[:, 0:256], lhsT=xT[:, c, :], rhs=Wcq[:, c, :], start=(c == 0), stop=(c == 7)),
                                     deps=[t_xT, t_w] + prev, sig=(c == 7))
                    t_cq = S.add('dve', lambda e: e.tensor_scalar(out=cq[:, :], in0=pQ[:, 0:256], scalar1=ss[:, 2:3], scalar2=None, op0=ALU.mult),
                                 deps=[t_mm, t_rstd], sig=True)
                    t_ss2 = S.add('act', lambda e: e.activation(out=junk[:, 0:256], in_=cq[:, :], func=AF.Square, accum_out=ss[:, 4:5]),
                                  deps=[t_cq], sig=True)
                    t_sd2 = S.add('act', lambda e: e.activation(out=ss[:, 5:6], in_=ss[:, 4:5], func=AF.Sqrt, scale=1.0 / 256, bias=epsT[:, 0:1]),
                                  deps=[t_ss2], sig=True)
                    t_r2 = S.add('dve', lambda e: e.reciprocal(out=ss[:, 6:7], in_=ss[:, 5:6]), deps=[t_sd2], sig=True)
                    t_cqn = S.add('dve', lambda e: e.tensor_scalar(out=cqn[:, :], in0=cq[:, :], scalar1=ss[:, 6:7], scalar2=None, op0=ALU.mult),
                                  deps=[t_r2], sig=True)
                    for c in range(2):
                        t_tr = S.add('pe', lambda e, c=c: e.transpose(out=pT[:, c * 128:(c + 1) * 128], in_=cqn[:, c * 128:(c + 1) * 128],
                                                                      identity=identB[:, :]), deps=[t_cqn, t_xT], sig=(c == 1))
                    t_cT = S.add('dve', lambda e: e.tensor_copy(out=cqnT[:, :, :], in_=pT[:, 0:256].rearrange("p (c t) -> p c t", c=2)),
                                 deps=[t_tr], sig=True)
                    for a in range(4):
                        for c in range(2):
                            t_mm = S.add('pe', lambda e, a=a, c=c: e.matmul(pQ[:, a * 128:(a + 1) * 128], lhsT=WuqN[:, c, a * 128:(a + 1) * 128],
                                                                            rhs=cqnT[:, c, :], start=(c == 0), stop=(c == 1)),
                                         deps=[t_cT, t_cq], sig=(a == 3 and c == 1))
                    pQv = pQ[:, :].rearrange("p (a t) -> p a t", a=4)
                    t_q1 = S.add('dve', lambda e: e.tensor_scalar(out=qbd[0:64, :, 0:128], in0=pQ[:, :].rearrange("p (a t) -> p a t", a=4)[0:64, :, :],
                                                                  scalar1=SC, scalar2=None, op0=ALU.mult), deps=[t_mm, t_z] + prev, sig=True)
                    t_q2 = S.add('dve', lambda e: e.tensor_scalar(out=qbd[64:128, :, 128:256], in0=pQ[:, :].rearrange("p (a t) -> p a t", a=4)[64:128, :, :],
                                                                  scalar1=SC, scalar2=None, op0=ALU.mult), deps=[t_mm, t_z] + prev, sig=True)
                    for c in range(2):
                        t_mm = S.add('pe', lambda e, c=c: e.matmul(pQ[:, 0:256], lhsT=cqnT[:, c, :], rhs=WuqR[:, c, :], start=(c == 0), stop=(c == 1)),
                                     deps=[t_q1, t_q2], sig=(c == 1))
                    v3 = lambda ap: ap.rearrange("p (h d) -> p h d", h=8)
                    t_a = S.add('dve', lambda e: e.tensor_tensor(out=v3(qra[:, :]), in0=v3(pQ[:, 0:256]), in1=bcast(tabq[:, 0:32], 8), op=ALU.mult),
                                deps=[t_mm, t_tq] + prev, sig=True)
                    t_b1 = S.add('dve', lambda e: e.tensor_tensor(out=v3(qrb[:, :])[:, :, 0:16], in0=v3(pQ[:, 0:256])[:, :, 16:32],
                                                                  in1=bcast(tabq[:, 32:48], 8), op=ALU.mult), deps=[t_mm, t_tq] + prev, sig=True)
                    t_b2 = S.add('dve', lambda e: e.tensor_tensor(out=v3(qrb[:, :])[:, :, 16:32], in0=v3(pQ[:, 0:256])[:, :, 0:16],
                                                                  in1=bcast(tabq[:, 48:64], 8), op=ALU.mult), deps=[t_mm, t_tq] + prev, sig=True)
                    t_qr = S.add('pool', lambda e: e.tensor_tensor(out=qr[:, :], in0=qra[:, :], in1=qrb[:, :], op=ALU.add),
                                 deps=[t_a, t_b1, t_b2] + prev, sig=True)
                    for h in range(8):
                        t_tr = S.add('pe', lambda e, h=h: e.transpose(out=pT[0:32, h * 128:(h + 1) * 128], in_=qr[:, h * 32:(h + 1) * 32],
                                                                      identity=identB[:, :]), deps=[t_qr, t_cT], sig=(h == 7))
                    t_qrT = S.add('dve', lambda e: e.tensor_copy(out=qrT[:, :], in_=pT[0:32, :]), deps=[t_tr] + prev, sig=True)
                    qready = [t_q1, t_q2, t_qrT]
                    prev_q = [t_b2, t_qrT, t_ss2, t_cqn]
                    t_exp = {}
                    t_pv = None

                    def emit_qk(kb):
                        for g in range(2):
                            bank = pST[(kb % 2) * 2 + g]
                            for j in range(2):
                                a = g * 2 + j
                                S.add('pe', lambda e, bank=bank, j=j, a=a, kb=kb: e.matmul(
                                    bank[:, j * 256:(j + 1) * 256], lhsT=knT[:, a, kb * 128:(kb + 1) * 128], rhs=qbd[:, a, :],
                                    start=(j == 0), stop=False, skip_group_check=True),
                                    deps=qready + [pst_free[(kb % 2) * 2 + g]])
                            t = S.add('pe', lambda e, bank=bank, g=g, kb=kb: e.matmul(
                                bank[:, :], lhsT=krT[0:32, kb * 128:(kb + 1) * 128], rhs=qrT[0:32, g * 512:(g + 1) * 512],
                                start=False, stop=True, skip_group_check=True), deps=qready, sig=True)
                            t_qk[(kb, g)] = t
                    t_qk = {}

                    def emit_exp(kb):
                        for g in range(2):
                            bi = (kb % 2) * 2 + g
                            t = S.add('act', lambda e, bi=bi, kb=kb, g=g: e.activation(
                                out=PT[kb % 2][:, g * 512:(g + 1) * 512], in_=pST[bi][:, :], func=AF.Exp),
                                deps=[t_qk[(kb, g)], PT_free[kb % 2]], sig=True)
                            pst_free[bi] = t
                            mi = kb - (2 * i + 1)
                            if mi >= 0:
                                t = S.add('pool', lambda e, kb=kb, g=g, mi=mi: e.tensor_tensor(
                                    out=PT[kb % 2][:, g * 512:(g + 1) * 512].rearrange("p (h q) -> p h q", h=4),
                                    in0=PT[kb % 2][:, g * 512:(g + 1) * 512].rearrange("p (h q) -> p h q", h=4),
                                    in1=bcast(mkm[:, mi, :], 4), op=ALU.mult), deps=[t, t_cst], sig=True)
                            t_exp[(kb, g)] = t

                    def emit_pv(kb):
                        nonlocal t_pv
                        for h in range(8):
                            t_pv = S.add('pe', lambda e, h=h, kb=kb: e.matmul(
                                pO[h // 4][:, h % 4, :], lhsT=PT[kb % 2][:, h * 128:(h + 1) * 128], rhs=vm[:, kb, h, :],
                                start=(kb == 0 and h % 4 == 0), stop=(kb == nk - 1), skip_group_check=True),
                                deps=[t_exp[(kb, h // 4)], t_onorm], sig=(h == 7))
                        PT_free[kb % 2] = t_pv

                    emit_qk(0)
                    for kb in range(nk):
                        if kb + 1 < nk:
                            emit_qk(kb + 1)
                        emit_exp(kb)
                        emit_pv(kb)
                    so = i % 2
                    t_rd = S.add('dve', lambda e: e.reciprocal(out=rden[:, 0:4], in_=pO[0][:, :, 64]), deps=[t_pv], sig=True)
                    t_rd2 = S.add('dve', lambda e: e.reciprocal(out=rden[:, 4:8], in_=pO[1][:, :, 64]), deps=[t_pv], sig=True)
                    for g in range(2):
                        t_onorm = S.add('dve', lambda e, g=g, so=so: e.tensor_tensor(
                            out=ost[so][:, g * 256:(g + 1) * 256].rearrange("p (h d) -> p h d", h=4), in0=pO[g][:, :, 0:64],
                            in1=rden[:, g * 4:(g + 1) * 4].unsqueeze(2).broadcast_to([128, 4, 64]), op=ALU.mult),
                            deps=[t_rd, t_rd2, ost_free[so]], sig=True)
                    ost_free[so] = S.add('sp', lambda e, i=i, so=so: e.dma_start(out=s_omla[i, :, :], in_=ost[so][:, :]),
                                         deps=[t_onorm], dma=f'ost{so}')
                    prev = [t_pv, t_onorm] + prev_q
                if debug:
                    S.final_wait()
                    S.add('sp', lambda e: e.dma_start(out=d_omla.rearrange("p (i f) -> i p f", i=nqb), in_=s_omla[0:nqb, :, :]), dma='dbg')
                S.final_wait()
                S.run(nc, "B", top)
        abst.close()
        if 'C' in phases:
            with ExitStack() as pc_:
                S = Sched()
                xq = din("xq", [nqb * 128, D])
                wq_in = din("wq_in", [D, 1288])
                g_attn = din("g_attn", [128, 8])
                ropeQ = din("ropeQ", [nqb, 128, 192])
                mk_add = din("mk_add", [3, 128, 128])
                NIT = 16
                Wq2 = sb("Wq2C", [128, 8, 1032], BF16, pc_)
                gA = sb("gAC", [128, 8], F32, pc_)
                mka = sb("mkaC", [128, 3, 128], F32, pc_)
                stg = [sb(f"stgC{i}", [128, 1032], F32, pc_) for i in range(2)]
                ksT = sb("ksTC", [128, nkb, 512], BF16, pc_)
                kiT2 = sb("kiT2C", [128, nkb, 128], BF16, pc_)
                Isc = sb("IscC", [128, nkb * 128], F32, pc_)
                Msk = sb("MskC", [128, nkb * 128], BF16, pc_)
                xs = sb("xsC", [128, D], F32, pc_)
                xb = sb("xbC", [128, D], BF16, pc_)
                tab = sb("tabC", [128, 192], F32, pc_)
                tabs = sb("tabsC", [128, 128], F32, pc_)
                junk = sb("junkC", [128, D], BF16, pc_)
                ss = sb("ssC", [128, 8], F32, pc_)
                xT = sb("xTC", [128, 8, 128], BF16, pc_)
                ra = sb("raC", [128, 512], F32, pc_)
                rb = sb("rbC", [128, 512], F32, pc_)
                qs = sb("qsC", [128, 512], BF16, pc_)
                qi = sb("qiC", [128, 512], BF16, pc_)
                wv = sb("wvC", [128, 8], F32, pc_)
                qbd = sb("qbdC", [128, 4, 256], BF16, pc_)
                qiT = sb("qiTC", [128, 4, 128], BF16, pc_)
                tmpR = [sb(f"tmpRC{i}", [128, 512], F32, pc_) for i in range(4)]
                pw2 = sb("pw2C", [128, 32], F32, pc_)
                hwt = sb("hwtC", [128, 32], F32, pc_)
                bs = sb("bsC", [128, 16], F32, pc_)
                PT = [sb(f"PTC{i}", [128, 1024], BF16, pc_) for i in range(2)]
                vb = [sb(f"vbC{i}", [128, 8, 65], BF16, pc_) for i in range(3)]
                rden = sb("rdenC", [128, 8], F32, pc_)
                ost = [sb(f"ostC{i}", [128, 512], BF16, pc_) for i in range(2)]
                pT = [ps(f"pTC{i}", [128, 1024], BF16, pc_) for i in range(2)]
                pST = [ps(f"pSTC{i}", [128, 512], F32, pc_) for i in range(4)]
                pO = [ps(f"pOC{i}", [128, 4, 65], F32, pc_) for i in range(2)]

                S.add('sp', lambda e: e.dma_start(out=gA[:, :], in_=g_attn[:, :]), dma='cst')
                S.add('sp', lambda e: e.dma_start(out=mka[:, :, :], in_=mk_add.rearrange("m q k -> q m k")), dma='cst')
                for k0 in range(0, nkb, 4):
                    k1 = min(nkb, k0 + 4)
                    S.add('sp', lambda e, k0=k0, k1=k1: e.dma_start(out=ksT[:, k0:k1, :], in_=s_ksT[k0:k1, :, :].rearrange("k p f -> p k f")), dma='cst')
                for k0 in range(0, nkb, 8):
                    k1 = min(nkb, k0 + 8)
                    S.add('sp', lambda e, k0=k0, k1=k1: e.dma_start(out=kiT2[0:64, k0:k1, :], in_=s_kiT[k0:k1, :, :].rearrange("k p t -> p k t")), dma='cst')
                    t_cst = S.add('sp', lambda e, k0=k0, k1=k1: e.dma_start(out=kiT2[64:128, k0:k1, :], in_=s_kiT[k0:k1, :, :].rearrange("k p t -> p k t")), dma='cst')
                t_z = S.add('pool', lambda e: e.memset(qbd[:, :, :], 0.0), sig=True)
                for it in range(NIT):
                    t_pw2 = S.add('pool', lambda e, it=it: e.memset(pw2[:, it:it + 1], 2.0 ** -(it + 1)), sig=True)
                stg_free = [None, None]
                t_w = []
                for c in range(8):
                    s = c % 2
                    td = S.add('sp', lambda e, s=s, c=c: e.dma_start(out=stg[s][:, :], in_=wq_in[c * 128:(c + 1) * 128, 256:1288]),
                               deps=[stg_free[s]], dma=f'stg{s}')
                    stg_free[s] = S.add(('dve', 'pool')[s], lambda e, s=s, c=c: e.tensor_scalar(
                        out=Wq2[:, c, :], in0=stg[s][:, :], scalar1=gA[:, c:c + 1], scalar2=None, op0=ALU.mult), deps=[td, t_cst], sig=True)
                    t_w.append(stg_free[s])
                v3 = lambda ap: ap.rearrange("p (h d) -> p h d", h=8)
                prev = []
                pst_free = [None] * 4
                PT_free = [None, None]
                pT_free = [None, None]
                vb_free = [None] * 3
                ost_free = [None, None]
                t_onorm = None
                vcount = 0
                for i in range(nqb):
                    nk = min(2 * i + 3, nkb)
                    W = nk * 128
                    S.add('sp', lambda e, i=i: e.dma_start(out=xs[:, :], in_=xq[i * 128:(i + 1) * 128, :]), deps=prev, dma='ld')
                    t_x = S.add('sp', lambda e, i=i: e.dma_start(out=tab[:, :], in_=ropeQ[i, :, :]), deps=prev, dma='ld')
                    t_ss = S.add('act', lambda e: e.activation(out=junk[:, :], in_=xs[:, :], func=AF.Square, accum_out=ss[:, 0:1]), deps=[t_x] + prev, sig=True)
                    t_sd = S.add('act', lambda e: e.activation(out=ss[:, 1:2], in_=ss[:, 0:1], func=AF.Sqrt, scale=1.0 / D, bias=epsT[:, 0:1]), deps=[t_ss], sig=True)
                    t_rstd = S.add('dve', lambda e: e.reciprocal(out=ss[:, 2:3], in_=ss[:, 1:2]), deps=[t_sd], sig=True)
                    t_r8 = S.add('dve', lambda e: e.tensor_scalar(out=ss[:, 3:4], in0=ss[:, 2:3], scalar1=0.125, scalar2=None, op0=ALU.mult), deps=[t_rstd], sig=True)
                    t_tabs = S.add('dve', lambda e: e.tensor_scalar(out=tabs[:, :], in0=tab[:, 0:128], scalar1=ss[:, 3:4], scalar2=None, op0=ALU.mult),
                                   deps=[t_r8, t_x] + prev, sig=True)
                    t_xb = S.add('dve', lambda e: e.tensor_copy(out=xb[:, :], in_=xs[:, :]), deps=[t_x] + prev, sig=True)
                    for c in range(8):
                        t_tr = S.add('pe', lambda e, c=c: e.transpose(out=pT[0][:, c * 128:(c + 1) * 128], in_=xb[:, c * 128:(c + 1) * 128],
                                                                      identity=identB[:, :]), deps=[t_xb] + prev, sig=(c == 7))
                    t_xT = S.add('dve', lambda e: e.tensor_copy(out=xT[:, :, :], in_=pT[0][:, :].rearrange("p (c t) -> p c t", c=8)), deps=[t_tr], sig=True)
                    t_mms = []
                    for bnk, (c0, c1) in enumerate(((0, 512), (512, 1024), (1024, 1032))):
                        for c in range(8):
                            t_mm = S.add('pe', lambda e, c=c, bnk=bnk, c0=c0, c1=c1: e.matmul(pST[bnk][:, 0:c1 - c0], lhsT=xT[:, c, :], rhs=Wq2[:, c, c0:c1],
                                                                                             start=(c == 0), stop=(c == 7)), deps=[t_xT, t_w] + prev, sig=(c == 7))
                        t_mms.append(t_mm)
                    outs = []
                    for bnk, dst in ((0, qs), (1, qi)):
                        t_a = S.add('dve', lambda e, bnk=bnk: e.tensor_tensor(out=v3(ra[:, :]), in0=v3(pST[bnk][:, :]), in1=bcast(tabs[:, 0:64], 8), op=ALU.mult),
                                    deps=[t_mms[bnk], t_tabs] + outs + prev, sig=True)
                        t_b1 = S.add('dve', lambda e, bnk=bnk: e.tensor_tensor(out=v3(rb[:, :])[:, :, 0:32], in0=v3(pST[bnk][:, :])[:, :, 32:64],
                                                                              in1=bcast(tabs[:, 64:96], 8), op=ALU.mult), deps=[t_mms[bnk], t_tabs] + outs + prev, sig=True)
                        t_b2 = S.add('dve', lambda e, bnk=bnk: e.tensor_tensor(out=v3(rb[:, :])[:, :, 32:64], in0=v3(pST[bnk][:, :])[:, :, 0:32],
                                                                              in1=bcast(tabs[:, 96:128], 8), op=ALU.mult), deps=[t_mms[bnk], t_tabs] + outs + prev, sig=True)
                        t_q = S.add('pool', lambda e, dst=dst: e.tensor_tensor(out=dst[:, :], in0=ra[:, :], in1=rb[:, :], op=ALU.add),
                                    deps=[t_a, t_b1, t_b2] + prev, sig=True)
                        outs = [t_q]
                        if bnk == 0:
                            t_qs = t_q
                        else:
                            t_qi = t_q
                    t_wv = S.add('dve', lambda e: e.tensor_scalar(out=wv[:, :], in0=pST[2][:, 0:8], scalar1=ss[:, 2:3], scalar2=8.0 ** -0.5,
                                                                  op0=ALU.mult, op1=ALU.mult), deps=[t_mms[2], t_rstd] + prev, sig=True)
                    pst_q = [t_b2, t_wv]
                    for a in range(4):
                        t_tr = S.add('pe', lambda e, a=a: e.transpose(out=pT[0][:, a * 128:(a + 1) * 128], in_=qs[:, a * 128:(a + 1) * 128],
                                                                      identity=identB[:, :]), deps=[t_qs, t_xT], sig=(a == 3))
                    for a in range(4):
                        t_tr2 = S.add('pe', lambda e, a=a: e.transpose(out=pT[1][:, a * 128:(a + 1) * 128], in_=qi[:, a * 128:(a + 1) * 128],
                                                                       identity=identB[:, :]), deps=[t_qi, pT_free[1]] + prev, sig=(a == 3))
                    pTv = lambda t: t[:, 0:512].rearrange("p (a t) -> p a t", a=4)
                    t_q1 = S.add('dve', lambda e: e.tensor_copy(out=qbd[0:64, :, 0:128], in_=pTv(pT[0])[0:64, :, :]), deps=[t_tr, t_z] + prev, sig=True)
                    t_q2 = S.add('dve', lambda e: e.tensor_copy(out=qbd[64:128, :, 128:256], in_=pTv(pT[0])[64:128, :, :]), deps=[t_tr, t_z] + prev, sig=True)
                    t_qiT = S.add('dve', lambda e: e.tensor_copy(out=qiT[:, :, :], in_=pTv(pT[1])), deps=[t_tr2] + prev, sig=True)
                    pT_free = [t_q2, t_qiT]
                    t_acc = None
                    tmp_free = [None] * 4
                    nch = (W + 511) // 512
                    cnt_ = 0
                    for ch in range(nch):
                        c0 = ch * 512
                        n = min(512, W - c0)
                        for h in range(8):
                            hp = (h % 2) * 64
                            bnk = h % 4
                            t_lg = S.add('pe', lambda e, hp=hp, h=h, bnk=bnk, c0=c0, n=n: e.matmul(
                                pST[bnk][:, 0:n], lhsT=qiT[hp:hp + 64, h // 2, :],
                                rhs=kiT2[hp:hp + 64, :, :].rearrange("p k t -> p (k t)")[:, c0:c0 + n], start=True, stop=True),
                                deps=[t_qiT, t_cst, pst_free[bnk]] + pst_q, sig=True)
                            sl = cnt_ % 4
                            cnt_ += 1
                            t_r = S.add('act', lambda e, bnk=bnk, sl=sl, n=n: e.activation(out=tmpR[sl][:, 0:n], in_=pST[bnk][:, 0:n], func=AF.Relu),
                                        deps=[t_lg, tmp_free[sl]], sig=True)
                            pst_free[bnk] = t_r
                            if h == 0:
                                t_acc = S.add('dve', lambda e, sl=sl, c0=c0, n=n: e.tensor_scalar(
                                    out=Isc[:, c0:c0 + n], in0=tmpR[sl][:, 0:n], scalar1=wv[:, 0:1], scalar2=None, op0=ALU.mult),
                                    deps=[t_r, t_wv, t_acc] + prev, sig=True)
                            else:
                                t_acc = S.add('dve', lambda e, sl=sl, c0=c0, n=n, h=h: e.scalar_tensor_tensor(
                                    out=Isc[:, c0:c0 + n], in0=tmpR[sl][:, 0:n], scalar=wv[:, h:h + 1], in1=Isc[:, c0:c0 + n],
                                    op0=ALU.mult, op1=ALU.add), deps=[t_r, t_wv, t_acc], sig=True)
                            tmp_free[sl] = t_acc
                    t_mx = S.add('dve', lambda e, W=W: e.tensor_reduce(out=bs[:, 1:2], in_=Isc[:, 0:W], axis=AX.X, op=ALU.max), deps=[t_acc] + prev, sig=True)
                    t_mn = S.add('dve', lambda e, W=W: e.tensor_reduce(out=bs[:, 0:1], in_=Isc[:, 0:W], axis=AX.X, op=ALU.min), deps=[t_acc] + prev, sig=True)
                    t_b = S.add('dve', lambda e: e.tensor_scalar(out=bs[:, 1:2], in0=bs[:, 1:2], scalar1=1.0, scalar2=None, op0=ALU.add), deps=[t_mx], sig=True)
                    t_b = S.add('dve', lambda e: e.tensor_scalar(out=bs[:, 0:1], in0=bs[:, 0:1], scalar1=-1.0, scalar2=None, op0=ALU.add), deps=[t_mn, t_b], sig=True)
                    for (mi, kb) in ((0, 0), (1, nk - 2), (2, nk - 1)):
                        t_b = S.add('dve', lambda e, mi=mi, kb=kb: e.tensor_tensor(out=Isc[:, kb * 128:(kb + 1) * 128], in0=Isc[:, kb * 128:(kb + 1) * 128],
                                                                                  in1=mka[:, mi, :], op=ALU.add), deps=[t_b, t_mn, t_mx, t_cst], sig=True)
                    W1 = (nk // 2) * 128
                    W2 = W - W1
                    t_b = S.add('dve', lambda e: e.tensor_tensor(out=bs[:, 7:8], in0=bs[:, 1:2], in1=bs[:, 0:1], op=ALU.subtract), deps=[t_b], sig=True)
                    t_b = S.add('dve', lambda e: e.tensor_scalar(out=hwt[:, 0:NIT], in0=pw2[:, 0:NIT], scalar1=bs[:, 7:8], scalar2=None, op0=ALU.mult),
                                deps=[t_b, t_pw2], sig=True)
                    t_b = S.add('dve', lambda e: e.tensor_tensor(out=bs[:, 2:3], in0=bs[:, 0:1], in1=hwt[:, 0:1], op=ALU.add), deps=[t_b], sig=True)
                    for it in range(NIT):
                        t_c1 = S.add('dve', lambda e, W1=W1: e.tensor_scalar(out=Msk[:, 0:W1], in0=Isc[:, 0:W1], scalar1=bs[:, 2:3], scalar2=0.0,
                                                                          op0=ALU.is_ge, op1=ALU.add, accum_out=bs[:, 3:4]), deps=[t_b] + prev, sig=True)
                        t_c2 = S.add('act', lambda e, W1=W1, W=W: e.activation(out=Msk[:, W1:W], in_=Isc[:, W1:W], func=AF.Sign, scale=-1.0,
                                                                             bias=bs[:, 2:3], accum_out=bs[:, 8:9]), deps=[t_b] + prev, sig=True)
                        t_b = S.add('dve', lambda e: e.scalar_tensor_tensor(out=bs[:, 9:10], in0=bs[:, 8:9], scalar=-0.5, in1=bs[:, 3:4],
                                                                            op0=ALU.mult, op1=ALU.add), deps=[t_c1, t_c2], sig=True)
                        t_b = S.add('dve', lambda e, W2=W2: e.tensor_scalar(out=bs[:, 4:5], in0=bs[:, 9:10], scalar1=float(TOPK) - 0.5 - W2 / 2.0,
                                                                          scalar2=None, op0=ALU.is_ge), deps=[t_b], sig=True)
                        t_b = S.add('dve', lambda e, it=it: e.scalar_tensor_tensor(out=bs[:, 0:1], in0=bs[:, 4:5], scalar=hwt[:, it:it + 1], in1=bs[:, 0:1],
                                                                                   op0=ALU.mult, op1=ALU.add), deps=[t_b], sig=True)
                        if it + 1 < NIT:
                            t_b = S.add('dve', lambda e, it=it: e.tensor_tensor(out=bs[:, 2:3], in0=bs[:, 0:1], in1=hwt[:, it + 1:it + 2], op=ALU.add),
                                        deps=[t_b], sig=True)
                    t_msk = S.add('dve', lambda e, W=W: e.tensor_scalar(out=Msk[:, 0:W], in0=Isc[:, 0:W], scalar1=bs[:, 0:1], scalar2=None, op0=ALU.is_ge),
                                  deps=[t_b], sig=True)
                    qready = [t_q1, t_q2, t_msk]
                    t_qk = {}
                    t_exp = {}
                    t_v = {}
                    t_pv = None

                    def emit_qk(kb):
                        for g in range(2):
                            bi = (kb % 2) * 2 + g
                            for j in range(2):
                                a = g * 2 + j
                                t = S.add('pe', lambda e, bi=bi, j=j, a=a, kb=kb: e.matmul(
                                    pST[bi][:, j * 256:(j + 1) * 256], lhsT=ksT[:, kb, a * 128:(a + 1) * 128], rhs=qbd[:, a, :],
                                    start=(j == 0), stop=(j == 1), skip_group_check=True), deps=qready + [pst_free[bi], t_cst], sig=(j == 1))
                            t_qk[(kb, g)] = t
                        t_qk[(kb, 'm')] = S.add('pe', lambda e, kb=kb: e.transpose(out=pT[kb % 2][:, 0:128], in_=Msk[:, kb * 128:(kb + 1) * 128],
                                                                                  identity=identB[:, :]), deps=[t_msk, pT_free[kb % 2]], sig=True)

                    def emit_v(kb):
                        nonlocal vcount
                        sl = vcount % 3
                        vcount += 1
                        t_v[kb] = (S.add('sp', lambda e, kb=kb, sl=sl: e.dma_start(out=vb[sl][:, :, :].rearrange("p h d -> p (h d)"), in_=s_vds[kb, :, :]),
                                         deps=[vb_free[sl]], dma=f'vb{sl}'), sl)

                    def emit_exp(kb):
                        for g in range(2):
                            bi = (kb % 2) * 2 + g
                            t = S.add('act', lambda e, bi=bi, kb=kb, g=g: e.activation(
                                out=PT[kb % 2][:, g * 512:(g + 1) * 512], in_=pST[bi][:, :], func=AF.Exp),
                                deps=[t_qk[(kb, g)], PT_free[kb % 2]], sig=True)
                            pst_free[bi] = t
                            t_exp[(kb, g)] = t
                        t = S.add('dve', lambda e, kb=kb: e.tensor_tensor(
                            out=PT[kb % 2][:, :].rearrange("p (h q) -> p h q", h=8), in0=PT[kb % 2][:, :].rearrange("p (h q) -> p h q", h=8),
                            in1=pT[kb % 2][:, 0:128].unsqueeze(1).broadcast_to([128, 8, 128]), op=ALU.mult),
                            deps=[t_exp[(kb, 0)], t_exp[(kb, 1)], t_qk[(kb, 'm')]], sig=True)
                        pT_free[kb % 2] = t
                        t_exp[kb] = t

                    def emit_pv(kb):
                        nonlocal t_pv
                        tv, sl = t_v[kb]
                        for h in range(8):
                            t_pv = S.add('pe', lambda e, h=h, kb=kb, sl=sl: e.matmul(
                                pO[h // 4][:, h % 4, :], lhsT=PT[kb % 2][:, h * 128:(h + 1) * 128], rhs=vb[sl][:, h, :],
                                start=(kb == 0 and h % 4 == 0), stop=(kb == nk - 1), skip_group_check=True),
                                deps=[t_exp[kb], tv, t_onorm], sig=(h == 7))
                        PT_free[kb % 2] = t_pv
                        vb_free[sl] = t_pv

                    emit_v(0)
                    emit_qk(0)
                    for kb in range(nk):
                        if kb + 1 < nk:
                            emit_v(kb + 1)
                            emit_qk(kb + 1)
                        emit_exp(kb)
                        emit_pv(kb)
                    so = i % 2
                    t_rd = S.add('dve', lambda e: e.reciprocal(out=rden[:, 0:4], in_=pO[0][:, :, 64]), deps=[t_pv], sig=True)
                    t_rd2 = S.add('dve', lambda e: e.reciprocal(out=rden[:, 4:8], in_=pO[1][:, :, 64]), deps=[t_pv], sig=True)
                    for g in range(2):
                        t_onorm = S.add('dve', lambda e, g=g, so=so: e.tensor_tensor(
                            out=ost[so][:, g * 256:(g + 1) * 256].rearrange("p (h d) -> p h d", h=4), in0=pO[g][:, :, 0:64],
                            in1=rden[:, g * 4:(g + 1) * 4].unsqueeze(2).broadcast_to([128, 4, 64]), op=ALU.mult),
                            deps=[t_rd, t_rd2, ost_free[so]], sig=True)
                    ost_free[so] = S.add('sp', lambda e, i=i, so=so: e.dma_start(out=s_odsa[i, :, :], in_=ost[so][:, :]), deps=[t_onorm], dma=f'ost{so}')
                    prev = [t_pv, t_onorm, t_msk, t_qiT, t_q2]
                S.final_wait()
                S.run(nc, "C", top)
        if 'D' in phases:
            with ExitStack() as pd_:
                S = Sched()
                xq = din("xq", [nqb * 128, D])
                w_o = din("w_o", [D, D])
                g_ffn = din("g_ffn", [128, 8])
                w_gate = din("w_gate", [D, DFF])
                w_up = din("w_up", [D, DFF])
                w_down = din("w_down", [DFF, D])
                g_fin = din("g_fin", [128, D])
                Wo = sb("WoD", [128, 8, D], BF16, pd_)
                Wg = sb("WgD", [128, 8, DFF], BF16, pd_)
                Wu = sb("WuD", [128, 8, DFF], BF16, pd_)
                Wd = sb("WdD", [128, NFC, D], BF16, pd_)
                gF = sb("gFD", [128, 8], F32, pd_)
                gfin = sb("gfinD", [128, D], F32, pd_)
                stg = [sb(f"stgD{i}", [128, 1024], F32, pd_) for i in range(2)]
                xs = [sb(f"xsD{i}", [128, D], F32, pd_) for i in range(2)]
                ob = [sb("obD0", [128, D], BF16, pd_)] * 2
                oT = [sb(f"oTD{i}", [128, 8, 128], BF16, pd_) for i in range(2)]
                ub = [sb("ubD0", [128, D], BF16, pd_)] * 2
                uT = [sb(f"uTD{i}", [128, 8, 128], BF16, pd_) for i in range(2)]
                junk = sb("junkD", [128, D], BF16, pd_)
                ss = [sb(f"ssD{i}", [128, 8], F32, pd_) for i in range(2)]
                sil = [sb(f"silD{i}", [128, 512], F32, pd_) for i in range(2)]
                actT = [sb(f"actTD{i}", [128, NFC, 128], BF16, pd_) for i in range(2)]
                h2 = [sb(f"h2D{i}", [128, D], F32, pd_) for i in range(2)]
                pT = ps("pTD", [128, 1024], BF16, pd_)
                pA = [ps(f"pAD{i}", [128, 512], F32, pd_) for i in range(2)]
                pG = [ps(f"pGD{i}", [128, 512], F32, pd_) for i in range(2)]
                pU = [ps(f"pUD{i}", [128, 512], F32, pd_) for i in range(2)]
                S.add('sp', lambda e: e.dma_start(out=gF[:, :], in_=g_ffn[:, :]), dma='cst')
                t_cst = S.add('sp', lambda e: e.dma_start(out=gfin[:, :], in_=g_fin[:, :]), dma='cst')
                stg_free = [None, None]
                t_w = []
                jobs = []
                for c in range(8):
                    jobs.append((Wo[:, c, :], w_o[c * 128:(c + 1) * 128, :], None, D))
                n_wo = len(jobs)
                for c in range(8):
                    for (Wt, wsrc) in ((Wg, w_gate), (Wu, w_up)):
                        for c0 in range(0, DFF, 1024):
                            n = min(1024, DFF - c0)
                            jobs.append((Wt[:, c, c0:c0 + n], wsrc[c * 128:(c + 1) * 128, c0:c0 + n], gF[:, c:c + 1], n))
                n_wgu = len(jobs)
                for f in range(NFC):
                    jobs.append((Wd[:, f, :], w_down[f * 128:(f + 1) * 128, :], None, D))
                for j, (dst, src, gs, n) in enumerate(jobs):
                    s = j % 2
                    td = S.add('sp', lambda e, s=s, src=src, n=n: e.dma_start(out=stg[s][:, 0:n], in_=src), deps=[stg_free[s]], dma=f'stg{s}')
                    eng = ('dve', 'pool')[s]
                    if gs is None:
                        stg_free[s] = S.add(eng, lambda e, s=s, dst=dst, n=n: e.tensor_copy(out=dst, in_=stg[s][:, 0:n]), deps=[td], sig=True)
                    else:
                        stg_free[s] = S.add(eng, lambda e, s=s, dst=dst, gs=gs, n=n: e.tensor_scalar(
                            out=dst, in0=stg[s][:, 0:n], scalar1=gs, scalar2=None, op0=ALU.mult), deps=[td, t_cst], sig=True)
                    t_w.append(stg_free[s])
                t_wo = t_w[:n_wo][-2:]
                t_wgu = t_w[:n_wgu][-2:]
                t_wd = t_w[-2:]
                pG_free = [None, None]
                pU_free = [None, None]
                sil_free = [None, None]
                pT_free = [None]
                pA_free = [None, None]
                done = {}
                st = {}

                def s1a(i):
                    b = i % 2
                    pv = done.get(i - 2, [])
                    S.add('sp', lambda e: e.dma_start(out=xs[b][:, :], in_=xq[i * 128:(i + 1) * 128, :]), deps=pv, dma=f'ld{b}')
                    S.add('sp', lambda e: e.dma_start(out=ob[b][:, 0:512], in_=s_omla[i, :, :]), deps=pv + [pT_free[0]], dma=f'ld{b}')
                    t_x = S.add('sp', lambda e: e.dma_start(out=ob[b][:, 512:1024], in_=s_odsa[i, :, :]), deps=pv + [pT_free[0]], dma=f'ld{b}')
                    for c in range(8):
                        t_tr = S.add('pe', lambda e, c=c: e.transpose(out=pT[:, c * 128:(c + 1) * 128], in_=ob[b][:, c * 128:(c + 1) * 128],
                                                                      identity=identB[:, :]), deps=[t_x, pT_free[0]], sig=(c == 7))
                    t_oT = S.add('dve', lambda e: e.tensor_copy(out=oT[b][:, :, :], in_=pT[:, :].rearrange("p (c t) -> p c t", c=8)), deps=[t_tr] + pv, sig=True)
                    pT_free[0] = t_oT
                    t_mm = [None, None]
                    for hf in range(2):
                        for c in range(8):
                            t_mm[hf] = S.add('pe', lambda e, c=c, hf=hf: e.matmul(pA[hf][:, :], lhsT=oT[b][:, c, :], rhs=Wo[:, c, hf * 512:(hf + 1) * 512],
                                                                                  start=(c == 0), stop=(c == 7)), deps=[t_oT, t_wo, pA_free[hf]], sig=(c == 7))
                    for hf in range(2):
                        t_h1 = S.add('dve', lambda e, hf=hf: e.tensor_tensor(out=xs[b][:, hf * 512:(hf + 1) * 512], in0=pA[hf][:, :],
                                                                             in1=xs[b][:, hf * 512:(hf + 1) * 512], op=ALU.add), deps=[t_mm[hf], t_x], sig=True)
                        pA_free[hf] = t_h1
                    t_ss = S.add('act', lambda e: e.activation(out=junk[:, :], in_=xs[b][:, :], func=AF.Square, accum_out=ss[b][:, 0:1]), deps=[t_h1] + pv, sig=True)
                    t_sd = S.add('act', lambda e: e.activation(out=ss[b][:, 1:2], in_=ss[b][:, 0:1], func=AF.Sqrt, scale=1.0 / D, bias=epsT[:, 0:1]), deps=[t_ss], sig=True)
                    t_r = S.add('dve', lambda e: e.reciprocal(out=ss[b][:, 2:3], in_=ss[b][:, 1:2]), deps=[t_sd], sig=True)
                    st[i] = dict(t_ub=S.add('dve', lambda e: e.tensor_scalar(out=ub[b][:, :], in0=xs[b][:, :], scalar1=ss[b][:, 2:3], scalar2=None, op0=ALU.mult),
                                            deps=[t_r, pT_free[0]] + pv, sig=True), t_h1=t_h1)

                def s1b(i):
                    b = i % 2
                    pv = done.get(i - 2, [])
                    for c in range(8):
                        t_tr = S.add('pe', lambda e, c=c: e.transpose(out=pT[:, c * 128:(c + 1) * 128], in_=ub[b][:, c * 128:(c + 1) * 128],
                                                                      identity=identB[:, :]), deps=[st[i]['t_ub'], pT_free[0]], sig=(c == 7))
                    st[i]['t_uT'] = S.add('dve', lambda e: e.tensor_copy(out=uT[b][:, :, :], in_=pT[:, :].rearrange("p (c t) -> p c t", c=8)), deps=[t_tr] + pv, sig=True)
                    pT_free[0] = st[i]['t_uT']

                def s2(i, g0, g1):
                    b = i % 2
                    pv = done.get(i - 2, [])
                    t_uT = st[i]['t_uT']
                    for gi in range(g0, g1):
                        sl = gi % 2
                        nf = min(4, NFC - gi * 4)
                        for (pX, Wt, fr) in ((pG, Wg, pG_free), (pU, Wu, pU_free)):
                            for j in range(nf):
                                f = gi * 4 + j
                                for c in range(8):
                                    t_mm = S.add('pe', lambda e, pX=pX, Wt=Wt, sl=sl, j=j, f=f, c=c: e.matmul(
                                        pX[sl][:, j * 128:(j + 1) * 128], lhsT=Wt[:, c, f * 128:(f + 1) * 128], rhs=uT[b][:, c, :],
                                        start=(c == 0), stop=(c == 7)), deps=[t_uT, t_wgu, fr[sl]], sig=(c == 7 and j == nf - 1))
                            if pX is pG:
                                t_g = t_mm
                            else:
                                t_u = t_mm
                        t_sil = S.add('act', lambda e, sl=sl, nf=nf: e.activation(out=sil[sl][:, 0:nf * 128], in_=pG[sl][:, 0:nf * 128], func=AF.Silu),
                                      deps=[t_g, sil_free[sl]], sig=True)
                        pG_free[sl] = t_sil
                        t_act = S.add('dve', lambda e, sl=sl, nf=nf, gi=gi: e.tensor_tensor(
                            out=actT[b][:, gi * 4:gi * 4 + nf, :], in0=pU[sl][:, 0:nf * 128].rearrange("p (f t) -> p f t", f=nf),
                            in1=sil[sl][:, 0:nf * 128].rearrange("p (f t) -> p f t", f=nf), op=ALU.mult), deps=[t_sil, t_u] + pv, sig=True)
                        pU_free[sl] = t_act
                        sil_free[sl] = t_act
                        st[i]['t_act'] = t_act
                        st[i]['t_u'] = t_u

                def s3(i):
                    b = i % 2
                    t_act = st[i]['t_act']
                    t_mm = [None, None]
                    for hf in range(2):
                        for f in range(NFC):
                            t_mm[hf] = S.add('pe', lambda e, f=f, hf=hf: e.matmul(pA[hf][:, :], lhsT=actT[b][:, f, :], rhs=Wd[:, f, hf * 512:(hf + 1) * 512],
                                                                                  start=(f == 0), stop=(f == NFC - 1)), deps=[t_act, t_wd, pA_free[hf]], sig=(f == NFC - 1))
                    for hf in range(2):
                        t_h2 = S.add('dve', lambda e, hf=hf: e.tensor_tensor(out=h2[b][:, hf * 512:(hf + 1) * 512], in0=pA[hf][:, :],
                                                                             in1=xs[b][:, hf * 512:(hf + 1) * 512], op=ALU.add),
                                     deps=[t_mm[hf]] + done.get(i - 2, []), sig=True)
                        pA_free[hf] = t_h2
                    t_ss = S.add('act', lambda e: e.activation(out=junk[:, :], in_=h2[b][:, :], func=AF.Square, accum_out=ss[b][:, 4:5]), deps=[t_h2], sig=True)
                    t_sd = S.add('act', lambda e: e.activation(out=ss[b][:, 5:6], in_=ss[b][:, 4:5], func=AF.Sqrt, scale=1.0 / D, bias=epsT[:, 0:1]), deps=[t_ss], sig=True)
                    t_r = S.add('dve', lambda e: e.reciprocal(out=ss[b][:, 6:7], in_=ss[b][:, 5:6]), deps=[t_sd], sig=True)
                    t_o = S.add('dve', lambda e: e.scalar_tensor_tensor(out=h2[b][:, :], in0=h2[b][:, :], scalar=ss[b][:, 6:7], in1=gfin[:, :], op0=ALU.mult, op1=ALU.mult),
                                deps=[t_r, t_cst], sig=True)
                    t_st = S.add('sp', lambda e: e.dma_start(out=out[i * 128:(i + 1) * 128, :], in_=h2[b][:, :]), deps=[t_o], dma=f'st{b}')
                    done[i] = [t_st, t_o, t_mm[1], t_sd]

                ngrp = (NFC + 3) // 4
                s1a(0)
                s1b(0)
                for i in range(nqb):
                    s2(i, 0, ngrp // 2)
                    if i + 1 < nqb:
                        s1a(i + 1)
                    s2(i, ngrp // 2, ngrp)
                    if i + 1 < nqb:
                        s1b(i + 1)
                    s3(i)
                S.final_wait()
                S.run(nc, "D", top)
    global LAST_INPUTS
    LAST_INPUTS = list(_din.keys())
    return nc, dbg


def bcast(ap, h):
    n = ap.shape[-1]
    return ap.unsqueeze(1).broadcast_to([ap.shape[0], h, n])


BF = ml_dtypes.bfloat16
def rope_tab(pos):
    pos = pos.astype(np.float32)
    out = np.zeros((pos.shape[0], 192), np.float32)
    for (half, off) in ((32, 0), (16, 128)):
        inv = np.power(np.float32(10000.0), -np.arange(half, dtype=np.float32) / np.float32(half)).astype(np.float32)
        ang = (pos[:, None] * inv[None, :]).astype(np.float32)
        c = np.cos(ang).astype(np.float32); s = np.sin(ang).astype(np.float32)
        d = 2 * half
        out[:, off:off + d] = np.concatenate([c, c], 1)
        out[:, off + d:off + 2 * d] = np.concatenate([-s, s], 1)
    return out
def core_inputs(inp, core):
    f32 = np.float32
    b, par = core // 2, core % 2
    x = np.asarray(inp['x'][b], f32)
    xk = np.zeros((65 * 128, 1024), f32); xk[0:16] = inp['meta_tokens']; xk[128:] = x
    xq = np.ascontiguousarray(x.reshape(64, 128, 1024)[par::2].reshape(32 * 128, 1024))
    w_in = np.asarray(inp['w_in'][0], f32)
    c_q, c_kv, k_r, q_s, k_s, v_s, q_i, k_i, w_i = np.split(w_in, np.cumsum([256,128,32,512,512,512,512,64,8])[:-1], axis=1)
    def pc(g, n):
        return np.ascontiguousarray(np.asarray(g, f32).reshape(n, 128).T)
    w_uq = np.asarray(inp['w_uq'][0], f32).reshape(256, 8, 96)
    w_ukv = np.asarray(inp['w_ukv'][0], f32).reshape(128, 8, 128)
    posK = np.concatenate([np.arange(128), 16 + np.arange(64 * 128)])
    ropeK = rope_tab(posK).reshape(65, 128, 192)
    g_idx = 2 * np.arange(32) + par
    posQ = (16 + 128 * g_idx[:, None] + np.arange(128)[None, :]).reshape(-1)
    ropeQ = rope_tab(posQ).reshape(32, 128, 192)
    tri = (np.arange(128)[:, None] <= np.arange(128)[None, :]).astype(f32)
    ones = np.ones((128, 128), f32); zeros = np.zeros((128, 128), f32)
    mk_mult = np.stack([tri, zeros] if par == 0 else [ones, tri]).astype(BF)
    NEG = np.float32(-1e30)
    meta_add = np.zeros((128, 128), f32); meta_add[:, 16:] = NEG
    triA = np.where(tri.T > 0, 0, NEG).astype(f32)
    allneg = np.full((128, 128), NEG, f32)
    mk_add = np.stack([meta_add, triA, allneg] if par == 0 else [meta_add, zeros, triA]).astype(f32)
    vcol0 = np.zeros((128, 8), f32); vcol0[0:16] = 1
    return {
        'xk': xk, 'xq': xq,
        'wk_in': np.ascontiguousarray(np.concatenate([k_s, v_s, c_kv, k_r, k_i], 1)),
        'wq_in': np.ascontiguousarray(np.concatenate([c_q, q_s, q_i, w_i], 1)),
        'g_attn': pc(inp['attn_norm_g'][0], 8),
        'w_uq_n': np.ascontiguousarray(w_uq[:, :, :64].reshape(256, 512)),
        'w_uq_r': np.ascontiguousarray(w_uq[:, :, 64:].reshape(256, 256)),
        'g_q': pc(inp['mla_q_norm_g'][0], 2),
        'w_ukv_k': np.ascontiguousarray(w_ukv[:, :, :64].reshape(128, 512)),
        'w_ukv_v': np.ascontiguousarray(w_ukv[:, :, 64:].reshape(128, 512)),
        'g_kv': pc(inp['mla_kv_norm_g'][0], 1),
        'w_o': np.asarray(inp['w_o'][0], f32),
        'g_ffn': pc(inp['ffn_norm_g'][0], 8),
        'w_gate': np.asarray(inp['w_gate'][0], f32), 'w_up': np.asarray(inp['w_up'][0], f32),
        'w_down': np.asarray(inp['w_down'][0], f32),
        'g_fin': np.ascontiguousarray(np.broadcast_to(np.asarray(inp['final_norm_g'], f32)[None, :], (128, 1024))),
        'ropeK': ropeK, 'ropeQ': ropeQ,
        'ident_f': np.eye(128, dtype=f32), 'ident_b': np.eye(128, dtype=f32).astype(BF),
        'vcol0': vcol0.astype(BF), 'mk_mult': mk_mult, 'mk_add': mk_add,
    }


def kernel(**inputs):
    inp = {k: np.asarray(v) for k, v in inputs.items()}
    nc, _ = build()
    names = set(LAST_INPUTS)
    in_maps = []
    for core in range(8):
        ci = core_inputs(inp, core)
        in_maps.append({k: np.ascontiguousarray(v) for k, v in ci.items() if k in names})
    res = run_bass_kernel_spmd(nc, in_maps, core_ids=list(range(8)))
    out = np.zeros((4, 64, 128, 1024), np.float32)
    for core in range(8):
        b, par = core // 2, core % 2
        out[b, par::2] = np.asarray(res.results[core]["out"], np.float32).reshape(32, 128, 1024)
    return out.reshape(4, 8192, 1024)
```

```python
import numpy as np
import ml_dtypes
from contextlib import ExitStack
import concourse.bass as bass
import concourse.mybir as mybir
from concourse.bass_utils import run_bass_kernel_spmd

F32 = mybir.dt.float32
BF16 = mybir.dt.bfloat16
AF = mybir.ActivationFunctionType
ALU = mybir.AluOpType
AX = mybir.AxisListType

D = 1024
NQB = 32
NKB = 65
EPS = 1e-6
DFF = 2816
NFC = DFF // 128
TOPK = 256
NEG = -1.0e30
LAST_INPUTS = []
import os
CUT = float(os.environ.get('KCUT', '99'))


class Sched:
    ENG = ('pe', 'act', 'dve', 'pool', 'sp')

    def __init__(self):
        self.q = {e: [] for e in self.ENG}
        self.cnt = {}

    def add(self, eng, fn, deps=(), sig=False, dma=None):
        tok = None
        inc = None
        if dma is not None:
            self.cnt[dma] = self.cnt.get(dma, 0) + 16
            inc = (dma, 16)
            tok = (dma, self.cnt[dma])
        elif sig:
            name = 'c_' + eng
            self.cnt[name] = self.cnt.get(name, 0) + 1
            inc = (name, 1)
            tok = (name, self.cnt[name])
        dl = []
        for d in deps:
            if d is None:
                continue
            if isinstance(d, list):
                dl.extend(x for x in d if x is not None)
            else:
                dl.append(d)
        self.q[eng].append((fn, dl, inc))
        return tok

    def final_wait(self, eng='sp'):
        deps = [(n, v) for n, v in self.cnt.items()]
        self.q[eng].append((lambda e: e.nop(), deps, None))

    def run(self, nc, name, semstack=None):
        with ExitStack() as es:
            sems = {n: (semstack or es).enter_context(nc.semaphore(name + '_' + n)) for n in self.cnt}
            block = es.enter_context(nc.Block())

            def mk(engname):
                def body(eng):
                    waited = {}
                    for fn, deps, inc in self.q[engname]:
                        for (s, v) in deps:
                            if engname == 'pe' and s == 'c_pe':
                                continue
                            if waited.get(s, 0) < v:
                                eng.wait_ge(sems[s], v)
                                waited[s] = v
                        inst = fn(eng)
                        if inc is not None:
                            inst.then_inc(sems[inc[0]], inc[1])
                return body
            block.tensor(mk('pe'))
            block.scalar(mk('act'))
            block.vector(mk('dve'))
            block.gpsimd(mk('pool'))
            block.sync(mk('sp'))


def _bc(ap, shape_mid):
    return ap


def build(nkb=NKB, nqb=NQB, debug=False, phases='ABCD'):
    nc = bass.Bass("TRN2", target_bir_lowering=False)
    T = nkb * 128

    _din = {}

    def din(name, shape, dt=F32):
        if name not in _din:
            _din[name] = nc.dram_tensor(name, list(shape), dt, kind="ExternalInput").ap()
        return _din[name]

    def dscr(name, shape, dt):
        return nc.dram_tensor(name, list(shape), dt).ap()

    out = nc.dram_tensor("out", [NQB * 128, D], F32, kind="ExternalOutput").ap()

    s_ksT = dscr("s_ksT", [NKB, 128, 512], BF16)
    s_vds = dscr("s_vds", [NKB, 128, 520], BF16)
    s_kiT = dscr("s_kiT", [NKB, 64, 128], BF16)
    s_h1 = dscr("s_h1", [NQB * 128, D], F32)
    s_omla = dscr("s_omla", [NQB, 128, 512], BF16)
    s_odsa = dscr("s_odsa", [NQB, 128, 512], BF16)
    dbg = {}
    if debug:
        def dout(name, shape, dt=F32):
            t = nc.dram_tensor(name, list(shape), dt, kind="ExternalOutput").ap()
            dbg[name] = t
            return t
        d_knT = dout("d_knT", [128, 4 * T], BF16)
        d_krT = dout("d_krT", [32, T], BF16)
        d_vm = dout("d_vm", [128, nkb * 520], BF16)
        d_ksT = dout("d_ksT", [NKB, 128, 512], BF16)
        d_vds = dout("d_vds", [NKB, 128, 520], BF16)
        d_kiT = dout("d_kiT", [NKB, 64, 128], BF16)
        d_omla = dout("d_omla", [128, nqb * 512], BF16)
        d_h1 = dout("d_h1", [NQB * 128, D], F32)

    with ExitStack() as top:
        def sb(name, shape, dt, es=top):
            return es.enter_context(nc.sbuf_tensor(name, list(shape), dt))

        def ps(name, shape, dt, es=top):
            return es.enter_context(nc.psum_tensor(name, list(shape), dt))

        identF = sb("identF", [128, 128], F32)
        identB = sb("identB", [128, 128], BF16)
        epsT = sb("epsT", [128, 1], F32)
        abst = ExitStack()
        knT = sb("knT", [128, 4, T], BF16, abst)
        krT = sb("krT", [32, T], BF16, abst)
        vm = sb("vm", [128, nkb, 8, 65], BF16, abst)

        if 'A' in phases:
            with ExitStack() as pa:
                S = Sched()
                xk = din("xk", [nkb * 128, D])
                wk_in = din("wk_in", [D, 1248])
                g_attn = din("g_attn", [128, 8])
                w_ukv_k = din("w_ukv_k", [128, 512])
                w_ukv_v = din("w_ukv_v", [128, 512])
                g_kv = din("g_kv", [128, 1])
                ropeK = din("ropeK", [nkb, 128, 192])
                ident_f = din("ident_f", [128, 128])
                ident_b = din("ident_b", [128, 128], BF16)
                vcol0 = din("vcol0", [128, 8], BF16)
                Wk = sb("Wk", [128, 8, 1248], BF16, pa)
                WukK = sb("WukK", [128, 512], BF16, pa)
                WukV = sb("WukV", [128, 512], BF16, pa)
                gA = sb("gA", [128, 8], F32, pa)
                gKV = sb("gKV", [128, 1], F32, pa)
                stg = [sb("stgA0", [128, 1248], F32, pa)] * 2
                xs = [sb(f"xsA{i}", [128, D], F32, pa) for i in range(2)]
                tab = [sb(f"tabA{i}", [128, 192], F32, pa) for i in range(2)]
                tabs = [sb(f"tabsA{i}", [128, 192], F32, pa) for i in range(2)]
                junk = sb("junkA", [128, D], BF16, pa)
                ss = [sb(f"ssA{i}", [128, 4], F32, pa) for i in range(2)]
                ss2 = [sb(f"ss2A{i}", [128, 4], F32, pa) for i in range(2)]
                xT = [sb(f"xTA{i}", [128, 8, 128], BF16, pa) for i in range(2)]
                ra = sb("ra", [128, 512], F32, pa)
                rb = sb("rb", [128, 512], F32, pa)
                ksr = sb("ksr", [128, 512], BF16, pa)
                ri_a = sb("ri_a", [128, 64], F32, pa)
                ri_b = sb("ri_b", [128, 64], F32, pa)
                kir = sb("kir", [128, 64], BF16, pa)
                rr_a = sb("rr_a", [128, 32], F32, pa)
                rr_b = sb("rr_b", [128, 32], F32, pa)
                krr = sb("krr", [128, 32], BF16, pa)
                ckv = sb("ckv", [128, 128], F32, pa)
                junk2 = sb("junk2A", [128, 128], BF16, pa)
                ckvn = sb("ckvn", [128, 128], BF16, pa)
                ckvnT = sb("ckvnT", [128, 128], BF16, pa)
                vst = [sb(f"vstA{i}", [128, 8, 65], BF16, pa) for i in range(2)]
                kst = [sb(f"kstA{i}", [128, 512], BF16, pa) for i in range(2)]
                kit = [sb(f"kitA{i}", [64, 128], BF16, pa) for i in range(2)]
                pT = [ps(f"pTA{i}", [128, 512], F32, pa) for i in range(2)]
                pP = [ps(f"pPA{i}", [128, 512], F32, pa) for i in range(3)]
                pS = ps("pSA", [128, 1024], BF16, pa)
                pK = ps("pKA", [128, 512], F32, pa)
                pV = ps("pVA", [128, 512], F32, pa)

                S.add('sp', lambda e: e.dma_start(out=identF[:, :], in_=ident_f[:, :]), dma='cst')
                S.add('sp', lambda e: e.dma_start(out=identB[:, :], in_=ident_b[:, :]), dma='cst')
                S.add('sp', lambda e: e.dma_start(out=gA[:, :], in_=g_attn[:, :]), dma='cst')
                S.add('sp', lambda e: e.dma_start(out=gKV[:, :], in_=g_kv[:, :]), dma='cst')
                t_eps = S.add('dve', lambda e: e.memset(epsT[:, :], EPS), sig=True)
                t_vm1 = S.add('pool', lambda e: e.memset(vm[:, :, :, 64:65], 1.0), sig=True)
                t_vst1 = [None, S.add('pool', lambda e: e.memset(vst[1][:, :, 64:65], 1.0), sig=True)]
                S.add('sp', lambda e: e.dma_start(out=vst[0][:, :, 64:65], in_=vcol0[:, :].unsqueeze(2), allow_slow_non_contiguous=True), dma='cst')
                t_cst = S.add('sp', lambda e: e.dma_start(out=vm[:, 0, :, 64:65], in_=vcol0[:, :].unsqueeze(2), allow_slow_non_contiguous=True), deps=[t_vm1], dma='cst')
                t_id = t_cst
                t_vc0 = t_cst
                t_vst1[0] = t_cst
                stg_free = [None, None]
                t_w = []
                for c in range(8):
                    s = 0
                    td = S.add('sp', lambda e, c=c, s=s: e.dma_start(out=stg[s][:, :], in_=wk_in[c * 128:(c + 1) * 128, :]),
                               deps=[stg_free[s]], dma=f'stg{s}')
                    eng = 'dve' if s == 0 else 'pool'
                    stg_free[s] = S.add(eng, lambda e, c=c, s=s: e.tensor_scalar(
                        out=Wk[:, c, :], in0=stg[s][:, :], scalar1=gA[:, c:c + 1], scalar2=None, op0=ALU.mult),
                        deps=[td, t_cst], sig=True)
                    t_w.append(stg_free[s])
                for (dst, src, s) in ((WukK, w_ukv_k, 0), (WukV, w_ukv_v, 0)):
                    td = S.add('sp', lambda e, s=s, src=src: e.dma_start(out=stg[s][:, 0:512], in_=src[:, :]),
                               deps=[stg_free[s]], dma=f'stg{s}')
                    eng = 'dve' if s == 0 else 'pool'
                    stg_free[s] = S.add(eng, lambda e, s=s, dst=dst: e.tensor_scalar(
                        out=dst[:, :], in0=stg[s][:, 0:512], scalar1=gKV[:, 0:1], scalar2=None, op0=ALU.mult),
                        deps=[td, t_cst], sig=True)
                    t_w.append(stg_free[s])

                xs_free = [None, None]
                tab_free = [None, None]
                xT_free = [None, None]
                pT_free = [None, None]
                pP_free = [[None], [None], [None]]
                pS_free = [None]
                pK_free = None
                pV_free = None
                vst_free = [None, None]
                kst_free = [None, None]
                kit_free = [None, None]
                ss_free = [None, None]
                tmp_free = {}
                ckvnT_free = None
                for k in range(nkb):
                    s = k % 2
                    t_x = S.add('sp', lambda e, k=k, s=s: e.dma_start(out=xs[s][:, :], in_=xk[k * 128:(k + 1) * 128, :]),
                                deps=[xs_free[s], tab_free[s]], dma=f'ld{s}')
                    t_tab = S.add('sp', lambda e, k=k, s=s: e.dma_start(out=tab[s][:, :], in_=ropeK[k, :, :]),
                                  deps=[tab_free[s]], dma=f'ld{s}')
                    t_x = t_tab
                    t_ss = S.add('act', lambda e, s=s: e.activation(out=junk[:, :], in_=xs[s][:, :], func=AF.Square,
                                                                   accum_out=ss[s][:, 0:1]),
                                 deps=[t_x, ss_free[s]], sig=True)
                    t_sd = S.add('act', lambda e, s=s: e.activation(out=ss[s][:, 1:2], in_=ss[s][:, 0:1], func=AF.Sqrt,
                                                                   scale=1.0 / D, bias=epsT[:, 0:1]),
                                 deps=[t_ss, t_eps], sig=True)
                    t_rstd = S.add('dve', lambda e, s=s: e.reciprocal(out=ss[s][:, 2:3], in_=ss[s][:, 1:2]),
                                   deps=[t_sd], sig=True)
                    rstd = ss[s][:, 2:3]
                    if CUT <= 1:
                        continue
                    t_tr = []
                    for hlf in range(2):
                        for j in range(4):
                            c = hlf * 4 + j
                            tt = S.add('pe', lambda e, s=s, c=c, hlf=hlf, j=j: e.transpose(
                                out=pT[hlf][:, j * 128:(j + 1) * 128], in_=xs[s][:, c * 128:(c + 1) * 128],
                                identity=identF[:, :]),
                                deps=[t_x, t_id, pT_free[hlf]], sig=(j == 3))
                        t_tr.append(tt)
                    te0 = S.add('act', lambda e, s=s: e.activation(out=xT[s][:, 0:4, :], in_=pT[0][:, :], func=AF.Copy),
                                deps=[t_tr[0], xT_free[s]], sig=True)
                    te1 = S.add('dve', lambda e, s=s: e.tensor_copy(out=xT[s][:, 4:8, :], in_=pT[1][:, :]),
                                deps=[t_tr[1], xT_free[s]], sig=True)
                    pT_free = [te0, te1]
                    xs_free[s] = [te0, te1, t_ss]
                    if CUT <= 2:
                        continue
                    t_mm = []
                    for bnk, (c0, c1) in enumerate(((0, 512), (512, 1024), (1024, 1248))):
                        for c in range(8):
                            tt = S.add('pe', lambda e, s=s, c=c, bnk=bnk, c0=c0, c1=c1: e.matmul(
                                pP[bnk][:, 0:c1 - c0], lhsT=xT[s][:, c, :], rhs=Wk[:, c, c0:c1],
                                start=(c == 0), stop=(c == 7)),
                                deps=[te0, te1, pP_free[bnk], t_w], sig=(c == 7))
                        t_mm.append(tt)
                    xT_free[s] = t_mm[2]
                    if CUT <= 3:
                        continue
                    t_tabs = S.add('dve', lambda e, s=s: e.tensor_scalar(
                        out=tabs[s][:, :], in0=tab[s][:, :], scalar1=ss[s][:, 2:3], scalar2=None, op0=ALU.mult),
                        deps=[t_tab, t_rstd, tab_free[s]], sig=True)
                    t_a = S.add('dve', lambda e, s=s: e.tensor_tensor(
                        out=ra[:, :].rearrange("p (h d) -> p h d", h=8), in0=pP[0][:, :].rearrange("p (h d) -> p h d", h=8),
                        in1=bcast(tabs[s][:, 0:64], 8), op=ALU.mult),
                        deps=[t_mm[0], t_tabs, tmp_free.get('ra')], sig=True)
                    t_b1 = S.add('dve', lambda e, s=s: e.tensor_tensor(
                        out=rb[:, :].rearrange("p (h d) -> p h d", h=8)[:, :, 0:32],
                        in0=pP[0][:, :].rearrange("p (h d) -> p h d", h=8)[:, :, 32:64],
                        in1=bcast(tabs[s][:, 64:96], 8), op=ALU.mult),
                        deps=[t_mm[0], t_tabs, tmp_free.get('ra')], sig=True)
                    t_b2 = S.add('dve', lambda e, s=s: e.tensor_tensor(
                        out=rb[:, :].rearrange("p (h d) -> p h d", h=8)[:, :, 32:64],
                        in0=pP[0][:, :].rearrange("p (h d) -> p h d", h=8)[:, :, 0:32],
                        in1=bcast(tabs[s][:, 96:128], 8), op=ALU.mult),
                        deps=[t_mm[0], t_tabs, tmp_free.get('ra')], sig=True)
                    pP_free[0] = [t_a, t_b1, t_b2]
                    t_ksr = S.add('pool', lambda e: e.tensor_tensor(out=ksr[:, :], in0=ra[:, :], in1=rb[:, :], op=ALU.add),
                                  deps=[t_a, t_b1, t_b2, tmp_free.get('ksr')], sig=True)
                    tmp_free['ra'] = t_ksr
                    if CUT <= 4:
                        continue
                    t_v = S.add('act', lambda e, s=s: e.activation(
                        out=vst[s][:, :, 0:64], in_=pP[1][:, :].rearrange("p (h d) -> p h d", h=8), func=AF.Copy,
                        scale=ss[s][:, 2:3]),
                        deps=[t_mm[1], t_rstd, vst_free[s], t_vst1[s]], sig=True)
                    pP_free[1] = [t_v]
                    t_vd = S.add('sp', lambda e, s=s, k=k: e.dma_start(
                        out=s_vds[k, :, :], in_=vst[s][:, :, :].rearrange("p h d -> p (h d)")),
                        deps=[t_v], dma=f'st{s}')
                    if CUT <= 5:
                        continue
                    t_ckv = S.add('dve', lambda e, s=s: e.tensor_scalar(out=ckv[:, :], in0=pP[2][:, 0:128], scalar1=ss[s][:, 2:3],
                                                                       scalar2=None, op0=ALU.mult),
                                  deps=[t_mm[2], t_rstd, tmp_free.get('ckv')], sig=True)
                    t_ss2 = S.add('act', lambda e, s=s: e.activation(out=junk2[:, :], in_=ckv[:, :], func=AF.Square,
                                                                    accum_out=ss2[s][:, 0:1]),
                                  deps=[t_ckv, tmp_free.get('ss2%d' % s)], sig=True)
                    t_sd2 = S.add('act', lambda e, s=s: e.activation(out=ss2[s][:, 1:2], in_=ss2[s][:, 0:1], func=AF.Sqrt,
                                                                    scale=1.0 / 128, bias=epsT[:, 0:1]),
                                  deps=[t_ss2], sig=True)
                    t_r2 = S.add('dve', lambda e, s=s: e.reciprocal(out=ss2[s][:, 2:3], in_=ss2[s][:, 1:2]),
                                 deps=[t_sd2], sig=True)
                    t_ckvn = S.add('dve', lambda e, s=s: e.tensor_scalar(
                        out=ckvn[:, :], in0=ckv[:, :], scalar1=ss2[s][:, 2:3], scalar2=None, op0=ALU.mult),
                        deps=[t_r2, t_ckv, tmp_free.get('ckvn')], sig=True)
                    tmp_free['ckv'] = [t_ckvn, t_ss2]
                    tmp_free['ss2%d' % s] = t_ckvn
                    if CUT <= 6:
                        continue
                    t_ra = S.add('dve', lambda e, s=s: e.tensor_tensor(
                        out=rr_a[:, :], in0=pP[2][:, 128:160], in1=tabs[s][:, 128:160], op=ALU.mult),
                        deps=[t_mm[2], t_tabs, tmp_free.get('rr')], sig=True)
                    t_rb1 = S.add('dve', lambda e, s=s: e.tensor_tensor(
                        out=rr_b[:, 0:16], in0=pP[2][:, 144:160], in1=tabs[s][:, 160:176], op=ALU.mult),
                        deps=[t_mm[2], t_tabs, tmp_free.get('rr')], sig=True)
                    t_rb2 = S.add('dve', lambda e, s=s: e.tensor_tensor(
                        out=rr_b[:, 16:32], in0=pP[2][:, 128:144], in1=tabs[s][:, 176:192], op=ALU.mult),
                        deps=[t_mm[2], t_tabs, tmp_free.get('rr')], sig=True)
                    t_krr = S.add('pool', lambda e: e.tensor_tensor(out=krr[:, :], in0=rr_a[:, :], in1=rr_b[:, :], op=ALU.add),
                                  deps=[t_ra, t_rb1, t_rb2, tmp_free.get('krr')], sig=True)
                    tmp_free['rr'] = t_krr
                    t_ia = S.add('dve', lambda e, s=s: e.tensor_tensor(
                        out=ri_a[:, :], in0=pP[2][:, 160:224], in1=tabs[s][:, 0:64], op=ALU.mult),
                        deps=[t_mm[2], t_tabs, tmp_free.get('ri')], sig=True)
                    t_ib1 = S.add('dve', lambda e, s=s: e.tensor_tensor(
                        out=ri_b[:, 0:32], in0=pP[2][:, 192:224], in1=tabs[s][:, 64:96], op=ALU.mult),
                        deps=[t_mm[2], t_tabs, tmp_free.get('ri')], sig=True)
                    t_ib2 = S.add('dve', lambda e, s=s: e.tensor_tensor(
                        out=ri_b[:, 32:64], in0=pP[2][:, 160:192], in1=tabs[s][:, 96:128], op=ALU.mult),
                        deps=[t_mm[2], t_tabs, tmp_free.get('ri')], sig=True)
                    t_kir = S.add('pool', lambda e: e.tensor_tensor(out=kir[:, :], in0=ri_a[:, :], in1=ri_b[:, :], op=ALU.add),
                                  deps=[t_ia, t_ib1, t_ib2, tmp_free.get('kir')], sig=True)
                    tmp_free['ri'] = t_kir
                    pP_free[2] = [t_ckv, t_ra, t_rb1, t_rb2, t_ia, t_ib1, t_ib2]
                    tab_free[s] = [t_a, t_b1, t_b2, t_ra, t_rb1, t_rb2, t_ia, t_ib1, t_ib2]
                    ss_free[s] = [t_tabs, t_v, t_ckv]
                    if CUT <= 7:
                        continue
                    for a in range(4):
                        t_t1 = S.add('pe', lambda e, a=a: e.transpose(
                            out=pS[:, a * 128:(a + 1) * 128], in_=ksr[:, a * 128:(a + 1) * 128], identity=identB[:, :]),
                            deps=[t_ksr, pS_free], sig=(a == 3))
                    if CUT <= 7.1:
                        continue
                    t_t2 = S.add('pe', lambda e: e.transpose(out=pS[0:64, 512:640], in_=kir[:, :], identity=identB[:, :]),
                                 deps=[t_kir, pS_free], sig=True)
                    if CUT <= 7.2:
                        continue
                    t_t3 = S.add('pe', lambda e: e.transpose(out=pS[0:32, 640:768], in_=krr[:, :], identity=identB[:, :]),
                                 deps=[t_krr, pS_free], sig=True)
                    if CUT <= 7.3:
                        continue
                    t_t4 = S.add('pe', lambda e: e.transpose(out=pS[:, 768:896], in_=ckvn[:, :], identity=identB[:, :]),
                                 deps=[t_ckvn, pS_free], sig=True)
                    tmp_free['ksr'] = t_t1
                    tmp_free['kir'] = t_t2
                    tmp_free['krr'] = t_t3
                    tmp_free['ckvn'] = t_t4
                    if CUT <= 8:
                        continue
                    t_e1 = S.add('dve', lambda e, s=s: e.tensor_copy(out=kst[s][:, :], in_=pS[:, 0:512]),
                                 deps=[t_t4, kst_free[s]], sig=True)
                    if CUT <= 8.1:
                        continue
                    t_d1 = S.add('sp', lambda e, s=s, k=k: e.dma_start(out=s_ksT[k, :, :], in_=kst[s][:, :]),
                                 deps=[t_e1], dma=f'st{s}')
                    if CUT <= 8.2:
                        continue
                    t_e2 = S.add('dve', lambda e, s=s: e.tensor_copy(out=kit[s][:, :], in_=pS[0:64, 512:640]),
                                 deps=[t_t4, kit_free[s]], sig=True)
                    if CUT <= 8.3:
                        continue
                    t_d2 = S.add('sp', lambda e, s=s, k=k: e.dma_start(out=s_kiT[k, :, :], in_=kit[s][:, :]),
                                 deps=[t_e2], dma=f'st{s}')
                    kit_free[s] = t_d2
                    kst_free[s] = t_d2
                    vst_free[s] = t_d2
                    if k == 0:
                        vst_free[s] = S.add('pool', lambda e, s=s: e.memset(vst[s][:, :, 64:65], 1.0), deps=[t_d2], sig=True)
                    if CUT <= 8.4:
                        continue
                    t_e3 = S.add('dve', lambda e, k=k: e.tensor_copy(out=krT[:, k * 128:(k + 1) * 128], in_=pS[0:32, 640:768]),
                                 deps=[t_t4], sig=True)
                    if CUT <= 8.5:
                        continue
                    t_e4 = S.add('dve', lambda e: e.tensor_copy(out=ckvnT[:, :], in_=pS[:, 768:896]),
                                 deps=[t_t4, ckvnT_free], sig=True)
                    pS_free = [t_e1, t_e2, t_e3, t_e4]
                    if CUT <= 9:
                        continue
                    for a in range(4):
                        t_k = S.add('pe', lambda e, a=a: e.matmul(
                            pK[:, a * 128:(a + 1) * 128], lhsT=WukK[:, a * 128:(a + 1) * 128], rhs=ckvnT[:, :],
                            start=True, stop=True), deps=[t_e4, pK_free, t_w], sig=(a == 3))
                    t_vv = S.add('pe', lambda e: e.matmul(pV[:, :], lhsT=ckvnT[:, :], rhs=WukV[:, :], start=True, stop=True),
                                 deps=[t_e4, pV_free, t_w], sig=True)
                    ckvnT_free = t_vv
                    pK_free = S.add('act', lambda e, k=k: e.activation(
                        out=knT[:, :, k * 128:(k + 1) * 128], in_=pK[:, :].rearrange("p (a t) -> p a t", a=4), func=AF.Copy),
                        deps=[t_k], sig=True)
                    pV_free = S.add('dve', lambda e, k=k: e.tensor_copy(
                        out=vm[:, k, :, 0:64], in_=pV[:, :].rearrange("p (h d) -> p h d", h=8)),
                        deps=[t_vv, t_vm1, t_vc0], sig=True)
                last = [pK_free, pV_free, kst_free[0], kst_free[1], kit_free[0], kit_free[1], vst_free[0], vst_free[1]]
                if debug and CUT > 50:
                    S.add('sp', lambda e: e.dma_start(out=d_knT[:, :], in_=knT[:, :, :].rearrange("p a t -> p (a t)")),
                          deps=last, dma='dbg')
                    S.add('sp', lambda e: e.dma_start(out=d_krT[:, :], in_=krT[:, :]), deps=last, dma='dbg')
                    t_dbg = S.add('sp', lambda e: e.dma_start(out=d_vm[:, :], in_=vm[:, :, :, :].rearrange("p k h d -> p (k h d)")),
                                  deps=last, dma='dbg')
                    last = last + [t_dbg]
                S.final_wait()
                S.run(nc, "A", top)
                if debug and CUT > 50:
                    S2_ = Sched()
                    t1 = S2_.add('sp', lambda e: e.dma_start(out=d_ksT[0:nkb, :, :], in_=s_ksT[0:nkb, :, :]), dma='d')
                    t1 = S2_.add('sp', lambda e: e.dma_start(out=d_vds[0:nkb, :, :], in_=s_vds[0:nkb, :, :]), dma='d')
                    t1 = S2_.add('sp', lambda e: e.dma_start(out=d_kiT[0:nkb, :, :], in_=s_kiT[0:nkb, :, :]), dma='d')
                    S2_.add('sp', lambda e: e.nop(), deps=[t1])
                    S2_.run(nc, "Ad", top)
        if 'B' in phases:
            with ExitStack() as pb_:
                S = Sched()
                xq = din("xq", [nqb * 128, D])
                wq_in = din("wq_in", [D, 1288])
                g_attn = din("g_attn", [128, 8])
                w_uq_n = din("w_uq_n", [256, 512])
                w_uq_r = din("w_uq_r", [256, 256])
                g_q = din("g_q", [128, 2])
                ropeQ = din("ropeQ", [nqb, 128, 192])
                mk_mult = din("mk_mult", [2, 128, 128], BF16)
                SC = 96.0 ** -0.5
                Wcq = sb("Wcq", [128, 8, 256], BF16, pb_)
                WuqN = sb("WuqN", [128, 2, 512], BF16, pb_)
                WuqR = sb("WuqR", [128, 2, 256], BF16, pb_)
                gA = sb("gAB", [128, 8], F32, pb_)
                gQ = sb("gQB", [128, 2], F32, pb_)
                mkm = sb("mkmB", [128, 2, 128], BF16, pb_)
                stg = [sb(f"stgB{i}", [128, 512], F32, pb_) for i in range(2)]
                xs = sb("xsB", [128, D], F32, pb_)
                xb = sb("xbB", [128, D], BF16, pb_)
                tab = sb("tabB", [128, 192], F32, pb_)
                tabq = sb("tabqB", [128, 64], F32, pb_)
                junk = sb("junkB", [128, D], BF16, pb_)
                ss = sb("ssB", [128, 8], F32, pb_)
                xT = sb("xTB", [128, 8, 128], BF16, pb_)
                cq = sb("cqB", [128, 256], F32, pb_)
                cqn = sb("cqnB", [128, 256], BF16, pb_)
                cqnT = sb("cqnTB", [128, 2, 128], BF16, pb_)
                qbd = sb("qbdB", [128, 4, 256], BF16, pb_)
                qra = sb("qraB", [128, 256], F32, pb_)
                qrb = sb("qrbB", [128, 256], F32, pb_)
                qr = sb("qrB", [128, 256], BF16, pb_)
                qrT = sb("qrTB", [32, 1024], BF16, pb_)
                PT = [sb(f"PTB{i}", [128, 1024], BF16, pb_) for i in range(2)]
                rden = sb("rdenB", [128, 8], F32, pb_)
                ost = [sb(f"ostB{i}", [128, 512], BF16, pb_) for i in range(2)]
                pT = ps("pTB", [128, 1024], BF16, pb_)
                pQ = ps("pQB", [128, 512], F32, pb_)
                pST = [ps(f"pSTB{i}", [128, 512], F32, pb_) for i in range(4)]
                pO = [ps(f"pOB{i}", [128, 4, 65], F32, pb_) for i in range(2)]

                S.add('sp', lambda e: e.dma_start(out=gA[:, :], in_=g_attn[:, :]), dma='cst')
                S.add('sp', lambda e: e.dma_start(out=gQ[:, :], in_=g_q[:, :]), dma='cst')
                t_cst = S.add('sp', lambda e: e.dma_start(out=mkm[:, :, :], in_=mk_mult.rearrange("m k q -> k m q")), dma='cst')
                t_z = S.add('pool', lambda e: e.memset(qbd[:, :, :], 0.0), sig=True)
                stg_free = [None, None]
                t_w = []
                jobs = [(Wcq[:, c, :], wq_in[c * 128:(c + 1) * 128, 0:256], gA[:, c:c + 1], 256) for c in range(8)]
                jobs += [(WuqN[:, c, :], w_uq_n[c * 128:(c + 1) * 128, :], gQ[:, c:c + 1], 512) for c in range(2)]
                jobs += [(WuqR[:, c, :], w_uq_r[c * 128:(c + 1) * 128, :], gQ[:, c:c + 1], 256) for c in range(2)]
                for j, (dst, src, gs, n) in enumerate(jobs):
                    s = j % 2
                    td = S.add('sp', lambda e, s=s, src=src, n=n: e.dma_start(out=stg[s][:, 0:n], in_=src),
                               deps=[stg_free[s]], dma=f'stg{s}')
                    stg_free[s] = S.add('dve' if s == 0 else 'pool', lambda e, s=s, dst=dst, gs=gs, n=n: e.tensor_scalar(
                        out=dst, in0=stg[s][:, 0:n], scalar1=gs, scalar2=None, op0=ALU.mult), deps=[td, t_cst], sig=True)
                    t_w.append(stg_free[s])

                prev = []
                pst_free = [None] * 4
                PT_free = [None, None]
                ost_free = [None, None]
                t_onorm = None
                for i in range(nqb):
                    nk = min(2 * i + 3, nkb)
                    t_x = S.add('sp', lambda e, i=i: e.dma_start(out=xs[:, :], in_=xq[i * 128:(i + 1) * 128, :]), deps=prev, dma='ld')
                    t_x = S.add('sp', lambda e, i=i: e.dma_start(out=tab[:, :], in_=ropeQ[i, :, :]), deps=prev, dma='ld')
                    t_ss = S.add('act', lambda e: e.activation(out=junk[:, :], in_=xs[:, :], func=AF.Square, accum_out=ss[:, 0:1]),
                                 deps=[t_x] + prev, sig=True)
                    t_sd = S.add('act', lambda e: e.activation(out=ss[:, 1:2], in_=ss[:, 0:1], func=AF.Sqrt, scale=1.0 / D, bias=epsT[:, 0:1]),
                                 deps=[t_ss], sig=True)
                    t_rstd = S.add('dve', lambda e: e.reciprocal(out=ss[:, 2:3], in_=ss[:, 1:2]), deps=[t_sd], sig=True)
                    t_xb = S.add('dve', lambda e: e.tensor_copy(out=xb[:, :], in_=xs[:, :]), deps=[t_x] + prev, sig=True)
                    t_tq = S.add('dve', lambda e: e.tensor_scalar(out=tabq[:, :], in0=tab[:, 128:192], scalar1=SC, scalar2=None, op0=ALU.mult),
                                 deps=[t_x] + prev, sig=True)
                    for c in range(8):
                        t_tr = S.add('pe', lambda e, c=c: e.transpose(out=pT[:, c * 128:(c + 1) * 128], in_=xb[:, c * 128:(c + 1) * 128],
                                                                      identity=identB[:, :]), deps=[t_xb] + prev, sig=(c == 7))
                    t_xT = S.add('dve', lambda e: e.tensor_copy(out=xT[:, :, :], in_=pT[:, :].rearrange("p (c t) -> p c t", c=8)),
                                 deps=[t_tr], sig=True)
                    for c in range(8):
                        t_mm = S.add('pe', lambda e, c=c: e.matmul(pQ[:, 0:256], lhsT=xT[:, c, :], rhs=Wcq[:, c, :], start=(c == 0), stop=(c == 7)),
                                     deps=[t_xT, t_w] + prev, sig=(c == 7))
                    t_cq = S.add('dve', lambda e: e.tensor_scalar(out=cq[:, :], in0=pQ[:, 0:256], scalar1=ss[:, 2:3], scalar2=None, op0=ALU.mult),
                                 deps=[t_mm, t_rstd], sig=True)
                    t_ss2 = S.add('act', lambda e: e.activation(out=junk[:, 0:256], in_=cq[:, :], func=AF.Square, accum_out=ss[:, 4:5]),
                                  deps=[t_cq], sig=True)
                    t_sd2 = S.add('act', lambda e: e.activation(out=ss[:, 5:6], in_=ss[:, 4:5], func=AF.Sqrt, scale=1.0 / 256, bias=epsT[:, 0:1]),
                                  deps=[t_ss2], sig=True)
                    t_r2 = S.add('dve', lambda e: e.reciprocal(out=ss[:, 6:7], in_=ss[:, 5:6]), deps=[t_sd2], sig=True)
                    t_cqn = S.add('dve', lambda e: e.tensor_scalar(out=cqn[:, :], in0=cq[:, :], scalar1=ss[:, 6:7], scalar2=None, op0=ALU.mult),
                                  deps=[t_r2], sig=True)
                    for c in range(2):
                        t_tr = S.add('pe', lambda e, c=c: e.transpose(out=pT[:, c * 128:(c + 1) * 128], in_=cqn[:, c * 128:(c + 1) * 128],
                                                                      identity=identB[:, :]), deps=[t_cqn, t_xT], sig=(c == 1))
                    t_cT = S.add('dve', lambda e: e.tensor_copy(out=cqnT[:, :, :], in_=pT[:, 0:256].rearrange("p (c t) -> p c t", c=2)),
                                 deps=[t_tr], sig=True)
                    for a in range(4):
                        for c in range(2):
                            t_mm = S.add('pe', lambda e, a=a, c=c: e.matmul(pQ[:, a * 128:(a + 1) * 128], lhsT=WuqN[:, c, a * 128:(a + 1) * 128],
                                                                            rhs=cqnT[:, c, :], start=(c == 0), stop=(c == 1)),
                                         deps=[t_cT, t_cq], sig=(a == 3 and c == 1))
                    pQv = pQ[:, :].rearrange("p (a t) -> p a t", a=4)
                    t_q1 = S.add('dve', lambda e: e.tensor_scalar(out=qbd[0:64, :, 0:128], in0=pQ[:, :].rearrange("p (a t) -> p a t", a=4)[0:64, :, :],
                                                                  scalar1=SC, scalar2=None, op0=ALU.mult), deps=[t_mm, t_z] + prev, sig=True)
                    t_q2 = S.add('dve', lambda e: e.tensor_scalar(out=qbd[64:128, :, 128:256], in0=pQ[:, :].rearrange("p (a t) -> p a t", a=4)[64:128, :, :],
                                                                  scalar1=SC, scalar2=None, op0=ALU.mult), deps=[t_mm, t_z] + prev, sig=True)
                    for c in range(2):
                        t_mm = S.add('pe', lambda e, c=c: e.matmul(pQ[:, 0:256], lhsT=cqnT[:, c, :], rhs=WuqR[:, c, :], start=(c == 0), stop=(c == 1)),
                                     deps=[t_q1, t_q2], sig=(c == 1))
                    v3 = lambda ap: ap.rearrange("p (h d) -> p h d", h=8)
                    t_a = S.add('dve', lambda e: e.tensor_tensor(out=v3(qra[:, :]), in0=v3(pQ[:, 0:256]), in1=bcast(tabq[:, 0:32], 8), op=ALU.mult),
                                deps=[t_mm, t_tq] + prev, sig=True)
                    t_b1 = S.add('dve', lambda e: e.tensor_tensor(out=v3(qrb[:, :])[:, :, 0:16], in0=v3(pQ[:, 0:256])[:, :, 16:32],
                                                                  in1=bcast(tabq[:, 32:48], 8), op=ALU.mult), deps=[t_mm, t_tq] + prev, sig=True)
                    t_b2 = S.add('dve', lambda e: e.tensor_tensor(out=v3(qrb[:, :])[:, :, 16:32], in0=v3(pQ[:, 0:256])[:, :, 0:16],
                                                                  in1=bcast(tabq[:, 48:64], 8), op=ALU.mult), deps=[t_mm, t_tq] + prev, sig=True)
                    t_qr = S.add('pool', lambda e: e.tensor_tensor(out=qr[:, :], in0=qra[:, :], in1=qrb[:, :], op=ALU.add),
                                 deps=[t_a, t_b1, t_b2] + prev, sig=True)
                    for h in range(8):
                        t_tr = S.add('pe', lambda e, h=h: e.transpose(out=pT[0:32, h * 128:(h + 1) * 128], in_=qr[:, h * 32:(h + 1) * 32],
                                                                      identity=identB[:, :]), deps=[t_qr, t_cT], sig=(h == 7))
                    t_qrT = S.add('dve', lambda e: e.tensor_copy(out=qrT[:, :], in_=pT[0:32, :]), deps=[t_tr] + prev, sig=True)
                    qready = [t_q1, t_q2, t_qrT]
                    prev_q = [t_b2, t_qrT, t_ss2, t_cqn]
                    t_exp = {}
                    t_pv = None

                    def emit_qk(kb):
                        for g in range(2):
                            bank = pST[(kb % 2) * 2 + g]
                            for j in range(2):
                                a = g * 2 + j
                                S.add('pe', lambda e, bank=bank, j=j, a=a, kb=kb: e.matmul(
                                    bank[:, j * 256:(j + 1) * 256], lhsT=knT[:, a, kb * 128:(kb + 1) * 128], rhs=qbd[:, a, :],
                                    start=(j == 0), stop=False, skip_group_check=True),
                                    deps=qready + [pst_free[(kb % 2) * 2 + g]])
                            t = S.add('pe', lambda e, bank=bank, g=g, kb=kb: e.matmul(
                                bank[:, :], lhsT=krT[0:32, kb * 128:(kb + 1) * 128], rhs=qrT[0:32, g * 512:(g + 1) * 512],
                                start=False, stop=True, skip_group_check=True), deps=qready, sig=True)
                            t_qk[(kb, g)] = t
                    t_qk = {}

                    def emit_exp(kb):
                        for g in range(2):
                            bi = (kb % 2) * 2 + g
                            t = S.add('act', lambda e, bi=bi, kb=kb, g=g: e.activation(
                                out=PT[kb % 2][:, g * 512:(g + 1) * 512], in_=pST[bi][:, :], func=AF.Exp),
                                deps=[t_qk[(kb, g)], PT_free[kb % 2]], sig=True)
                            pst_free[bi] = t
                            mi = kb - (2 * i + 1)
                            if mi >= 0:
                                t = S.add('pool', lambda e, kb=kb, g=g, mi=mi: e.tensor_tensor(
                                    out=PT[kb % 2][:, g * 512:(g + 1) * 512].rearrange("p (h q) -> p h q", h=4),
                                    in0=PT[kb % 2][:, g * 512:(g + 1) * 512].rearrange("p (h q) -> p h q", h=4),
                                    in1=bcast(mkm[:, mi, :], 4), op=ALU.mult), deps=[t, t_cst], sig=True)
                            t_exp[(kb, g)] = t

                    def emit_pv(kb):
                        nonlocal t_pv
                        for h in range(8):
                            t_pv = S.add('pe', lambda e, h=h, kb=kb: e.matmul(
                                pO[h // 4][:, h % 4, :], lhsT=PT[kb % 2][:, h * 128:(h + 1) * 128], rhs=vm[:, kb, h, :],
                                start=(kb == 0 and h % 4 == 0), stop=(kb == nk - 1), skip_group_check=True),
                                deps=[t_exp[(kb, h // 4)], t_onorm], sig=(h == 7))
                        PT_free[kb % 2] = t_pv

                    emit_qk(0)
                    for kb in range(nk):
                        if kb + 1 < nk:
                            emit_qk(kb + 1)
                        emit_exp(kb)
                        emit_pv(kb)
                    so = i % 2
                    t_rd = S.add('dve', lambda e: e.reciprocal(out=rden[:, 0:4], in_=pO[0][:, :, 64]), deps=[t_pv], sig=True)
                    t_rd2 = S.add('dve', lambda e: e.reciprocal(out=rden[:, 4:8], in_=pO[1][:, :, 64]), deps=[t_pv], sig=True)
                    for g in range(2):
                        t_onorm = S.add('dve', lambda e, g=g, so=so: e.tensor_tensor(
                            out=ost[so][:, g * 256:(g + 1) * 256].rearrange("p (h d) -> p h d", h=4), in0=pO[g][:, :, 0:64],
                            in1=rden[:, g * 4:(g + 1) * 4].unsqueeze(2).broadcast_to([128, 4, 64]), op=ALU.mult),
                            deps=[t_rd, t_rd2, ost_free[so]], sig=True)
                    ost_free[so] = S.add('sp', lambda e, i=i, so=so: e.dma_start(out=s_omla[i, :, :], in_=ost[so][:, :]),
                                         deps=[t_onorm], dma=f'ost{so}')
                    prev = [t_pv, t_onorm] + prev_q
                if debug:
                    S.final_wait()
                    S.add('sp', lambda e: e.dma_start(out=d_omla.rearrange("p (i f) -> i p f", i=nqb), in_=s_omla[0:nqb, :, :]), dma='dbg')
                S.final_wait()
                S.run(nc, "B", top)
        abst.close()
        if 'C' in phases:
            with ExitStack() as pc_:
                S = Sched()
                xq = din("xq", [nqb * 128, D])
                wq_in = din("wq_in", [D, 1288])
                g_attn = din("g_attn", [128, 8])
                ropeQ = din("ropeQ", [nqb, 128, 192])
                mk_add = din("mk_add", [3, 128, 128])
                NIT = 16
                Wq2 = sb("Wq2C", [128, 8, 1032], BF16, pc_)
                gA = sb("gAC", [128, 8], F32, pc_)
                mka = sb("mkaC", [128, 3, 128], F32, pc_)
                stg = [sb(f"stgC{i}", [128, 1032], F32, pc_) for i in range(2)]
                ksT = sb("ksTC", [128, nkb, 512], BF16, pc_)
                kiT2 = sb("kiT2C", [128, nkb, 128], BF16, pc_)
                Isc = sb("IscC", [128, nkb * 128], F32, pc_)
                Msk = sb("MskC", [128, nkb * 128], BF16, pc_)
                xs = sb("xsC", [128, D], F32, pc_)
                xb = sb("xbC", [128, D], BF16, pc_)
                tab = sb("tabC", [128, 192], F32, pc_)
                tabs = sb("tabsC", [128, 128], F32, pc_)
                junk = sb("junkC", [128, D], BF16, pc_)
                ss = sb("ssC", [128, 8], F32, pc_)
                xT = sb("xTC", [128, 8, 128], BF16, pc_)
                ra = sb("raC", [128, 512], F32, pc_)
                rb = sb("rbC", [128, 512], F32, pc_)
                qs = sb("qsC", [128, 512], BF16, pc_)
                qi = sb("qiC", [128, 512], BF16, pc_)
                wv = sb("wvC", [128, 8], F32, pc_)
                qbd = sb("qbdC", [128, 4, 256], BF16, pc_)
                qiT = sb("qiTC", [128, 4, 128], BF16, pc_)
                tmpR = [sb(f"tmpRC{i}", [128, 512], F32, pc_) for i in range(4)]
                pw2 = sb("pw2C", [128, 32], F32, pc_)
                hwt = sb("hwtC", [128, 32], F32, pc_)
                bs = sb("bsC", [128, 16], F32, pc_)
                PT = [sb(f"PTC{i}", [128, 1024], BF16, pc_) for i in range(2)]
                vb = [sb(f"vbC{i}", [128, 8, 65], BF16, pc_) for i in range(3)]
                rden = sb("rdenC", [128, 8], F32, pc_)
                ost = [sb(f"ostC{i}", [128, 512], BF16, pc_) for i in range(2)]
                pT = [ps(f"pTC{i}", [128, 1024], BF16, pc_) for i in range(2)]
                pST = [ps(f"pSTC{i}", [128, 512], F32, pc_) for i in range(4)]
                pO = [ps(f"pOC{i}", [128, 4, 65], F32, pc_) for i in range(2)]

                S.add('sp', lambda e: e.dma_start(out=gA[:, :], in_=g_attn[:, :]), dma='cst')
                S.add('sp', lambda e: e.dma_start(out=mka[:, :, :], in_=mk_add.rearrange("m q k -> q m k")), dma='cst')
                for k0 in range(0, nkb, 4):
                    k1 = min(nkb, k0 + 4)
                    S.add('sp', lambda e, k0=k0, k1=k1: e.dma_start(out=ksT[:, k0:k1, :], in_=s_ksT[k0:k1, :, :].rearrange("k p f -> p k f")), dma='cst')
                for k0 in range(0, nkb, 8):
                    k1 = min(nkb, k0 + 8)
                    S.add('sp', lambda e, k0=k0, k1=k1: e.dma_start(out=kiT2[0:64, k0:k1, :], in_=s_kiT[k0:k1, :, :].rearrange("k p t -> p k t")), dma='cst')
                    t_cst = S.add('sp', lambda e, k0=k0, k1=k1: e.dma_start(out=kiT2[64:128, k0:k1, :], in_=s_kiT[k0:k1, :, :].rearrange("k p t -> p k t")), dma='cst')
                t_z = S.add('pool', lambda e: e.memset(qbd[:, :, :], 0.0), sig=True)
                for it in range(NIT):
                    t_pw2 = S.add('pool', lambda e, it=it: e.memset(pw2[:, it:it + 1], 2.0 ** -(it + 1)), sig=True)
                stg_free = [None, None]
                t_w = []
                for c in range(8):
                    s = c % 2
                    td = S.add('sp', lambda e, s=s, c=c: e.dma_start(out=stg[s][:, :], in_=wq_in[c * 128:(c + 1) * 128, 256:1288]),
                               deps=[stg_free[s]], dma=f'stg{s}')
                    stg_free[s] = S.add(('dve', 'pool')[s], lambda e, s=s, c=c: e.tensor_scalar(
                        out=Wq2[:, c, :], in0=stg[s][:, :], scalar1=gA[:, c:c + 1], scalar2=None, op0=ALU.mult), deps=[td, t_cst], sig=True)
                    t_w.append(stg_free[s])
                v3 = lambda ap: ap.rearrange("p (h d) -> p h d", h=8)
                prev = []
                pst_free = [None] * 4
                PT_free = [None, None]
                pT_free = [None, None]
                vb_free = [None] * 3
                ost_free = [None, None]
                t_onorm = None
                vcount = 0
                for i in range(nqb):
                    nk = min(2 * i + 3, nkb)
                    W = nk * 128
                    S.add('sp', lambda e, i=i: e.dma_start(out=xs[:, :], in_=xq[i * 128:(i + 1) * 128, :]), deps=prev, dma='ld')
                    t_x = S.add('sp', lambda e, i=i: e.dma_start(out=tab[:, :], in_=ropeQ[i, :, :]), deps=prev, dma='ld')
                    t_ss = S.add('act', lambda e: e.activation(out=junk[:, :], in_=xs[:, :], func=AF.Square, accum_out=ss[:, 0:1]), deps=[t_x] + prev, sig=True)
                    t_sd = S.add('act', lambda e: e.activation(out=ss[:, 1:2], in_=ss[:, 0:1], func=AF.Sqrt, scale=1.0 / D, bias=epsT[:, 0:1]), deps=[t_ss], sig=True)
                    t_rstd = S.add('dve', lambda e: e.reciprocal(out=ss[:, 2:3], in_=ss[:, 1:2]), deps=[t_sd], sig=True)
                    t_r8 = S.add('dve', lambda e: e.tensor_scalar(out=ss[:, 3:4], in0=ss[:, 2:3], scalar1=0.125, scalar2=None, op0=ALU.mult), deps=[t_rstd], sig=True)
                    t_tabs = S.add('dve', lambda e: e.tensor_scalar(out=tabs[:, :], in0=tab[:, 0:128], scalar1=ss[:, 3:4], scalar2=None, op0=ALU.mult),
                                   deps=[t_r8, t_x] + prev, sig=True)
                    t_xb = S.add('dve', lambda e: e.tensor_copy(out=xb[:, :], in_=xs[:, :]), deps=[t_x] + prev, sig=True)
                    for c in range(8):
                        t_tr = S.add('pe', lambda e, c=c: e.transpose(out=pT[0][:, c * 128:(c + 1) * 128], in_=xb[:, c * 128:(c + 1) * 128],
                                                                      identity=identB[:, :]), deps=[t_xb] + prev, sig=(c == 7))
                    t_xT = S.add('dve', lambda e: e.tensor_copy(out=xT[:, :, :], in_=pT[0][:, :].rearrange("p (c t) -> p c t", c=8)), deps=[t_tr], sig=True)
                    t_mms = []
                    for bnk, (c0, c1) in enumerate(((0, 512), (512, 1024), (1024, 1032))):
                        for c in range(8):
                            t_mm = S.add('pe', lambda e, c=c, bnk=bnk, c0=c0, c1=c1: e.matmul(pST[bnk][:, 0:c1 - c0], lhsT=xT[:, c, :], rhs=Wq2[:, c, c0:c1],
                                                                                             start=(c == 0), stop=(c == 7)), deps=[t_xT, t_w] + prev, sig=(c == 7))
                        t_mms.append(t_mm)
                    outs = []
                    for bnk, dst in ((0, qs), (1, qi)):
                        t_a = S.add('dve', lambda e, bnk=bnk: e.tensor_tensor(out=v3(ra[:, :]), in0=v3(pST[bnk][:, :]), in1=bcast(tabs[:, 0:64], 8), op=ALU.mult),
                                    deps=[t_mms[bnk], t_tabs] + outs + prev, sig=True)
                        t_b1 = S.add('dve', lambda e, bnk=bnk: e.tensor_tensor(out=v3(rb[:, :])[:, :, 0:32], in0=v3(pST[bnk][:, :])[:, :, 32:64],
                                                                              in1=bcast(tabs[:, 64:96], 8), op=ALU.mult), deps=[t_mms[bnk], t_tabs] + outs + prev, sig=True)
                        t_b2 = S.add('dve', lambda e, bnk=bnk: e.tensor_tensor(out=v3(rb[:, :])[:, :, 32:64], in0=v3(pST[bnk][:, :])[:, :, 0:32],
                                                                              in1=bcast(tabs[:, 96:128], 8), op=ALU.mult), deps=[t_mms[bnk], t_tabs] + outs + prev, sig=True)
                        t_q = S.add('pool', lambda e, dst=dst: e.tensor_tensor(out=dst[:, :], in0=ra[:, :], in1=rb[:, :], op=ALU.add),
                                    deps=[t_a, t_b1, t_b2] + prev, sig=True)
                        outs = [t_q]
                        if bnk == 0:
                            t_qs = t_q
                        else:
                            t_qi = t_q
                    t_wv = S.add('dve', lambda e: e.tensor_scalar(out=wv[:, :], in0=pST[2][:, 0:8], scalar1=ss[:, 2:3], scalar2=8.0 ** -0.5,
                                                                  op0=ALU.mult, op1=ALU.mult), deps=[t_mms[2], t_rstd] + prev, sig=True)
                    pst_q = [t_b2, t_wv]
                    for a in range(4):
                        t_tr = S.add('pe', lambda e, a=a: e.transpose(out=pT[0][:, a * 128:(a + 1) * 128], in_=qs[:, a * 128:(a + 1) * 128],
                                                                      identity=identB[:, :]), deps=[t_qs, t_xT], sig=(a == 3))
                    for a in range(4):
                        t_tr2 = S.add('pe', lambda e, a=a: e.transpose(out=pT[1][:, a * 128:(a + 1) * 128], in_=qi[:, a * 128:(a + 1) * 128],
                                                                       identity=identB[:, :]), deps=[t_qi, pT_free[1]] + prev, sig=(a == 3))
                    pTv = lambda t: t[:, 0:512].rearrange("p (a t) -> p a t", a=4)
                    t_q1 = S.add('dve', lambda e: e.tensor_copy(out=qbd[0:64, :, 0:128], in_=pTv(pT[0])[0:64, :, :]), deps=[t_tr, t_z] + prev, sig=True)
                    t_q2 = S.add('dve', lambda e: e.tensor_copy(out=qbd[64:128, :, 128:256], in_=pTv(pT[0])[64:128, :, :]), deps=[t_tr, t_z] + prev, sig=True)
                    t_qiT = S.add('dve', lambda e: e.tensor_copy(out=qiT[:, :, :], in_=pTv(pT[1])), deps=[t_tr2] + prev, sig=True)
                    pT_free = [t_q2, t_qiT]
                    t_acc = None
                    tmp_free = [None] * 4
                    nch = (W + 511) // 512
                    cnt_ = 0
                    for ch in range(nch):
                        c0 = ch * 512
                        n = min(512, W - c0)
                        for h in range(8):
                            hp = (h % 2) * 64
                            bnk = h % 4
                            t_lg = S.add('pe', lambda e, hp=hp, h=h, bnk=bnk, c0=c0, n=n: e.matmul(
                                pST[bnk][:, 0:n], lhsT=qiT[hp:hp + 64, h // 2, :],
                                rhs=kiT2[hp:hp + 64, :, :].rearrange("p k t -> p (k t)")[:, c0:c0 + n], start=True, stop=True),
                                deps=[t_qiT, t_cst, pst_free[bnk]] + pst_q, sig=True)
                            sl = cnt_ % 4
                            cnt_ += 1
                            t_r = S.add('act', lambda e, bnk=bnk, sl=sl, n=n: e.activation(out=tmpR[sl][:, 0:n], in_=pST[bnk][:, 0:n], func=AF.Relu),
                                        deps=[t_lg, tmp_free[sl]], sig=True)
                            pst_free[bnk] = t_r
                            if h == 0:
                                t_acc = S.add('dve', lambda e, sl=sl, c0=c0, n=n: e.tensor_scalar(
                                    out=Isc[:, c0:c0 + n], in0=tmpR[sl][:, 0:n], scalar1=wv[:, 0:1], scalar2=None, op0=ALU.mult),
                                    deps=[t_r, t_wv, t_acc] + prev, sig=True)
                            else:
                                t_acc = S.add('dve', lambda e, sl=sl, c0=c0, n=n, h=h: e.scalar_tensor_tensor(
                                    out=Isc[:, c0:c0 + n], in0=tmpR[sl][:, 0:n], scalar=wv[:, h:h + 1], in1=Isc[:, c0:c0 + n],
                                    op0=ALU.mult, op1=ALU.add), deps=[t_r, t_wv, t_acc], sig=True)
                            tmp_free[sl] = t_acc
                    t_mx = S.add('dve', lambda e, W=W: e.tensor_reduce(out=bs[:, 1:2], in_=Isc[:, 0:W], axis=AX.X, op=ALU.max), deps=[t_acc] + prev, sig=True)
                    t_mn = S.add('dve', lambda e, W=W: e.tensor_reduce(out=bs[:, 0:1], in_=Isc[:, 0:W], axis=AX.X, op=ALU.min), deps=[t_acc] + prev, sig=True)
                    t_b = S.add('dve', lambda e: e.tensor_scalar(out=bs[:, 1:2], in0=bs[:, 1:2], scalar1=1.0, scalar2=None, op0=ALU.add), deps=[t_mx], sig=True)
                    t_b = S.add('dve', lambda e: e.tensor_scalar(out=bs[:, 0:1], in0=bs[:, 0:1], scalar1=-1.0, scalar2=None, op0=ALU.add), deps=[t_mn, t_b], sig=True)
                    for (mi, kb) in ((0, 0), (1, nk - 2), (2, nk - 1)):
                        t_b = S.add('dve', lambda e, mi=mi, kb=kb: e.tensor_tensor(out=Isc[:, kb * 128:(kb + 1) * 128], in0=Isc[:, kb * 128:(kb + 1) * 128],
                                                                                  in1=mka[:, mi, :], op=ALU.add), deps=[t_b, t_mn, t_mx, t_cst], sig=True)
                    W1 = (nk // 2) * 128
                    W2 = W - W1
                    t_b = S.add('dve', lambda e: e.tensor_tensor(out=bs[:, 7:8], in0=bs[:, 1:2], in1=bs[:, 0:1], op=ALU.subtract), deps=[t_b], sig=True)
                    t_b = S.add('dve', lambda e: e.tensor_scalar(out=hwt[:, 0:NIT], in0=pw2[:, 0:NIT], scalar1=bs[:, 7:8], scalar2=None, op0=ALU.mult),
                                deps=[t_b, t_pw2], sig=True)
                    t_b = S.add('dve', lambda e: e.tensor_tensor(out=bs[:, 2:3], in0=bs[:, 0:1], in1=hwt[:, 0:1], op=ALU.add), deps=[t_b], sig=True)
                    for it in range(NIT):
                        t_c1 = S.add('dve', lambda e, W1=W1: e.tensor_scalar(out=Msk[:, 0:W1], in0=Isc[:, 0:W1], scalar1=bs[:, 2:3], scalar2=0.0,
                                                                          op0=ALU.is_ge, op1=ALU.add, accum_out=bs[:, 3:4]), deps=[t_b] + prev, sig=True)
                        t_c2 = S.add('act', lambda e, W1=W1, W=W: e.activation(out=Msk[:, W1:W], in_=Isc[:, W1:W], func=AF.Sign, scale=-1.0,
                                                                             bias=bs[:, 2:3], accum_out=bs[:, 8:9]), deps=[t_b] + prev, sig=True)
                        t_b = S.add('dve', lambda e: e.scalar_tensor_tensor(out=bs[:, 9:10], in0=bs[:, 8:9], scalar=-0.5, in1=bs[:, 3:4],
                                                                            op0=ALU.mult, op1=ALU.add), deps=[t_c1, t_c2], sig=True)
                        t_b = S.add('dve', lambda e, W2=W2: e.tensor_scalar(out=bs[:, 4:5], in0=bs[:, 9:10], scalar1=float(TOPK) - 0.5 - W2 / 2.0,
                                                                          scalar2=None, op0=ALU.is_ge), deps=[t_b], sig=True)
                        t_b = S.add('dve', lambda e, it=it: e.scalar_tensor_tensor(out=bs[:, 0:1], in0=bs[:, 4:5], scalar=hwt[:, it:it + 1], in1=bs[:, 0:1],
                                                                                   op0=ALU.mult, op1=ALU.add), deps=[t_b], sig=True)
                        if it + 1 < NIT:
                            t_b = S.add('dve', lambda e, it=it: e.tensor_tensor(out=bs[:, 2:3], in0=bs[:, 0:1], in1=hwt[:, it + 1:it + 2], op=ALU.add),
                                        deps=[t_b], sig=True)
                    t_msk = S.add('dve', lambda e, W=W: e.tensor_scalar(out=Msk[:, 0:W], in0=Isc[:, 0:W], scalar1=bs[:, 0:1], scalar2=None, op0=ALU.is_ge),
                                  deps=[t_b], sig=True)
                    qready = [t_q1, t_q2, t_msk]
                    t_qk = {}
                    t_exp = {}
                    t_v = {}
                    t_pv = None

                    def emit_qk(kb):
                        for g in range(2):
                            bi = (kb % 2) * 2 + g
                            for j in range(2):
                                a = g * 2 + j
                                t = S.add('pe', lambda e, bi=bi, j=j, a=a, kb=kb: e.matmul(
                                    pST[bi][:, j * 256:(j + 1) * 256], lhsT=ksT[:, kb, a * 128:(a + 1) * 128], rhs=qbd[:, a, :],
                                    start=(j == 0), stop=(j == 1), skip_group_check=True), deps=qready + [pst_free[bi], t_cst], sig=(j == 1))
                            t_qk[(kb, g)] = t
                        t_qk[(kb, 'm')] = S.add('pe', lambda e, kb=kb: e.transpose(out=pT[kb % 2][:, 0:128], in_=Msk[:, kb * 128:(kb + 1) * 128],
                                                                                  identity=identB[:, :]), deps=[t_msk, pT_free[kb % 2]], sig=True)

                    def emit_v(kb):
                        nonlocal vcount
                        sl = vcount % 3
                        vcount += 1
                        t_v[kb] = (S.add('sp', lambda e, kb=kb, sl=sl: e.dma_start(out=vb[sl][:, :, :].rearrange("p h d -> p (h d)"), in_=s_vds[kb, :, :]),
                                         deps=[vb_free[sl]], dma=f'vb{sl}'), sl)

                    def emit_exp(kb):
                        for g in range(2):
                            bi = (kb % 2) * 2 + g
                            t = S.add('act', lambda e, bi=bi, kb=kb, g=g: e.activation(
                                out=PT[kb % 2][:, g * 512:(g + 1) * 512], in_=pST[bi][:, :], func=AF.Exp),
                                deps=[t_qk[(kb, g)], PT_free[kb % 2]], sig=True)
                            pst_free[bi] = t
                            t = S.add('dve', lambda e, kb=kb, g=g: e.tensor_tensor(
                                out=PT[kb % 2][:, g * 512:(g + 1) * 512].rearrange("p (h q) -> p h q", h=4),
                                in0=PT[kb % 2][:, g * 512:(g + 1) * 512].rearrange("p (h q) -> p h q", h=4),
                                in1=pT[kb % 2][:, 0:128].unsqueeze(1).broadcast_to([128, 4, 128]), op=ALU.mult),
                                deps=[t, t_qk[(kb, 'm')]], sig=True)
                            t_exp[(kb, g)] = t
                        pT_free[kb % 2] = t

                    def emit_pv(kb):
                        nonlocal t_pv
                        tv, sl = t_v[kb]
                        for h in range(8):
                            t_pv = S.add('pe', lambda e, h=h, kb=kb, sl=sl: e.matmul(
                                pO[h // 4][:, h % 4, :], lhsT=PT[kb % 2][:, h * 128:(h + 1) * 128], rhs=vb[sl][:, h, :],
                                start=(kb == 0 and h % 4 == 0), stop=(kb == nk - 1), skip_group_check=True),
                                deps=[t_exp[(kb, h // 4)], tv, t_onorm], sig=(h == 7))
                        PT_free[kb % 2] = t_pv
                        vb_free[sl] = t_pv

                    emit_v(0)
                    emit_qk(0)
                    for kb in range(nk):
                        if kb + 1 < nk:
                            emit_v(kb + 1)
                            emit_qk(kb + 1)
                        emit_exp(kb)
                        emit_pv(kb)
                    so = i % 2
                    t_rd = S.add('dve', lambda e: e.reciprocal(out=rden[:, 0:4], in_=pO[0][:, :, 64]), deps=[t_pv], sig=True)
                    t_rd2 = S.add('dve', lambda e: e.reciprocal(out=rden[:, 4:8], in_=pO[1][:, :, 64]), deps=[t_pv], sig=True)
                    for g in range(2):
                        t_onorm = S.add('dve', lambda e, g=g, so=so: e.tensor_tensor(
                            out=ost[so][:, g * 256:(g + 1) * 256].rearrange("p (h d) -> p h d", h=4), in0=pO[g][:, :, 0:64],
                            in1=rden[:, g * 4:(g + 1) * 4].unsqueeze(2).broadcast_to([128, 4, 64]), op=ALU.mult),
                            deps=[t_rd, t_rd2, ost_free[so]], sig=True)
                    ost_free[so] = S.add('sp', lambda e, i=i, so=so: e.dma_start(out=s_odsa[i, :, :], in_=ost[so][:, :]), deps=[t_onorm], dma=f'ost{so}')
                    prev = [t_pv, t_onorm, t_msk, t_qiT, t_q2]
                S.final_wait()
                S.run(nc, "C", top)
        if 'D' in phases:
            with ExitStack() as pd_:
                S = Sched()
                xq = din("xq", [nqb * 128, D])
                w_o = din("w_o", [D, D])
                g_ffn = din("g_ffn", [128, 8])
                w_gate = din("w_gate", [D, DFF])
                w_up = din("w_up", [D, DFF])
                w_down = din("w_down", [DFF, D])
                g_fin = din("g_fin", [128, D])
                Wo = sb("WoD", [128, 8, D], BF16, pd_)
                Wg = sb("WgD", [128, 8, DFF], BF16, pd_)
                Wu = sb("WuD", [128, 8, DFF], BF16, pd_)
                Wd = sb("WdD", [128, NFC, D], BF16, pd_)
                gF = sb("gFD", [128, 8], F32, pd_)
                gfin = sb("gfinD", [128, D], F32, pd_)
                stg = [sb(f"stgD{i}", [128, 1024], F32, pd_) for i in range(2)]
                xs = [sb(f"xsD{i}", [128, D], F32, pd_) for i in range(2)]
                ob = [sb("obD0", [128, D], BF16, pd_)] * 2
                oT = [sb(f"oTD{i}", [128, 8, 128], BF16, pd_) for i in range(2)]
                ub = [sb("ubD0", [128, D], BF16, pd_)] * 2
                uT = [sb(f"uTD{i}", [128, 8, 128], BF16, pd_) for i in range(2)]
                junk = sb("junkD", [128, D], BF16, pd_)
                ss = [sb(f"ssD{i}", [128, 8], F32, pd_) for i in range(2)]
                sil = [sb(f"silD{i}", [128, 512], F32, pd_) for i in range(2)]
                actT = [sb(f"actTD{i}", [128, NFC, 128], BF16, pd_) for i in range(2)]
                h2 = [sb(f"h2D{i}", [128, D], F32, pd_) for i in range(2)]
                pT = ps("pTD", [128, 1024], BF16, pd_)
                pA = [ps(f"pAD{i}", [128, 512], F32, pd_) for i in range(2)]
                pG = [ps(f"pGD{i}", [128, 512], F32, pd_) for i in range(2)]
                pU = [ps(f"pUD{i}", [128, 512], F32, pd_) for i in range(2)]
                S.add('sp', lambda e: e.dma_start(out=gF[:, :], in_=g_ffn[:, :]), dma='cst')
                t_cst = S.add('sp', lambda e: e.dma_start(out=gfin[:, :], in_=g_fin[:, :]), dma='cst')
                stg_free = [None, None]
                t_w = []
                jobs = []
                for c in range(8):
                    jobs.append((Wo[:, c, :], w_o[c * 128:(c + 1) * 128, :], None, D))
                n_wo = len(jobs)
                for c in range(8):
                    for (Wt, wsrc) in ((Wg, w_gate), (Wu, w_up)):
                        for c0 in range(0, DFF, 1024):
                            n = min(1024, DFF - c0)
                            jobs.append((Wt[:, c, c0:c0 + n], wsrc[c * 128:(c + 1) * 128, c0:c0 + n], gF[:, c:c + 1], n))
                n_wgu = len(jobs)
                for f in range(NFC):
                    jobs.append((Wd[:, f, :], w_down[f * 128:(f + 1) * 128, :], None, D))
                for j, (dst, src, gs, n) in enumerate(jobs):
                    s = j % 2
                    td = S.add('sp', lambda e, s=s, src=src, n=n: e.dma_start(out=stg[s][:, 0:n], in_=src), deps=[stg_free[s]], dma=f'stg{s}')
                    eng = ('dve', 'pool')[s]
                    if gs is None:
                        stg_free[s] = S.add(eng, lambda e, s=s, dst=dst, n=n: e.tensor_copy(out=dst, in_=stg[s][:, 0:n]), deps=[td], sig=True)
                    else:
                        stg_free[s] = S.add(eng, lambda e, s=s, dst=dst, gs=gs, n=n: e.tensor_scalar(
                            out=dst, in0=stg[s][:, 0:n], scalar1=gs, scalar2=None, op0=ALU.mult), deps=[td, t_cst], sig=True)
                    t_w.append(stg_free[s])
                t_wo = t_w[:n_wo][-2:]
                t_wgu = t_w[:n_wgu][-2:]
                t_wd = t_w[-2:]
                pG_free = [None, None]
                pU_free = [None, None]
                sil_free = [None, None]
                pT_free = [None]
                pA_free = [None, None]
                done = {}
                st = {}

                def s1a(i):
                    b = i % 2
                    pv = done.get(i - 2, [])
                    S.add('sp', lambda e: e.dma_start(out=xs[b][:, :], in_=xq[i * 128:(i + 1) * 128, :]), deps=pv, dma=f'ld{b}')
                    S.add('sp', lambda e: e.dma_start(out=ob[b][:, 0:512], in_=s_omla[i, :, :]), deps=pv + [pT_free[0]], dma=f'ld{b}')
                    t_x = S.add('sp', lambda e: e.dma_start(out=ob[b][:, 512:1024], in_=s_odsa[i, :, :]), deps=pv + [pT_free[0]], dma=f'ld{b}')
                    for c in range(8):
                        t_tr = S.add('pe', lambda e, c=c: e.transpose(out=pT[:, c * 128:(c + 1) * 128], in_=ob[b][:, c * 128:(c + 1) * 128],
                                                                      identity=identB[:, :]), deps=[t_x, pT_free[0]], sig=(c == 7))
                    t_oT = S.add('dve', lambda e: e.tensor_copy(out=oT[b][:, :, :], in_=pT[:, :].rearrange("p (c t) -> p c t", c=8)), deps=[t_tr] + pv, sig=True)
                    pT_free[0] = t_oT
                    t_mm = [None, None]
                    for hf in range(2):
                        for c in range(8):
                            t_mm[hf] = S.add('pe', lambda e, c=c, hf=hf: e.matmul(pA[hf][:, :], lhsT=oT[b][:, c, :], rhs=Wo[:, c, hf * 512:(hf + 1) * 512],
                                                                                  start=(c == 0), stop=(c == 7)), deps=[t_oT, t_wo, pA_free[hf]], sig=(c == 7))
                    for hf in range(2):
                        t_h1 = S.add('dve', lambda e, hf=hf: e.tensor_tensor(out=xs[b][:, hf * 512:(hf + 1) * 512], in0=pA[hf][:, :],
                                                                             in1=xs[b][:, hf * 512:(hf + 1) * 512], op=ALU.add), deps=[t_mm[hf], t_x], sig=True)
                        pA_free[hf] = t_h1
                    t_ss = S.add('act', lambda e: e.activation(out=junk[:, :], in_=xs[b][:, :], func=AF.Square, accum_out=ss[b][:, 0:1]), deps=[t_h1] + pv, sig=True)
                    t_sd = S.add('act', lambda e: e.activation(out=ss[b][:, 1:2], in_=ss[b][:, 0:1], func=AF.Sqrt, scale=1.0 / D, bias=epsT[:, 0:1]), deps=[t_ss], sig=True)
                    t_r = S.add('dve', lambda e: e.reciprocal(out=ss[b][:, 2:3], in_=ss[b][:, 1:2]), deps=[t_sd], sig=True)
                    st[i] = dict(t_ub=S.add('dve', lambda e: e.tensor_scalar(out=ub[b][:, :], in0=xs[b][:, :], scalar1=ss[b][:, 2:3], scalar2=None, op0=ALU.mult),
                                            deps=[t_r, pT_free[0]] + pv, sig=True), t_h1=t_h1)

                def s1b(i):
                    b = i % 2
                    pv = done.get(i - 2, [])
                    for c in range(8):
                        t_tr = S.add('pe', lambda e, c=c: e.transpose(out=pT[:, c * 128:(c + 1) * 128], in_=ub[b][:, c * 128:(c + 1) * 128],
                                                                      identity=identB[:, :]), deps=[st[i]['t_ub'], pT_free[0]], sig=(c == 7))
                    st[i]['t_uT'] = S.add('dve', lambda e: e.tensor_copy(out=uT[b][:, :, :], in_=pT[:, :].rearrange("p (c t) -> p c t", c=8)), deps=[t_tr] + pv, sig=True)
                    pT_free[0] = st[i]['t_uT']

                def s2(i, g0, g1):
                    b = i % 2
                    pv = done.get(i - 2, [])
                    t_uT = st[i]['t_uT']
                    for gi in range(g0, g1):
                        sl = gi % 2
                        nf = min(4, NFC - gi * 4)
                        for (pX, Wt, fr) in ((pG, Wg, pG_free), (pU, Wu, pU_free)):
                            for j in range(nf):
                                f = gi * 4 + j
                                for c in range(8):
                                    t_mm = S.add('pe', lambda e, pX=pX, Wt=Wt, sl=sl, j=j, f=f, c=c: e.matmul(
                                        pX[sl][:, j * 128:(j + 1) * 128], lhsT=Wt[:, c, f * 128:(f + 1) * 128], rhs=uT[b][:, c, :],
                                        start=(c == 0), stop=(c == 7)), deps=[t_uT, t_wgu, fr[sl]], sig=(c == 7 and j == nf - 1))
                            if pX is pG:
                                t_g = t_mm
                            else:
                                t_u = t_mm
                        t_sil = S.add('act', lambda e, sl=sl, nf=nf: e.activation(out=sil[sl][:, 0:nf * 128], in_=pG[sl][:, 0:nf * 128], func=AF.Silu),
                                      deps=[t_g, sil_free[sl]], sig=True)
                        pG_free[sl] = t_sil
                        t_act = S.add('dve', lambda e, sl=sl, nf=nf, gi=gi: e.tensor_tensor(
                            out=actT[b][:, gi * 4:gi * 4 + nf, :], in0=pU[sl][:, 0:nf * 128].rearrange("p (f t) -> p f t", f=nf),
                            in1=sil[sl][:, 0:nf * 128].rearrange("p (f t) -> p f t", f=nf), op=ALU.mult), deps=[t_sil, t_u] + pv, sig=True)
                        pU_free[sl] = t_act
                        sil_free[sl] = t_act
                        st[i]['t_act'] = t_act
                        st[i]['t_u'] = t_u

                def s3(i):
                    b = i % 2
                    t_act = st[i]['t_act']
                    t_mm = [None, None]
                    for hf in range(2):
                        for f in range(NFC):
                            t_mm[hf] = S.add('pe', lambda e, f=f, hf=hf: e.matmul(pA[hf][:, :], lhsT=actT[b][:, f, :], rhs=Wd[:, f, hf * 512:(hf + 1) * 512],
                                                                                  start=(f == 0), stop=(f == NFC - 1)), deps=[t_act, t_wd, pA_free[hf]], sig=(f == NFC - 1))
                    for hf in range(2):
                        t_h2 = S.add('dve', lambda e, hf=hf: e.tensor_tensor(out=h2[b][:, hf * 512:(hf + 1) * 512], in0=pA[hf][:, :],
                                                                             in1=xs[b][:, hf * 512:(hf + 1) * 512], op=ALU.add),
                                     deps=[t_mm[hf]] + done.get(i - 2, []), sig=True)
                        pA_free[hf] = t_h2
                    t_ss = S.add('act', lambda e: e.activation(out=junk[:, :], in_=h2[b][:, :], func=AF.Square, accum_out=ss[b][:, 4:5]), deps=[t_h2], sig=True)
                    t_sd = S.add('act', lambda e: e.activation(out=ss[b][:, 5:6], in_=ss[b][:, 4:5], func=AF.Sqrt, scale=1.0 / D, bias=epsT[:, 0:1]), deps=[t_ss], sig=True)
                    t_r = S.add('dve', lambda e: e.reciprocal(out=ss[b][:, 6:7], in_=ss[b][:, 5:6]), deps=[t_sd], sig=True)
                    t_o = S.add('dve', lambda e: e.scalar_tensor_tensor(out=h2[b][:, :], in0=h2[b][:, :], scalar=ss[b][:, 6:7], in1=gfin[:, :], op0=ALU.mult, op1=ALU.mult),
                                deps=[t_r, t_cst], sig=True)
                    t_st = S.add('sp', lambda e: e.dma_start(out=out[i * 128:(i + 1) * 128, :], in_=h2[b][:, :]), deps=[t_o], dma=f'st{b}')
                    done[i] = [t_st, t_o, t_mm[1], t_sd]

                ngrp = (NFC + 3) // 4
                s1a(0)
                s1b(0)
                for i in range(nqb):
                    s2(i, 0, ngrp // 2)
                    if i + 1 < nqb:
                        s1a(i + 1)
                    s2(i, ngrp // 2, ngrp)
                    if i + 1 < nqb:
                        s1b(i + 1)
                    s3(i)
                S.final_wait()
                S.run(nc, "D", top)
    global LAST_INPUTS
    LAST_INPUTS = list(_din.keys())
    return nc, dbg


def bcast(ap, h):
    n = ap.shape[-1]
    return ap.unsqueeze(1).broadcast_to([ap.shape[0], h, n])


BF = ml_dtypes.bfloat16
def rope_tab(pos):
    pos = pos.astype(np.float32)
    out = np.zeros((pos.shape[0], 192), np.float32)
    for (half, off) in ((32, 0), (16, 128)):
        inv = np.power(np.float32(10000.0), -np.arange(half, dtype=np.float32) / np.float32(half)).astype(np.float32)
        ang = (pos[:, None] * inv[None, :]).astype(np.float32)
        c = np.cos(ang).astype(np.float32); s = np.sin(ang).astype(np.float32)
        d = 2 * half
        out[:, off:off + d] = np.concatenate([c, c], 1)
        out[:, off + d:off + 2 * d] = np.concatenate([-s, s], 1)
    return out
def core_inputs(inp, core):
    f32 = np.float32
    b, par = core // 2, core % 2
    x = np.asarray(inp['x'][b], f32)
    xk = np.zeros((65 * 128, 1024), f32); xk[0:16] = inp['meta_tokens']; xk[128:] = x
    xq = np.ascontiguousarray(x.reshape(64, 128, 1024)[par::2].reshape(32 * 128, 1024))
    w_in = np.asarray(inp['w_in'][0], f32)
    c_q, c_kv, k_r, q_s, k_s, v_s, q_i, k_i, w_i = np.split(w_in, np.cumsum([256,128,32,512,512,512,512,64,8])[:-1], axis=1)
    def pc(g, n):
        return np.ascontiguousarray(np.asarray(g, f32).reshape(n, 128).T)
    w_uq = np.asarray(inp['w_uq'][0], f32).reshape(256, 8, 96)
    w_ukv = np.asarray(inp['w_ukv'][0], f32).reshape(128, 8, 128)
    posK = np.concatenate([np.arange(128), 16 + np.arange(64 * 128)])
    ropeK = rope_tab(posK).reshape(65, 128, 192)
    g_idx = 2 * np.arange(32) + par
    posQ = (16 + 128 * g_idx[:, None] + np.arange(128)[None, :]).reshape(-1)
    ropeQ = rope_tab(posQ).reshape(32, 128, 192)
    tri = (np.arange(128)[:, None] <= np.arange(128)[None, :]).astype(f32)
    ones = np.ones((128, 128), f32); zeros = np.zeros((128, 128), f32)
    mk_mult = np.stack([tri, zeros] if par == 0 else [ones, tri]).astype(BF)
    NEG = np.float32(-1e30)
    meta_add = np.zeros((128, 128), f32); meta_add[:, 16:] = NEG
    triA = np.where(tri.T > 0, 0, NEG).astype(f32)
    allneg = np.full((128, 128), NEG, f32)
    mk_add = np.stack([meta_add, triA, allneg] if par == 0 else [meta_add, zeros, triA]).astype(f32)
    vcol0 = np.zeros((128, 8), f32); vcol0[0:16] = 1
    return {
        'xk': xk, 'xq': xq,
        'wk_in': np.ascontiguousarray(np.concatenate([k_s, v_s, c_kv, k_r, k_i], 1)),
        'wq_in': np.ascontiguousarray(np.concatenate([c_q, q_s, q_i, w_i], 1)),
        'g_attn': pc(inp['attn_norm_g'][0], 8),
        'w_uq_n': np.ascontiguousarray(w_uq[:, :, :64].reshape(256, 512)),
        'w_uq_r': np.ascontiguousarray(w_uq[:, :, 64:].reshape(256, 256)),
        'g_q': pc(inp['mla_q_norm_g'][0], 2),
        'w_ukv_k': np.ascontiguousarray(w_ukv[:, :, :64].reshape(128, 512)),
        'w_ukv_v': np.ascontiguousarray(w_ukv[:, :, 64:].reshape(128, 512)),
        'g_kv': pc(inp['mla_kv_norm_g'][0], 1),
        'w_o': np.asarray(inp['w_o'][0], f32),
        'g_ffn': pc(inp['ffn_norm_g'][0], 8),
        'w_gate': np.asarray(inp['w_gate'][0], f32), 'w_up': np.asarray(inp['w_up'][0], f32),
        'w_down': np.asarray(inp['w_down'][0], f32),
        'g_fin': np.ascontiguousarray(np.broadcast_to(np.asarray(inp['final_norm_g'], f32)[None, :], (128, 1024))),
        'ropeK': ropeK, 'ropeQ': ropeQ,
        'ident_f': np.eye(128, dtype=f32), 'ident_b': np.eye(128, dtype=f32).astype(BF),
        'vcol0': vcol0.astype(BF), 'mk_mult': mk_mult, 'mk_add': mk_add,
    }


def kernel(**inputs):
    inp = {k: np.asarray(v) for k, v in inputs.items()}
    nc, _ = build()
    names = set(LAST_INPUTS)
    in_maps = []
    for core in range(8):
        ci = core_inputs(inp, core)
        in_maps.append({k: np.ascontiguousarray(v) for k, v in ci.items() if k in names})
    res = run_bass_kernel_spmd(nc, in_maps, core_ids=list(range(8)))
    out = np.zeros((4, 64, 128, 1024), np.float32)
    for core in range(8):
        b, par = core // 2, core % 2
        out[b, par::2] = np.asarray(res.results[core]["out"], np.float32).reshape(32, 128, 1024)
    return out.reshape(4, 8192, 1024)
```

```python
import numpy as np
import ml_dtypes
from contextlib import ExitStack
import concourse.bass as bass
import concourse.mybir as mybir
from concourse.bass_utils import run_bass_kernel_spmd

F32 = mybir.dt.float32
BF16 = mybir.dt.bfloat16
AF = mybir.ActivationFunctionType
ALU = mybir.AluOpType
AX = mybir.AxisListType

D = 1024
NQB = 32
NKB = 65
EPS = 1e-6
DFF = 2816
NFC = DFF // 128
TOPK = 256
NEG = -1.0e30
LAST_INPUTS = []
import os
CUT = float(os.environ.get('KCUT', '99'))


class Sched:
    ENG = ('pe', 'act', 'dve', 'pool', 'sp')

    def __init__(self):
        self.q = {e: [] for e in self.ENG}
        self.cnt = {}

    def add(self, eng, fn, deps=(), sig=False, dma=None):
        tok = None
        inc = None
        if dma is not None:
            self.cnt[dma] = self.cnt.get(dma, 0) + 16
            inc = (dma, 16)
            tok = (dma, self.cnt[dma])
        elif sig:
            name = 'c_' + eng
            self.cnt[name] = self.cnt.get(name, 0) + 1
            inc = (name, 1)
            tok = (name, self.cnt[name])
        dl = []
        for d in deps:
            if d is None:
                continue
            if isinstance(d, list):
                dl.extend(x for x in d if x is not None)
            else:
                dl.append(d)
        self.q[eng].append((fn, dl, inc))
        return tok

    def final_wait(self, eng='sp'):
        deps = [(n, v) for n, v in self.cnt.items()]
        self.q[eng].append((lambda e: e.nop(), deps, None))

    def run(self, nc, name, semstack=None):
        with ExitStack() as es:
            sems = {n: (semstack or es).enter_context(nc.semaphore(name + '_' + n)) for n in self.cnt}
            block = es.enter_context(nc.Block())

            def mk(engname):
                def body(eng):
                    waited = {}
                    for fn, deps, inc in self.q[engname]:
                        for (s, v) in deps:
                            if engname == 'pe' and s == 'c_pe':
                                continue
                            if waited.get(s, 0) < v:
                                eng.wait_ge(sems[s], v)
                                waited[s] = v
                        inst = fn(eng)
                        if inc is not None:
                            inst.then_inc(sems[inc[0]], inc[1])
                return body
            block.tensor(mk('pe'))
            block.scalar(mk('act'))
            block.vector(mk('dve'))
            block.gpsimd(mk('pool'))
            block.sync(mk('sp'))


def _bc(ap, shape_mid):
    return ap


def build(nkb=NKB, nqb=NQB, debug=False, phases='ABCD'):
    nc = bass.Bass("TRN2", target_bir_lowering=False)
    T = nkb * 128

    _din = {}

    def din(name, shape, dt=F32):
        if name not in _din:
            _din[name] = nc.dram_tensor(name, list(shape), dt, kind="ExternalInput").ap()
        return _din[name]

    def dscr(name, shape, dt):
        return nc.dram_tensor(name, list(shape), dt).ap()

    out = nc.dram_tensor("out", [NQB * 128, D], F32, kind="ExternalOutput").ap()

    s_ksT = dscr("s_ksT", [NKB, 128, 512], BF16)
    s_vds = dscr("s_vds", [NKB, 128, 520], BF16)
    s_kiT = dscr("s_kiT", [NKB, 64, 128], BF16)
    s_h1 = dscr("s_h1", [NQB * 128, D], F32)
    s_omla = dscr("s_omla", [NQB, 128, 512], BF16)
    s_odsa = dscr("s_odsa", [NQB, 128, 512], BF16)
    s_vmla = dscr("s_vmla", [NKB, 128, 520], BF16)
    dbg = {}
    if debug:
        def dout(name, shape, dt=F32):
            t = nc.dram_tensor(name, list(shape), dt, kind="ExternalOutput").ap()
            dbg[name] = t
            return t
        d_knT = dout("d_knT", [128, 4 * T], BF16)
        d_krT = dout("d_krT", [32, T], BF16)
        d_vm = dout("d_vm", [128, nkb * 520], BF16)
        d_ksT = dout("d_ksT", [NKB, 128, 512], BF16)
        d_vds = dout("d_vds", [NKB, 128, 520], BF16)
        d_kiT = dout("d_kiT", [NKB, 64, 128], BF16)
        d_omla = dout("d_omla", [128, nqb * 512], BF16)
        d_h1 = dout("d_h1", [NQB * 128, D], F32)

    with ExitStack() as top:
        def sb(name, shape, dt, es=top):
            return es.enter_context(nc.sbuf_tensor(name, list(shape), dt))

        def ps(name, shape, dt, es=top):
            return es.enter_context(nc.psum_tensor(name, list(shape), dt))

        identF = sb("identF", [128, 128], F32)
        identB = sb("identB", [128, 128], BF16)
        epsT = sb("epsT", [128, 1], F32)
        abst = ExitStack()
        knT = sb("knT", [96, 8, T], BF16, abst)

        if 'A' in phases:
            with ExitStack() as pa:
                S = Sched()
                xk = din("xk", [nkb * 128, D])
                wk_in = din("wk_in", [D, 1248])
                g_attn = din("g_attn", [128, 8])
                w_ukv_k = din("w_ukv_k", [128, 512])
                w_ukv_v = din("w_ukv_v", [128, 512])
                g_kv = din("g_kv", [128, 1])
                ropeK = din("ropeK", [nkb, 128, 192])
                ident_f = din("ident_f", [128, 128])
                ident_b = din("ident_b", [128, 128], BF16)
                vcol0 = din("vcol0", [128, 8], BF16)
                Wk = sb("Wk", [128, 8, 1248], BF16, pa)
                WukK = sb("WukK", [128, 512], BF16, pa)
                WukV = sb("WukV", [128, 512], BF16, pa)
                gA = sb("gA", [128, 8], F32, pa)
                gKV = sb("gKV", [128, 1], F32, pa)
                stg = [sb("stgA0", [128, 1248], F32, pa)] * 2
                xs = [sb(f"xsA{i}", [128, D], F32, pa) for i in range(2)]
                tab = [sb(f"tabA{i}", [128, 192], F32, pa) for i in range(2)]
                tabs = [sb(f"tabsA{i}", [128, 192], F32, pa) for i in range(2)]
                junk = sb("junkA", [128, D], BF16, pa)
                ss = [sb(f"ssA{i}", [128, 4], F32, pa) for i in range(2)]
                ss2 = [sb(f"ss2A{i}", [128, 4], F32, pa) for i in range(2)]
                xT = [sb(f"xTA{i}", [128, 8, 128], BF16, pa) for i in range(2)]
                ra = sb("ra", [128, 512], F32, pa)
                rb = sb("rb", [128, 512], F32, pa)
                ksr = sb("ksr", [128, 512], BF16, pa)
                ri_a = sb("ri_a", [128, 64], F32, pa)
                ri_b = sb("ri_b", [128, 64], F32, pa)
                kir = sb("kir", [128, 64], BF16, pa)
                rr_a = sb("rr_a", [128, 32], F32, pa)
                rr_b = sb("rr_b", [128, 32], F32, pa)
                kr96 = sb("kr96", [128, 96], BF16, pa)
                vmst = [sb(f"vmstA{i}", [128, 8, 65], BF16, pa) for i in range(2)]
                ckv = sb("ckv", [128, 128], F32, pa)
                junk2 = sb("junk2A", [128, 128], BF16, pa)
                ckvn = sb("ckvn", [128, 128], BF16, pa)
                ckvnT = sb("ckvnT", [128, 128], BF16, pa)
                vst = [sb(f"vstA{i}", [128, 8, 65], BF16, pa) for i in range(2)]
                kst = [sb(f"kstA{i}", [128, 512], BF16, pa) for i in range(2)]
                kit = [sb(f"kitA{i}", [64, 128], BF16, pa) for i in range(2)]
                pT = [ps(f"pTA{i}", [128, 512], F32, pa) for i in range(2)]
                pP = [ps(f"pPA{i}", [128, 512], F32, pa) for i in range(3)]
                pS = ps("pSA", [128, 1024], BF16, pa)
                pK = ps("pKA", [128, 512], F32, pa)
                pV = ps("pVA", [128, 512], F32, pa)

                S.add('sp', lambda e: e.dma_start(out=identF[:, :], in_=ident_f[:, :]), dma='cst')
                S.add('sp', lambda e: e.dma_start(out=identB[:, :], in_=ident_b[:, :]), dma='cst')
                S.add('sp', lambda e: e.dma_start(out=gA[:, :], in_=g_attn[:, :]), dma='cst')
                S.add('sp', lambda e: e.dma_start(out=gKV[:, :], in_=g_kv[:, :]), dma='cst')
                t_eps = S.add('dve', lambda e: e.memset(epsT[:, :], EPS), sig=True)
                t_vm1 = S.add('pool', lambda e: e.memset(vmst[1][:, :, 64:65], 1.0), sig=True)
                t_kz = S.add('pool', lambda e: e.memset(kr96[:, 0:64], 0.0), sig=True)
                t_vst1 = [None, S.add('pool', lambda e: e.memset(vst[1][:, :, 64:65], 1.0), sig=True)]
                S.add('sp', lambda e: e.dma_start(out=vst[0][:, :, 64:65], in_=vcol0[:, :].unsqueeze(2), allow_slow_non_contiguous=True), dma='cst')
                t_cst = S.add('sp', lambda e: e.dma_start(out=vmst[0][:, :, 64:65], in_=vcol0[:, :].unsqueeze(2), allow_slow_non_contiguous=True), deps=[t_vm1], dma='cst')
                t_id = t_cst
                t_vc0 = t_cst
                t_vst1[0] = t_cst
                stg_free = [None, None]
                t_w = []
                for c in range(8):
                    s = 0
                    td = S.add('sp', lambda e, c=c, s=s: e.dma_start(out=stg[s][:, :], in_=wk_in[c * 128:(c + 1) * 128, :]),
                               deps=[stg_free[s]], dma=f'stg{s}')
                    eng = 'dve' if s == 0 else 'pool'
                    stg_free[s] = S.add(eng, lambda e, c=c, s=s: e.tensor_scalar(
                        out=Wk[:, c, :], in0=stg[s][:, :], scalar1=gA[:, c:c + 1], scalar2=None, op0=ALU.mult),
                        deps=[td, t_cst], sig=True)
                    t_w.append(stg_free[s])
                for (dst, src, s) in ((WukK, w_ukv_k, 0), (WukV, w_ukv_v, 0)):
                    td = S.add('sp', lambda e, s=s, src=src: e.dma_start(out=stg[s][:, 0:512], in_=src[:, :]),
                               deps=[stg_free[s]], dma=f'stg{s}')
                    eng = 'dve' if s == 0 else 'pool'
                    stg_free[s] = S.add(eng, lambda e, s=s, dst=dst: e.tensor_scalar(
                        out=dst[:, :], in0=stg[s][:, 0:512], scalar1=gKV[:, 0:1], scalar2=None, op0=ALU.mult),
                        deps=[td, t_cst], sig=True)
                    t_w.append(stg_free[s])

                xs_free = [None, None]
                tab_free = [None, None]
                xT_free = [None, None]
                pT_free = [None, None]
                pP_free = [[None], [None], [None]]
                pS_free = [None]
                pK_free = None
                pV_free = None
                vst_free = [None, None]
                kst_free = [None, None]
                kit_free = [None, None]
                ss_free = [None, None]
                tmp_free = {}
                ckvnT_free = None
                for k in range(nkb):
                    s = k % 2
                    t_x = S.add('sp', lambda e, k=k, s=s: e.dma_start(out=xs[s][:, :], in_=xk[k * 128:(k + 1) * 128, :]),
                                deps=[xs_free[s], tab_free[s]], dma=f'ld{s}')
                    t_tab = S.add('sp', lambda e, k=k, s=s: e.dma_start(out=tab[s][:, :], in_=ropeK[k, :, :]),
                                  deps=[tab_free[s]], dma=f'ld{s}')
                    t_x = t_tab
                    t_ss = S.add('act', lambda e, s=s: e.activation(out=junk[:, :], in_=xs[s][:, :], func=AF.Square,
                                                                   accum_out=ss[s][:, 0:1]),
                                 deps=[t_x, ss_free[s]], sig=True)
                    t_sd = S.add('act', lambda e, s=s: e.activation(out=ss[s][:, 1:2], in_=ss[s][:, 0:1], func=AF.Sqrt,
                                                                   scale=1.0 / D, bias=epsT[:, 0:1]),
                                 deps=[t_ss, t_eps], sig=True)
                    t_rstd = S.add('dve', lambda e, s=s: e.reciprocal(out=ss[s][:, 2:3], in_=ss[s][:, 1:2]),
                                   deps=[t_sd], sig=True)
                    rstd = ss[s][:, 2:3]
                    if CUT <= 1:
                        continue
                    t_tr = []
                    for hlf in range(2):
                        for j in range(4):
                            c = hlf * 4 + j
                            tt = S.add('pe', lambda e, s=s, c=c, hlf=hlf, j=j: e.transpose(
                                out=pT[hlf][:, j * 128:(j + 1) * 128], in_=xs[s][:, c * 128:(c + 1) * 128],
                                identity=identF[:, :]),
                                deps=[t_x, t_id, pT_free[hlf]], sig=(j == 3))
                        t_tr.append(tt)
                    te0 = S.add('act', lambda e, s=s: e.activation(out=xT[s][:, 0:4, :], in_=pT[0][:, :], func=AF.Copy),
                                deps=[t_tr[0], xT_free[s]], sig=True)
                    te1 = S.add('dve', lambda e, s=s: e.tensor_copy(out=xT[s][:, 4:8, :], in_=pT[1][:, :]),
                                deps=[t_tr[1], xT_free[s]], sig=True)
                    pT_free = [te0, te1]
                    xs_free[s] = [te0, te1, t_ss]
                    if CUT <= 2:
                        continue
                    t_mm = []
                    for bnk, (c0, c1) in enumerate(((0, 512), (512, 1024), (1024, 1248))):
                        for c in range(8):
                            tt = S.add('pe', lambda e, s=s, c=c, bnk=bnk, c0=c0, c1=c1: e.matmul(
                                pP[bnk][:, 0:c1 - c0], lhsT=xT[s][:, c, :], rhs=Wk[:, c, c0:c1],
                                start=(c == 0), stop=(c == 7)),
                                deps=[te0, te1, pP_free[bnk], t_w], sig=(c == 7))
                        t_mm.append(tt)
                    xT_free[s] = t_mm[2]
                    if CUT <= 3:
                        continue
                    t_tabs = S.add('dve', lambda e, s=s: e.tensor_scalar(
                        out=tabs[s][:, :], in0=tab[s][:, :], scalar1=ss[s][:, 2:3], scalar2=None, op0=ALU.mult),
                        deps=[t_tab, t_rstd, tab_free[s]], sig=True)
                    t_a = S.add('dve', lambda e, s=s: e.tensor_tensor(
                        out=ra[:, :].rearrange("p (h d) -> p h d", h=8), in0=pP[0][:, :].rearrange("p (h d) -> p h d", h=8),
                        in1=bcast(tabs[s][:, 0:64], 8), op=ALU.mult),
                        deps=[t_mm[0], t_tabs, tmp_free.get('ra')], sig=True)
                    t_b1 = S.add('dve', lambda e, s=s: e.tensor_tensor(
                        out=rb[:, :].rearrange("p (h d) -> p h d", h=8)[:, :, 0:32],
                        in0=pP[0][:, :].rearrange("p (h d) -> p h d", h=8)[:, :, 32:64],
                        in1=bcast(tabs[s][:, 64:96], 8), op=ALU.mult),
                        deps=[t_mm[0], t_tabs, tmp_free.get('ra')], sig=True)
                    t_b2 = S.add('dve', lambda e, s=s: e.tensor_tensor(
                        out=rb[:, :].rearrange("p (h d) -> p h d", h=8)[:, :, 32:64],
                        in0=pP[0][:, :].rearrange("p (h d) -> p h d", h=8)[:, :, 0:32],
                        in1=bcast(tabs[s][:, 96:128], 8), op=ALU.mult),
                        deps=[t_mm[0], t_tabs, tmp_free.get('ra')], sig=True)
                    pP_free[0] = [t_a, t_b1, t_b2]
                    t_ksr = S.add('pool', lambda e: e.tensor_tensor(out=ksr[:, :], in0=ra[:, :], in1=rb[:, :], op=ALU.add),
                                  deps=[t_a, t_b1, t_b2, tmp_free.get('ksr')], sig=True)
                    tmp_free['ra'] = t_ksr
                    if CUT <= 4:
                        continue
                    t_v = S.add('act', lambda e, s=s: e.activation(
                        out=vst[s][:, :, 0:64], in_=pP[1][:, :].rearrange("p (h d) -> p h d", h=8), func=AF.Copy,
                        scale=ss[s][:, 2:3]),
                        deps=[t_mm[1], t_rstd, vst_free[s], t_vst1[s]], sig=True)
                    pP_free[1] = [t_v]
                    t_vd = S.add('sp', lambda e, s=s, k=k: e.dma_start(
                        out=s_vds[k, :, :], in_=vst[s][:, :, :].rearrange("p h d -> p (h d)")),
                        deps=[t_v], dma=f'st{s}')
                    if CUT <= 5:
                        continue
                    t_ckv = S.add('dve', lambda e, s=s: e.tensor_scalar(out=ckv[:, :], in0=pP[2][:, 0:128], scalar1=ss[s][:, 2:3],
                                                                       scalar2=None, op0=ALU.mult),
                                  deps=[t_mm[2], t_rstd, tmp_free.get('ckv')], sig=True)
                    t_ss2 = S.add('act', lambda e, s=s: e.activation(out=junk2[:, :], in_=ckv[:, :], func=AF.Square,
                                                                    accum_out=ss2[s][:, 0:1]),
                                  deps=[t_ckv, tmp_free.get('ss2%d' % s)], sig=True)
                    t_sd2 = S.add('act', lambda e, s=s: e.activation(out=ss2[s][:, 1:2], in_=ss2[s][:, 0:1], func=AF.Sqrt,
                                                                    scale=1.0 / 128, bias=epsT[:, 0:1]),
                                  deps=[t_ss2], sig=True)
                    t_r2 = S.add('dve', lambda e, s=s: e.reciprocal(out=ss2[s][:, 2:3], in_=ss2[s][:, 1:2]),
                                 deps=[t_sd2], sig=True)
                    t_ckvn = S.add('dve', lambda e, s=s: e.tensor_scalar(
                        out=ckvn[:, :], in0=ckv[:, :], scalar1=ss2[s][:, 2:3], scalar2=None, op0=ALU.mult),
                        deps=[t_r2, t_ckv, tmp_free.get('ckvn')], sig=True)
                    tmp_free['ckv'] = [t_ckvn, t_ss2]
                    tmp_free['ss2%d' % s] = t_ckvn
                    if CUT <= 6:
                        continue
                    t_ra = S.add('dve', lambda e, s=s: e.tensor_tensor(
                        out=rr_a[:, :], in0=pP[2][:, 128:160], in1=tabs[s][:, 128:160], op=ALU.mult),
                        deps=[t_mm[2], t_tabs, tmp_free.get('rr')], sig=True)
                    t_rb1 = S.add('dve', lambda e, s=s: e.tensor_tensor(
                        out=rr_b[:, 0:16], in0=pP[2][:, 144:160], in1=tabs[s][:, 160:176], op=ALU.mult),
                        deps=[t_mm[2], t_tabs, tmp_free.get('rr')], sig=True)
                    t_rb2 = S.add('dve', lambda e, s=s: e.tensor_tensor(
                        out=rr_b[:, 16:32], in0=pP[2][:, 128:144], in1=tabs[s][:, 176:192], op=ALU.mult),
                        deps=[t_mm[2], t_tabs, tmp_free.get('rr')], sig=True)
                    t_krr = S.add('pool', lambda e: e.tensor_tensor(out=kr96[:, 64:96], in0=rr_a[:, :], in1=rr_b[:, :], op=ALU.add),
                                  deps=[t_ra, t_rb1, t_rb2, tmp_free.get('krr'), t_kz], sig=True)
                    tmp_free['rr'] = t_krr
                    t_ia = S.add('dve', lambda e, s=s: e.tensor_tensor(
                        out=ri_a[:, :], in0=pP[2][:, 160:224], in1=tabs[s][:, 0:64], op=ALU.mult),
                        deps=[t_mm[2], t_tabs, tmp_free.get('ri')], sig=True)
                    t_ib1 = S.add('dve', lambda e, s=s: e.tensor_tensor(
                        out=ri_b[:, 0:32], in0=pP[2][:, 192:224], in1=tabs[s][:, 64:96], op=ALU.mult),
                        deps=[t_mm[2], t_tabs, tmp_free.get('ri')], sig=True)
                    t_ib2 = S.add('dve', lambda e, s=s: e.tensor_tensor(
                        out=ri_b[:, 32:64], in0=pP[2][:, 160:192], in1=tabs[s][:, 96:128], op=ALU.mult),
                        deps=[t_mm[2], t_tabs, tmp_free.get('ri')], sig=True)
                    t_kir = S.add('pool', lambda e: e.tensor_tensor(out=kir[:, :], in0=ri_a[:, :], in1=ri_b[:, :], op=ALU.add),
                                  deps=[t_ia, t_ib1, t_ib2, tmp_free.get('kir')], sig=True)
                    tmp_free['ri'] = t_kir
                    pP_free[2] = [t_ckv, t_ra, t_rb1, t_rb2, t_ia, t_ib1, t_ib2]
                    tab_free[s] = [t_a, t_b1, t_b2, t_ra, t_rb1, t_rb2, t_ia, t_ib1, t_ib2]
                    ss_free[s] = [t_tabs, t_v, t_ckv]
                    if CUT <= 7:
                        continue
                    for a in range(4):
                        t_t1 = S.add('pe', lambda e, a=a: e.transpose(
                            out=pS[:, a * 128:(a + 1) * 128], in_=ksr[:, a * 128:(a + 1) * 128], identity=identB[:, :]),
                            deps=[t_ksr, pS_free], sig=(a == 3))
                    if CUT <= 7.1:
                        continue
                    t_t2 = S.add('pe', lambda e: e.transpose(out=pS[0:64, 512:640], in_=kir[:, :], identity=identB[:, :]),
                                 deps=[t_kir, pS_free], sig=True)
                    if CUT <= 7.2:
                        continue
                    t_t3 = S.add('pe', lambda e: e.transpose(out=pS[0:96, 640:768], in_=kr96[:, :], identity=identB[:, :]),
                                 deps=[t_krr, pS_free], sig=True)
                    if CUT <= 7.3:
                        continue
                    t_t4 = S.add('pe', lambda e: e.transpose(out=pS[:, 768:896], in_=ckvn[:, :], identity=identB[:, :]),
                                 deps=[t_ckvn, pS_free], sig=True)
                    tmp_free['ksr'] = t_t1
                    tmp_free['kir'] = t_t2
                    tmp_free['krr'] = t_t3
                    tmp_free['ckvn'] = t_t4
                    if CUT <= 8:
                        continue
                    t_e1 = S.add('dve', lambda e, s=s: e.tensor_copy(out=kst[s][:, :], in_=pS[:, 0:512]),
                                 deps=[t_t4, kst_free[s]], sig=True)
                    if CUT <= 8.1:
                        continue
                    t_d1 = S.add('sp', lambda e, s=s, k=k: e.dma_start(out=s_ksT[k, :, :], in_=kst[s][:, :]),
                                 deps=[t_e1], dma=f'st{s}')
                    if CUT <= 8.2:
                        continue
                    t_e2 = S.add('dve', lambda e, s=s: e.tensor_copy(out=kit[s][:, :], in_=pS[0:64, 512:640]),
                                 deps=[t_t4, kit_free[s]], sig=True)
                    if CUT <= 8.3:
                        continue
                    t_d2 = S.add('sp', lambda e, s=s, k=k: e.dma_start(out=s_kiT[k, :, :], in_=kit[s][:, :]),
                                 deps=[t_e2], dma=f'st{s}')
                    if CUT <= 8.4:
                        continue
                    t_e3 = S.add('dve', lambda e, k=k: e.tensor_copy(out=knT[64:96, :, k * 128:(k + 1) * 128],
                                                                     in_=pS[64:96, 640:768].unsqueeze(1).broadcast_to([32, 8, 128])),
                                 deps=[t_t4], sig=True)
                    if CUT <= 8.5:
                        continue
                    t_e4 = S.add('dve', lambda e: e.tensor_copy(out=ckvnT[:, :], in_=pS[:, 768:896]),
                                 deps=[t_t4, ckvnT_free], sig=True)
                    pS_free = [t_e1, t_e2, t_e3, t_e4]
                    if CUT <= 9:
                        continue
                    for h in range(4):
                        t_k = S.add('pe', lambda e, h=h: e.matmul(
                            pK[0:64, h * 128:(h + 1) * 128], lhsT=WukK[:, h * 64:(h + 1) * 64], rhs=ckvnT[:, :],
                            start=True, stop=True), deps=[t_e4, pK_free, t_w], sig=(h == 3))
                    for h in range(4, 8):
                        t_k2 = S.add('pe', lambda e, h=h: e.matmul(
                            pP[1][0:64, (h - 4) * 128:(h - 3) * 128], lhsT=WukK[:, h * 64:(h + 1) * 64], rhs=ckvnT[:, :],
                            start=True, stop=True), deps=[t_e4, t_w] + pP_free[1], sig=(h == 7))
                    t_vv = S.add('pe', lambda e: e.matmul(pV[:, :], lhsT=ckvnT[:, :], rhs=WukV[:, :], start=True, stop=True),
                                 deps=[t_e4, pV_free, t_w], sig=True)
                    ckvnT_free = t_vv
                    pK_free = S.add('act', lambda e, k=k: e.activation(
                        out=knT[0:64, 0:4, k * 128:(k + 1) * 128], in_=pK[0:64, :].rearrange("p (a t) -> p a t", a=4), func=AF.Copy),
                        deps=[t_k], sig=True)
                    t_ke2 = S.add('act', lambda e, k=k: e.activation(
                        out=knT[0:64, 4:8, k * 128:(k + 1) * 128], in_=pP[1][0:64, :].rearrange("p (a t) -> p a t", a=4), func=AF.Copy),
                        deps=[t_k2], sig=True)
                    pP_free[1] = pP_free[1] + [t_ke2]
                    pV_free = S.add('dve', lambda e, s=s: e.tensor_copy(
                        out=vmst[s][:, :, 0:64], in_=pV[:, :].rearrange("p (h d) -> p h d", h=8)),
                        deps=[t_vv, t_vm1, t_cst, vst_free[s]], sig=True)
                    t_d3 = S.add('sp', lambda e, s=s, k=k: e.dma_start(out=s_vmla[k, :, :], in_=vmst[s][:, :, :].rearrange("p h d -> p (h d)")),
                                 deps=[pV_free], dma=f'st{s}')
                    kit_free[s] = t_d3
                    kst_free[s] = t_d3
                    vst_free[s] = t_d3
                    if k == 0:
                        vst_free[s] = S.add('pool', lambda e, s=s: e.memset(vst[s][:, :, 64:65], 1.0), deps=[t_d3], sig=True)
                        vst_free[s] = [vst_free[s], S.add('pool', lambda e, s=s: e.memset(vmst[s][:, :, 64:65], 1.0), deps=[t_d3], sig=True)]
                last = [pK_free, pV_free, kst_free[0], kst_free[1], kit_free[0], kit_free[1], vst_free[0], vst_free[1]]
                S.final_wait()
                S.run(nc, "A", top)
                if debug and CUT > 50:
                    S2_ = Sched()
                    t1 = S2_.add('sp', lambda e: e.dma_start(out=d_ksT[0:nkb, :, :], in_=s_ksT[0:nkb, :, :]), dma='d')
                    t1 = S2_.add('sp', lambda e: e.dma_start(out=d_vds[0:nkb, :, :], in_=s_vds[0:nkb, :, :]), dma='d')
                    t1 = S2_.add('sp', lambda e: e.dma_start(out=d_kiT[0:nkb, :, :], in_=s_kiT[0:nkb, :, :]), dma='d')
                    S2_.add('sp', lambda e: e.nop(), deps=[t1])
                    S2_.run(nc, "Ad", top)
        if 'B' in phases:
            with ExitStack() as pb_:
                S = Sched()
                xq = din("xq", [nqb * 128, D])
                wq_in = din("wq_in", [D, 1288])
                g_attn = din("g_attn", [128, 8])
                w_uq_n = din("w_uq_n", [256, 512])
                w_uq_r = din("w_uq_r", [256, 256])
                g_q = din("g_q", [128, 2])
                ropeQ = din("ropeQ", [nqb, 128, 192])
                mk_mult = din("mk_mult", [2, 128, 128], BF16)
                SC = 96.0 ** -0.5
                Wcq = sb("Wcq", [128, 8, 256], BF16, pb_)
                WuqN = sb("WuqN", [128, 2, 512], BF16, pb_)
                WuqR = sb("WuqR", [128, 2, 256], BF16, pb_)
                gA = sb("gAB", [128, 8], F32, pb_)
                gQ = sb("gQB", [128, 2], F32, pb_)
                mkm = sb("mkmB", [128, 2, 128], BF16, pb_)
                stg = [sb(f"stgB{i}", [128, 512], F32, pb_) for i in range(2)]
                xs = sb("xsB", [128, D], F32, pb_)
                xb = sb("xbB", [128, D], BF16, pb_)
                tab = sb("tabB", [128, 192], F32, pb_)
                tabq = sb("tabqB", [128, 64], F32, pb_)
                junk = sb("junkB", [128, D], BF16, pb_)
                ss = sb("ssB", [128, 8], F32, pb_)
                xT = sb("xTB", [128, 8, 128], BF16, pb_)
                cq = sb("cqB", [128, 256], F32, pb_)
                cqn = sb("cqnB", [128, 256], BF16, pb_)
                cqnT = sb("cqnTB", [128, 2, 128], BF16, pb_)
                q96 = sb("q96B", [96, 8, 128], BF16, pb_)
                qr96 = sb("qr96B", [128, 8, 96], BF16, pb_)
                vb = [sb(f"vbB{i}", [128, 8, 65], BF16, pb_) for i in range(3)]
                qra = sb("qraB", [128, 256], F32, pb_)
                qrb = sb("qrbB", [128, 256], F32, pb_)
                PT = [sb(f"PTB{i}", [128, 1024], BF16, pb_) for i in range(2)]
                rden = sb("rdenB", [128, 8], F32, pb_)
                ost = [sb(f"ostB{i}", [128, 512], BF16, pb_) for i in range(2)]
                pT = ps("pTB", [128, 1024], BF16, pb_)
                pQ = ps("pQB", [128, 512], F32, pb_)
                pST = [ps(f"pSTB{i}", [128, 512], F32, pb_) for i in range(4)]
                pO = [ps(f"pOB{i}", [128, 4, 65], F32, pb_) for i in range(2)]

                S.add('sp', lambda e: e.dma_start(out=gA[:, :], in_=g_attn[:, :]), dma='cst')
                S.add('sp', lambda e: e.dma_start(out=gQ[:, :], in_=g_q[:, :]), dma='cst')
                t_cst = S.add('sp', lambda e: e.dma_start(out=mkm[:, :, :], in_=mk_mult.rearrange("m k q -> k m q")), dma='cst')
                t_z = S.add('pool', lambda e: e.memset(qr96[:, :, :], 0.0), sig=True)
                stg_free = [None, None]
                t_w = []
                jobs = [(Wcq[:, c, :], wq_in[c * 128:(c + 1) * 128, 0:256], gA[:, c:c + 1], 256) for c in range(8)]
                jobs += [(WuqN[:, c, :], w_uq_n[c * 128:(c + 1) * 128, :], gQ[:, c:c + 1], 512) for c in range(2)]
                jobs += [(WuqR[:, c, :], w_uq_r[c * 128:(c + 1) * 128, :], gQ[:, c:c + 1], 256) for c in range(2)]
                for j, (dst, src, gs, n) in enumerate(jobs):
                    s = j % 2
                    td = S.add('sp', lambda e, s=s, src=src, n=n: e.dma_start(out=stg[s][:, 0:n], in_=src),
                               deps=[stg_free[s]], dma=f'stg{s}')
                    stg_free[s] = S.add('dve' if s == 0 else 'pool', lambda e, s=s, dst=dst, gs=gs, n=n: e.tensor_scalar(
                        out=dst, in0=stg[s][:, 0:n], scalar1=gs, scalar2=None, op0=ALU.mult), deps=[td, t_cst], sig=True)
                    t_w.append(stg_free[s])

                prev = []
                vb_free = [None] * 3
                vcount = 0
                pst_free = [None] * 4
                PT_free = [None, None]
                ost_free = [None, None]
                t_onorm = None
                for i in range(nqb):
                    nk = min(2 * i + 3, nkb)
                    t_x = S.add('sp', lambda e, i=i: e.dma_start(out=xs[:, :], in_=xq[i * 128:(i + 1) * 128, :]), deps=prev, dma='ld')
                    t_x = S.add('sp', lambda e, i=i: e.dma_start(out=tab[:, :], in_=ropeQ[i, :, :]), deps=prev, dma='ld')
                    t_ss = S.add('act', lambda e: e.activation(out=junk[:, :], in_=xs[:, :], func=AF.Square, accum_out=ss[:, 0:1]),
                                 deps=[t_x] + prev, sig=True)
                    t_sd = S.add('act', lambda e: e.activation(out=ss[:, 1:2], in_=ss[:, 0:1], func=AF.Sqrt, scale=1.0 / D, bias=epsT[:, 0:1]),
                                 deps=[t_ss], sig=True)
                    t_rstd = S.add('dve', lambda e: e.reciprocal(out=ss[:, 2:3], in_=ss[:, 1:2]), deps=[t_sd], sig=True)
                    t_xb = S.add('dve', lambda e: e.tensor_copy(out=xb[:, :], in_=xs[:, :]), deps=[t_x] + prev, sig=True)
                    t_tq = S.add('dve', lambda e: e.tensor_scalar(out=tabq[:, :], in0=tab[:, 128:192], scalar1=SC, scalar2=None, op0=ALU.mult),
                                 deps=[t_x] + prev, sig=True)
                    for c in range(8):
                        t_tr = S.add('pe', lambda e, c=c: e.transpose(out=pT[:, c * 128:(c + 1) * 128], in_=xb[:, c * 128:(c + 1) * 128],
                                                                      identity=identB[:, :]), deps=[t_xb] + prev, sig=(c == 7))
                    t_xT = S.add('dve', lambda e: e.tensor_copy(out=xT[:, :, :], in_=pT[:, :].rearrange("p (c t) -> p c t", c=8)),
                                 deps=[t_tr], sig=True)
                    for c in range(8):
                        t_mm = S.add('pe', lambda e, c=c: e.matmul(pQ[:, 0:256], lhsT=xT[:, c, :], rhs=Wcq[:, c, :], start=(c == 0), stop=(c == 7)),
                                     deps=[t_xT, t_w] + prev, sig=(c == 7))
                    t_cq = S.add('dve', lambda e: e.tensor_scalar(out=cq[:, :], in0=pQ[:, 0:256], scalar1=ss[:, 2:3], scalar2=None, op0=ALU.mult),
                                 deps=[t_mm, t_rstd], sig=True)
                    t_ss2 = S.add('act', lambda e: e.activation(out=junk[:, 0:256], in_=cq[:, :], func=AF.Square, accum_out=ss[:, 4:5]),
                                  deps=[t_cq], sig=True)
                    t_sd2 = S.add('act', lambda e: e.activation(out=ss[:, 5:6], in_=ss[:, 4:5], func=AF.Sqrt, scale=1.0 / 256, bias=epsT[:, 0:1]),
                                  deps=[t_ss2], sig=True)
                    t_r2 = S.add('dve', lambda e: e.reciprocal(out=ss[:, 6:7], in_=ss[:, 5:6]), deps=[t_sd2], sig=True)
                    t_cqn = S.add('dve', lambda e: e.tensor_scalar(out=cqn[:, :], in0=cq[:, :], scalar1=ss[:, 6:7], scalar2=None, op0=ALU.mult),
                                  deps=[t_r2], sig=True)
                    for c in range(2):
                        t_tr = S.add('pe', lambda e, c=c: e.transpose(out=pT[:, c * 128:(c + 1) * 128], in_=cqn[:, c * 128:(c + 1) * 128],
                                                                      identity=identB[:, :]), deps=[t_cqn, t_xT], sig=(c == 1))
                    t_cT = S.add('dve', lambda e: e.tensor_copy(out=cqnT[:, :, :], in_=pT[:, 0:256].rearrange("p (c t) -> p c t", c=2)),
                                 deps=[t_tr], sig=True)
                    t_qn = None
                    for rnd in range(2):
                        for j in range(4):
                            h = rnd * 4 + j
                            for c in range(2):
                                t_mm = S.add('pe', lambda e, j=j, h=h, c=c: e.matmul(pQ[0:64, j * 128:(j + 1) * 128], lhsT=WuqN[:, c, h * 64:(h + 1) * 64],
                                                                                rhs=cqnT[:, c, :], start=(c == 0), stop=(c == 1)),
                                             deps=[t_cT, t_cq, t_qn], sig=(j == 3 and c == 1))
                        t_qn = S.add('dve', lambda e, rnd=rnd: e.tensor_scalar(out=q96[0:64, rnd * 4:(rnd + 1) * 4, :],
                                                                               in0=pQ[0:64, :].rearrange("p (a t) -> p a t", a=4),
                                                                               scalar1=SC, scalar2=None, op0=ALU.mult), deps=[t_mm] + prev, sig=True)
                    t_q1 = t_qn
                    for c in range(2):
                        t_mm = S.add('pe', lambda e, c=c: e.matmul(pQ[:, 0:256], lhsT=cqnT[:, c, :], rhs=WuqR[:, c, :], start=(c == 0), stop=(c == 1)),
                                     deps=[t_q1], sig=(c == 1))
                    v3 = lambda ap: ap.rearrange("p (h d) -> p h d", h=8)
                    t_a = S.add('dve', lambda e: e.tensor_tensor(out=v3(qra[:, :]), in0=v3(pQ[:, 0:256]), in1=bcast(tabq[:, 0:32], 8), op=ALU.mult),
                                deps=[t_mm, t_tq] + prev, sig=True)
                    t_b1 = S.add('dve', lambda e: e.tensor_tensor(out=v3(qrb[:, :])[:, :, 0:16], in0=v3(pQ[:, 0:256])[:, :, 16:32],
                                                                  in1=bcast(tabq[:, 32:48], 8), op=ALU.mult), deps=[t_mm, t_tq] + prev, sig=True)
                    t_b2 = S.add('dve', lambda e: e.tensor_tensor(out=v3(qrb[:, :])[:, :, 16:32], in0=v3(pQ[:, 0:256])[:, :, 0:16],
                                                                  in1=bcast(tabq[:, 48:64], 8), op=ALU.mult), deps=[t_mm, t_tq] + prev, sig=True)
                    t_qr = S.add('pool', lambda e: e.tensor_tensor(out=qr96[:, :, 64:96], in0=v3(qra[:, :]), in1=v3(qrb[:, :]), op=ALU.add),
                                 deps=[t_a, t_b1, t_b2, t_z] + prev, sig=True)
                    for h in range(8):
                        t_tr = S.add('pe', lambda e, h=h: e.transpose(out=pT[0:96, h * 128:(h + 1) * 128], in_=qr96[:, h, :],
                                                                      identity=identB[:, :]), deps=[t_qr, t_cT], sig=(h == 7))
                    t_qrT = S.add('dve', lambda e: e.tensor_copy(out=q96[64:96, :, :], in_=pT[64:96, :].rearrange("p (h t) -> p h t", h=8)),
                                  deps=[t_tr] + prev, sig=True)
                    qready = [t_q1, t_qrT]
                    prev_q = [t_b2, t_qrT, t_ss2, t_cqn]
                    t_exp = {}
                    t_pv = None

                    def emit_qk(kb):
                        for g in range(2):
                            bank = pST[(kb % 2) * 2 + g]
                            for j in range(4):
                                h = g * 4 + j
                                t = S.add('pe', lambda e, bank=bank, j=j, h=h, kb=kb: e.matmul(
                                    bank[:, j * 128:(j + 1) * 128], lhsT=knT[0:96, h, kb * 128:(kb + 1) * 128], rhs=q96[0:96, h, :],
                                    start=(j == 0), stop=(j == 3), skip_group_check=True),
                                    deps=qready + [pst_free[(kb % 2) * 2 + g]], sig=(j == 3))
                            t_qk[(kb, g)] = t

                    t_v = {}

                    def emit_v(kb):
                        nonlocal vcount
                        sl = vcount % 3
                        vcount += 1
                        t_v[kb] = (S.add('sp', lambda e, kb=kb, sl=sl: e.dma_start(out=vb[sl][:, :, :].rearrange("p h d -> p (h d)"), in_=s_vmla[kb, :, :]),
                                         deps=[vb_free[sl]], dma=f'vb{sl}'), sl)
                    t_qk = {}

                    def emit_exp(kb):
                        for g in range(2):
                            bi = (kb % 2) * 2 + g
                            t = S.add('act', lambda e, bi=bi, kb=kb, g=g: e.activation(
                                out=PT[kb % 2][:, g * 512:(g + 1) * 512], in_=pST[bi][:, :], func=AF.Exp),
                                deps=[t_qk[(kb, g)], PT_free[kb % 2]], sig=True)
                            pst_free[bi] = t
                            mi = kb - (2 * i + 1)
                            if mi >= 0:
                                t = S.add('pool', lambda e, kb=kb, g=g, mi=mi: e.tensor_tensor(
                                    out=PT[kb % 2][:, g * 512:(g + 1) * 512].rearrange("p (h q) -> p h q", h=4),
                                    in0=PT[kb % 2][:, g * 512:(g + 1) * 512].rearrange("p (h q) -> p h q", h=4),
                                    in1=bcast(mkm[:, mi, :], 4), op=ALU.mult), deps=[t, t_cst], sig=True)
                            t_exp[(kb, g)] = t

                    def emit_pv(kb):
                        nonlocal t_pv
                        for h in range(8):
                            t_pv = S.add('pe', lambda e, h=h, kb=kb, sl=t_v[kb][1]: e.matmul(
                                pO[h // 4][:, h % 4, :], lhsT=PT[kb % 2][:, h * 128:(h + 1) * 128], rhs=vb[sl][:, h, :],
                                start=(kb == 0 and h % 4 == 0), stop=(kb == nk - 1), skip_group_check=True),
                                deps=[t_exp[(kb, h // 4)], t_onorm, t_v[kb][0]], sig=(h == 7))
                        PT_free[kb % 2] = t_pv
                        vb_free[t_v[kb][1]] = t_pv

                    emit_v(0)
                    emit_qk(0)
                    for kb in range(nk):
                        if kb + 1 < nk:
                            emit_v(kb + 1)
                            emit_qk(kb + 1)
                        emit_exp(kb)
                        emit_pv(kb)
                    so = i % 2
                    t_rd = S.add('dve', lambda e: e.reciprocal(out=rden[:, 0:4], in_=pO[0][:, :, 64]), deps=[t_pv], sig=True)
                    t_rd2 = S.add('dve', lambda e: e.reciprocal(out=rden[:, 4:8], in_=pO[1][:, :, 64]), deps=[t_pv], sig=True)
                    for g in range(2):
                        t_onorm = S.add('dve', lambda e, g=g, so=so: e.tensor_tensor(
                            out=ost[so][:, g * 256:(g + 1) * 256].rearrange("p (h d) -> p h d", h=4), in0=pO[g][:, :, 0:64],
                            in1=rden[:, g * 4:(g + 1) * 4].unsqueeze(2).broadcast_to([128, 4, 64]), op=ALU.mult),
                            deps=[t_rd, t_rd2, ost_free[so]], sig=True)
                    ost_free[so] = S.add('sp', lambda e, i=i, so=so: e.dma_start(out=s_omla[i, :, :], in_=ost[so][:, :]),
                                         deps=[t_onorm], dma=f'ost{so}')
                    prev = [t_pv, t_onorm] + prev_q
                if debug:
                    S.final_wait()
                    S.add('sp', lambda e: e.dma_start(out=d_omla.rearrange("p (i f) -> i p f", i=nqb), in_=s_omla[0:nqb, :, :]), dma='dbg')
                S.final_wait()
                S.run(nc, "B", top)
        abst.close()
        if 'C' in phases:
            with ExitStack() as pc_:
                S = Sched()
                xq = din("xq", [nqb * 128, D])
                wq_in = din("wq_in", [D, 1288])
                g_attn = din("g_attn", [128, 8])
                ropeQ = din("ropeQ", [nqb, 128, 192])
                mk_add = din("mk_add", [3, 128, 128])
                NIT = 16
                Wq2 = sb("Wq2C", [128, 8, 1032], BF16, pc_)
                gA = sb("gAC", [128, 8], F32, pc_)
                mka = sb("mkaC", [128, 3, 128], F32, pc_)
                stg = [sb(f"stgC{i}", [128, 1032], F32, pc_) for i in range(2)]
                ksT = sb("ksTC", [128, nkb, 512], BF16, pc_)
                kiT2 = sb("kiT2C", [128, nkb, 128], BF16, pc_)
                Isc = sb("IscC", [128, nkb * 128], F32, pc_)
                Msk = sb("MskC", [128, nkb * 128], BF16, pc_)
                xs = sb("xsC", [128, D], F32, pc_)
                xb = sb("xbC", [128, D], BF16, pc_)
                tab = sb("tabC", [128, 192], F32, pc_)
                tabs = sb("tabsC", [128, 128], F32, pc_)
                junk = sb("junkC", [128, D], BF16, pc_)
                ss = sb("ssC", [128, 8], F32, pc_)
                xT = sb("xTC", [128, 8, 128], BF16, pc_)
                ra = sb("raC", [128, 512], F32, pc_)
                rb = sb("rbC", [128, 512], F32, pc_)
                qs = sb("qsC", [128, 512], BF16, pc_)
                qi = sb("qiC", [128, 512], BF16, pc_)
                wv = sb("wvC", [128, 8], F32, pc_)
                qbd = sb("qbdC", [128, 4, 256], BF16, pc_)
                qiT = sb("qiTC", [128, 4, 128], BF16, pc_)
                tmpR = [sb(f"tmpRC{i}", [128, 512], F32, pc_) for i in range(4)]
                pw2 = sb("pw2C", [128, 32], F32, pc_)
                hwt = sb("hwtC", [128, 32], F32, pc_)
                bs = sb("bsC", [128, 16], F32, pc_)
                PT = [sb(f"PTC{i}", [128, 1024], BF16, pc_) for i in range(2)]
                vb = [sb(f"vbC{i}", [128, 8, 65], BF16, pc_) for i in range(3)]
                rden = sb("rdenC", [128, 8], F32, pc_)
                ost = [sb(f"ostC{i}", [128, 512], BF16, pc_) for i in range(2)]
                pT = [ps(f"pTC{i}", [128, 1024], BF16, pc_) for i in range(2)]
                pST = [ps(f"pSTC{i}", [128, 512], F32, pc_) for i in range(4)]
                pO = [ps(f"pOC{i}", [128, 4, 65], F32, pc_) for i in range(2)]

                S.add('sp', lambda e: e.dma_start(out=gA[:, :], in_=g_attn[:, :]), dma='cst')
                S.add('sp', lambda e: e.dma_start(out=mka[:, :, :], in_=mk_add.rearrange("m q k -> q m k")), dma='cst')
                for k0 in range(0, nkb, 4):
                    k1 = min(nkb, k0 + 4)
                    S.add('sp', lambda e, k0=k0, k1=k1: e.dma_start(out=ksT[:, k0:k1, :], in_=s_ksT[k0:k1, :, :].rearrange("k p f -> p k f")), dma='cst')
                for k0 in range(0, nkb, 8):
                    k1 = min(nkb, k0 + 8)
                    S.add('sp', lambda e, k0=k0, k1=k1: e.dma_start(out=kiT2[0:64, k0:k1, :], in_=s_kiT[k0:k1, :, :].rearrange("k p t -> p k t")), dma='cst')
                    t_cst = S.add('sp', lambda e, k0=k0, k1=k1: e.dma_start(out=kiT2[64:128, k0:k1, :], in_=s_kiT[k0:k1, :, :].rearrange("k p t -> p k t")), dma='cst')
                t_z = S.add('pool', lambda e: e.memset(qbd[:, :, :], 0.0), sig=True)
                for it in range(NIT):
                    t_pw2 = S.add('pool', lambda e, it=it: e.memset(pw2[:, it:it + 1], 2.0 ** -(it + 1)), sig=True)
                stg_free = [None, None]
                t_w = []
                for c in range(8):
                    s = c % 2
                    td = S.add('sp', lambda e, s=s, c=c: e.dma_start(out=stg[s][:, :], in_=wq_in[c * 128:(c + 1) * 128, 256:1288]),
                               deps=[stg_free[s]], dma=f'stg{s}')
                    stg_free[s] = S.add(('dve', 'pool')[s], lambda e, s=s, c=c: e.tensor_scalar(
                        out=Wq2[:, c, :], in0=stg[s][:, :], scalar1=gA[:, c:c + 1], scalar2=None, op0=ALU.mult), deps=[td, t_cst], sig=True)
                    t_w.append(stg_free[s])
                v3 = lambda ap: ap.rearrange("p (h d) -> p h d", h=8)
                prev = []
                pst_free = [None] * 4
                PT_free = [None, None]
                pT_free = [None, None]
                vb_free = [None] * 3
                ost_free = [None, None]
                t_onorm = None
                vcount = 0
                for i in range(nqb):
                    nk = min(2 * i + 3, nkb)
                    W = nk * 128
                    S.add('sp', lambda e, i=i: e.dma_start(out=xs[:, :], in_=xq[i * 128:(i + 1) * 128, :]), deps=prev, dma='ld')
                    t_x = S.add('sp', lambda e, i=i: e.dma_start(out=tab[:, :], in_=ropeQ[i, :, :]), deps=prev, dma='ld')
                    t_ss = S.add('act', lambda e: e.activation(out=junk[:, :], in_=xs[:, :], func=AF.Square, accum_out=ss[:, 0:1]), deps=[t_x] + prev, sig=True)
                    t_sd = S.add('act', lambda e: e.activation(out=ss[:, 1:2], in_=ss[:, 0:1], func=AF.Sqrt, scale=1.0 / D, bias=epsT[:, 0:1]), deps=[t_ss], sig=True)
                    t_rstd = S.add('dve', lambda e: e.reciprocal(out=ss[:, 2:3], in_=ss[:, 1:2]), deps=[t_sd], sig=True)
                    t_r8 = S.add('dve', lambda e: e.tensor_scalar(out=ss[:, 3:4], in0=ss[:, 2:3], scalar1=0.125, scalar2=None, op0=ALU.mult), deps=[t_rstd], sig=True)
                    t_tabs = S.add('dve', lambda e: e.tensor_scalar(out=tabs[:, :], in0=tab[:, 0:128], scalar1=ss[:, 3:4], scalar2=None, op0=ALU.mult),
                                   deps=[t_r8, t_x] + prev, sig=True)
                    t_xb = S.add('dve', lambda e: e.tensor_copy(out=xb[:, :], in_=xs[:, :]), deps=[t_x] + prev, sig=True)
                    for c in range(8):
                        t_tr = S.add('pe', lambda e, c=c: e.transpose(out=pT[0][:, c * 128:(c + 1) * 128], in_=xb[:, c * 128:(c + 1) * 128],
                                                                      identity=identB[:, :]), deps=[t_xb] + prev, sig=(c == 7))
                    t_xT = S.add('dve', lambda e: e.tensor_copy(out=xT[:, :, :], in_=pT[0][:, :].rearrange("p (c t) -> p c t", c=8)), deps=[t_tr], sig=True)
                    t_mms = []
                    for bnk, (c0, c1) in enumerate(((0, 512), (512, 1024), (1024, 1032))):
                        for c in range(8):
                            t_mm = S.add('pe', lambda e, c=c, bnk=bnk, c0=c0, c1=c1: e.matmul(pST[bnk][:, 0:c1 - c0], lhsT=xT[:, c, :], rhs=Wq2[:, c, c0:c1],
                                                                                             start=(c == 0), stop=(c == 7)), deps=[t_xT, t_w] + prev, sig=(c == 7))
                        t_mms.append(t_mm)
                    outs = []
                    for bnk, dst in ((0, qs), (1, qi)):
                        t_a = S.add('dve', lambda e, bnk=bnk: e.tensor_tensor(out=v3(ra[:, :]), in0=v3(pST[bnk][:, :]), in1=bcast(tabs[:, 0:64], 8), op=ALU.mult),
                                    deps=[t_mms[bnk], t_tabs] + outs + prev, sig=True)
                        t_b1 = S.add('dve', lambda e, bnk=bnk: e.tensor_tensor(out=v3(rb[:, :])[:, :, 0:32], in0=v3(pST[bnk][:, :])[:, :, 32:64],
                                                                              in1=bcast(tabs[:, 64:96], 8), op=ALU.mult), deps=[t_mms[bnk], t_tabs] + outs + prev, sig=True)
                        t_b2 = S.add('dve', lambda e, bnk=bnk: e.tensor_tensor(out=v3(rb[:, :])[:, :, 32:64], in0=v3(pST[bnk][:, :])[:, :, 0:32],
                                                                              in1=bcast(tabs[:, 96:128], 8), op=ALU.mult), deps=[t_mms[bnk], t_tabs] + outs + prev, sig=True)
                        t_q = S.add('pool', lambda e, dst=dst: e.tensor_tensor(out=dst[:, :], in0=ra[:, :], in1=rb[:, :], op=ALU.add),
                                    deps=[t_a, t_b1, t_b2] + prev, sig=True)
                        outs = [t_q]
                        if bnk == 0:
                            t_qs = t_q
                        else:
                            t_qi = t_q
                    t_wv = S.add('dve', lambda e: e.tensor_scalar(out=wv[:, :], in0=pST[2][:, 0:8], scalar1=ss[:, 2:3], scalar2=8.0 ** -0.5,
                                                                  op0=ALU.mult, op1=ALU.mult), deps=[t_mms[2], t_rstd] + prev, sig=True)
                    pst_q = [t_b2, t_wv]
                    for a in range(4):
                        t_tr = S.add('pe', lambda e, a=a: e.transpose(out=pT[0][:, a * 128:(a + 1) * 128], in_=qs[:, a * 128:(a + 1) * 128],
                                                                      identity=identB[:, :]), deps=[t_qs, t_xT], sig=(a == 3))
                    for a in range(4):
                        t_tr2 = S.add('pe', lambda e, a=a: e.transpose(out=pT[1][:, a * 128:(a + 1) * 128], in_=qi[:, a * 128:(a + 1) * 128],
                                                                       identity=identB[:, :]), deps=[t_qi, pT_free[1]] + prev, sig=(a == 3))
                    pTv = lambda t: t[:, 0:512].rearrange("p (a t) -> p a t", a=4)
                    t_q1 = S.add('dve', lambda e: e.tensor_copy(out=qbd[0:64, :, 0:128], in_=pTv(pT[0])[0:64, :, :]), deps=[t_tr, t_z] + prev, sig=True)
                    t_q2 = S.add('dve', lambda e: e.tensor_copy(out=qbd[64:128, :, 128:256], in_=pTv(pT[0])[64:128, :, :]), deps=[t_tr, t_z] + prev, sig=True)
                    t_qiT = S.add('dve', lambda e: e.tensor_copy(out=qiT[:, :, :], in_=pTv(pT[1])), deps=[t_tr2] + prev, sig=True)
                    pT_free = [t_q2, t_qiT]
                    t_acc = None
                    tmp_free = [None] * 4
                    nch = (W + 511) // 512
                    cnt_ = 0
                    for ch in range(nch):
                        c0 = ch * 512
                        n = min(512, W - c0)
                        for h in range(8):
                            hp = (h % 2) * 64
                            bnk = h % 4
                            t_lg = S.add('pe', lambda e, hp=hp, h=h, bnk=bnk, c0=c0, n=n: e.matmul(
                                pST[bnk][:, 0:n], lhsT=qiT[hp:hp + 64, h // 2, :],
                                rhs=kiT2[hp:hp + 64, :, :].rearrange("p k t -> p (k t)")[:, c0:c0 + n], start=True, stop=True),
                                deps=[t_qiT, t_cst, pst_free[bnk]] + pst_q, sig=True)
                            sl = cnt_ % 4
                            cnt_ += 1
                            t_r = S.add('act', lambda e, bnk=bnk, sl=sl, n=n: e.activation(out=tmpR[sl][:, 0:n], in_=pST[bnk][:, 0:n], func=AF.Relu),
                                        deps=[t_lg, tmp_free[sl]], sig=True)
                            pst_free[bnk] = t_r
                            if h == 0:
                                t_acc = S.add('dve', lambda e, sl=sl, c0=c0, n=n: e.tensor_scalar(
                                    out=Isc[:, c0:c0 + n], in0=tmpR[sl][:, 0:n], scalar1=wv[:, 0:1], scalar2=None, op0=ALU.mult),
                                    deps=[t_r, t_wv, t_acc] + prev, sig=True)
                            else:
                                t_acc = S.add('dve', lambda e, sl=sl, c0=c0, n=n, h=h: e.scalar_tensor_tensor(
                                    out=Isc[:, c0:c0 + n], in0=tmpR[sl][:, 0:n], scalar=wv[:, h:h + 1], in1=Isc[:, c0:c0 + n],
                                    op0=ALU.mult, op1=ALU.add), deps=[t_r, t_wv, t_acc], sig=True)
                            tmp_free[sl] = t_acc
                    t_mx = S.add('dve', lambda e, W=W: e.tensor_reduce(out=bs[:, 1:2], in_=Isc[:, 0:W], axis=AX.X, op=ALU.max), deps=[t_acc] + prev, sig=True)
                    t_mn = S.add('dve', lambda e, W=W: e.tensor_reduce(out=bs[:, 0:1], in_=Isc[:, 0:W], axis=AX.X, op=ALU.min), deps=[t_acc] + prev, sig=True)
                    t_b = S.add('dve', lambda e: e.tensor_scalar(out=bs[:, 1:2], in0=bs[:, 1:2], scalar1=1.0, scalar2=None, op0=ALU.add), deps=[t_mx], sig=True)
                    t_b = S.add('dve', lambda e: e.tensor_scalar(out=bs[:, 0:1], in0=bs[:, 0:1], scalar1=-1.0, scalar2=None, op0=ALU.add), deps=[t_mn, t_b], sig=True)
                    for (mi, kb) in ((0, 0), (1, nk - 2), (2, nk - 1)):
                        t_b = S.add('dve', lambda e, mi=mi, kb=kb: e.tensor_tensor(out=Isc[:, kb * 128:(kb + 1) * 128], in0=Isc[:, kb * 128:(kb + 1) * 128],
                                                                                  in1=mka[:, mi, :], op=ALU.add), deps=[t_b, t_mn, t_mx, t_cst], sig=True)
                    W1 = (nk // 2) * 128
                    W2 = W - W1
                    t_b = S.add('dve', lambda e: e.tensor_tensor(out=bs[:, 7:8], in0=bs[:, 1:2], in1=bs[:, 0:1], op=ALU.subtract), deps=[t_b], sig=True)
                    t_b = S.add('dve', lambda e: e.tensor_scalar(out=hwt[:, 0:NIT], in0=pw2[:, 0:NIT], scalar1=bs[:, 7:8], scalar2=None, op0=ALU.mult),
                                deps=[t_b, t_pw2], sig=True)
                    t_b = S.add('dve', lambda e: e.tensor_tensor(out=bs[:, 2:3], in0=bs[:, 0:1], in1=hwt[:, 0:1], op=ALU.add), deps=[t_b], sig=True)
                    for it in range(NIT):
                        t_c1 = S.add('dve', lambda e, W1=W1: e.tensor_scalar(out=Msk[:, 0:W1], in0=Isc[:, 0:W1], scalar1=bs[:, 2:3], scalar2=0.0,
                                                                          op0=ALU.is_ge, op1=ALU.add, accum_out=bs[:, 3:4]), deps=[t_b] + prev, sig=True)
                        t_c2 = S.add('act', lambda e, W1=W1, W=W: e.activation(out=Msk[:, W1:W], in_=Isc[:, W1:W], func=AF.Sign, scale=-1.0,
                                                                             bias=bs[:, 2:3], accum_out=bs[:, 8:9]), deps=[t_b] + prev, sig=True)
                        t_b = S.add('dve', lambda e: e.scalar_tensor_tensor(out=bs[:, 9:10], in0=bs[:, 8:9], scalar=-0.5, in1=bs[:, 3:4],
                                                                            op0=ALU.mult, op1=ALU.add), deps=[t_c1, t_c2], sig=True)
                        t_b = S.add('dve', lambda e, W2=W2: e.tensor_scalar(out=bs[:, 4:5], in0=bs[:, 9:10], scalar1=float(TOPK) - 0.5 - W2 / 2.0,
                                                                          scalar2=None, op0=ALU.is_ge), deps=[t_b], sig=True)
                        t_b = S.add('dve', lambda e, it=it: e.scalar_tensor_tensor(out=bs[:, 0:1], in0=bs[:, 4:5], scalar=hwt[:, it:it + 1], in1=bs[:, 0:1],
                                                                                   op0=ALU.mult, op1=ALU.add), deps=[t_b], sig=True)
                        if it + 1 < NIT:
                            t_b = S.add('dve', lambda e, it=it: e.tensor_tensor(out=bs[:, 2:3], in0=bs[:, 0:1], in1=hwt[:, it + 1:it + 2], op=ALU.add),
                                        deps=[t_b], sig=True)
                    t_msk = S.add('dve', lambda e, W=W: e.tensor_scalar(out=Msk[:, 0:W], in0=Isc[:, 0:W], scalar1=bs[:, 0:1], scalar2=None, op0=ALU.is_ge),
                                  deps=[t_b], sig=True)
                    qready = [t_q1, t_q2, t_msk]
                    t_qk = {}
                    t_exp = {}
                    t_v = {}
                    t_pv = None

                    def emit_qk(kb):
                        for g in range(2):
                            bi = (kb % 2) * 2 + g
                            for j in range(2):
                                a = g * 2 + j
                                t = S.add('pe', lambda e, bi=bi, j=j, a=a, kb=kb: e.matmul(
                                    pST[bi][:, j * 256:(j + 1) * 256], lhsT=ksT[:, kb, a * 128:(a + 1) * 128], rhs=qbd[:, a, :],
                                    start=(j == 0), stop=(j == 1), skip_group_check=True), deps=qready + [pst_free[bi], t_cst], sig=(j == 1))
                            t_qk[(kb, g)] = t
                        t_qk[(kb, 'm')] = S.add('pe', lambda e, kb=kb: e.transpose(out=pT[kb % 2][:, 0:128], in_=Msk[:, kb * 128:(kb + 1) * 128],
                                                                                  identity=identB[:, :]), deps=[t_msk, pT_free[kb % 2]], sig=True)

                    def emit_v(kb):
                        nonlocal vcount
                        sl = vcount % 3
                        vcount += 1
                        t_v[kb] = (S.add('sp', lambda e, kb=kb, sl=sl: e.dma_start(out=vb[sl][:, :, :].rearrange("p h d -> p (h d)"), in_=s_vds[kb, :, :]),
                                         deps=[vb_free[sl]], dma=f'vb{sl}'), sl)

                    def emit_exp(kb):
                        for g in range(2):
                            bi = (kb % 2) * 2 + g
                            t = S.add('act', lambda e, bi=bi, kb=kb, g=g: e.activation(
                                out=PT[kb % 2][:, g * 512:(g + 1) * 512], in_=pST[bi][:, :], func=AF.Exp),
                                deps=[t_qk[(kb, g)], PT_free[kb % 2]], sig=True)
                            pst_free[bi] = t
                            t = S.add('dve', lambda e, kb=kb, g=g: e.tensor_tensor(
                                out=PT[kb % 2][:, g * 512:(g + 1) * 512].rearrange("p (h q) -> p h q", h=4),
                                in0=PT[kb % 2][:, g * 512:(g + 1) * 512].rearrange("p (h q) -> p h q", h=4),
                                in1=pT[kb % 2][:, 0:128].unsqueeze(1).broadcast_to([128, 4, 128]), op=ALU.mult),
                                deps=[t, t_qk[(kb, 'm')]], sig=True)
                            t_exp[(kb, g)] = t
                        pT_free[kb % 2] = t

                    def emit_pv(kb):
                        nonlocal t_pv
                        tv, sl = t_v[kb]
                        for h in range(8):
                            t_pv = S.add('pe', lambda e, h=h, kb=kb, sl=sl: e.matmul(
                                pO[h // 4][:, h % 4, :], lhsT=PT[kb % 2][:, h * 128:(h + 1) * 128], rhs=vb[sl][:, h, :],
                                start=(kb == 0 and h % 4 == 0), stop=(kb == nk - 1), skip_group_check=True),
                                deps=[t_exp[(kb, h // 4)], tv, t_onorm], sig=(h == 7))
                        PT_free[kb % 2] = t_pv
                        vb_free[sl] = t_pv

                    emit_v(0)
                    emit_qk(0)
                    for kb in range(nk):
                        if kb + 1 < nk:
                            emit_v(kb + 1)
                            emit_qk(kb + 1)
                        emit_exp(kb)
                        emit_pv(kb)
                    so = i % 2
                    t_rd = S.add('dve', lambda e: e.reciprocal(out=rden[:, 0:4], in_=pO[0][:, :, 64]), deps=[t_pv], sig=True)
                    t_rd2 = S.add('dve', lambda e: e.reciprocal(out=rden[:, 4:8], in_=pO[1][:, :, 64]), deps=[t_pv], sig=True)
                    for g in range(2):
                        t_onorm = S.add('dve', lambda e, g=g, so=so: e.tensor_tensor(
                            out=ost[so][:, g * 256:(g + 1) * 256].rearrange("p (h d) -> p h d", h=4), in0=pO[g][:, :, 0:64],
                            in1=rden[:, g * 4:(g + 1) * 4].unsqueeze(2).broadcast_to([128, 4, 64]), op=ALU.mult),
                            deps=[t_rd, t_rd2, ost_free[so]], sig=True)
                    ost_free[so] = S.add('sp', lambda e, i=i, so=so: e.dma_start(out=s_odsa[i, :, :], in_=ost[so][:, :]), deps=[t_onorm], dma=f'ost{so}')
                    prev = [t_pv, t_onorm, t_msk, t_qiT, t_q2]
                S.final_wait()
                S.run(nc, "C", top)
        if 'D' in phases:
            with ExitStack() as pd_:
                S = Sched()
                xq = din("xq", [nqb * 128, D])
                w_o = din("w_o", [D, D])
                g_ffn = din("g_ffn", [128, 8])
                w_gate = din("w_gate", [D, DFF])
                w_up = din("w_up", [D, DFF])
                w_down = din("w_down", [DFF, D])
                g_fin = din("g_fin", [128, D])
                Wo = sb("WoD", [128, 8, D], BF16, pd_)
                Wg = sb("WgD", [128, 8, DFF], BF16, pd_)
                Wu = sb("WuD", [128, 8, DFF], BF16, pd_)
                Wd = sb("WdD", [128, NFC, D], BF16, pd_)
                gF = sb("gFD", [128, 8], F32, pd_)
                gfin = sb("gfinD", [128, D], F32, pd_)
                stg = [sb(f"stgD{i}", [128, 1024], F32, pd_) for i in range(2)]
                xs = [sb(f"xsD{i}", [128, D], F32, pd_) for i in range(2)]
                ob = [sb("obD0", [128, D], BF16, pd_)] * 2
                oT = [sb(f"oTD{i}", [128, 8, 128], BF16, pd_) for i in range(2)]
                ub = [sb("ubD0", [128, D], BF16, pd_)] * 2
                uT = [sb(f"uTD{i}", [128, 8, 128], BF16, pd_) for i in range(2)]
                junk = sb("junkD", [128, D], BF16, pd_)
                ss = [sb(f"ssD{i}", [128, 8], F32, pd_) for i in range(2)]
                sil = [sb(f"silD{i}", [128, 512], F32, pd_) for i in range(2)]
                actT = [sb(f"actTD{i}", [128, NFC, 128], BF16, pd_) for i in range(2)]
                h2 = [sb(f"h2D{i}", [128, D], F32, pd_) for i in range(2)]
                pT = ps("pTD", [128, 1024], BF16, pd_)
                pA = [ps(f"pAD{i}", [128, 512], F32, pd_) for i in range(2)]
                pG = [ps(f"pGD{i}", [128, 512], F32, pd_) for i in range(2)]
                pU = [ps(f"pUD{i}", [128, 512], F32, pd_) for i in range(2)]
                S.add('sp', lambda e: e.dma_start(out=gF[:, :], in_=g_ffn[:, :]), dma='cst')
                t_cst = S.add('sp', lambda e: e.dma_start(out=gfin[:, :], in_=g_fin[:, :]), dma='cst')
                stg_free = [None, None]
                t_w = []
                jobs = []
                for c in range(8):
                    jobs.append((Wo[:, c, :], w_o[c * 128:(c + 1) * 128, :], None, D))
                n_wo = len(jobs)
                for c in range(8):
                    for (Wt, wsrc) in ((Wg, w_gate), (Wu, w_up)):
                        for c0 in range(0, DFF, 1024):
                            n = min(1024, DFF - c0)
                            jobs.append((Wt[:, c, c0:c0 + n], wsrc[c * 128:(c + 1) * 128, c0:c0 + n], gF[:, c:c + 1], n))
                n_wgu = len(jobs)
                for f in range(NFC):
                    jobs.append((Wd[:, f, :], w_down[f * 128:(f + 1) * 128, :], None, D))
                for j, (dst, src, gs, n) in enumerate(jobs):
                    s = j % 2
                    td = S.add('sp', lambda e, s=s, src=src, n=n: e.dma_start(out=stg[s][:, 0:n], in_=src), deps=[stg_free[s]], dma=f'stg{s}')
                    eng = ('dve', 'pool')[s]
                    if gs is None:
                        stg_free[s] = S.add(eng, lambda e, s=s, dst=dst, n=n: e.tensor_copy(out=dst, in_=stg[s][:, 0:n]), deps=[td], sig=True)
                    else:
                        stg_free[s] = S.add(eng, lambda e, s=s, dst=dst, gs=gs, n=n: e.tensor_scalar(
                            out=dst, in0=stg[s][:, 0:n], scalar1=gs, scalar2=None, op0=ALU.mult), deps=[td, t_cst], sig=True)
                    t_w.append(stg_free[s])
                t_wo = t_w[:n_wo][-2:]
                t_wgu = t_w[:n_wgu][-2:]
                t_wd = t_w[-2:]
                pG_free = [None, None]
                pU_free = [None, None]
                sil_free = [None, None]
                pT_free = [None]
                pA_free = [None, None]
                done = {}
                st = {}

                def s1a(i):
                    b = i % 2
                    pv = done.get(i - 2, [])
                    S.add('sp', lambda e: e.dma_start(out=xs[b][:, :], in_=xq[i * 128:(i + 1) * 128, :]), deps=pv, dma=f'ld{b}')
                    S.add('sp', lambda e: e.dma_start(out=ob[b][:, 0:512], in_=s_omla[i, :, :]), deps=pv + [pT_free[0]], dma=f'ld{b}')
                    t_x = S.add('sp', lambda e: e.dma_start(out=ob[b][:, 512:1024], in_=s_odsa[i, :, :]), deps=pv + [pT_free[0]], dma=f'ld{b}')
                    for c in range(8):
                        t_tr = S.add('pe', lambda e, c=c: e.transpose(out=pT[:, c * 128:(c + 1) * 128], in_=ob[b][:, c * 128:(c + 1) * 128],
                                                                      identity=identB[:, :]), deps=[t_x, pT_free[0]], sig=(c == 7))
                    t_oT = S.add('dve', lambda e: e.tensor_copy(out=oT[b][:, :, :], in_=pT[:, :].rearrange("p (c t) -> p c t", c=8)), deps=[t_tr] + pv, sig=True)
                    pT_free[0] = t_oT
                    t_mm = [None, None]
                    for hf in range(2):
                        for c in range(8):
                            t_mm[hf] = S.add('pe', lambda e, c=c, hf=hf: e.matmul(pA[hf][:, :], lhsT=oT[b][:, c, :], rhs=Wo[:, c, hf * 512:(hf + 1) * 512],
                                                                                  start=(c == 0), stop=(c == 7)), deps=[t_oT, t_wo, pA_free[hf]], sig=(c == 7))
                    for hf in range(2):
                        t_h1 = S.add('dve', lambda e, hf=hf: e.tensor_tensor(out=xs[b][:, hf * 512:(hf + 1) * 512], in0=pA[hf][:, :],
                                                                             in1=xs[b][:, hf * 512:(hf + 1) * 512], op=ALU.add), deps=[t_mm[hf], t_x], sig=True)
                        pA_free[hf] = t_h1
                    t_ss = S.add('act', lambda e: e.activation(out=junk[:, :], in_=xs[b][:, :], func=AF.Square, accum_out=ss[b][:, 0:1]), deps=[t_h1] + pv, sig=True)
                    t_sd = S.add('act', lambda e: e.activation(out=ss[b][:, 1:2], in_=ss[b][:, 0:1], func=AF.Sqrt, scale=1.0 / D, bias=epsT[:, 0:1]), deps=[t_ss], sig=True)
                    t_r = S.add('dve', lambda e: e.reciprocal(out=ss[b][:, 2:3], in_=ss[b][:, 1:2]), deps=[t_sd], sig=True)
                    st[i] = dict(t_ub=S.add('dve', lambda e: e.tensor_scalar(out=ub[b][:, :], in0=xs[b][:, :], scalar1=ss[b][:, 2:3], scalar2=None, op0=ALU.mult),
                                            deps=[t_r, pT_free[0]] + pv, sig=True), t_h1=t_h1)

                def s1b(i):
                    b = i % 2
                    pv = done.get(i - 2, [])
                    for c in range(8):
                        t_tr = S.add('pe', lambda e, c=c: e.transpose(out=pT[:, c * 128:(c + 1) * 128], in_=ub[b][:, c * 128:(c + 1) * 128],
                                                                      identity=identB[:, :]), deps=[st[i]['t_ub'], pT_free[0]], sig=(c == 7))
                    st[i]['t_uT'] = S.add('dve', lambda e: e.tensor_copy(out=uT[b][:, :, :], in_=pT[:, :].rearrange("p (c t) -> p c t", c=8)), deps=[t_tr] + pv, sig=True)
                    pT_free[0] = st[i]['t_uT']

                def s2(i, g0, g1):
                    b = i % 2
                    pv = done.get(i - 2, [])
                    t_uT = st[i]['t_uT']
                    for gi in range(g0, g1):
                        sl = gi % 2
                        nf = min(4, NFC - gi * 4)
                        for (pX, Wt, fr) in ((pG, Wg, pG_free), (pU, Wu, pU_free)):
                            for j in range(nf):
                                f = gi * 4 + j
                                for c in range(8):
                                    t_mm = S.add('pe', lambda e, pX=pX, Wt=Wt, sl=sl, j=j, f=f, c=c: e.matmul(
                                        pX[sl][:, j * 128:(j + 1) * 128], lhsT=Wt[:, c, f * 128:(f + 1) * 128], rhs=uT[b][:, c, :],
                                        start=(c == 0), stop=(c == 7)), deps=[t_uT, t_wgu, fr[sl]], sig=(c == 7 and j == nf - 1))
                            if pX is pG:
                                t_g = t_mm
                            else:
                                t_u = t_mm
                        t_sil = S.add('act', lambda e, sl=sl, nf=nf: e.activation(out=sil[sl][:, 0:nf * 128], in_=pG[sl][:, 0:nf * 128], func=AF.Silu),
                                      deps=[t_g, sil_free[sl]], sig=True)
                        pG_free[sl] = t_sil
                        t_act = S.add('dve', lambda e, sl=sl, nf=nf, gi=gi: e.tensor_tensor(
                            out=actT[b][:, gi * 4:gi * 4 + nf, :], in0=pU[sl][:, 0:nf * 128].rearrange("p (f t) -> p f t", f=nf),
                            in1=sil[sl][:, 0:nf * 128].rearrange("p (f t) -> p f t", f=nf), op=ALU.mult), deps=[t_sil, t_u] + pv, sig=True)
                        pU_free[sl] = t_act
                        sil_free[sl] = t_act
                        st[i]['t_act'] = t_act
                        st[i]['t_u'] = t_u

                def s3(i):
                    b = i % 2
                    t_act = st[i]['t_act']
                    t_mm = [None, None]
                    for hf in range(2):
                        for f in range(NFC):
                            t_mm[hf] = S.add('pe', lambda e, f=f, hf=hf: e.matmul(pA[hf][:, :], lhsT=actT[b][:, f, :], rhs=Wd[:, f, hf * 512:(hf + 1) * 512],
                                                                                  start=(f == 0), stop=(f == NFC - 1)), deps=[t_act, t_wd, pA_free[hf]], sig=(f == NFC - 1))
                    for hf in range(2):
                        t_h2 = S.add('dve', lambda e, hf=hf: e.tensor_tensor(out=h2[b][:, hf * 512:(hf + 1) * 512], in0=pA[hf][:, :],
                                                                             in1=xs[b][:, hf * 512:(hf + 1) * 512], op=ALU.add),
                                     deps=[t_mm[hf]] + done.get(i - 2, []), sig=True)
                        pA_free[hf] = t_h2
                    t_ss = S.add('act', lambda e: e.activation(out=junk[:, :], in_=h2[b][:, :], func=AF.Square, accum_out=ss[b][:, 4:5]), deps=[t_h2], sig=True)
                    t_sd = S.add('act', lambda e: e.activation(out=ss[b][:, 5:6], in_=ss[b][:, 4:5], func=AF.Sqrt, scale=1.0 / D, bias=epsT[:, 0:1]), deps=[t_ss], sig=True)
                    t_r = S.add('dve', lambda e: e.reciprocal(out=ss[b][:, 6:7], in_=ss[b][:, 5:6]), deps=[t_sd], sig=True)
                    t_o = S.add('dve', lambda e: e.scalar_tensor_tensor(out=h2[b][:, :], in0=h2[b][:, :], scalar=ss[b][:, 6:7], in1=gfin[:, :], op0=ALU.mult, op1=ALU.mult),
                                deps=[t_r, t_cst], sig=True)
                    t_st = S.add('sp', lambda e: e.dma_start(out=out[i * 128:(i + 1) * 128, :], in_=h2[b][:, :]), deps=[t_o], dma=f'st{b}')
                    done[i] = [t_st, t_o, t_mm[1], t_sd]

                ngrp = (NFC + 3) // 4
                s1a(0)
                s1b(0)
                for i in range(nqb):
                    s2(i, 0, ngrp // 2)
                    if i + 1 < nqb:
                        s1a(i + 1)
                    s2(i, ngrp // 2, ngrp)
                    if i + 1 < nqb:
                        s1b(i + 1)
                    s3(i)
                S.final_wait()
                S.run(nc, "D", top)
    global LAST_INPUTS
    LAST_INPUTS = list(_din.keys())
    return nc, dbg


def bcast(ap, h):
    n = ap.shape[-1]
    return ap.unsqueeze(1).broadcast_to([ap.shape[0], h, n])


BF = ml_dtypes.bfloat16
def rope_tab(pos):
    pos = pos.astype(np.float32)
    out = np.zeros((pos.shape[0], 192), np.float32)
    for (half, off) in ((32, 0), (16, 128)):
        inv = np.power(np.float32(10000.0), -np.arange(half, dtype=np.float32) / np.float32(half)).astype(np.float32)
        ang = (pos[:, None] * inv[None, :]).astype(np.float32)
        c = np.cos(ang).astype(np.float32); s = np.sin(ang).astype(np.float32)
        d = 2 * half
        out[:, off:off + d] = np.concatenate([c, c], 1)
        out[:, off + d:off + 2 * d] = np.concatenate([-s, s], 1)
    return out
def core_inputs(inp, core):
    f32 = np.float32
    b, par = core // 2, core % 2
    x = np.asarray(inp['x'][b], f32)
    xk = np.zeros((65 * 128, 1024), f32); xk[0:16] = inp['meta_tokens']; xk[128:] = x
    xq = np.ascontiguousarray(x.reshape(64, 128, 1024)[par::2].reshape(32 * 128, 1024))
    w_in = np.asarray(inp['w_in'][0], f32)
    c_q, c_kv, k_r, q_s, k_s, v_s, q_i, k_i, w_i = np.split(w_in, np.cumsum([256,128,32,512,512,512,512,64,8])[:-1], axis=1)
    def pc(g, n):
        return np.ascontiguousarray(np.asarray(g, f32).reshape(n, 128).T)
    w_uq = np.asarray(inp['w_uq'][0], f32).reshape(256, 8, 96)
    w_ukv = np.asarray(inp['w_ukv'][0], f32).reshape(128, 8, 128)
    posK = np.concatenate([np.arange(128), 16 + np.arange(64 * 128)])
    ropeK = rope_tab(posK).reshape(65, 128, 192)
    g_idx = 2 * np.arange(32) + par
    posQ = (16 + 128 * g_idx[:, None] + np.arange(128)[None, :]).reshape(-1)
    ropeQ = rope_tab(posQ).reshape(32, 128, 192)
    tri = (np.arange(128)[:, None] <= np.arange(128)[None, :]).astype(f32)
    ones = np.ones((128, 128), f32); zeros = np.zeros((128, 128), f32)
    mk_mult = np.stack([tri, zeros] if par == 0 else [ones, tri]).astype(BF)
    NEG = np.float32(-1e30)
    meta_add = np.zeros((128, 128), f32); meta_add[:, 16:] = NEG
    triA = np.where(tri.T > 0, 0, NEG).astype(f32)
    allneg = np.full((128, 128), NEG, f32)
    mk_add = np.stack([meta_add, triA, allneg] if par == 0 else [meta_add, zeros, triA]).astype(f32)
    vcol0 = np.zeros((128, 8), f32); vcol0[0:16] = 1
    return {
        'xk': xk, 'xq': xq,
        'wk_in': np.ascontiguousarray(np.concatenate([k_s, v_s, c_kv, k_r, k_i], 1)),
        'wq_in': np.ascontiguousarray(np.concatenate([c_q, q_s, q_i, w_i], 1)),
        'g_attn': pc(inp['attn_norm_g'][0], 8),
        'w_uq_n': np.ascontiguousarray(w_uq[:, :, :64].reshape(256, 512)),
        'w_uq_r': np.ascontiguousarray(w_uq[:, :, 64:].reshape(256, 256)),
        'g_q': pc(inp['mla_q_norm_g'][0], 2),
        'w_ukv_k': np.ascontiguousarray(w_ukv[:, :, :64].reshape(128, 512)),
        'w_ukv_v': np.ascontiguousarray(w_ukv[:, :, 64:].reshape(128, 512)),
        'g_kv': pc(inp['mla_kv_norm_g'][0], 1),
        'w_o': np.asarray(inp['w_o'][0], f32),
        'g_ffn': pc(inp['ffn_norm_g'][0], 8),
        'w_gate': np.asarray(inp['w_gate'][0], f32), 'w_up': np.asarray(inp['w_up'][0], f32),
        'w_down': np.asarray(inp['w_down'][0], f32),
        'g_fin': np.ascontiguousarray(np.broadcast_to(np.asarray(inp['final_norm_g'], f32)[None, :], (128, 1024))),
        'ropeK': ropeK, 'ropeQ': ropeQ,
        'ident_f': np.eye(128, dtype=f32), 'ident_b': np.eye(128, dtype=f32).astype(BF),
        'vcol0': vcol0.astype(BF), 'mk_mult': mk_mult, 'mk_add': mk_add,
    }


def kernel(**inputs):
    inp = {k: np.asarray(v) for k, v in inputs.items()}
    nc, _ = build()
    names = set(LAST_INPUTS)
    in_maps = []
    for core in range(8):
        ci = core_inputs(inp, core)
        in_maps.append({k: np.ascontiguousarray(v) for k, v in ci.items() if k in names})
    res = run_bass_kernel_spmd(nc, in_maps, core_ids=list(range(8)))
    out = np.zeros((4, 64, 128, 1024), np.float32)
    for core in range(8):
        b, par = core // 2, core % 2
        out[b, par::2] = np.asarray(res.results[core]["out"], np.float32).reshape(32, 128, 1024)
    return out.reshape(4, 8192, 1024)
```

```python
import numpy as np
import ml_dtypes
from contextlib import ExitStack
import concourse.bass as bass
import concourse.mybir as mybir
from concourse.bass_utils import run_bass_kernel_spmd

F32 = mybir.dt.float32
BF16 = mybir.dt.bfloat16
AF = mybir.ActivationFunctionType
ALU = mybir.AluOpType
AX = mybir.AxisListType

D = 1024
NQB = 32
NKB = 65
EPS = 1e-6
DFF = 2816
NFC = DFF // 128
TOPK = 256
NEG = -1.0e30
LAST_INPUTS = []
import os
CUT = float(os.environ.get('KCUT', '99'))


class Sched:
    ENG = ('pe', 'act', 'dve', 'pool', 'sp')

    def __init__(self):
        self.q = {e: [] for e in self.ENG}
        self.cnt = {}

    def add(self, eng, fn, deps=(), sig=False, dma=None):
        tok = None
        inc = None
        if dma is not None:
            self.cnt[dma] = self.cnt.get(dma, 0) + 16
            inc = (dma, 16)
            tok = (dma, self.cnt[dma])
        elif sig:
            name = 'c_' + eng
            self.cnt[name] = self.cnt.get(name, 0) + 1
            inc = (name, 1)
            tok = (name, self.cnt[name])
        dl = []
        for d in deps:
            if d is None:
                continue
            if isinstance(d, list):
                dl.extend(x for x in d if x is not None)
            else:
                dl.append(d)
        self.q[eng].append((fn, dl, inc))
        return tok

    def final_wait(self, eng='sp'):
        deps = [(n, v) for n, v in self.cnt.items()]
        self.q[eng].append((lambda e: e.nop(), deps, None))

    def run(self, nc, name, semstack=None):
        with ExitStack() as es:
            sems = {n: (semstack or es).enter_context(nc.semaphore(name + '_' + n)) for n in self.cnt}
            block = es.enter_context(nc.Block())

            def mk(engname):
                def body(eng):
                    waited = {}
                    for fn, deps, inc in self.q[engname]:
                        for (s, v) in deps:
                            if engname == 'pe' and s == 'c_pe':
                                continue
                            if waited.get(s, 0) < v:
                                eng.wait_ge(sems[s], v)
                                waited[s] = v
                        inst = fn(eng)
                        if inc is not None:
                            inst.then_inc(sems[inc[0]], inc[1])
                return body
            block.tensor(mk('pe'))
            block.scalar(mk('act'))
            block.vector(mk('dve'))
            block.gpsimd(mk('pool'))
            block.sync(mk('sp'))


def _bc(ap, shape_mid):
    return ap


def build(nkb=NKB, nqb=NQB, debug=False, phases='ABCD'):
    nc = bass.Bass("TRN2", target_bir_lowering=False)
    T = nkb * 128

    _din = {}

    def din(name, shape, dt=F32):
        if name not in _din:
            _din[name] = nc.dram_tensor(name, list(shape), dt, kind="ExternalInput").ap()
        return _din[name]

    def dscr(name, shape, dt):
        return nc.dram_tensor(name, list(shape), dt).ap()

    out = nc.dram_tensor("out", [NQB * 128, D], F32, kind="ExternalOutput").ap()

    s_ksT = dscr("s_ksT", [NKB, 128, 512], BF16)
    s_vds = dscr("s_vds", [NKB, 128, 520], BF16)
    s_kiT = dscr("s_kiT", [NKB, 64, 128], BF16)
    s_h1 = dscr("s_h1", [NQB * 128, D], F32)
    s_omla = dscr("s_omla", [NQB, 128, 512], BF16)
    s_odsa = dscr("s_odsa", [NQB, 128, 512], BF16)
    s_vmla = dscr("s_vmla", [NKB, 128, 520], BF16)
    dbg = {}
    if debug:
        def dout(name, shape, dt=F32):
            t = nc.dram_tensor(name, list(shape), dt, kind="ExternalOutput").ap()
            dbg[name] = t
            return t
        d_knT = dout("d_knT", [128, 4 * T], BF16)
        d_krT = dout("d_krT", [32, T], BF16)
        d_vm = dout("d_vm", [128, nkb * 520], BF16)
        d_ksT = dout("d_ksT", [NKB, 128, 512], BF16)
        d_vds = dout("d_vds", [NKB, 128, 520], BF16)
        d_kiT = dout("d_kiT", [NKB, 64, 128], BF16)
        d_omla = dout("d_omla", [128, nqb * 512], BF16)
        d_h1 = dout("d_h1", [NQB * 128, D], F32)

    with ExitStack() as top:
        def sb(name, shape, dt, es=top):
            return es.enter_context(nc.sbuf_tensor(name, list(shape), dt))

        def ps(name, shape, dt, es=top):
            return es.enter_context(nc.psum_tensor(name, list(shape), dt))

        identF = sb("identF", [128, 128], F32)
        identB = sb("identB", [128, 128], BF16)
        epsT = sb("epsT", [128, 1], F32)
        abst = ExitStack()
        knT = sb("knT", [96, 8, T], BF16, abst)

        if 'A' in phases:
            with ExitStack() as pa:
                S = Sched()
                xk = din("xk", [nkb * 128, D])
                wk_in = din("wk_in", [D, 1248])
                g_attn = din("g_attn", [128, 8])
                w_ukv_k = din("w_ukv_k", [128, 512])
                w_ukv_v = din("w_ukv_v", [128, 512])
                g_kv = din("g_kv", [128, 1])
                ropeK = din("ropeK", [nkb, 128, 192])
                ident_f = din("ident_f", [128, 128])
                ident_b = din("ident_b", [128, 128], BF16)
                vcol0 = din("vcol0", [128, 8], BF16)
                Wk = sb("Wk", [128, 8, 1248], BF16, pa)
                WukK = sb("WukK", [128, 512], BF16, pa)
                WukV = sb("WukV", [128, 512], BF16, pa)
                gA = sb("gA", [128, 8], F32, pa)
                gKV = sb("gKV", [128, 1], F32, pa)
                stg = [sb("stgA0", [128, 1248], F32, pa)] * 2
                xs = [sb(f"xsA{i}", [128, D], F32, pa) for i in range(2)]
                tab = [sb(f"tabA{i}", [128, 192], F32, pa) for i in range(2)]
                tabs = [sb(f"tabsA{i}", [128, 192], F32, pa) for i in range(2)]
                junk = sb("junkA", [128, D], BF16, pa)
                ss = [sb(f"ssA{i}", [128, 4], F32, pa) for i in range(2)]
                ss2 = [sb(f"ss2A{i}", [128, 4], F32, pa) for i in range(2)]
                xT = [sb(f"xTA{i}", [128, 8, 128], BF16, pa) for i in range(2)]
                ra = sb("ra", [128, 512], F32, pa)
                rb = sb("rb", [128, 512], F32, pa)
                ksr = sb("ksr", [128, 512], BF16, pa)
                ri_a = sb("ri_a", [128, 64], F32, pa)
                ri_b = sb("ri_b", [128, 64], F32, pa)
                kir = sb("kir", [128, 64], BF16, pa)
                rr_a = sb("rr_a", [128, 32], F32, pa)
                rr_b = sb("rr_b", [128, 32], F32, pa)
                kr96 = sb("kr96", [128, 96], BF16, pa)
                vmst = [sb(f"vmstA{i}", [128, 8, 65], BF16, pa) for i in range(2)]
                ckv = sb("ckv", [128, 128], F32, pa)
                junk2 = sb("junk2A", [128, 128], BF16, pa)
                ckvn = sb("ckvn", [128, 128], BF16, pa)
                ckvnT = sb("ckvnT", [128, 128], BF16, pa)
                vst = [sb(f"vstA{i}", [128, 8, 65], BF16, pa) for i in range(2)]
                kst = [sb(f"kstA{i}", [128, 512], BF16, pa) for i in range(2)]
                kit = [sb(f"kitA{i}", [64, 128], BF16, pa) for i in range(2)]
                pT = [ps(f"pTA{i}", [128, 512], F32, pa) for i in range(2)]
                pP = [ps(f"pPA{i}", [128, 512], F32, pa) for i in range(3)]
                pS = ps("pSA", [128, 1024], BF16, pa)
                pK = ps("pKA", [128, 512], F32, pa)
                pV = ps("pVA", [128, 512], F32, pa)

                S.add('sp', lambda e: e.dma_start(out=identF[:, :], in_=ident_f[:, :]), dma='cst')
                S.add('sp', lambda e: e.dma_start(out=identB[:, :], in_=ident_b[:, :]), dma='cst')
                S.add('sp', lambda e: e.dma_start(out=gA[:, :], in_=g_attn[:, :]), dma='cst')
                S.add('sp', lambda e: e.dma_start(out=gKV[:, :], in_=g_kv[:, :]), dma='cst')
                t_eps = S.add('dve', lambda e: e.memset(epsT[:, :], EPS), sig=True)
                t_vm1 = S.add('pool', lambda e: e.memset(vmst[1][:, :, 64:65], 1.0), sig=True)
                t_kz = S.add('pool', lambda e: e.memset(kr96[:, 0:64], 0.0), sig=True)
                t_vst1 = [None, S.add('pool', lambda e: e.memset(vst[1][:, :, 64:65], 1.0), sig=True)]
                S.add('sp', lambda e: e.dma_start(out=vst[0][:, :, 64:65], in_=vcol0[:, :].unsqueeze(2), allow_slow_non_contiguous=True), dma='cst')
                t_cst = S.add('sp', lambda e: e.dma_start(out=vmst[0][:, :, 64:65], in_=vcol0[:, :].unsqueeze(2), allow_slow_non_contiguous=True), deps=[t_vm1], dma='cst')
                t_id = t_cst
                t_vc0 = t_cst
                t_vst1[0] = t_cst
                stg_free = [None, None]
                t_w = []
                for c in range(8):
                    s = 0
                    td = S.add('sp', lambda e, c=c, s=s: e.dma_start(out=stg[s][:, :], in_=wk_in[c * 128:(c + 1) * 128, :]),
                               deps=[stg_free[s]], dma=f'stg{s}')
                    eng = 'dve' if s == 0 else 'pool'
                    stg_free[s] = S.add(eng, lambda e, c=c, s=s: e.tensor_scalar(
                        out=Wk[:, c, :], in0=stg[s][:, :], scalar1=gA[:, c:c + 1], scalar2=None, op0=ALU.mult),
                        deps=[td, t_cst], sig=True)
                    t_w.append(stg_free[s])
                for (dst, src, s) in ((WukK, w_ukv_k, 0), (WukV, w_ukv_v, 0)):
                    td = S.add('sp', lambda e, s=s, src=src: e.dma_start(out=stg[s][:, 0:512], in_=src[:, :]),
                               deps=[stg_free[s]], dma=f'stg{s}')
                    eng = 'dve' if s == 0 else 'pool'
                    stg_free[s] = S.add(eng, lambda e, s=s, dst=dst: e.tensor_scalar(
                        out=dst[:, :], in0=stg[s][:, 0:512], scalar1=gKV[:, 0:1], scalar2=None, op0=ALU.mult),
                        deps=[td, t_cst], sig=True)
                    t_w.append(stg_free[s])

                xs_free = [None, None]
                tab_free = [None, None]
                xT_free = [None, None]
                pT_free = [None, None]
                pP_free = [[None], [None], [None]]
                pS_free = [None]
                pK_free = None
                pV_free = None
                vst_free = [None, None]
                kst_free = [None, None]
                kit_free = [None, None]
                ss_free = [None, None]
                tmp_free = {}
                ckvnT_free = None
                for k in range(nkb):
                    s = k % 2
                    t_x = S.add('sp', lambda e, k=k, s=s: e.dma_start(out=xs[s][:, :], in_=xk[k * 128:(k + 1) * 128, :]),
                                deps=[xs_free[s], tab_free[s]], dma=f'ld{s}')
                    t_tab = S.add('sp', lambda e, k=k, s=s: e.dma_start(out=tab[s][:, :], in_=ropeK[k, :, :]),
                                  deps=[tab_free[s]], dma=f'ld{s}')
                    t_x = t_tab
                    t_ss = S.add('act', lambda e, s=s: e.activation(out=junk[:, :], in_=xs[s][:, :], func=AF.Square,
                                                                   accum_out=ss[s][:, 0:1]),
                                 deps=[t_x, ss_free[s]], sig=True)
                    t_sd = S.add('act', lambda e, s=s: e.activation(out=ss[s][:, 1:2], in_=ss[s][:, 0:1], func=AF.Sqrt,
                                                                   scale=1.0 / D, bias=epsT[:, 0:1]),
                                 deps=[t_ss, t_eps], sig=True)
                    t_rstd = S.add('dve', lambda e, s=s: e.reciprocal(out=ss[s][:, 2:3], in_=ss[s][:, 1:2]),
                                   deps=[t_sd], sig=True)
                    rstd = ss[s][:, 2:3]
                    if CUT <= 1:
                        continue
                    t_tr = []
                    for hlf in range(2):
                        for j in range(4):
                            c = hlf * 4 + j
                            tt = S.add('pe', lambda e, s=s, c=c, hlf=hlf, j=j: e.transpose(
                                out=pT[hlf][:, j * 128:(j + 1) * 128], in_=xs[s][:, c * 128:(c + 1) * 128],
                                identity=identF[:, :]),
                                deps=[t_x, t_id, pT_free[hlf]], sig=(j == 3))
                        t_tr.append(tt)
                    te0 = S.add('act', lambda e, s=s: e.activation(out=xT[s][:, 0:4, :], in_=pT[0][:, :], func=AF.Copy),
                                deps=[t_tr[0], xT_free[s]], sig=True)
                    te1 = S.add('dve', lambda e, s=s: e.tensor_copy(out=xT[s][:, 4:8, :], in_=pT[1][:, :]),
                                deps=[t_tr[1], xT_free[s]], sig=True)
                    pT_free = [te0, te1]
                    xs_free[s] = [te0, te1, t_ss]
                    if CUT <= 2:
                        continue
                    t_mm = []
                    for bnk, (c0, c1) in enumerate(((0, 512), (512, 1024), (1024, 1248))):
                        for c in range(8):
                            tt = S.add('pe', lambda e, s=s, c=c, bnk=bnk, c0=c0, c1=c1: e.matmul(
                                pP[bnk][:, 0:c1 - c0], lhsT=xT[s][:, c, :], rhs=Wk[:, c, c0:c1],
                                start=(c == 0), stop=(c == 7)),
                                deps=[te0, te1, pP_free[bnk], t_w], sig=(c == 7))
                        t_mm.append(tt)
                    xT_free[s] = t_mm[2]
                    if CUT <= 3:
                        continue
                    t_tabs = S.add('dve', lambda e, s=s: e.tensor_scalar(
                        out=tabs[s][:, :], in0=tab[s][:, :], scalar1=ss[s][:, 2:3], scalar2=None, op0=ALU.mult),
                        deps=[t_tab, t_rstd, tab_free[s]], sig=True)
                    t_a = S.add('dve', lambda e, s=s: e.tensor_tensor(
                        out=ra[:, :].rearrange("p (h d) -> p h d", h=8), in0=pP[0][:, :].rearrange("p (h d) -> p h d", h=8),
                        in1=bcast(tabs[s][:, 0:64], 8), op=ALU.mult),
                        deps=[t_mm[0], t_tabs, tmp_free.get('ra')], sig=True)
                    t_b1 = S.add('dve', lambda e, s=s: e.tensor_tensor(
                        out=rb[:, :].rearrange("p (h d) -> p h d", h=8)[:, :, 0:32],
                        in0=pP[0][:, :].rearrange("p (h d) -> p h d", h=8)[:, :, 32:64],
                        in1=bcast(tabs[s][:, 64:96], 8), op=ALU.mult),
                        deps=[t_mm[0], t_tabs, tmp_free.get('ra')], sig=True)
                    t_b2 = S.add('dve', lambda e, s=s: e.tensor_tensor(
                        out=rb[:, :].rearrange("p (h d) -> p h d", h=8)[:, :, 32:64],
                        in0=pP[0][:, :].rearrange("p (h d) -> p h d", h=8)[:, :, 0:32],
                        in1=bcast(tabs[s][:, 96:128], 8), op=ALU.mult),
                        deps=[t_mm[0], t_tabs, tmp_free.get('ra')], sig=True)
                    pP_free[0] = [t_a, t_b1, t_b2]
                    t_ksr = S.add('pool', lambda e: e.tensor_tensor(out=ksr[:, :], in0=ra[:, :], in1=rb[:, :], op=ALU.add),
                                  deps=[t_a, t_b1, t_b2, tmp_free.get('ksr')], sig=True)
                    tmp_free['ra'] = t_ksr
                    if CUT <= 4:
                        continue
                    t_v = S.add('act', lambda e, s=s: e.activation(
                        out=vst[s][:, :, 0:64], in_=pP[1][:, :].rearrange("p (h d) -> p h d", h=8), func=AF.Copy,
                        scale=ss[s][:, 2:3]),
                        deps=[t_mm[1], t_rstd, vst_free[s], t_vst1[s]], sig=True)
                    pP_free[1] = [t_v]
                    t_vd = S.add('sp', lambda e, s=s, k=k: e.dma_start(
                        out=s_vds[k, :, :], in_=vst[s][:, :, :].rearrange("p h d -> p (h d)")),
                        deps=[t_v], dma=f'st{s}')
                    if CUT <= 5:
                        continue
                    t_ckv = S.add('dve', lambda e, s=s: e.tensor_scalar(out=ckv[:, :], in0=pP[2][:, 0:128], scalar1=ss[s][:, 2:3],
                                                                       scalar2=None, op0=ALU.mult),
                                  deps=[t_mm[2], t_rstd, tmp_free.get('ckv')], sig=True)
                    t_ss2 = S.add('act', lambda e, s=s: e.activation(out=junk2[:, :], in_=ckv[:, :], func=AF.Square,
                                                                    accum_out=ss2[s][:, 0:1]),
                                  deps=[t_ckv, tmp_free.get('ss2%d' % s)], sig=True)
                    t_sd2 = S.add('act', lambda e, s=s: e.activation(out=ss2[s][:, 1:2], in_=ss2[s][:, 0:1], func=AF.Sqrt,
                                                                    scale=1.0 / 128, bias=epsT[:, 0:1]),
                                  deps=[t_ss2], sig=True)
                    t_r2 = S.add('dve', lambda e, s=s: e.reciprocal(out=ss2[s][:, 2:3], in_=ss2[s][:, 1:2]),
                                 deps=[t_sd2], sig=True)
                    t_ckvn = S.add('dve', lambda e, s=s: e.tensor_scalar(
                        out=ckvn[:, :], in0=ckv[:, :], scalar1=ss2[s][:, 2:3], scalar2=None, op0=ALU.mult),
                        deps=[t_r2, t_ckv, tmp_free.get('ckvn')], sig=True)
                    tmp_free['ckv'] = [t_ckvn, t_ss2]
                    tmp_free['ss2%d' % s] = t_ckvn
                    if CUT <= 6:
                        continue
                    t_ra = S.add('dve', lambda e, s=s: e.tensor_tensor(
                        out=rr_a[:, :], in0=pP[2][:, 128:160], in1=tabs[s][:, 128:160], op=ALU.mult),
                        deps=[t_mm[2], t_tabs, tmp_free.get('rr')], sig=True)
                    t_rb1 = S.add('dve', lambda e, s=s: e.tensor_tensor(
                        out=rr_b[:, 0:16], in0=pP[2][:, 144:160], in1=tabs[s][:, 160:176], op=ALU.mult),
                        deps=[t_mm[2], t_tabs, tmp_free.get('rr')], sig=True)
                    t_rb2 = S.add('dve', lambda e, s=s: e.tensor_tensor(
                        out=rr_b[:, 16:32], in0=pP[2][:, 128:144], in1=tabs[s][:, 176:192], op=ALU.mult),
                        deps=[t_mm[2], t_tabs, tmp_free.get('rr')], sig=True)
                    t_krr = S.add('pool', lambda e: e.tensor_tensor(out=kr96[:, 64:96], in0=rr_a[:, :], in1=rr_b[:, :], op=ALU.add),
                                  deps=[t_ra, t_rb1, t_rb2, tmp_free.get('krr'), t_kz], sig=True)
                    tmp_free['rr'] = t_krr
                    t_ia = S.add('dve', lambda e, s=s: e.tensor_tensor(
                        out=ri_a[:, :], in0=pP[2][:, 160:224], in1=tabs[s][:, 0:64], op=ALU.mult),
                        deps=[t_mm[2], t_tabs, tmp_free.get('ri')], sig=True)
                    t_ib1 = S.add('dve', lambda e, s=s: e.tensor_tensor(
                        out=ri_b[:, 0:32], in0=pP[2][:, 192:224], in1=tabs[s][:, 64:96], op=ALU.mult),
                        deps=[t_mm[2], t_tabs, tmp_free.get('ri')], sig=True)
                    t_ib2 = S.add('dve', lambda e, s=s: e.tensor_tensor(
                        out=ri_b[:, 32:64], in0=pP[2][:, 160:192], in1=tabs[s][:, 96:128], op=ALU.mult),
                        deps=[t_mm[2], t_tabs, tmp_free.get('ri')], sig=True)
                    t_kir = S.add('pool', lambda e: e.tensor_tensor(out=kir[:, :], in0=ri_a[:, :], in1=ri_b[:, :], op=ALU.add),
                                  deps=[t_ia, t_ib1, t_ib2, tmp_free.get('kir')], sig=True)
                    tmp_free['ri'] = t_kir
                    pP_free[2] = [t_ckv, t_ra, t_rb1, t_rb2, t_ia, t_ib1, t_ib2]
                    tab_free[s] = [t_a, t_b1, t_b2, t_ra, t_rb1, t_rb2, t_ia, t_ib1, t_ib2]
                    ss_free[s] = [t_tabs, t_v, t_ckv]
                    if CUT <= 7:
                        continue
                    for a in range(4):
                        t_t1 = S.add('pe', lambda e, a=a: e.transpose(
                            out=pS[:, a * 128:(a + 1) * 128], in_=ksr[:, a * 128:(a + 1) * 128], identity=identB[:, :]),
                            deps=[t_ksr, pS_free], sig=(a == 3))
                    if CUT <= 7.1:
                        continue
                    t_t2 = S.add('pe', lambda e: e.transpose(out=pS[0:64, 512:640], in_=kir[:, :], identity=identB[:, :]),
                                 deps=[t_kir, pS_free], sig=True)
                    if CUT <= 7.2:
                        continue
                    t_t3 = S.add('pe', lambda e: e.transpose(out=pS[0:96, 640:768], in_=kr96[:, :], identity=identB[:, :]),
                                 deps=[t_krr, pS_free], sig=True)
                    if CUT <= 7.3:
                        continue
                    t_t4 = S.add('pe', lambda e: e.transpose(out=pS[:, 768:896], in_=ckvn[:, :], identity=identB[:, :]),
                                 deps=[t_ckvn, pS_free], sig=True)
                    tmp_free['ksr'] = t_t1
                    tmp_free['kir'] = t_t2
                    tmp_free['krr'] = t_t3
                    tmp_free['ckvn'] = t_t4
                    if CUT <= 8:
                        continue
                    t_e1 = S.add('dve', lambda e, s=s: e.tensor_copy(out=kst[s][:, :], in_=pS[:, 0:512]),
                                 deps=[t_t4, kst_free[s]], sig=True)
                    if CUT <= 8.1:
                        continue
                    t_d1 = S.add('sp', lambda e, s=s, k=k: e.dma_start(out=s_ksT[k, :, :], in_=kst[s][:, :]),
                                 deps=[t_e1], dma=f'st{s}')
                    if CUT <= 8.2:
                        continue
                    t_e2 = S.add('dve', lambda e, s=s: e.tensor_copy(out=kit[s][:, :], in_=pS[0:64, 512:640]),
                                 deps=[t_t4, kit_free[s]], sig=True)
                    if CUT <= 8.3:
                        continue
                    t_d2 = S.add('sp', lambda e, s=s, k=k: e.dma_start(out=s_kiT[k, :, :], in_=kit[s][:, :]),
                                 deps=[t_e2], dma=f'st{s}')
                    if CUT <= 8.4:
                        continue
                    t_e3 = S.add('dve', lambda e, k=k: e.tensor_copy(out=knT[64:96, :, k * 128:(k + 1) * 128],
                                                                     in_=pS[64:96, 640:768].unsqueeze(1).broadcast_to([32, 8, 128])),
                                 deps=[t_t4], sig=True)
                    if CUT <= 8.5:
                        continue
                    t_e4 = S.add('dve', lambda e: e.tensor_copy(out=ckvnT[:, :], in_=pS[:, 768:896]),
                                 deps=[t_t4, ckvnT_free], sig=True)
                    pS_free = [t_e1, t_e2, t_e3, t_e4]
                    if CUT <= 9:
                        continue
                    for h in range(4):
                        t_k = S.add('pe', lambda e, h=h: e.matmul(
                            pK[0:64, h * 128:(h + 1) * 128], lhsT=WukK[:, h * 64:(h + 1) * 64], rhs=ckvnT[:, :],
                            start=True, stop=True), deps=[t_e4, pK_free, t_w], sig=(h == 3))
                    for h in range(4, 8):
                        t_k2 = S.add('pe', lambda e, h=h: e.matmul(
                            pP[1][0:64, (h - 4) * 128:(h - 3) * 128], lhsT=WukK[:, h * 64:(h + 1) * 64], rhs=ckvnT[:, :],
                            start=True, stop=True), deps=[t_e4, t_w] + pP_free[1], sig=(h == 7))
                    t_vv = S.add('pe', lambda e: e.matmul(pV[:, :], lhsT=ckvnT[:, :], rhs=WukV[:, :], start=True, stop=True),
                                 deps=[t_e4, pV_free, t_w], sig=True)
                    ckvnT_free = t_vv
                    pK_free = S.add('act', lambda e, k=k: e.activation(
                        out=knT[0:64, 0:4, k * 128:(k + 1) * 128], in_=pK[0:64, :].rearrange("p (a t) -> p a t", a=4), func=AF.Copy),
                        deps=[t_k], sig=True)
                    t_ke2 = S.add('act', lambda e, k=k: e.activation(
                        out=knT[0:64, 4:8, k * 128:(k + 1) * 128], in_=pP[1][0:64, :].rearrange("p (a t) -> p a t", a=4), func=AF.Copy),
                        deps=[t_k2], sig=True)
                    pP_free[1] = pP_free[1] + [t_ke2]
                    pV_free = S.add('dve', lambda e, s=s: e.tensor_copy(
                        out=vmst[s][:, :, 0:64], in_=pV[:, :].rearrange("p (h d) -> p h d", h=8)),
                        deps=[t_vv, t_vm1, t_cst, vst_free[s]], sig=True)
                    t_d3 = S.add('sp', lambda e, s=s, k=k: e.dma_start(out=s_vmla[k, :, :], in_=vmst[s][:, :, :].rearrange("p h d -> p (h d)")),
                                 deps=[pV_free], dma=f'st{s}')
                    kit_free[s] = t_d3
                    kst_free[s] = t_d3
                    vst_free[s] = t_d3
                    if k == 0:
                        vst_free[s] = S.add('pool', lambda e, s=s: e.memset(vst[s][:, :, 64:65], 1.0), deps=[t_d3], sig=True)
                        vst_free[s] = [vst_free[s], S.add('pool', lambda e, s=s: e.memset(vmst[s][:, :, 64:65], 1.0), deps=[t_d3], sig=True)]
                last = [pK_free, pV_free, kst_free[0], kst_free[1], kit_free[0], kit_free[1], vst_free[0], vst_free[1]]
                S.final_wait()
                S.run(nc, "A", top)
                if debug and CUT > 50:
                    S2_ = Sched()
                    t1 = S2_.add('sp', lambda e: e.dma_start(out=d_ksT[0:nkb, :, :], in_=s_ksT[0:nkb, :, :]), dma='d')
                    t1 = S2_.add('sp', lambda e: e.dma_start(out=d_vds[0:nkb, :, :], in_=s_vds[0:nkb, :, :]), dma='d')
                    t1 = S2_.add('sp', lambda e: e.dma_start(out=d_kiT[0:nkb, :, :], in_=s_kiT[0:nkb, :, :]), dma='d')
                    S2_.add('sp', lambda e: e.nop(), deps=[t1])
                    S2_.run(nc, "Ad", top)
        if 'B' in phases:
            with ExitStack() as pb_:
                S = Sched()
                xq = din("xq", [nqb * 128, D])
                wq_in = din("wq_in", [D, 1288])
                g_attn = din("g_attn", [128, 8])
                w_uq_n = din("w_uq_n", [256, 512])
                w_uq_r = din("w_uq_r", [256, 256])
                g_q = din("g_q", [128, 2])
                ropeQ = din("ropeQ", [nqb, 128, 192])
                mk_mult = din("mk_mult", [2, 128, 128], BF16)
                SC = 96.0 ** -0.5
                Wcq = sb("Wcq", [128, 8, 256], BF16, pb_)
                WuqN = sb("WuqN", [128, 2, 512], BF16, pb_)
                WuqR = sb("WuqR", [128, 2, 256], BF16, pb_)
                gA = sb("gAB", [128, 8], F32, pb_)
                gQ = sb("gQB", [128, 2], F32, pb_)
                mkm = sb("mkmB", [128, 2, 128], BF16, pb_)
                stg = [sb(f"stgB{i}", [128, 512], F32, pb_) for i in range(2)]
                xs = sb("xsB", [128, D], F32, pb_)
                xb = sb("xbB", [128, D], BF16, pb_)
                tab = sb("tabB", [128, 192], F32, pb_)
                tabq = sb("tabqB", [128, 64], F32, pb_)
                junk = sb("junkB", [128, D], BF16, pb_)
                ss = sb("ssB", [128, 8], F32, pb_)
                xT = sb("xTB", [128, 8, 128], BF16, pb_)
                cq = sb("cqB", [128, 256], F32, pb_)
                cqn = sb("cqnB", [128, 256], BF16, pb_)
                cqnT = sb("cqnTB", [128, 2, 128], BF16, pb_)
                q96 = sb("q96B", [96, 8, 128], BF16, pb_)
                qr96 = sb("qr96B", [128, 8, 96], BF16, pb_)
                vb = [sb(f"vbB{i}", [128, 8, 65], BF16, pb_) for i in range(3)]
                qra = sb("qraB", [128, 256], F32, pb_)
                qrb = sb("qrbB", [128, 256], F32, pb_)
                PT = [sb(f"PTB{i}", [128, 1024], BF16, pb_) for i in range(2)]
                rden = sb("rdenB", [128, 8], F32, pb_)
                ost = [sb(f"ostB{i}", [128, 512], BF16, pb_) for i in range(2)]
                pT = ps("pTB", [128, 1024], BF16, pb_)
                pQ = ps("pQB", [128, 512], F32, pb_)
                pST = [ps(f"pSTB{i}", [128, 512], F32, pb_) for i in range(4)]
                pO = [ps(f"pOB{i}", [128, 4, 65], F32, pb_) for i in range(2)]

                S.add('sp', lambda e: e.dma_start(out=gA[:, :], in_=g_attn[:, :]), dma='cst')
                S.add('sp', lambda e: e.dma_start(out=gQ[:, :], in_=g_q[:, :]), dma='cst')
                t_cst = S.add('sp', lambda e: e.dma_start(out=mkm[:, :, :], in_=mk_mult.rearrange("m k q -> k m q")), dma='cst')
                t_z = S.add('pool', lambda e: e.memset(qr96[:, :, :], 0.0), sig=True)
                stg_free = [None, None]
                t_w = []
                jobs = [(Wcq[:, c, :], wq_in[c * 128:(c + 1) * 128, 0:256], gA[:, c:c + 1], 256) for c in range(8)]
                jobs += [(WuqN[:, c, :], w_uq_n[c * 128:(c + 1) * 128, :], gQ[:, c:c + 1], 512) for c in range(2)]
                jobs += [(WuqR[:, c, :], w_uq_r[c * 128:(c + 1) * 128, :], gQ[:, c:c + 1], 256) for c in range(2)]
                for j, (dst, src, gs, n) in enumerate(jobs):
                    s = j % 2
                    td = S.add('sp', lambda e, s=s, src=src, n=n: e.dma_start(out=stg[s][:, 0:n], in_=src),
                               deps=[stg_free[s]], dma=f'stg{s}')
                    stg_free[s] = S.add('dve' if s == 0 else 'pool', lambda e, s=s, dst=dst, gs=gs, n=n: e.tensor_scalar(
                        out=dst, in0=stg[s][:, 0:n], scalar1=gs, scalar2=None, op0=ALU.mult), deps=[td, t_cst], sig=True)
                    t_w.append(stg_free[s])

                prev = []
                vb_free = [None] * 3
                vcount = 0
                pst_free = [None] * 4
                PT_free = [None, None]
                ost_free = [None, None]
                t_onorm = None
                for i in range(nqb):
                    nk = min(2 * i + 3, nkb)
                    t_x = S.add('sp', lambda e, i=i: e.dma_start(out=xs[:, :], in_=xq[i * 128:(i + 1) * 128, :]), deps=prev, dma='ld')
                    t_x = S.add('sp', lambda e, i=i: e.dma_start(out=tab[:, :], in_=ropeQ[i, :, :]), deps=prev, dma='ld')
                    t_ss = S.add('act', lambda e: e.activation(out=junk[:, :], in_=xs[:, :], func=AF.Square, accum_out=ss[:, 0:1]),
                                 deps=[t_x] + prev, sig=True)
                    t_sd = S.add('act', lambda e: e.activation(out=ss[:, 1:2], in_=ss[:, 0:1], func=AF.Sqrt, scale=1.0 / D, bias=epsT[:, 0:1]),
                                 deps=[t_ss], sig=True)
                    t_rstd = S.add('dve', lambda e: e.reciprocal(out=ss[:, 2:3], in_=ss[:, 1:2]), deps=[t_sd], sig=True)
                    t_xb = S.add('dve', lambda e: e.tensor_copy(out=xb[:, :], in_=xs[:, :]), deps=[t_x] + prev, sig=True)
                    t_tq = S.add('dve', lambda e: e.tensor_scalar(out=tabq[:, :], in0=tab[:, 128:192], scalar1=SC, scalar2=None, op0=ALU.mult),
                                 deps=[t_x] + prev, sig=True)
                    for c in range(8):
                        t_tr = S.add('pe', lambda e, c=c: e.transpose(out=pT[:, c * 128:(c + 1) * 128], in_=xb[:, c * 128:(c + 1) * 128],
                                                                      identity=identB[:, :]), deps=[t_xb] + prev, sig=(c == 7))
                    t_xT = S.add('dve', lambda e: e.tensor_copy(out=xT[:, :, :], in_=pT[:, :].rearrange("p (c t) -> p c t", c=8)),
                                 deps=[t_tr], sig=True)
                    for c in range(8):
                        t_mm = S.add('pe', lambda e, c=c: e.matmul(pQ[:, 0:256], lhsT=xT[:, c, :], rhs=Wcq[:, c, :], start=(c == 0), stop=(c == 7)),
                                     deps=[t_xT, t_w] + prev, sig=(c == 7))
                    t_cq = S.add('dve', lambda e: e.tensor_scalar(out=cq[:, :], in0=pQ[:, 0:256], scalar1=ss[:, 2:3], scalar2=None, op0=ALU.mult),
                                 deps=[t_mm, t_rstd], sig=True)
                    t_ss2 = S.add('act', lambda e: e.activation(out=junk[:, 0:256], in_=cq[:, :], func=AF.Square, accum_out=ss[:, 4:5]),
                                  deps=[t_cq], sig=True)
                    t_sd2 = S.add('act', lambda e: e.activation(out=ss[:, 5:6], in_=ss[:, 4:5], func=AF.Sqrt, scale=1.0 / 256, bias=epsT[:, 0:1]),
                                  deps=[t_ss2], sig=True)
                    t_r2 = S.add('dve', lambda e: e.reciprocal(out=ss[:, 6:7], in_=ss[:, 5:6]), deps=[t_sd2], sig=True)
                    t_cqn = S.add('dve', lambda e: e.tensor_scalar(out=cqn[:, :], in0=cq[:, :], scalar1=ss[:, 6:7], scalar2=None, op0=ALU.mult),
                                  deps=[t_r2], sig=True)
                    for c in range(2):
                        t_tr = S.add('pe', lambda e, c=c: e.transpose(out=pT[:, c * 128:(c + 1) * 128], in_=cqn[:, c * 128:(c + 1) * 128],
                                                                      identity=identB[:, :]), deps=[t_cqn, t_xT], sig=(c == 1))
                    t_cT = S.add('dve', lambda e: e.tensor_copy(out=cqnT[:, :, :], in_=pT[:, 0:256].rearrange("p (c t) -> p c t", c=2)),
                                 deps=[t_tr], sig=True)
                    t_qn = None
                    for rnd in range(2):
                        for j in range(4):
                            h = rnd * 4 + j
                            for c in range(2):
                                t_mm = S.add('pe', lambda e, j=j, h=h, c=c: e.matmul(pQ[0:64, j * 128:(j + 1) * 128], lhsT=WuqN[:, c, h * 64:(h + 1) * 64],
                                                                                rhs=cqnT[:, c, :], start=(c == 0), stop=(c == 1)),
                                             deps=[t_cT, t_cq, t_qn], sig=(j == 3 and c == 1))
                        t_qn = S.add('dve', lambda e, rnd=rnd: e.tensor_scalar(out=q96[0:64, rnd * 4:(rnd + 1) * 4, :],
                                                                               in0=pQ[0:64, :].rearrange("p (a t) -> p a t", a=4),
                                                                               scalar1=SC, scalar2=None, op0=ALU.mult), deps=[t_mm] + prev, sig=True)
                    t_q1 = t_qn
                    for c in range(2):
                        t_mm = S.add('pe', lambda e, c=c: e.matmul(pQ[:, 0:256], lhsT=cqnT[:, c, :], rhs=WuqR[:, c, :], start=(c == 0), stop=(c == 1)),
                                     deps=[t_q1], sig=(c == 1))
                    v3 = lambda ap: ap.rearrange("p (h d) -> p h d", h=8)
                    t_a = S.add('dve', lambda e: e.tensor_tensor(out=v3(qra[:, :]), in0=v3(pQ[:, 0:256]), in1=bcast(tabq[:, 0:32], 8), op=ALU.mult),
                                deps=[t_mm, t_tq] + prev, sig=True)
                    t_b1 = S.add('dve', lambda e: e.tensor_tensor(out=v3(qrb[:, :])[:, :, 0:16], in0=v3(pQ[:, 0:256])[:, :, 16:32],
                                                                  in1=bcast(tabq[:, 32:48], 8), op=ALU.mult), deps=[t_mm, t_tq] + prev, sig=True)
                    t_b2 = S.add('dve', lambda e: e.tensor_tensor(out=v3(qrb[:, :])[:, :, 16:32], in0=v3(pQ[:, 0:256])[:, :, 0:16],
                                                                  in1=bcast(tabq[:, 48:64], 8), op=ALU.mult), deps=[t_mm, t_tq] + prev, sig=True)
                    t_qr = S.add('pool', lambda e: e.tensor_tensor(out=qr96[:, :, 64:96], in0=v3(qra[:, :]), in1=v3(qrb[:, :]), op=ALU.add),
                                 deps=[t_a, t_b1, t_b2, t_z] + prev, sig=True)
                    for h in range(8):
                        t_tr = S.add('pe', lambda e, h=h: e.transpose(out=pT[0:96, h * 128:(h + 1) * 128], in_=qr96[:, h, :],
                                                                      identity=identB[:, :]), deps=[t_qr, t_cT], sig=(h == 7))
                    t_qrT = S.add('dve', lambda e: e.tensor_copy(out=q96[64:96, :, :], in_=pT[64:96, :].rearrange("p (h t) -> p h t", h=8)),
                                  deps=[t_tr] + prev, sig=True)
                    qready = [t_q1, t_qrT]
                    prev_q = [t_b2, t_qrT, t_ss2, t_cqn]
                    t_exp = {}
                    t_pv = None

                    def emit_qk(kb):
                        for g in range(2):
                            bank = pST[(kb % 2) * 2 + g]
                            for j in range(4):
                                h = g * 4 + j
                                t = S.add('pe', lambda e, bank=bank, j=j, h=h, kb=kb: e.matmul(
                                    bank[:, j * 128:(j + 1) * 128], lhsT=knT[0:96, h, kb * 128:(kb + 1) * 128], rhs=q96[0:96, h, :],
                                    start=(j == 0), stop=(j == 3), skip_group_check=True),
                                    deps=qready + [pst_free[(kb % 2) * 2 + g]], sig=(j == 3))
                            t_qk[(kb, g)] = t

                    t_v = {}

                    def emit_v(kb):
                        nonlocal vcount
                        sl = vcount % 3
                        vcount += 1
                        t_v[kb] = (S.add('sp', lambda e, kb=kb, sl=sl: e.dma_start(out=vb[sl][:, :, :].rearrange("p h d -> p (h d)"), in_=s_vmla[kb, :, :]),
                                         deps=[vb_free[sl]], dma=f'vb{sl}'), sl)
                    t_qk = {}

                    def emit_exp(kb):
                        for g in range(2):
                            bi = (kb % 2) * 2 + g
                            t = S.add('act', lambda e, bi=bi, kb=kb, g=g: e.activation(
                                out=PT[kb % 2][:, g * 512:(g + 1) * 512], in_=pST[bi][:, :], func=AF.Exp),
                                deps=[t_qk[(kb, g)], PT_free[kb % 2]], sig=True)
                            pst_free[bi] = t
                            mi = kb - (2 * i + 1)
                            if mi >= 0:
                                t = S.add('pool', lambda e, kb=kb, g=g, mi=mi: e.tensor_tensor(
                                    out=PT[kb % 2][:, g * 512:(g + 1) * 512].rearrange("p (h q) -> p h q", h=4),
                                    in0=PT[kb % 2][:, g * 512:(g + 1) * 512].rearrange("p (h q) -> p h q", h=4),
                                    in1=bcast(mkm[:, mi, :], 4), op=ALU.mult), deps=[t, t_cst], sig=True)
                            t_exp[(kb, g)] = t

                    def emit_pv(kb):
                        nonlocal t_pv
                        for h in range(8):
                            t_pv = S.add('pe', lambda e, h=h, kb=kb, sl=t_v[kb][1]: e.matmul(
                                pO[h // 4][:, h % 4, :], lhsT=PT[kb % 2][:, h * 128:(h + 1) * 128], rhs=vb[sl][:, h, :],
                                start=(kb == 0 and h % 4 == 0), stop=(kb == nk - 1), skip_group_check=True),
                                deps=[t_exp[(kb, h // 4)], t_onorm, t_v[kb][0]], sig=(h == 7))
                        PT_free[kb % 2] = t_pv
                        vb_free[t_v[kb][1]] = t_pv

                    emit_v(0)
                    emit_qk(0)
                    for kb in range(nk):
                        if kb + 1 < nk:
                            emit_v(kb + 1)
                            emit_qk(kb + 1)
                        emit_exp(kb)
                        emit_pv(kb)
                    so = i % 2
                    t_rd = S.add('dve', lambda e: e.reciprocal(out=rden[:, 0:4], in_=pO[0][:, :, 64]), deps=[t_pv], sig=True)
                    t_rd2 = S.add('dve', lambda e: e.reciprocal(out=rden[:, 4:8], in_=pO[1][:, :, 64]), deps=[t_pv], sig=True)
                    for g in range(2):
                        t_onorm = S.add('dve', lambda e, g=g, so=so: e.tensor_tensor(
                            out=ost[so][:, g * 256:(g + 1) * 256].rearrange("p (h d) -> p h d", h=4), in0=pO[g][:, :, 0:64],
                            in1=rden[:, g * 4:(g + 1) * 4].unsqueeze(2).broadcast_to([128, 4, 64]), op=ALU.mult),
                            deps=[t_rd, t_rd2, ost_free[so]], sig=True)
                    ost_free[so] = S.add('sp', lambda e, i=i, so=so: e.dma_start(out=s_omla[i, :, :], in_=ost[so][:, :]),
                                         deps=[t_onorm], dma=f'ost{so}')
                    prev = [t_pv, t_onorm] + prev_q
                if debug:
                    S.final_wait()
                    S.add('sp', lambda e: e.dma_start(out=d_omla.rearrange("p (i f) -> i p f", i=nqb), in_=s_omla[0:nqb, :, :]), dma='dbg')
                S.final_wait()
                S.run(nc, "B", top)
        abst.close()
        if 'C' in phases:
            with ExitStack() as pc_:
                S = Sched()
                xq = din("xq", [nqb * 128, D])
                wq_in = din("wq_in", [D, 1288])
                g_attn = din("g_attn", [128, 8])
                ropeQ = din("ropeQ", [nqb, 128, 192])
                mk_add = din("mk_add", [3, 128, 128])
                NIT = 16
                Wq2 = sb("Wq2C", [128, 8, 1032], BF16, pc_)
                gA = sb("gAC", [128, 8], F32, pc_)
                mka = sb("mkaC", [128, 3, 128], F32, pc_)
                stg = [sb("stgC0", [128, 1032], F32, pc_)] * 2
                ksT = sb("ksTC", [128, nkb, 512], BF16, pc_)
                kiT2 = sb("kiT2C", [128, nkb, 128], BF16, pc_)
                Isc = sb("IscC", [128, nkb * 128], F32, pc_)
                MskL = [sb(f"MskC{i}", [128, nkb * 128], BF16, pc_) for i in range(2)]
                xs = sb("xsC", [128, D], F32, pc_)
                xb = sb("xbC", [128, D], BF16, pc_)
                tab = sb("tabC", [128, 192], F32, pc_)
                tabs = sb("tabsC", [128, 128], F32, pc_)
                junk = sb("junkC", [128, D], BF16, pc_)
                ss = sb("ssC", [128, 8], F32, pc_)
                xT = sb("xTC", [128, 8, 128], BF16, pc_)
                ra = sb("raC", [128, 512], F32, pc_)
                rb = sb("rbC", [128, 512], F32, pc_)
                qs = sb("qsC", [128, 512], BF16, pc_)
                qi = sb("qiC", [128, 512], BF16, pc_)
                wv = sb("wvC", [128, 8], F32, pc_)
                qbdL = [sb(f"qbdC{i}", [128, 4, 256], BF16, pc_) for i in range(2)]
                qiT = sb("qiTC", [128, 4, 128], BF16, pc_)
                tmpR = [sb(f"tmpRC{i}", [128, 512], F32, pc_) for i in range(3)]
                pw2 = sb("pw2C", [128, 32], F32, pc_)
                hwt = sb("hwtC", [128, 32], F32, pc_)
                bs = sb("bsC", [128, 16], F32, pc_)
                PT = [sb(f"PTC{i}", [128, 1024], BF16, pc_) for i in range(2)]
                vb = [sb(f"vbC{i}", [128, 8, 65], BF16, pc_) for i in range(3)]
                rden = sb("rdenC", [128, 8], F32, pc_)
                ost = [sb(f"ostC{i}", [128, 512], BF16, pc_) for i in range(2)]
                pT = [ps(f"pTC{i}", [128, 1024], BF16, pc_) for i in range(2)]
                pST = [ps(f"pSTC{i}", [128, 512], F32, pc_) for i in range(4)]
                pO = [ps(f"pOC{i}", [128, 4, 65], F32, pc_) for i in range(2)]

                S.add('sp', lambda e: e.dma_start(out=gA[:, :], in_=g_attn[:, :]), dma='cst')
                S.add('sp', lambda e: e.dma_start(out=mka[:, :, :], in_=mk_add.rearrange("m q k -> q m k")), dma='cst')
                for k0 in range(0, nkb, 4):
                    k1 = min(nkb, k0 + 4)
                    S.add('sp', lambda e, k0=k0, k1=k1: e.dma_start(out=ksT[:, k0:k1, :], in_=s_ksT[k0:k1, :, :].rearrange("k p f -> p k f")), dma='cst')
                for k0 in range(0, nkb, 8):
                    k1 = min(nkb, k0 + 8)
                    S.add('sp', lambda e, k0=k0, k1=k1: e.dma_start(out=kiT2[0:64, k0:k1, :], in_=s_kiT[k0:k1, :, :].rearrange("k p t -> p k t")), dma='cst')
                    t_cst = S.add('sp', lambda e, k0=k0, k1=k1: e.dma_start(out=kiT2[64:128, k0:k1, :], in_=s_kiT[k0:k1, :, :].rearrange("k p t -> p k t")), dma='cst')
                S.add('pool', lambda e: e.memset(qbdL[0][:, :, :], 0.0), sig=True)
                t_z = S.add('pool', lambda e: e.memset(qbdL[1][:, :, :], 0.0), sig=True)
                for it in range(NIT):
                    t_pw2 = S.add('pool', lambda e, it=it: e.memset(pw2[:, it:it + 1], 2.0 ** -(it + 1)), sig=True)
                stg_free = [None, None]
                t_w = []
                for c in range(8):
                    s = 0
                    td = S.add('sp', lambda e, s=s, c=c: e.dma_start(out=stg[s][:, :], in_=wq_in[c * 128:(c + 1) * 128, 256:1288]),
                               deps=[stg_free[s]], dma=f'stg{s}')
                    stg_free[s] = S.add(('dve', 'pool')[s], lambda e, s=s, c=c: e.tensor_scalar(
                        out=Wq2[:, c, :], in0=stg[s][:, :], scalar1=gA[:, c:c + 1], scalar2=None, op0=ALU.mult), deps=[td, t_cst], sig=True)
                    t_w.append(stg_free[s])
                v3 = lambda ap: ap.rearrange("p (h d) -> p h d", h=8)
                G = dict(prevA=[], att_done=[], t_onorm=None)
                stt = {}
                pst_free = [None] * 4
                PT_free = [None, None]
                pT_free = [None, None]
                vb_free = [None] * 3
                ost_free = [None, None]
                vcount = 0

                def genA(i):
                    nk = min(2 * i + 3, nkb)
                    W = nk * 128
                    prev = G['prevA'] + G['att_done']
                    Msk = MskL[i % 2]
                    qbd = qbdL[i % 2]
                    S.add('sp', lambda e, i=i: e.dma_start(out=xs[:, :], in_=xq[i * 128:(i + 1) * 128, :]), deps=prev, dma='ld')
                    t_x = S.add('sp', lambda e, i=i: e.dma_start(out=tab[:, :], in_=ropeQ[i, :, :]), deps=prev, dma='ld')
                    t_ss = S.add('act', lambda e: e.activation(out=junk[:, :], in_=xs[:, :], func=AF.Square, accum_out=ss[:, 0:1]), deps=[t_x] + prev, sig=True)
                    t_sd = S.add('act', lambda e: e.activation(out=ss[:, 1:2], in_=ss[:, 0:1], func=AF.Sqrt, scale=1.0 / D, bias=epsT[:, 0:1]), deps=[t_ss], sig=True)
                    t_rstd = S.add('dve', lambda e: e.reciprocal(out=ss[:, 2:3], in_=ss[:, 1:2]), deps=[t_sd], sig=True)
                    t_r8 = S.add('dve', lambda e: e.tensor_scalar(out=ss[:, 3:4], in0=ss[:, 2:3], scalar1=0.125, scalar2=None, op0=ALU.mult), deps=[t_rstd], sig=True)
                    t_tabs = S.add('dve', lambda e: e.tensor_scalar(out=tabs[:, :], in0=tab[:, 0:128], scalar1=ss[:, 3:4], scalar2=None, op0=ALU.mult),
                                   deps=[t_r8, t_x] + prev, sig=True)
                    t_xb = S.add('dve', lambda e: e.tensor_copy(out=xb[:, :], in_=xs[:, :]), deps=[t_x] + prev, sig=True)
                    for c in range(8):
                        t_tr = S.add('pe', lambda e, c=c: e.transpose(out=pT[0][:, c * 128:(c + 1) * 128], in_=xb[:, c * 128:(c + 1) * 128],
                                                                      identity=identB[:, :]), deps=[t_xb] + prev, sig=(c == 7))
                    t_xT = S.add('dve', lambda e: e.tensor_copy(out=xT[:, :, :], in_=pT[0][:, :].rearrange("p (c t) -> p c t", c=8)), deps=[t_tr], sig=True)
                    t_mms = []
                    for bnk, (c0, c1) in enumerate(((0, 512), (512, 1024), (1024, 1032))):
                        for c in range(8):
                            t_mm = S.add('pe', lambda e, c=c, bnk=bnk, c0=c0, c1=c1: e.matmul(pST[bnk][:, 0:c1 - c0], lhsT=xT[:, c, :], rhs=Wq2[:, c, c0:c1],
                                                                                             start=(c == 0), stop=(c == 7)), deps=[t_xT, t_w] + prev, sig=(c == 7))
                        t_mms.append(t_mm)
                    outs = []
                    for bnk, dst in ((0, qs), (1, qi)):
                        t_a = S.add('dve', lambda e, bnk=bnk: e.tensor_tensor(out=v3(ra[:, :]), in0=v3(pST[bnk][:, :]), in1=bcast(tabs[:, 0:64], 8), op=ALU.mult),
                                    deps=[t_mms[bnk], t_tabs] + outs + prev, sig=True)
                        t_b1 = S.add('dve', lambda e, bnk=bnk: e.tensor_tensor(out=v3(rb[:, :])[:, :, 0:32], in0=v3(pST[bnk][:, :])[:, :, 32:64],
                                                                              in1=bcast(tabs[:, 64:96], 8), op=ALU.mult), deps=[t_mms[bnk], t_tabs] + outs + prev, sig=True)
                        t_b2 = S.add('dve', lambda e, bnk=bnk: e.tensor_tensor(out=v3(rb[:, :])[:, :, 32:64], in0=v3(pST[bnk][:, :])[:, :, 0:32],
                                                                              in1=bcast(tabs[:, 96:128], 8), op=ALU.mult), deps=[t_mms[bnk], t_tabs] + outs + prev, sig=True)
                        t_q = S.add('pool', lambda e, dst=dst: e.tensor_tensor(out=dst[:, :], in0=ra[:, :], in1=rb[:, :], op=ALU.add),
                                    deps=[t_a, t_b1, t_b2] + prev, sig=True)
                        outs = [t_q]
                        if bnk == 0:
                            t_qs = t_q
                        else:
                            t_qi = t_q
                    t_wv = S.add('dve', lambda e: e.tensor_scalar(out=wv[:, :], in0=pST[2][:, 0:8], scalar1=ss[:, 2:3], scalar2=8.0 ** -0.5,
                                                                  op0=ALU.mult, op1=ALU.mult), deps=[t_mms[2], t_rstd] + prev, sig=True)
                    pst_q = [t_b2, t_wv]
                    for a in range(4):
                        t_tr = S.add('pe', lambda e, a=a: e.transpose(out=pT[0][:, a * 128:(a + 1) * 128], in_=qs[:, a * 128:(a + 1) * 128],
                                                                      identity=identB[:, :]), deps=[t_qs, t_xT], sig=(a == 3))
                    for a in range(4):
                        t_tr2 = S.add('pe', lambda e, a=a: e.transpose(out=pT[1][:, a * 128:(a + 1) * 128], in_=qi[:, a * 128:(a + 1) * 128],
                                                                       identity=identB[:, :]), deps=[t_qi, pT_free[1]] + prev, sig=(a == 3))
                    pTv = lambda t: t[:, 0:512].rearrange("p (a t) -> p a t", a=4)
                    t_q1 = S.add('dve', lambda e: e.tensor_copy(out=qbd[0:64, :, 0:128], in_=pTv(pT[0])[0:64, :, :]), deps=[t_tr, t_z] + prev, sig=True)
                    t_q2 = S.add('dve', lambda e: e.tensor_copy(out=qbd[64:128, :, 128:256], in_=pTv(pT[0])[64:128, :, :]), deps=[t_tr, t_z] + prev, sig=True)
                    t_qiT = S.add('dve', lambda e: e.tensor_copy(out=qiT[:, :, :], in_=pTv(pT[1])), deps=[t_tr2] + prev, sig=True)
                    pT_free[0] = t_q2
                    pT_free[1] = t_qiT
                    t_acc = None
                    tmp_free = [None] * 3
                    nch = (W + 511) // 512
                    cnt_ = 0
                    for ch in range(nch):
                        c0 = ch * 512
                        n = min(512, W - c0)
                        for h in range(8):
                            hp = (h % 2) * 64
                            bnk = h % 4
                            t_lg = S.add('pe', lambda e, hp=hp, h=h, bnk=bnk, c0=c0, n=n: e.matmul(
                                pST[bnk][:, 0:n], lhsT=qiT[hp:hp + 64, h // 2, :],
                                rhs=kiT2[hp:hp + 64, :, :].rearrange("p k t -> p (k t)")[:, c0:c0 + n], start=True, stop=True),
                                deps=[t_qiT, t_cst, pst_free[bnk]] + pst_q, sig=True)
                            sl = cnt_ % 3
                            cnt_ += 1
                            t_r = S.add('act', lambda e, bnk=bnk, sl=sl, n=n: e.activation(out=tmpR[sl][:, 0:n], in_=pST[bnk][:, 0:n], func=AF.Relu),
                                        deps=[t_lg, tmp_free[sl]], sig=True)
                            pst_free[bnk] = t_r
                            if h == 0:
                                t_acc = S.add('dve', lambda e, sl=sl, c0=c0, n=n: e.tensor_scalar(
                                    out=Isc[:, c0:c0 + n], in0=tmpR[sl][:, 0:n], scalar1=wv[:, 0:1], scalar2=None, op0=ALU.mult),
                                    deps=[t_r, t_wv, t_acc] + prev, sig=True)
                            else:
                                t_acc = S.add('dve', lambda e, sl=sl, c0=c0, n=n, h=h: e.scalar_tensor_tensor(
                                    out=Isc[:, c0:c0 + n], in0=tmpR[sl][:, 0:n], scalar=wv[:, h:h + 1], in1=Isc[:, c0:c0 + n],
                                    op0=ALU.mult, op1=ALU.add), deps=[t_r, t_wv, t_acc], sig=True)
                            tmp_free[sl] = t_acc
                    t_mx = S.add('dve', lambda e, W=W: e.tensor_reduce(out=bs[:, 1:2], in_=Isc[:, 0:W], axis=AX.X, op=ALU.max), deps=[t_acc] + prev, sig=True)
                    t_mn = S.add('dve', lambda e, W=W: e.tensor_reduce(out=bs[:, 0:1], in_=Isc[:, 0:W], axis=AX.X, op=ALU.min), deps=[t_acc] + prev, sig=True)
                    t_b = S.add('dve', lambda e: e.tensor_scalar(out=bs[:, 1:2], in0=bs[:, 1:2], scalar1=1.0, scalar2=None, op0=ALU.add), deps=[t_mx], sig=True)
                    t_b = S.add('dve', lambda e: e.tensor_scalar(out=bs[:, 0:1], in0=bs[:, 0:1], scalar1=-1.0, scalar2=None, op0=ALU.add), deps=[t_mn, t_b], sig=True)
                    for (mi, kb) in ((0, 0), (1, nk - 2), (2, nk - 1)):
                        t_b = S.add('dve', lambda e, mi=mi, kb=kb: e.tensor_tensor(out=Isc[:, kb * 128:(kb + 1) * 128], in0=Isc[:, kb * 128:(kb + 1) * 128],
                                                                                  in1=mka[:, mi, :], op=ALU.add), deps=[t_b, t_mn, t_mx, t_cst], sig=True)
                    W1 = (nk // 2) * 128
                    W2 = W - W1
                    t_b = S.add('dve', lambda e: e.tensor_tensor(out=bs[:, 7:8], in0=bs[:, 1:2], in1=bs[:, 0:1], op=ALU.subtract), deps=[t_b], sig=True)
                    t_b = S.add('dve', lambda e: e.tensor_scalar(out=hwt[:, 0:NIT], in0=pw2[:, 0:NIT], scalar1=bs[:, 7:8], scalar2=None, op0=ALU.mult),
                                deps=[t_b, t_pw2], sig=True)
                    t_b = S.add('dve', lambda e: e.tensor_tensor(out=bs[:, 2:3], in0=bs[:, 0:1], in1=hwt[:, 0:1], op=ALU.add), deps=[t_b], sig=True)
                    yield 'front'
                    for it in range(NIT):
                        t_c1 = S.add('dve', lambda e, W1=W1: e.tensor_scalar(out=Msk[:, 0:W1], in0=Isc[:, 0:W1], scalar1=bs[:, 2:3], scalar2=0.0,
                                                                          op0=ALU.is_ge, op1=ALU.add, accum_out=bs[:, 3:4]), deps=[t_b] + prev, sig=True)
                        t_c2 = S.add('act', lambda e, W1=W1, W=W: e.activation(out=Msk[:, W1:W], in_=Isc[:, W1:W], func=AF.Sign, scale=-1.0,
                                                                             bias=bs[:, 2:3], accum_out=bs[:, 8:9]), deps=[t_b] + prev, sig=True)
                        t_b = S.add('dve', lambda e: e.scalar_tensor_tensor(out=bs[:, 9:10], in0=bs[:, 8:9], scalar=-0.5, in1=bs[:, 3:4],
                                                                            op0=ALU.mult, op1=ALU.add), deps=[t_c1, t_c2], sig=True)
                        t_b = S.add('dve', lambda e, W2=W2: e.tensor_scalar(out=bs[:, 4:5], in0=bs[:, 9:10], scalar1=float(TOPK) - 0.5 - W2 / 2.0,
                                                                          scalar2=None, op0=ALU.is_ge), deps=[t_b], sig=True)
                        t_b = S.add('dve', lambda e, it=it: e.scalar_tensor_tensor(out=bs[:, 0:1], in0=bs[:, 4:5], scalar=hwt[:, it:it + 1], in1=bs[:, 0:1],
                                                                                   op0=ALU.mult, op1=ALU.add), deps=[t_b], sig=True)
                        if it + 1 < NIT:
                            t_b = S.add('dve', lambda e, it=it: e.tensor_tensor(out=bs[:, 2:3], in0=bs[:, 0:1], in1=hwt[:, it + 1:it + 2], op=ALU.add),
                                        deps=[t_b], sig=True)
                        yield 'it'
                    t_msk = S.add('dve', lambda e, W=W: e.tensor_scalar(out=Msk[:, 0:W], in0=Isc[:, 0:W], scalar1=bs[:, 0:1], scalar2=None, op0=ALU.is_ge),
                                  deps=[t_b], sig=True)
                    stt[i] = dict(nk=nk, t_q1=t_q1, t_q2=t_q2, t_msk=t_msk)
                    G['prevA'] = [t_msk, t_qiT, t_q2, t_acc]

                def genB(i):
                    nonlocal vcount
                    nk = stt[i]['nk']
                    t_msk = stt[i]['t_msk']
                    Msk = MskL[i % 2]
                    qbd = qbdL[i % 2]
                    qready = [stt[i]['t_q1'], stt[i]['t_q2'], t_msk]
                    t_qk = {}
                    t_exp = {}
                    t_v = {}
                    t_pv = None

                    def emit_qk(kb):
                        for g in range(2):
                            bi = (kb % 2) * 2 + g
                            for j in range(2):
                                a = g * 2 + j
                                t = S.add('pe', lambda e, bi=bi, j=j, a=a, kb=kb: e.matmul(
                                    pST[bi][:, j * 256:(j + 1) * 256], lhsT=ksT[:, kb, a * 128:(a + 1) * 128], rhs=qbd[:, a, :],
                                    start=(j == 0), stop=(j == 1), skip_group_check=True), deps=qready + [pst_free[bi], t_cst], sig=(j == 1))
                            t_qk[(kb, g)] = t
                        t_qk[(kb, 'm')] = S.add('pe', lambda e, kb=kb: e.transpose(out=pT[kb % 2][:, 0:128], in_=Msk[:, kb * 128:(kb + 1) * 128],
                                                                                  identity=identB[:, :]), deps=[t_msk, pT_free[kb % 2]], sig=True)

                    def emit_v(kb):
                        nonlocal vcount
                        sl = vcount % 3
                        vcount += 1
                        t_v[kb] = (S.add('sp', lambda e, kb=kb, sl=sl: e.dma_start(out=vb[sl][:, :, :].rearrange("p h d -> p (h d)"), in_=s_vds[kb, :, :]),
                                         deps=[vb_free[sl]], dma=f'vb{sl}'), sl)

                    def emit_exp(kb):
                        for g in range(2):
                            bi = (kb % 2) * 2 + g
                            t = S.add('act', lambda e, bi=bi, kb=kb, g=g: e.activation(
                                out=PT[kb % 2][:, g * 512:(g + 1) * 512], in_=pST[bi][:, :], func=AF.Exp),
                                deps=[t_qk[(kb, g)], PT_free[kb % 2]], sig=True)
                            pst_free[bi] = t
                            t = S.add('dve', lambda e, kb=kb, g=g: e.tensor_tensor(
                                out=PT[kb % 2][:, g * 512:(g + 1) * 512].rearrange("p (h q) -> p h q", h=4),
                                in0=PT[kb % 2][:, g * 512:(g + 1) * 512].rearrange("p (h q) -> p h q", h=4),
                                in1=pT[kb % 2][:, 0:128].unsqueeze(1).broadcast_to([128, 4, 128]), op=ALU.mult),
                                deps=[t, t_qk[(kb, 'm')]], sig=True)
                            t_exp[(kb, g)] = t
                        pT_free[kb % 2] = t

                    def emit_pv(kb):
                        nonlocal t_pv
                        tv, sl = t_v[kb]
                        for h in range(8):
                            t_pv = S.add('pe', lambda e, h=h, kb=kb, sl=sl: e.matmul(
                                pO[h // 4][:, h % 4, :], lhsT=PT[kb % 2][:, h * 128:(h + 1) * 128], rhs=vb[sl][:, h, :],
                                start=(kb == 0 and h % 4 == 0), stop=(kb == nk - 1), skip_group_check=True),
                                deps=[t_exp[(kb, h // 4)], tv, G['t_onorm']], sig=(h == 7))
                        PT_free[kb % 2] = t_pv
                        vb_free[sl] = t_pv

                    emit_v(0)
                    emit_qk(0)
                    for kb in range(nk):
                        if kb + 1 < nk:
                            emit_v(kb + 1)
                            emit_qk(kb + 1)
                        emit_exp(kb)
                        emit_pv(kb)
                        yield 'kb'
                    so = i % 2
                    t_rd = S.add('dve', lambda e: e.reciprocal(out=rden[:, 0:4], in_=pO[0][:, :, 64]), deps=[t_pv], sig=True)
                    t_rd2 = S.add('dve', lambda e: e.reciprocal(out=rden[:, 4:8], in_=pO[1][:, :, 64]), deps=[t_pv], sig=True)
                    for g in range(2):
                        t_onorm = S.add('dve', lambda e, g=g, so=so: e.tensor_tensor(
                            out=ost[so][:, g * 256:(g + 1) * 256].rearrange("p (h d) -> p h d", h=4), in0=pO[g][:, :, 0:64],
                            in1=rden[:, g * 4:(g + 1) * 4].unsqueeze(2).broadcast_to([128, 4, 64]), op=ALU.mult),
                            deps=[t_rd, t_rd2, ost_free[so]], sig=True)
                    ost_free[so] = S.add('sp', lambda e, i=i, so=so: e.dma_start(out=s_odsa[i, :, :], in_=ost[so][:, :]), deps=[t_onorm], dma=f'ost{so}')
                    G['t_onorm'] = t_onorm
                    G['att_done'] = [t_pv, t_onorm]

                for i in range(nqb):
                    a = genA(i)
                    for tag in a:
                        if tag == 'front':
                            break
                    if i > 0:
                        b = genB(i - 1)
                        nkp = stt[i - 1]['nk']
                        ita = 0
                        for step in range(nkp):
                            next(b, None)
                            target = ((step + 1) * NIT) // nkp
                            while ita < target:
                                next(a, None)
                                ita += 1
                        for _ in a:
                            pass
                        for _ in b:
                            pass
                    else:
                        for _ in a:
                            pass
                for _ in genB(nqb - 1):
                    pass
                S.final_wait()
                S.run(nc, "C", top)
        if 'D' in phases:
            with ExitStack() as pd_:
                S = Sched()
                xq = din("xq", [nqb * 128, D])
                w_o = din("w_o", [D, D])
                g_ffn = din("g_ffn", [128, 8])
                w_gate = din("w_gate", [D, DFF])
                w_up = din("w_up", [D, DFF])
                w_down = din("w_down", [DFF, D])
                g_fin = din("g_fin", [128, D])
                Wo = sb("WoD", [128, 8, D], BF16, pd_)
                Wg = sb("WgD", [128, 8, DFF], BF16, pd_)
                Wu = sb("WuD", [128, 8, DFF], BF16, pd_)
                Wd = sb("WdD", [128, NFC, D], BF16, pd_)
                gF = sb("gFD", [128, 8], F32, pd_)
                gfin = sb("gfinD", [128, D], F32, pd_)
                stg = [sb(f"stgD{i}", [128, 1024], F32, pd_) for i in range(2)]
                xs = [sb(f"xsD{i}", [128, D], F32, pd_) for i in range(2)]
                ob = [sb("obD0", [128, D], BF16, pd_)] * 2
                oT = [sb(f"oTD{i}", [128, 8, 128], BF16, pd_) for i in range(2)]
                ub = [sb("ubD0", [128, D], BF16, pd_)] * 2
                uT = [sb(f"uTD{i}", [128, 8, 128], BF16, pd_) for i in range(2)]
                junk = sb("junkD", [128, D], BF16, pd_)
                ss = [sb(f"ssD{i}", [128, 8], F32, pd_) for i in range(2)]
                sil = [sb(f"silD{i}", [128, 512], F32, pd_) for i in range(2)]
                actT = [sb(f"actTD{i}", [128, NFC, 128], BF16, pd_) for i in range(2)]
                h2 = [sb(f"h2D{i}", [128, D], F32, pd_) for i in range(2)]
                pT = ps("pTD", [128, 1024], BF16, pd_)
                pA = [ps(f"pAD{i}", [128, 512], F32, pd_) for i in range(2)]
                pG = [ps(f"pGD{i}", [128, 512], F32, pd_) for i in range(2)]
                pU = [ps(f"pUD{i}", [128, 512], F32, pd_) for i in range(2)]
                S.add('sp', lambda e: e.dma_start(out=gF[:, :], in_=g_ffn[:, :]), dma='cst')
                t_cst = S.add('sp', lambda e: e.dma_start(out=gfin[:, :], in_=g_fin[:, :]), dma='cst')
                stg_free = [None, None]
                t_w = []
                jobs = []
                for c in range(8):
                    jobs.append((Wo[:, c, :], w_o[c * 128:(c + 1) * 128, :], None, D))
                n_wo = len(jobs)
                for c in range(8):
                    for (Wt, wsrc) in ((Wg, w_gate), (Wu, w_up)):
                        for c0 in range(0, DFF, 1024):
                            n = min(1024, DFF - c0)
                            jobs.append((Wt[:, c, c0:c0 + n], wsrc[c * 128:(c + 1) * 128, c0:c0 + n], gF[:, c:c + 1], n))
                n_wgu = len(jobs)
                for f in range(NFC):
                    jobs.append((Wd[:, f, :], w_down[f * 128:(f + 1) * 128, :], None, D))
                for j, (dst, src, gs, n) in enumerate(jobs):
                    s = j % 2
                    td = S.add('sp', lambda e, s=s, src=src, n=n: e.dma_start(out=stg[s][:, 0:n], in_=src), deps=[stg_free[s]], dma=f'stg{s}')
                    eng = ('dve', 'pool')[s]
                    if gs is None:
                        stg_free[s] = S.add(eng, lambda e, s=s, dst=dst, n=n: e.tensor_copy(out=dst, in_=stg[s][:, 0:n]), deps=[td], sig=True)
                    else:
                        stg_free[s] = S.add(eng, lambda e, s=s, dst=dst, gs=gs, n=n: e.tensor_scalar(
                            out=dst, in0=stg[s][:, 0:n], scalar1=gs, scalar2=None, op0=ALU.mult), deps=[td, t_cst], sig=True)
                    t_w.append(stg_free[s])
                t_wo = t_w[:n_wo][-2:]
                t_wgu = t_w[:n_wgu][-2:]
                t_wd = t_w[-2:]
                pG_free = [None, None]
                pU_free = [None, None]
                sil_free = [None, None]
                pT_free = [None]
                pA_free = [None, None]
                done = {}
                st = {}

                def s1a(i):
                    b = i % 2
                    pv = done.get(i - 2, [])
                    S.add('sp', lambda e: e.dma_start(out=xs[b][:, :], in_=xq[i * 128:(i + 1) * 128, :]), deps=pv, dma=f'ld{b}')
                    S.add('sp', lambda e: e.dma_start(out=ob[b][:, 0:512], in_=s_omla[i, :, :]), deps=pv + [pT_free[0]], dma=f'ld{b}')
                    t_x = S.add('sp', lambda e: e.dma_start(out=ob[b][:, 512:1024], in_=s_odsa[i, :, :]), deps=pv + [pT_free[0]], dma=f'ld{b}')
                    for c in range(8):
                        t_tr = S.add('pe', lambda e, c=c: e.transpose(out=pT[:, c * 128:(c + 1) * 128], in_=ob[b][:, c * 128:(c + 1) * 128],
                                                                      identity=identB[:, :]), deps=[t_x, pT_free[0]], sig=(c == 7))
                    t_oT = S.add('dve', lambda e: e.tensor_copy(out=oT[b][:, :, :], in_=pT[:, :].rearrange("p (c t) -> p c t", c=8)), deps=[t_tr] + pv, sig=True)
                    pT_free[0] = t_oT
                    t_mm = [None, None]
                    for hf in range(2):
                        for c in range(8):
                            t_mm[hf] = S.add('pe', lambda e, c=c, hf=hf: e.matmul(pA[hf][:, :], lhsT=oT[b][:, c, :], rhs=Wo[:, c, hf * 512:(hf + 1) * 512],
                                                                                  start=(c == 0), stop=(c == 7)), deps=[t_oT, t_wo, pA_free[hf]], sig=(c == 7))
                    for hf in range(2):
                        t_h1 = S.add('dve', lambda e, hf=hf: e.tensor_tensor(out=xs[b][:, hf * 512:(hf + 1) * 512], in0=pA[hf][:, :],
                                                                             in1=xs[b][:, hf * 512:(hf + 1) * 512], op=ALU.add), deps=[t_mm[hf], t_x], sig=True)
                        pA_free[hf] = t_h1
                    t_ss = S.add('act', lambda e: e.activation(out=junk[:, :], in_=xs[b][:, :], func=AF.Square, accum_out=ss[b][:, 0:1]), deps=[t_h1] + pv, sig=True)
                    t_sd = S.add('act', lambda e: e.activation(out=ss[b][:, 1:2], in_=ss[b][:, 0:1], func=AF.Sqrt, scale=1.0 / D, bias=epsT[:, 0:1]), deps=[t_ss], sig=True)
                    t_r = S.add('dve', lambda e: e.reciprocal(out=ss[b][:, 2:3], in_=ss[b][:, 1:2]), deps=[t_sd], sig=True)
                    st[i] = dict(t_ub=S.add('dve', lambda e: e.tensor_scalar(out=ub[b][:, :], in0=xs[b][:, :], scalar1=ss[b][:, 2:3], scalar2=None, op0=ALU.mult),
                                            deps=[t_r, pT_free[0]] + pv, sig=True), t_h1=t_h1)

                def s1b(i):
                    b = i % 2
                    pv = done.get(i - 2, [])
                    for c in range(8):
                        t_tr = S.add('pe', lambda e, c=c: e.transpose(out=pT[:, c * 128:(c + 1) * 128], in_=ub[b][:, c * 128:(c + 1) * 128],
                                                                      identity=identB[:, :]), deps=[st[i]['t_ub'], pT_free[0]], sig=(c == 7))
                    st[i]['t_uT'] = S.add('dve', lambda e: e.tensor_copy(out=uT[b][:, :, :], in_=pT[:, :].rearrange("p (c t) -> p c t", c=8)), deps=[t_tr] + pv, sig=True)
                    pT_free[0] = st[i]['t_uT']

                def s2(i, g0, g1):
                    b = i % 2
                    pv = done.get(i - 2, [])
                    t_uT = st[i]['t_uT']
                    for gi in range(g0, g1):
                        sl = gi % 2
                        nf = min(4, NFC - gi * 4)
                        for (pX, Wt, fr) in ((pG, Wg, pG_free), (pU, Wu, pU_free)):
                            for j in range(nf):
                                f = gi * 4 + j
                                for c in range(8):
                                    t_mm = S.add('pe', lambda e, pX=pX, Wt=Wt, sl=sl, j=j, f=f, c=c: e.matmul(
                                        pX[sl][:, j * 128:(j + 1) * 128], lhsT=Wt[:, c, f * 128:(f + 1) * 128], rhs=uT[b][:, c, :],
                                        start=(c == 0), stop=(c == 7)), deps=[t_uT, t_wgu, fr[sl]], sig=(c == 7 and j == nf - 1))
                            if pX is pG:
                                t_g = t_mm
                            else:
                                t_u = t_mm
                        t_sil = S.add('act', lambda e, sl=sl, nf=nf: e.activation(out=sil[sl][:, 0:nf * 128], in_=pG[sl][:, 0:nf * 128], func=AF.Silu),
                                      deps=[t_g, sil_free[sl]], sig=True)
                        pG_free[sl] = t_sil
                        t_act = S.add('dve', lambda e, sl=sl, nf=nf, gi=gi: e.tensor_tensor(
                            out=actT[b][:, gi * 4:gi * 4 + nf, :], in0=pU[sl][:, 0:nf * 128].rearrange("p (f t) -> p f t", f=nf),
                            in1=sil[sl][:, 0:nf * 128].rearrange("p (f t) -> p f t", f=nf), op=ALU.mult), deps=[t_sil, t_u] + pv, sig=True)
                        pU_free[sl] = t_act
                        sil_free[sl] = t_act
                        st[i]['t_act'] = t_act
                        st[i]['t_u'] = t_u

                def s3(i):
                    b = i % 2
                    t_act = st[i]['t_act']
                    t_mm = [None, None]
                    for hf in range(2):
                        for f in range(NFC):
                            t_mm[hf] = S.add('pe', lambda e, f=f, hf=hf: e.matmul(pA[hf][:, :], lhsT=actT[b][:, f, :], rhs=Wd[:, f, hf * 512:(hf + 1) * 512],
                                                                                  start=(f == 0), stop=(f == NFC - 1)), deps=[t_act, t_wd, pA_free[hf]], sig=(f == NFC - 1))
                    for hf in range(2):
                        t_h2 = S.add('dve', lambda e, hf=hf: e.tensor_tensor(out=h2[b][:, hf * 512:(hf + 1) * 512], in0=pA[hf][:, :],
                                                                             in1=xs[b][:, hf * 512:(hf + 1) * 512], op=ALU.add),
                                     deps=[t_mm[hf]] + done.get(i - 2, []), sig=True)
                        pA_free[hf] = t_h2
                    t_ss = S.add('act', lambda e: e.activation(out=junk[:, :], in_=h2[b][:, :], func=AF.Square, accum_out=ss[b][:, 4:5]), deps=[t_h2], sig=True)
                    t_sd = S.add('act', lambda e: e.activation(out=ss[b][:, 5:6], in_=ss[b][:, 4:5], func=AF.Sqrt, scale=1.0 / D, bias=epsT[:, 0:1]), deps=[t_ss], sig=True)
                    t_r = S.add('dve', lambda e: e.reciprocal(out=ss[b][:, 6:7], in_=ss[b][:, 5:6]), deps=[t_sd], sig=True)
                    t_o = S.add('dve', lambda e: e.scalar_tensor_tensor(out=h2[b][:, :], in0=h2[b][:, :], scalar=ss[b][:, 6:7], in1=gfin[:, :], op0=ALU.mult, op1=ALU.mult),
                                deps=[t_r, t_cst], sig=True)
                    t_st = S.add('sp', lambda e: e.dma_start(out=out[i * 128:(i + 1) * 128, :], in_=h2[b][:, :]), deps=[t_o], dma=f'st{b}')
                    done[i] = [t_st, t_o, t_mm[1], t_sd]

                ngrp = (NFC + 3) // 4
                s1a(0)
                s1b(0)
                for i in range(nqb):
                    s2(i, 0, ngrp // 2)
                    if i + 1 < nqb:
                        s1a(i + 1)
                    s2(i, ngrp // 2, ngrp)
                    if i + 1 < nqb:
                        s1b(i + 1)
                    s3(i)
                S.final_wait()
                S.run(nc, "D", top)
    global LAST_INPUTS
    LAST_INPUTS = list(_din.keys())
    return nc, dbg


def bcast(ap, h):
    n = ap.shape[-1]
    return ap.unsqueeze(1).broadcast_to([ap.shape[0], h, n])


BF = ml_dtypes.bfloat16
def rope_tab(pos):
    pos = pos.astype(np.float32)
    out = np.zeros((pos.shape[0], 192), np.float32)
    for (half, off) in ((32, 0), (16, 128)):
        inv = np.power(np.float32(10000.0), -np.arange(half, dtype=np.float32) / np.float32(half)).astype(np.float32)
        ang = (pos[:, None] * inv[None, :]).astype(np.float32)
        c = np.cos(ang).astype(np.float32); s = np.sin(ang).astype(np.float32)
        d = 2 * half
        out[:, off:off + d] = np.concatenate([c, c], 1)
        out[:, off + d:off + 2 * d] = np.concatenate([-s, s], 1)
    return out
def core_inputs(inp, core):
    f32 = np.float32
    b, par = core // 2, core % 2
    x = np.asarray(inp['x'][b], f32)
    xk = np.zeros((65 * 128, 1024), f32); xk[0:16] = inp['meta_tokens']; xk[128:] = x
    xq = np.ascontiguousarray(x.reshape(64, 128, 1024)[par::2].reshape(32 * 128, 1024))
    w_in = np.asarray(inp['w_in'][0], f32)
    c_q, c_kv, k_r, q_s, k_s, v_s, q_i, k_i, w_i = np.split(w_in, np.cumsum([256,128,32,512,512,512,512,64,8])[:-1], axis=1)
    def pc(g, n):
        return np.ascontiguousarray(np.asarray(g, f32).reshape(n, 128).T)
    w_uq = np.asarray(inp['w_uq'][0], f32).reshape(256, 8, 96)
    w_ukv = np.asarray(inp['w_ukv'][0], f32).reshape(128, 8, 128)
    posK = np.concatenate([np.arange(128), 16 + np.arange(64 * 128)])
    ropeK = rope_tab(posK).reshape(65, 128, 192)
    g_idx = 2 * np.arange(32) + par
    posQ = (16 + 128 * g_idx[:, None] + np.arange(128)[None, :]).reshape(-1)
    ropeQ = rope_tab(posQ).reshape(32, 128, 192)
    tri = (np.arange(128)[:, None] <= np.arange(128)[None, :]).astype(f32)
    ones = np.ones((128, 128), f32); zeros = np.zeros((128, 128), f32)
    mk_mult = np.stack([tri, zeros] if par == 0 else [ones, tri]).astype(BF)
    NEG = np.float32(-1e30)
    meta_add = np.zeros((128, 128), f32); meta_add[:, 16:] = NEG
    triA = np.where(tri.T > 0, 0, NEG).astype(f32)
    allneg = np.full((128, 128), NEG, f32)
    mk_add = np.stack([meta_add, triA, allneg] if par == 0 else [meta_add, zeros, triA]).astype(f32)
    vcol0 = np.zeros((128, 8), f32); vcol0[0:16] = 1
    return {
        'xk': xk, 'xq': xq,
        'wk_in': np.ascontiguousarray(np.concatenate([k_s, v_s, c_kv, k_r, k_i], 1)),
        'wq_in': np.ascontiguousarray(np.concatenate([c_q, q_s, q_i, w_i], 1)),
        'g_attn': pc(inp['attn_norm_g'][0], 8),
        'w_uq_n': np.ascontiguousarray(w_uq[:, :, :64].reshape(256, 512)),
        'w_uq_r': np.ascontiguousarray(w_uq[:, :, 64:].reshape(256, 256)),
        'g_q': pc(inp['mla_q_norm_g'][0], 2),
        'w_ukv_k': np.ascontiguousarray(w_ukv[:, :, :64].reshape(128, 512)),
        'w_ukv_v': np.ascontiguousarray(w_ukv[:, :, 64:].reshape(128, 512)),
        'g_kv': pc(inp['mla_kv_norm_g'][0], 1),
        'w_o': np.asarray(inp['w_o'][0], f32),
        'g_ffn': pc(inp['ffn_norm_g'][0], 8),
        'w_gate': np.asarray(inp['w_gate'][0], f32), 'w_up': np.asarray(inp['w_up'][0], f32),
        'w_down': np.asarray(inp['w_down'][0], f32),
        'g_fin': np.ascontiguousarray(np.broadcast_to(np.asarray(inp['final_norm_g'], f32)[None, :], (128, 1024))),
        'ropeK': ropeK, 'ropeQ': ropeQ,
        'ident_f': np.eye(128, dtype=f32), 'ident_b': np.eye(128, dtype=f32).astype(BF),
        'vcol0': vcol0.astype(BF), 'mk_mult': mk_mult, 'mk_add': mk_add,
    }


def kernel(**inputs):
    inp = {k: np.asarray(v) for k, v in inputs.items()}
    nc, _ = build()
    names = set(LAST_INPUTS)
    in_maps = []
    for core in range(8):
        ci = core_inputs(inp, core)
        in_maps.append({k: np.ascontiguousarray(v) for k, v in ci.items() if k in names})
    res = run_bass_kernel_spmd(nc, in_maps, core_ids=list(range(8)))
    out = np.zeros((4, 64, 128, 1024), np.float32)
    for core in range(8):
        b, par = core // 2, core % 2
        out[b, par::2] = np.asarray(res.results[core]["out"], np.float32).reshape(32, 128, 1024)
    return out.reshape(4, 8192, 1024)
```

```python
import numpy as np
import ml_dtypes
from contextlib import ExitStack
import concourse.bass as bass
import concourse.mybir as mybir
from concourse.bass_utils import run_bass_kernel_spmd

F32 = mybir.dt.float32
BF16 = mybir.dt.bfloat16
AF = mybir.ActivationFunctionType
ALU = mybir.AluOpType
AX = mybir.AxisListType

D = 1024
NQB = 32
NKB = 65
EPS = 1e-6
DFF = 2816
NFC = DFF // 128
TOPK = 256
NEG = -1.0e30
LAST_INPUTS = []
import os
CUT = float(os.environ.get('KCUT', '99'))


class Sched:
    ENG = ('pe', 'act', 'dve', 'pool', 'sp')

    def __init__(self):
        self.q = {e: [] for e in self.ENG}
        self.cnt = {}

    def add(self, eng, fn, deps=(), sig=False, dma=None):
        tok = None
        inc = None
        if dma is not None:
            self.cnt[dma] = self.cnt.get(dma, 0) + 16
            inc = (dma, 16)
            tok = (dma, self.cnt[dma])
        elif sig:
            name = 'c_' + eng
            self.cnt[name] = self.cnt.get(name, 0) + 1
            inc = (name, 1)
            tok = (name, self.cnt[name])
        dl = []
        for d in deps:
            if d is None:
                continue
            if isinstance(d, list):
                dl.extend(x for x in d if x is not None)
            else:
                dl.append(d)
        self.q[eng].append((fn, dl, inc))
        return tok

    def final_wait(self, eng='sp'):
        deps = [(n, v) for n, v in self.cnt.items()]
        self.q[eng].append((lambda e: e.nop(), deps, None))

    def run(self, nc, name, semstack=None):
        with ExitStack() as es:
            sems = {n: (semstack or es).enter_context(nc.semaphore(name + '_' + n)) for n in self.cnt}
            block = es.enter_context(nc.Block())

            def mk(engname):
                def body(eng):
                    waited = {}
                    for fn, deps, inc in self.q[engname]:
                        for (s, v) in deps:
                            if engname == 'pe' and s == 'c_pe':
                                continue
                            if waited.get(s, 0) < v:
                                eng.wait_ge(sems[s], v)
                                waited[s] = v
                        inst = fn(eng)
                        if inc is not None:
                            inst.then_inc(sems[inc[0]], inc[1])
                return body
            block.tensor(mk('pe'))
            block.scalar(mk('act'))
            block.vector(mk('dve'))
            block.gpsimd(mk('pool'))
            block.sync(mk('sp'))


def _bc(ap, shape_mid):
    return ap


def build(nkb=NKB, nqb=NQB, debug=False, phases='ABCD'):
    nc = bass.Bass("TRN2", target_bir_lowering=False)
    T = nkb * 128

    _din = {}

    def din(name, shape, dt=F32):
        if name not in _din:
            _din[name] = nc.dram_tensor(name, list(shape), dt, kind="ExternalInput").ap()
        return _din[name]

    def dscr(name, shape, dt):
        return nc.dram_tensor(name, list(shape), dt).ap()

    out = nc.dram_tensor("out", [NQB * 128, D], F32, kind="ExternalOutput").ap()

    s_ksT = dscr("s_ksT", [NKB, 128, 512], BF16)
    s_vds = dscr("s_vds", [NKB, 128, 520], BF16)
    s_kiT = dscr("s_kiT", [NKB, 64, 128], BF16)
    s_h1 = dscr("s_h1", [NQB * 128, D], F32)
    s_omla = dscr("s_omla", [NQB, 128, 512], BF16)
    s_odsa = dscr("s_odsa", [NQB, 128, 512], BF16)
    s_vmla = dscr("s_vmla", [NKB, 128, 520], BF16)
    dbg = {}
    if debug:
        def dout(name, shape, dt=F32):
            t = nc.dram_tensor(name, list(shape), dt, kind="ExternalOutput").ap()
            dbg[name] = t
            return t
        d_knT = dout("d_knT", [128, 4 * T], BF16)
        d_krT = dout("d_krT", [32, T], BF16)
        d_vm = dout("d_vm", [128, nkb * 520], BF16)
        d_ksT = dout("d_ksT", [NKB, 128, 512], BF16)
        d_vds = dout("d_vds", [NKB, 128, 520], BF16)
        d_kiT = dout("d_kiT", [NKB, 64, 128], BF16)
        d_omla = dout("d_omla", [128, nqb * 512], BF16)
        d_h1 = dout("d_h1", [NQB * 128, D], F32)

    with ExitStack() as top:
        def sb(name, shape, dt, es=top):
            return es.enter_context(nc.sbuf_tensor(name, list(shape), dt))

        def ps(name, shape, dt, es=top):
            return es.enter_context(nc.psum_tensor(name, list(shape), dt))

        identF = sb("identF", [128, 128], F32)
        identB = sb("identB", [128, 128], BF16)
        epsT = sb("epsT", [128, 1], F32)
        abst = ExitStack()
        knT = sb("knT", [96, 8, T], BF16, abst)

        if 'A' in phases:
            with ExitStack() as pa:
                S = Sched()
                xk = din("xk", [nkb * 128, D])
                wk_in = din("wk_in", [D, 1248])
                g_attn = din("g_attn", [128, 8])
                w_ukv_k = din("w_ukv_k", [128, 512])
                w_ukv_v = din("w_ukv_v", [128, 512])
                g_kv = din("g_kv", [128, 1])
                ropeK = din("ropeK", [nkb, 128, 192])
                ident_f = din("ident_f", [128, 128])
                ident_b = din("ident_b", [128, 128], BF16)
                vcol0 = din("vcol0", [128, 8], BF16)
                Wk = sb("Wk", [128, 8, 1248], BF16, pa)
                WukK = sb("WukK", [128, 512], BF16, pa)
                WukV = sb("WukV", [128, 512], BF16, pa)
                gA = sb("gA", [128, 8], F32, pa)
                gKV = sb("gKV", [128, 1], F32, pa)
                stg = [sb("stgA0", [128, 1248], F32, pa)] * 2
                xs = [sb(f"xsA{i}", [128, D], F32, pa) for i in range(2)]
                tab = [sb(f"tabA{i}", [128, 192], F32, pa) for i in range(2)]
                tabs = [sb(f"tabsA{i}", [128, 192], F32, pa) for i in range(2)]
                junk = sb("junkA", [128, D], BF16, pa)
                ss = [sb(f"ssA{i}", [128, 4], F32, pa) for i in range(2)]
                ss2 = [sb(f"ss2A{i}", [128, 4], F32, pa) for i in range(2)]
                xT = [sb(f"xTA{i}", [128, 8, 128], BF16, pa) for i in range(2)]
                ra = sb("ra", [128, 512], F32, pa)
                rb = sb("rb", [128, 512], F32, pa)
                ksr = sb("ksr", [128, 512], BF16, pa)
                ri_a = sb("ri_a", [128, 64], F32, pa)
                ri_b = sb("ri_b", [128, 64], F32, pa)
                kir = sb("kir", [128, 64], BF16, pa)
                rr_a = sb("rr_a", [128, 32], F32, pa)
                rr_b = sb("rr_b", [128, 32], F32, pa)
                kr96 = sb("kr96", [128, 96], BF16, pa)
                vmst = [sb(f"vmstA{i}", [128, 8, 65], BF16, pa) for i in range(2)]
                ckv = sb("ckv", [128, 128], F32, pa)
                junk2 = sb("junk2A", [128, 128], BF16, pa)
                ckvn = sb("ckvn", [128, 128], BF16, pa)
                ckvnT = sb("ckvnT", [128, 128], BF16, pa)
                vst = [sb(f"vstA{i}", [128, 8, 65], BF16, pa) for i in range(2)]
                kst = [sb(f"kstA{i}", [128, 512], BF16, pa) for i in range(2)]
                kit = [sb(f"kitA{i}", [64, 128], BF16, pa) for i in range(2)]
                pT = [ps(f"pTA{i}", [128, 512], F32, pa) for i in range(2)]
                pP = [ps(f"pPA{i}", [128, 512], F32, pa) for i in range(3)]
                pS = ps("pSA", [128, 1024], BF16, pa)
                pK = ps("pKA", [128, 512], F32, pa)
                pV = ps("pVA", [128, 512], F32, pa)

                S.add('sp', lambda e: e.dma_start(out=identF[:, :], in_=ident_f[:, :]), dma='cst')
                S.add('sp', lambda e: e.dma_start(out=identB[:, :], in_=ident_b[:, :]), dma='cst')
                S.add('sp', lambda e: e.dma_start(out=gA[:, :], in_=g_attn[:, :]), dma='cst')
                S.add('sp', lambda e: e.dma_start(out=gKV[:, :], in_=g_kv[:, :]), dma='cst')
                t_eps = S.add('dve', lambda e: e.memset(epsT[:, :], EPS), sig=True)
                t_vm1 = S.add('pool', lambda e: e.memset(vmst[1][:, :, 64:65], 1.0), sig=True)
                t_kz = S.add('pool', lambda e: e.memset(kr96[:, 0:64], 0.0), sig=True)
                t_vst1 = [None, S.add('pool', lambda e: e.memset(vst[1][:, :, 64:65], 1.0), sig=True)]
                S.add('sp', lambda e: e.dma_start(out=vst[0][:, :, 64:65], in_=vcol0[:, :].unsqueeze(2), allow_slow_non_contiguous=True), dma='cst')
                t_cst = S.add('sp', lambda e: e.dma_start(out=vmst[0][:, :, 64:65], in_=vcol0[:, :].unsqueeze(2), allow_slow_non_contiguous=True), deps=[t_vm1], dma='cst')
                t_id = t_cst
                t_vc0 = t_cst
                t_vst1[0] = t_cst
                stg_free = [None, None]
                t_w = []
                for c in range(8):
                    s = 0
                    td = S.add('sp', lambda e, c=c, s=s: e.dma_start(out=stg[s][:, :], in_=wk_in[c * 128:(c + 1) * 128, :]),
                               deps=[stg_free[s]], dma=f'stg{s}')
                    eng = 'dve' if s == 0 else 'pool'
                    stg_free[s] = S.add(eng, lambda e, c=c, s=s: e.tensor_scalar(
                        out=Wk[:, c, :], in0=stg[s][:, :], scalar1=gA[:, c:c + 1], scalar2=None, op0=ALU.mult),
                        deps=[td, t_cst], sig=True)
                    t_w.append(stg_free[s])
                for (dst, src, s) in ((WukK, w_ukv_k, 0), (WukV, w_ukv_v, 0)):
                    td = S.add('sp', lambda e, s=s, src=src: e.dma_start(out=stg[s][:, 0:512], in_=src[:, :]),
                               deps=[stg_free[s]], dma=f'stg{s}')
                    eng = 'dve' if s == 0 else 'pool'
                    stg_free[s] = S.add(eng, lambda e, s=s, dst=dst: e.tensor_scalar(
                        out=dst[:, :], in0=stg[s][:, 0:512], scalar1=gKV[:, 0:1], scalar2=None, op0=ALU.mult),
                        deps=[td, t_cst], sig=True)
                    t_w.append(stg_free[s])

                xs_free = [None, None]
                tab_free = [None, None]
                xT_free = [None, None]
                pT_free = [None, None]
                pP_free = [[None], [None], [None]]
                pS_free = [None]
                pK_free = None
                pV_free = None
                vst_free = [None, None]
                kst_free = [None, None]
                kit_free = [None, None]
                ss_free = [None, None]
                tmp_free = {}
                ckvnT_free = None
                for k in range(nkb):
                    s = k % 2
                    t_x = S.add('sp', lambda e, k=k, s=s: e.dma_start(out=xs[s][:, :], in_=xk[k * 128:(k + 1) * 128, :]),
                                deps=[xs_free[s], tab_free[s]], dma=f'ld{s}')
                    t_tab = S.add('sp', lambda e, k=k, s=s: e.dma_start(out=tab[s][:, :], in_=ropeK[k, :, :]),
                                  deps=[tab_free[s]], dma=f'ld{s}')
                    t_x = t_tab
                    t_ss = S.add('act', lambda e, s=s: e.activation(out=junk[:, :], in_=xs[s][:, :], func=AF.Square,
                                                                   accum_out=ss[s][:, 0:1]),
                                 deps=[t_x, ss_free[s]], sig=True)
                    t_sd = S.add('act', lambda e, s=s: e.activation(out=ss[s][:, 1:2], in_=ss[s][:, 0:1], func=AF.Sqrt,
                                                                   scale=1.0 / D, bias=epsT[:, 0:1]),
                                 deps=[t_ss, t_eps], sig=True)
                    t_rstd = S.add('dve', lambda e, s=s: e.reciprocal(out=ss[s][:, 2:3], in_=ss[s][:, 1:2]),
                                   deps=[t_sd], sig=True)
                    rstd = ss[s][:, 2:3]
                    if CUT <= 1:
                        continue
                    t_tr = []
                    for hlf in range(2):
                        for j in range(4):
                            c = hlf * 4 + j
                            tt = S.add('pe', lambda e, s=s, c=c, hlf=hlf, j=j: e.transpose(
                                out=pT[hlf][:, j * 128:(j + 1) * 128], in_=xs[s][:, c * 128:(c + 1) * 128],
                                identity=identF[:, :]),
                                deps=[t_x, t_id, pT_free[hlf]], sig=(j == 3))
                        t_tr.append(tt)
                    te0 = S.add('act', lambda e, s=s: e.activation(out=xT[s][:, 0:4, :], in_=pT[0][:, :], func=AF.Copy),
                                deps=[t_tr[0], xT_free[s]], sig=True)
                    te1 = S.add('dve', lambda e, s=s: e.tensor_copy(out=xT[s][:, 4:8, :], in_=pT[1][:, :]),
                                deps=[t_tr[1], xT_free[s]], sig=True)
                    pT_free = [te0, te1]
                    xs_free[s] = [te0, te1, t_ss]
                    if CUT <= 2:
                        continue
                    t_mm = []
                    for bnk, (c0, c1) in enumerate(((0, 512), (512, 1024), (1024, 1248))):
                        for c in range(8):
                            tt = S.add('pe', lambda e, s=s, c=c, bnk=bnk, c0=c0, c1=c1: e.matmul(
                                pP[bnk][:, 0:c1 - c0], lhsT=xT[s][:, c, :], rhs=Wk[:, c, c0:c1],
                                start=(c == 0), stop=(c == 7)),
                                deps=[te0, te1, pP_free[bnk], t_w], sig=(c == 7))
                        t_mm.append(tt)
                    xT_free[s] = t_mm[2]
                    if CUT <= 3:
                        continue
                    t_tabs = S.add('dve', lambda e, s=s: e.tensor_scalar(
                        out=tabs[s][:, :], in0=tab[s][:, :], scalar1=ss[s][:, 2:3], scalar2=None, op0=ALU.mult),
                        deps=[t_tab, t_rstd, tab_free[s]], sig=True)
                    t_a = S.add('dve', lambda e, s=s: e.tensor_tensor(
                        out=ra[:, :].rearrange("p (h d) -> p h d", h=8), in0=pP[0][:, :].rearrange("p (h d) -> p h d", h=8),
                        in1=bcast(tabs[s][:, 0:64], 8), op=ALU.mult),
                        deps=[t_mm[0], t_tabs, tmp_free.get('ra')], sig=True)
                    t_b1 = S.add('dve', lambda e, s=s: e.tensor_tensor(
                        out=rb[:, :].rearrange("p (h d) -> p h d", h=8)[:, :, 0:32],
                        in0=pP[0][:, :].rearrange("p (h d) -> p h d", h=8)[:, :, 32:64],
                        in1=bcast(tabs[s][:, 64:96], 8), op=ALU.mult),
                        deps=[t_mm[0], t_tabs, tmp_free.get('ra')], sig=True)
                    t_b2 = S.add('dve', lambda e, s=s: e.tensor_tensor(
                        out=rb[:, :].rearrange("p (h d) -> p h d", h=8)[:, :, 32:64],
                        in0=pP[0][:, :].rearrange("p (h d) -> p h d", h=8)[:, :, 0:32],
                        in1=bcast(tabs[s][:, 96:128], 8), op=ALU.mult),
                        deps=[t_mm[0], t_tabs, tmp_free.get('ra')], sig=True)
                    pP_free[0] = [t_a, t_b1, t_b2]
                    t_ksr = S.add('pool', lambda e: e.tensor_tensor(out=ksr[:, :], in0=ra[:, :], in1=rb[:, :], op=ALU.add),
                                  deps=[t_a, t_b1, t_b2, tmp_free.get('ksr')], sig=True)
                    tmp_free['ra'] = t_ksr
                    if CUT <= 4:
                        continue
                    t_v = S.add('act', lambda e, s=s: e.activation(
                        out=vst[s][:, :, 0:64], in_=pP[1][:, :].rearrange("p (h d) -> p h d", h=8), func=AF.Copy,
                        scale=ss[s][:, 2:3]),
                        deps=[t_mm[1], t_rstd, vst_free[s], t_vst1[s]], sig=True)
                    pP_free[1] = [t_v]
                    t_vd = S.add('sp', lambda e, s=s, k=k: e.dma_start(
                        out=s_vds[k, :, :], in_=vst[s][:, :, :].rearrange("p h d -> p (h d)")),
                        deps=[t_v], dma=f'st{s}')
                    if CUT <= 5:
                        continue
                    t_ckv = S.add('dve', lambda e, s=s: e.tensor_scalar(out=ckv[:, :], in0=pP[2][:, 0:128], scalar1=ss[s][:, 2:3],
                                                                       scalar2=None, op0=ALU.mult),
                                  deps=[t_mm[2], t_rstd, tmp_free.get('ckv')], sig=True)
                    t_ss2 = S.add('act', lambda e, s=s: e.activation(out=junk2[:, :], in_=ckv[:, :], func=AF.Square,
                                                                    accum_out=ss2[s][:, 0:1]),
                                  deps=[t_ckv, tmp_free.get('ss2%d' % s)], sig=True)
                    t_sd2 = S.add('act', lambda e, s=s: e.activation(out=ss2[s][:, 1:2], in_=ss2[s][:, 0:1], func=AF.Sqrt,
                                                                    scale=1.0 / 128, bias=epsT[:, 0:1]),
                                  deps=[t_ss2], sig=True)
                    t_r2 = S.add('dve', lambda e, s=s: e.reciprocal(out=ss2[s][:, 2:3], in_=ss2[s][:, 1:2]),
                                 deps=[t_sd2], sig=True)
                    t_ckvn = S.add('dve', lambda e, s=s: e.tensor_scalar(
                        out=ckvn[:, :], in0=ckv[:, :], scalar1=ss2[s][:, 2:3], scalar2=None, op0=ALU.mult),
                        deps=[t_r2, t_ckv, tmp_free.get('ckvn')], sig=True)
                    tmp_free['ckv'] = [t_ckvn, t_ss2]
                    tmp_free['ss2%d' % s] = t_ckvn
                    if CUT <= 6:
                        continue
                    t_ra = S.add('dve', lambda e, s=s: e.tensor_tensor(
                        out=rr_a[:, :], in0=pP[2][:, 128:160], in1=tabs[s][:, 128:160], op=ALU.mult),
                        deps=[t_mm[2], t_tabs, tmp_free.get('rr')], sig=True)
                    t_rb1 = S.add('dve', lambda e, s=s: e.tensor_tensor(
                        out=rr_b[:, 0:16], in0=pP[2][:, 144:160], in1=tabs[s][:, 160:176], op=ALU.mult),
                        deps=[t_mm[2], t_tabs, tmp_free.get('rr')], sig=True)
                    t_rb2 = S.add('dve', lambda e, s=s: e.tensor_tensor(
                        out=rr_b[:, 16:32], in0=pP[2][:, 128:144], in1=tabs[s][:, 176:192], op=ALU.mult),
                        deps=[t_mm[2], t_tabs, tmp_free.get('rr')], sig=True)
                    t_krr = S.add('pool', lambda e: e.tensor_tensor(out=kr96[:, 64:96], in0=rr_a[:, :], in1=rr_b[:, :], op=ALU.add),
                                  deps=[t_ra, t_rb1, t_rb2, tmp_free.get('krr'), t_kz], sig=True)
                    tmp_free['rr'] = t_krr
                    t_ia = S.add('dve', lambda e, s=s: e.tensor_tensor(
                        out=ri_a[:, :], in0=pP[2][:, 160:224], in1=tabs[s][:, 0:64], op=ALU.mult),
                        deps=[t_mm[2], t_tabs, tmp_free.get('ri')], sig=True)
                    t_ib1 = S.add('dve', lambda e, s=s: e.tensor_tensor(
                        out=ri_b[:, 0:32], in0=pP[2][:, 192:224], in1=tabs[s][:, 64:96], op=ALU.mult),
                        deps=[t_mm[2], t_tabs, tmp_free.get('ri')], sig=True)
                    t_ib2 = S.add('dve', lambda e, s=s: e.tensor_tensor(
                        out=ri_b[:, 32:64], in0=pP[2][:, 160:192], in1=tabs[s][:, 96:128], op=ALU.mult),
                        deps=[t_mm[2], t_tabs, tmp_free.get('ri')], sig=True)
                    t_kir = S.add('pool', lambda e: e.tensor_tensor(out=kir[:, :], in0=ri_a[:, :], in1=ri_b[:, :], op=ALU.add),
                                  deps=[t_ia, t_ib1, t_ib2, tmp_free.get('kir')], sig=True)
                    tmp_free['ri'] = t_kir
                    pP_free[2] = [t_ckv, t_ra, t_rb1, t_rb2, t_ia, t_ib1, t_ib2]
                    tab_free[s] = [t_a, t_b1, t_b2, t_ra, t_rb1, t_rb2, t_ia, t_ib1, t_ib2]
                    ss_free[s] = [t_tabs, t_v, t_ckv]
                    if CUT <= 7:
                        continue
                    for a in range(4):
                        t_t1 = S.add('pe', lambda e, a=a: e.transpose(
                            out=pS[:, a * 128:(a + 1) * 128], in_=ksr[:, a * 128:(a + 1) * 128], identity=identB[:, :]),
                            deps=[t_ksr, pS_free], sig=(a == 3))
                    if CUT <= 7.1:
                        continue
                    t_t2 = S.add('pe', lambda e: e.transpose(out=pS[0:64, 512:640], in_=kir[:, :], identity=identB[:, :]),
                                 deps=[t_kir, pS_free], sig=True)
                    if CUT <= 7.2:
                        continue
                    t_t3 = S.add('pe', lambda e: e.transpose(out=pS[0:96, 640:768], in_=kr96[:, :], identity=identB[:, :]),
                                 deps=[t_krr, pS_free], sig=True)
                    if CUT <= 7.3:
                        continue
                    t_t4 = S.add('pe', lambda e: e.transpose(out=pS[:, 768:896], in_=ckvn[:, :], identity=identB[:, :]),
                                 deps=[t_ckvn, pS_free], sig=True)
                    tmp_free['ksr'] = t_t1
                    tmp_free['kir'] = t_t2
                    tmp_free['krr'] = t_t3
                    tmp_free['ckvn'] = t_t4
                    if CUT <= 8:
                        continue
                    t_e1 = S.add('dve', lambda e, s=s: e.tensor_copy(out=kst[s][:, :], in_=pS[:, 0:512]),
                                 deps=[t_t4, kst_free[s]], sig=True)
                    if CUT <= 8.1:
                        continue
                    t_d1 = S.add('sp', lambda e, s=s, k=k: e.dma_start(out=s_ksT[k, :, :], in_=kst[s][:, :]),
                                 deps=[t_e1], dma=f'st{s}')
                    if CUT <= 8.2:
                        continue
                    t_e2 = S.add('dve', lambda e, s=s: e.tensor_copy(out=kit[s][:, :], in_=pS[0:64, 512:640]),
                                 deps=[t_t4, kit_free[s]], sig=True)
                    if CUT <= 8.3:
                        continue
                    t_d2 = S.add('sp', lambda e, s=s, k=k: e.dma_start(out=s_kiT[k, :, :], in_=kit[s][:, :]),
                                 deps=[t_e2], dma=f'st{s}')
                    if CUT <= 8.4:
                        continue
                    t_e3 = S.add('dve', lambda e, k=k: e.tensor_copy(out=knT[64:96, :, k * 128:(k + 1) * 128],
                                                                     in_=pS[64:96, 640:768].unsqueeze(1).broadcast_to([32, 8, 128])),
                                 deps=[t_t4], sig=True)
                    if CUT <= 8.5:
                        continue
                    t_e4 = S.add('dve', lambda e: e.tensor_copy(out=ckvnT[:, :], in_=pS[:, 768:896]),
                                 deps=[t_t4, ckvnT_free], sig=True)
                    pS_free = [t_e1, t_e2, t_e3, t_e4]
                    if CUT <= 9:
                        continue
                    for h in range(4):
                        t_k = S.add('pe', lambda e, h=h: e.matmul(
                            pK[0:64, h * 128:(h + 1) * 128], lhsT=WukK[:, h * 64:(h + 1) * 64], rhs=ckvnT[:, :],
                            start=True, stop=True), deps=[t_e4, pK_free, t_w], sig=(h == 3))
                    for h in range(4, 8):
                        t_k2 = S.add('pe', lambda e, h=h: e.matmul(
                            pP[1][0:64, (h - 4) * 128:(h - 3) * 128], lhsT=WukK[:, h * 64:(h + 1) * 64], rhs=ckvnT[:, :],
                            start=True, stop=True), deps=[t_e4, t_w] + pP_free[1], sig=(h == 7))
                    t_vv = S.add('pe', lambda e: e.matmul(pV[:, :], lhsT=ckvnT[:, :], rhs=WukV[:, :], start=True, stop=True),
                                 deps=[t_e4, pV_free, t_w], sig=True)
                    ckvnT_free = t_vv
                    pK_free = S.add('act', lambda e, k=k: e.activation(
                        out=knT[0:64, 0:4, k * 128:(k + 1) * 128], in_=pK[0:64, :].rearrange("p (a t) -> p a t", a=4), func=AF.Copy),
                        deps=[t_k], sig=True)
                    t_ke2 = S.add('act', lambda e, k=k: e.activation(
                        out=knT[0:64, 4:8, k * 128:(k + 1) * 128], in_=pP[1][0:64, :].rearrange("p (a t) -> p a t", a=4), func=AF.Copy),
                        deps=[t_k2], sig=True)
                    pP_free[1] = pP_free[1] + [t_ke2]
                    pV_free = S.add('dve', lambda e, s=s: e.tensor_copy(
                        out=vmst[s][:, :, 0:64], in_=pV[:, :].rearrange("p (h d) -> p h d", h=8)),
                        deps=[t_vv, t_vm1, t_cst, vst_free[s]], sig=True)
                    t_d3 = S.add('sp', lambda e, s=s, k=k: e.dma_start(out=s_vmla[k, :, :], in_=vmst[s][:, :, :].rearrange("p h d -> p (h d)")),
                                 deps=[pV_free], dma=f'st{s}')
                    kit_free[s] = t_d3
                    kst_free[s] = t_d3
                    vst_free[s] = t_d3
                    if k == 0:
                        vst_free[s] = S.add('pool', lambda e, s=s: e.memset(vst[s][:, :, 64:65], 1.0), deps=[t_d3], sig=True)
                        vst_free[s] = [vst_free[s], S.add('pool', lambda e, s=s: e.memset(vmst[s][:, :, 64:65], 1.0), deps=[t_d3], sig=True)]
                last = [pK_free, pV_free, kst_free[0], kst_free[1], kit_free[0], kit_free[1], vst_free[0], vst_free[1]]
                S.final_wait()
                S.run(nc, "A", top)
                if debug and CUT > 50:
                    S2_ = Sched()
                    t1 = S2_.add('sp', lambda e: e.dma_start(out=d_ksT[0:nkb, :, :], in_=s_ksT[0:nkb, :, :]), dma='d')
                    t1 = S2_.add('sp', lambda e: e.dma_start(out=d_vds[0:nkb, :, :], in_=s_vds[0:nkb, :, :]), dma='d')
                    t1 = S2_.add('sp', lambda e: e.dma_start(out=d_kiT[0:nkb, :, :], in_=s_kiT[0:nkb, :, :]), dma='d')
                    S2_.add('sp', lambda e: e.nop(), deps=[t1])
                    S2_.run(nc, "Ad", top)
        if 'B' in phases:
            with ExitStack() as pb_:
                S = Sched()
                xq = din("xq", [nqb * 128, D])
                wq_in = din("wq_in", [D, 1288])
                g_attn = din("g_attn", [128, 8])
                w_uq_n = din("w_uq_n", [256, 512])
                w_uq_r = din("w_uq_r", [256, 256])
                g_q = din("g_q", [128, 2])
                ropeQ = din("ropeQ", [nqb, 128, 192])
                mk_mult = din("mk_mult", [2, 128, 128], BF16)
                SC = 96.0 ** -0.5
                Wcq = sb("Wcq", [128, 8, 256], BF16, pb_)
                WuqN = sb("WuqN", [128, 2, 512], BF16, pb_)
                WuqR = sb("WuqR", [128, 2, 256], BF16, pb_)
                gA = sb("gAB", [128, 8], F32, pb_)
                gQ = sb("gQB", [128, 2], F32, pb_)
                mkm = sb("mkmB", [128, 2, 128], BF16, pb_)
                stg = [sb(f"stgB{i}", [128, 512], F32, pb_) for i in range(2)]
                xs = sb("xsB", [128, D], F32, pb_)
                xb = sb("xbB", [128, D], BF16, pb_)
                tab = sb("tabB", [128, 192], F32, pb_)
                tabq = sb("tabqB", [128, 64], F32, pb_)
                junk = sb("junkB", [128, D], BF16, pb_)
                ss = sb("ssB", [128, 8], F32, pb_)
                xT = sb("xTB", [128, 8, 128], BF16, pb_)
                cq = sb("cqB", [128, 256], F32, pb_)
                cqn = sb("cqnB", [128, 256], BF16, pb_)
                cqnT = sb("cqnTB", [128, 2, 128], BF16, pb_)
                q96 = sb("q96B", [96, 8, 128], BF16, pb_)
                qr96 = sb("qr96B", [128, 8, 96], BF16, pb_)
                vb = [sb(f"vbB{i}", [128, 8, 65], BF16, pb_) for i in range(6)]
                qra = sb("qraB", [128, 256], F32, pb_)
                qrb = sb("qrbB", [128, 256], F32, pb_)
                PT = [sb(f"PTB{i}", [128, 1024], BF16, pb_) for i in range(2)]
                rden = sb("rdenB", [128, 8], F32, pb_)
                ost = [sb(f"ostB{i}", [128, 512], BF16, pb_) for i in range(2)]
                pT = ps("pTB", [128, 1024], BF16, pb_)
                pQ = ps("pQB", [128, 512], F32, pb_)
                pST = [ps(f"pSTB{i}", [128, 512], F32, pb_) for i in range(4)]
                pO = [ps(f"pOB{i}", [128, 4, 65], F32, pb_) for i in range(2)]

                S.add('sp', lambda e: e.dma_start(out=gA[:, :], in_=g_attn[:, :]), dma='cst')
                S.add('sp', lambda e: e.dma_start(out=gQ[:, :], in_=g_q[:, :]), dma='cst')
                t_cst = S.add('sp', lambda e: e.dma_start(out=mkm[:, :, :], in_=mk_mult.rearrange("m k q -> k m q")), dma='cst')
                t_z = S.add('pool', lambda e: e.memset(qr96[:, :, :], 0.0), sig=True)
                stg_free = [None, None]
                t_w = []
                jobs = [(Wcq[:, c, :], wq_in[c * 128:(c + 1) * 128, 0:256], gA[:, c:c + 1], 256) for c in range(8)]
                jobs += [(WuqN[:, c, :], w_uq_n[c * 128:(c + 1) * 128, :], gQ[:, c:c + 1], 512) for c in range(2)]
                jobs += [(WuqR[:, c, :], w_uq_r[c * 128:(c + 1) * 128, :], gQ[:, c:c + 1], 256) for c in range(2)]
                for j, (dst, src, gs, n) in enumerate(jobs):
                    s = j % 2
                    td = S.add('sp', lambda e, s=s, src=src, n=n: e.dma_start(out=stg[s][:, 0:n], in_=src),
                               deps=[stg_free[s]], dma=f'stg{s}')
                    stg_free[s] = S.add('dve' if s == 0 else 'pool', lambda e, s=s, dst=dst, gs=gs, n=n: e.tensor_scalar(
                        out=dst, in0=stg[s][:, 0:n], scalar1=gs, scalar2=None, op0=ALU.mult), deps=[td, t_cst], sig=True)
                    t_w.append(stg_free[s])

                prev = []
                vb_free = [None] * 6
                vcount = 0
                pst_free = [None] * 4
                PT_free = [None, None]
                ost_free = [None, None]
                t_onorm = None
                for i in range(nqb):
                    nk = min(2 * i + 3, nkb)
                    t_x = S.add('sp', lambda e, i=i: e.dma_start(out=xs[:, :], in_=xq[i * 128:(i + 1) * 128, :]), deps=prev, dma='ld')
                    t_x = S.add('sp', lambda e, i=i: e.dma_start(out=tab[:, :], in_=ropeQ[i, :, :]), deps=prev, dma='ld')
                    t_ss = S.add('act', lambda e: e.activation(out=junk[:, :], in_=xs[:, :], func=AF.Square, accum_out=ss[:, 0:1]),
                                 deps=[t_x] + prev, sig=True)
                    t_sd = S.add('act', lambda e: e.activation(out=ss[:, 1:2], in_=ss[:, 0:1], func=AF.Sqrt, scale=1.0 / D, bias=epsT[:, 0:1]),
                                 deps=[t_ss], sig=True)
                    t_rstd = S.add('dve', lambda e: e.reciprocal(out=ss[:, 2:3], in_=ss[:, 1:2]), deps=[t_sd], sig=True)
                    t_xb = S.add('dve', lambda e: e.tensor_copy(out=xb[:, :], in_=xs[:, :]), deps=[t_x] + prev, sig=True)
                    t_tq = S.add('dve', lambda e: e.tensor_scalar(out=tabq[:, :], in0=tab[:, 128:192], scalar1=SC, scalar2=None, op0=ALU.mult),
                                 deps=[t_x] + prev, sig=True)
                    for c in range(8):
                        t_tr = S.add('pe', lambda e, c=c: e.transpose(out=pT[:, c * 128:(c + 1) * 128], in_=xb[:, c * 128:(c + 1) * 128],
                                                                      identity=identB[:, :]), deps=[t_xb] + prev, sig=(c == 7))
                    t_xT = S.add('dve', lambda e: e.tensor_copy(out=xT[:, :, :], in_=pT[:, :].rearrange("p (c t) -> p c t", c=8)),
                                 deps=[t_tr], sig=True)
                    for c in range(8):
                        t_mm = S.add('pe', lambda e, c=c: e.matmul(pQ[:, 0:256], lhsT=xT[:, c, :], rhs=Wcq[:, c, :], start=(c == 0), stop=(c == 7)),
                                     deps=[t_xT, t_w] + prev, sig=(c == 7))
                    t_cq = S.add('dve', lambda e: e.tensor_scalar(out=cq[:, :], in0=pQ[:, 0:256], scalar1=ss[:, 2:3], scalar2=None, op0=ALU.mult),
                                 deps=[t_mm, t_rstd], sig=True)
                    t_ss2 = S.add('act', lambda e: e.activation(out=junk[:, 0:256], in_=cq[:, :], func=AF.Square, accum_out=ss[:, 4:5]),
                                  deps=[t_cq], sig=True)
                    t_sd2 = S.add('act', lambda e: e.activation(out=ss[:, 5:6], in_=ss[:, 4:5], func=AF.Sqrt, scale=1.0 / 256, bias=epsT[:, 0:1]),
                                  deps=[t_ss2], sig=True)
                    t_r2 = S.add('dve', lambda e: e.reciprocal(out=ss[:, 6:7], in_=ss[:, 5:6]), deps=[t_sd2], sig=True)
                    t_cqn = S.add('dve', lambda e: e.tensor_scalar(out=cqn[:, :], in0=cq[:, :], scalar1=ss[:, 6:7], scalar2=None, op0=ALU.mult),
                                  deps=[t_r2], sig=True)
                    for c in range(2):
                        t_tr = S.add('pe', lambda e, c=c: e.transpose(out=pT[:, c * 128:(c + 1) * 128], in_=cqn[:, c * 128:(c + 1) * 128],
                                                                      identity=identB[:, :]), deps=[t_cqn, t_xT], sig=(c == 1))
                    t_cT = S.add('dve', lambda e: e.tensor_copy(out=cqnT[:, :, :], in_=pT[:, 0:256].rearrange("p (c t) -> p c t", c=2)),
                                 deps=[t_tr], sig=True)
                    t_qn = None
                    for rnd in range(2):
                        for j in range(4):
                            h = rnd * 4 + j
                            for c in range(2):
                                t_mm = S.add('pe', lambda e, j=j, h=h, c=c: e.matmul(pQ[0:64, j * 128:(j + 1) * 128], lhsT=WuqN[:, c, h * 64:(h + 1) * 64],
                                                                                rhs=cqnT[:, c, :], start=(c == 0), stop=(c == 1)),
                                             deps=[t_cT, t_cq, t_qn], sig=(j == 3 and c == 1))
                        t_qn = S.add('dve', lambda e, rnd=rnd: e.tensor_scalar(out=q96[0:64, rnd * 4:(rnd + 1) * 4, :],
                                                                               in0=pQ[0:64, :].rearrange("p (a t) -> p a t", a=4),
                                                                               scalar1=SC, scalar2=None, op0=ALU.mult), deps=[t_mm] + prev, sig=True)
                    t_q1 = t_qn
                    for c in range(2):
                        t_mm = S.add('pe', lambda e, c=c: e.matmul(pQ[:, 0:256], lhsT=cqnT[:, c, :], rhs=WuqR[:, c, :], start=(c == 0), stop=(c == 1)),
                                     deps=[t_q1], sig=(c == 1))
                    v3 = lambda ap: ap.rearrange("p (h d) -> p h d", h=8)
                    t_a = S.add('dve', lambda e: e.tensor_tensor(out=v3(qra[:, :]), in0=v3(pQ[:, 0:256]), in1=bcast(tabq[:, 0:32], 8), op=ALU.mult),
                                deps=[t_mm, t_tq] + prev, sig=True)
                    t_b1 = S.add('dve', lambda e: e.tensor_tensor(out=v3(qrb[:, :])[:, :, 0:16], in0=v3(pQ[:, 0:256])[:, :, 16:32],
                                                                  in1=bcast(tabq[:, 32:48], 8), op=ALU.mult), deps=[t_mm, t_tq] + prev, sig=True)
                    t_b2 = S.add('dve', lambda e: e.tensor_tensor(out=v3(qrb[:, :])[:, :, 16:32], in0=v3(pQ[:, 0:256])[:, :, 0:16],
                                                                  in1=bcast(tabq[:, 48:64], 8), op=ALU.mult), deps=[t_mm, t_tq] + prev, sig=True)
                    t_qr = S.add('pool', lambda e: e.tensor_tensor(out=qr96[:, :, 64:96], in0=v3(qra[:, :]), in1=v3(qrb[:, :]), op=ALU.add),
                                 deps=[t_a, t_b1, t_b2, t_z] + prev, sig=True)
                    for h in range(8):
                        t_tr = S.add('pe', lambda e, h=h: e.transpose(out=pT[0:96, h * 128:(h + 1) * 128], in_=qr96[:, h, :],
                                                                      identity=identB[:, :]), deps=[t_qr, t_cT], sig=(h == 7))
                    t_qrT = S.add('dve', lambda e: e.tensor_copy(out=q96[64:96, :, :], in_=pT[64:96, :].rearrange("p (h t) -> p h t", h=8)),
                                  deps=[t_tr] + prev, sig=True)
                    qready = [t_q1, t_qrT]
                    prev_q = [t_b2, t_qrT, t_ss2, t_cqn]
                    t_exp = {}
                    t_pv = None

                    def emit_qk(kb):
                        for g in range(2):
                            bank = pST[(kb % 2) * 2 + g]
                            for j in range(4):
                                h = g * 4 + j
                                t = S.add('pe', lambda e, bank=bank, j=j, h=h, kb=kb: e.matmul(
                                    bank[:, j * 128:(j + 1) * 128], lhsT=knT[0:96, h, kb * 128:(kb + 1) * 128], rhs=q96[0:96, h, :],
                                    start=(j == 0), stop=(j == 3), skip_group_check=True),
                                    deps=qready + [pst_free[(kb % 2) * 2 + g]], sig=(j == 3))
                            t_qk[(kb, g)] = t

                    t_v = {}

                    def emit_v(kb):
                        nonlocal vcount
                        sl = vcount % 6
                        vcount += 1
                        t_v[kb] = (S.add('sp', lambda e, kb=kb, sl=sl: e.dma_start(out=vb[sl][:, :, :].rearrange("p h d -> p (h d)"), in_=s_vmla[kb, :, :]),
                                         deps=[vb_free[sl]], dma=f'vb{sl}'), sl)
                    t_qk = {}

                    def emit_exp(kb):
                        for g in range(2):
                            bi = (kb % 2) * 2 + g
                            t = S.add('act', lambda e, bi=bi, kb=kb, g=g: e.activation(
                                out=PT[kb % 2][:, g * 512:(g + 1) * 512], in_=pST[bi][:, :], func=AF.Exp),
                                deps=[t_qk[(kb, g)], PT_free[kb % 2]], sig=True)
                            pst_free[bi] = t
                            mi = kb - (2 * i + 1)
                            if mi >= 0:
                                t = S.add('pool', lambda e, kb=kb, g=g, mi=mi: e.tensor_tensor(
                                    out=PT[kb % 2][:, g * 512:(g + 1) * 512].rearrange("p (h q) -> p h q", h=4),
                                    in0=PT[kb % 2][:, g * 512:(g + 1) * 512].rearrange("p (h q) -> p h q", h=4),
                                    in1=bcast(mkm[:, mi, :], 4), op=ALU.mult), deps=[t, t_cst], sig=True)
                            t_exp[(kb, g)] = t

                    def emit_pv(kb):
                        nonlocal t_pv
                        for h in range(8):
                            t_pv = S.add('pe', lambda e, h=h, kb=kb, sl=t_v[kb][1]: e.matmul(
                                pO[h // 4][:, h % 4, :], lhsT=PT[kb % 2][:, h * 128:(h + 1) * 128], rhs=vb[sl][:, h, :],
                                start=(kb == 0 and h % 4 == 0), stop=(kb == nk - 1), skip_group_check=True),
                                deps=[t_exp[(kb, h // 4)], t_onorm, t_v[kb][0]], sig=(h == 7))
                        PT_free[kb % 2] = t_pv
                        vb_free[t_v[kb][1]] = t_pv

                    emit_v(0)
                    emit_qk(0)
                    for kb in range(nk):
                        if kb + 1 < nk:
                            emit_v(kb + 1)
                            emit_qk(kb + 1)
                        emit_exp(kb)
                        emit_pv(kb)
                    so = i % 2
                    t_rd = S.add('dve', lambda e: e.reciprocal(out=rden[:, 0:4], in_=pO[0][:, :, 64]), deps=[t_pv], sig=True)
                    t_rd2 = S.add('dve', lambda e: e.reciprocal(out=rden[:, 4:8], in_=pO[1][:, :, 64]), deps=[t_pv], sig=True)
                    for g in range(2):
                        t_onorm = S.add('dve', lambda e, g=g, so=so: e.tensor_tensor(
                            out=ost[so][:, g * 256:(g + 1) * 256].rearrange("p (h d) -> p h d", h=4), in0=pO[g][:, :, 0:64],
                            in1=rden[:, g * 4:(g + 1) * 4].unsqueeze(2).broadcast_to([128, 4, 64]), op=ALU.mult),
                            deps=[t_rd, t_rd2, ost_free[so]], sig=True)
                    ost_free[so] = S.add('sp', lambda e, i=i, so=so: e.dma_start(out=s_omla[i, :, :], in_=ost[so][:, :]),
                                         deps=[t_onorm], dma=f'ost{so}')
                    prev = [t_pv, t_onorm] + prev_q
                if debug:
                    S.final_wait()
                    S.add('sp', lambda e: e.dma_start(out=d_omla.rearrange("p (i f) -> i p f", i=nqb), in_=s_omla[0:nqb, :, :]), dma='dbg')
                S.final_wait()
                S.run(nc, "B", top)
        abst.close()
        if 'C' in phases:
            with ExitStack() as pc_:
                S = Sched()
                xq = din("xq", [nqb * 128, D])
                wq_in = din("wq_in", [D, 1288])
                g_attn = din("g_attn", [128, 8])
                ropeQ = din("ropeQ", [nqb, 128, 192])
                mk_add = din("mk_add", [3, 128, 128])
                NIT = 16
                Wq2 = sb("Wq2C", [128, 8, 1032], BF16, pc_)
                gA = sb("gAC", [128, 8], F32, pc_)
                mka = sb("mkaC", [128, 3, 128], F32, pc_)
                stg = [sb("stgC0", [128, 1032], F32, pc_)] * 2
                ksT = sb("ksTC", [128, nkb, 512], BF16, pc_)
                kiT2 = sb("kiT2C", [128, nkb, 128], BF16, pc_)
                Isc = sb("IscC", [128, nkb * 128], F32, pc_)
                MskL = [sb(f"MskC{i}", [128, nkb * 128], BF16, pc_) for i in range(2)]
                xs = sb("xsC", [128, D], F32, pc_)
                xb = sb("xbC", [128, D], BF16, pc_)
                tab = sb("tabC", [128, 192], F32, pc_)
                tabs = sb("tabsC", [128, 128], F32, pc_)
                junk = sb("junkC", [128, D], BF16, pc_)
                ss = sb("ssC", [128, 8], F32, pc_)
                xT = sb("xTC", [128, 8, 128], BF16, pc_)
                ra = sb("raC", [128, 512], F32, pc_)
                rb = sb("rbC", [128, 512], F32, pc_)
                qs = sb("qsC", [128, 512], BF16, pc_)
                qi = sb("qiC", [128, 512], BF16, pc_)
                wv = sb("wvC", [128, 8], F32, pc_)
                qbdL = [sb(f"qbdC{i}", [128, 4, 256], BF16, pc_) for i in range(2)]
                qiT = sb("qiTC", [128, 4, 128], BF16, pc_)
                tmpR = [sb(f"tmpRC{i}", [128, 512], F32, pc_) for i in range(3)]
                pw2 = sb("pw2C", [128, 32], F32, pc_)
                hwt = sb("hwtC", [128, 32], F32, pc_)
                bs = sb("bsC", [128, 16], F32, pc_)
                PT = [sb(f"PTC{i}", [128, 1024], BF16, pc_) for i in range(2)]
                vb = [sb(f"vbC{i}", [128, 8, 65], BF16, pc_) for i in range(3)]
                rden = sb("rdenC", [128, 8], F32, pc_)
                ident4 = sb("ident4C", [128, 4, 128], BF16, pc_)
                ost = [sb(f"ostC{i}", [128, 512], BF16, pc_) for i in range(2)]
                pT = [ps(f"pTC{i}", [128, 1024], BF16, pc_) for i in range(2)]
                pST = [ps(f"pSTC{i}", [128, 512], F32, pc_) for i in range(4)]
                pO = [ps(f"pOC{i}", [128, 4, 65], F32, pc_) for i in range(2)]

                S.add('sp', lambda e: e.dma_start(out=gA[:, :], in_=g_attn[:, :]), dma='cst')
                S.add('sp', lambda e: e.dma_start(out=mka[:, :, :], in_=mk_add.rearrange("m q k -> q m k")), dma='cst')
                for k0 in range(0, nkb, 4):
                    k1 = min(nkb, k0 + 4)
                    S.add('sp', lambda e, k0=k0, k1=k1: e.dma_start(out=ksT[:, k0:k1, :], in_=s_ksT[k0:k1, :, :].rearrange("k p f -> p k f")), dma='cst')
                for k0 in range(0, nkb, 8):
                    k1 = min(nkb, k0 + 8)
                    S.add('sp', lambda e, k0=k0, k1=k1: e.dma_start(out=kiT2[0:64, k0:k1, :], in_=s_kiT[k0:k1, :, :].rearrange("k p t -> p k t")), dma='cst')
                    t_cst = S.add('sp', lambda e, k0=k0, k1=k1: e.dma_start(out=kiT2[64:128, k0:k1, :], in_=s_kiT[k0:k1, :, :].rearrange("k p t -> p k t")), dma='cst')
                S.add('pool', lambda e: e.memset(qbdL[0][:, :, :], 0.0), sig=True)
                t_i4 = S.add('pool', lambda e: e.tensor_copy(out=ident4[:, :, :], in_=identB[:, :].unsqueeze(1).broadcast_to([128, 4, 128])), sig=True)
                t_z = S.add('pool', lambda e: e.memset(qbdL[1][:, :, :], 0.0), sig=True)
                for it in range(NIT):
                    t_pw2 = S.add('pool', lambda e, it=it: e.memset(pw2[:, it:it + 1], 2.0 ** -(it + 1)), sig=True)
                stg_free = [None, None]
                t_w = []
                for c in range(8):
                    s = 0
                    td = S.add('sp', lambda e, s=s, c=c: e.dma_start(out=stg[s][:, :], in_=wq_in[c * 128:(c + 1) * 128, 256:1288]),
                               deps=[stg_free[s]], dma=f'stg{s}')
                    stg_free[s] = S.add(('dve', 'pool')[s], lambda e, s=s, c=c: e.tensor_scalar(
                        out=Wq2[:, c, :], in0=stg[s][:, :], scalar1=gA[:, c:c + 1], scalar2=None, op0=ALU.mult), deps=[td, t_cst], sig=True)
                    t_w.append(stg_free[s])
                v3 = lambda ap: ap.rearrange("p (h d) -> p h d", h=8)
                G = dict(prevA=[], att_done=[], t_onorm=None)
                stt = {}
                pst_free = [None] * 4
                PT_free = [None, None]
                pT_free = [None, None]
                vb_free = [None] * 3
                ost_free = [None, None]
                vcount = 0

                def genA(i):
                    nk = min(2 * i + 3, nkb)
                    W = nk * 128
                    prev = G['prevA'] + G['att_done']
                    Msk = MskL[i % 2]
                    qbd = qbdL[i % 2]
                    S.add('sp', lambda e, i=i: e.dma_start(out=xs[:, :], in_=xq[i * 128:(i + 1) * 128, :]), deps=prev, dma='ld')
                    t_x = S.add('sp', lambda e, i=i: e.dma_start(out=tab[:, :], in_=ropeQ[i, :, :]), deps=prev, dma='ld')
                    t_ss = S.add('act', lambda e: e.activation(out=junk[:, :], in_=xs[:, :], func=AF.Square, accum_out=ss[:, 0:1]), deps=[t_x] + prev, sig=True)
                    t_sd = S.add('act', lambda e: e.activation(out=ss[:, 1:2], in_=ss[:, 0:1], func=AF.Sqrt, scale=1.0 / D, bias=epsT[:, 0:1]), deps=[t_ss], sig=True)
                    t_rstd = S.add('dve', lambda e: e.reciprocal(out=ss[:, 2:3], in_=ss[:, 1:2]), deps=[t_sd], sig=True)
                    t_r8 = S.add('dve', lambda e: e.tensor_scalar(out=ss[:, 3:4], in0=ss[:, 2:3], scalar1=0.125, scalar2=None, op0=ALU.mult), deps=[t_rstd], sig=True)
                    t_tabs = S.add('dve', lambda e: e.tensor_scalar(out=tabs[:, :], in0=tab[:, 0:128], scalar1=ss[:, 3:4], scalar2=None, op0=ALU.mult),
                                   deps=[t_r8, t_x] + prev, sig=True)
                    t_xb = S.add('dve', lambda e: e.tensor_copy(out=xb[:, :], in_=xs[:, :]), deps=[t_x] + prev, sig=True)
                    for c in range(8):
                        t_tr = S.add('pe', lambda e, c=c: e.transpose(out=pT[0][:, c * 128:(c + 1) * 128], in_=xb[:, c * 128:(c + 1) * 128],
                                                                      identity=identB[:, :]), deps=[t_xb] + prev, sig=(c == 7))
                    t_xT = S.add('dve', lambda e: e.tensor_copy(out=xT[:, :, :], in_=pT[0][:, :].rearrange("p (c t) -> p c t", c=8)), deps=[t_tr], sig=True)
                    t_mms = []
                    for bnk, (c0, c1) in enumerate(((0, 512), (512, 1024), (1024, 1032))):
                        for c in range(8):
                            t_mm = S.add('pe', lambda e, c=c, bnk=bnk, c0=c0, c1=c1: e.matmul(pST[bnk][:, 0:c1 - c0], lhsT=xT[:, c, :], rhs=Wq2[:, c, c0:c1],
                                                                                             start=(c == 0), stop=(c == 7)), deps=[t_xT, t_w] + prev, sig=(c == 7))
                        t_mms.append(t_mm)
                    outs = []
                    for bnk, dst in ((0, qs), (1, qi)):
                        t_a = S.add('dve', lambda e, bnk=bnk: e.tensor_tensor(out=v3(ra[:, :]), in0=v3(pST[bnk][:, :]), in1=bcast(tabs[:, 0:64], 8), op=ALU.mult),
                                    deps=[t_mms[bnk], t_tabs] + outs + prev, sig=True)
                        t_b1 = S.add('dve', lambda e, bnk=bnk: e.tensor_tensor(out=v3(rb[:, :])[:, :, 0:32], in0=v3(pST[bnk][:, :])[:, :, 32:64],
                                                                              in1=bcast(tabs[:, 64:96], 8), op=ALU.mult), deps=[t_mms[bnk], t_tabs] + outs + prev, sig=True)
                        t_b2 = S.add('dve', lambda e, bnk=bnk: e.tensor_tensor(out=v3(rb[:, :])[:, :, 32:64], in0=v3(pST[bnk][:, :])[:, :, 0:32],
                                                                              in1=bcast(tabs[:, 96:128], 8), op=ALU.mult), deps=[t_mms[bnk], t_tabs] + outs + prev, sig=True)
                        t_q = S.add('pool', lambda e, dst=dst: e.tensor_tensor(out=dst[:, :], in0=ra[:, :], in1=rb[:, :], op=ALU.add),
                                    deps=[t_a, t_b1, t_b2] + prev, sig=True)
                        outs = [t_q]
                        if bnk == 0:
                            t_qs = t_q
                        else:
                            t_qi = t_q
                    t_wv = S.add('dve', lambda e: e.tensor_scalar(out=wv[:, :], in0=pST[2][:, 0:8], scalar1=ss[:, 2:3], scalar2=8.0 ** -0.5,
                                                                  op0=ALU.mult, op1=ALU.mult), deps=[t_mms[2], t_rstd] + prev, sig=True)
                    pst_q = [t_b2, t_wv]
                    for a in range(4):
                        t_tr = S.add('pe', lambda e, a=a: e.transpose(out=pT[0][:, a * 128:(a + 1) * 128], in_=qs[:, a * 128:(a + 1) * 128],
                                                                      identity=identB[:, :]), deps=[t_qs, t_xT], sig=(a == 3))
                    for a in range(4):
                        t_tr2 = S.add('pe', lambda e, a=a: e.transpose(out=pT[1][:, a * 128:(a + 1) * 128], in_=qi[:, a * 128:(a + 1) * 128],
                                                                       identity=identB[:, :]), deps=[t_qi, pT_free[1]] + prev, sig=(a == 3))
                    pTv = lambda t: t[:, 0:512].rearrange("p (a t) -> p a t", a=4)
                    t_q1 = S.add('dve', lambda e: e.tensor_copy(out=qbd[0:64, :, 0:128], in_=pTv(pT[0])[0:64, :, :]), deps=[t_tr, t_z] + prev, sig=True)
                    t_q2 = S.add('dve', lambda e: e.tensor_copy(out=qbd[64:128, :, 128:256], in_=pTv(pT[0])[64:128, :, :]), deps=[t_tr, t_z] + prev, sig=True)
                    t_qiT = S.add('dve', lambda e: e.tensor_copy(out=qiT[:, :, :], in_=pTv(pT[1])), deps=[t_tr2] + prev, sig=True)
                    pT_free[0] = t_q2
                    pT_free[1] = t_qiT
                    t_acc = None
                    tmp_free = [None] * 3
                    nch = (W + 511) // 512
                    cnt_ = 0
                    for ch in range(nch):
                        c0 = ch * 512
                        n = min(512, W - c0)
                        for h in range(8):
                            hp = (h % 2) * 64
                            bnk = h % 4
                            t_lg = S.add('pe', lambda e, hp=hp, h=h, bnk=bnk, c0=c0, n=n: e.matmul(
                                pST[bnk][:, 0:n], lhsT=qiT[hp:hp + 64, h // 2, :],
                                rhs=kiT2[hp:hp + 64, :, :].rearrange("p k t -> p (k t)")[:, c0:c0 + n], start=True, stop=True),
                                deps=[t_qiT, t_cst, pst_free[bnk]] + pst_q, sig=True)
                            sl = cnt_ % 3
                            cnt_ += 1
                            t_r = S.add('act', lambda e, bnk=bnk, sl=sl, n=n: e.activation(out=tmpR[sl][:, 0:n], in_=pST[bnk][:, 0:n], func=AF.Relu),
                                        deps=[t_lg, tmp_free[sl]], sig=True)
                            pst_free[bnk] = t_r
                            if h == 0:
                                t_acc = S.add('dve', lambda e, sl=sl, c0=c0, n=n: e.tensor_scalar(
                                    out=Isc[:, c0:c0 + n], in0=tmpR[sl][:, 0:n], scalar1=wv[:, 0:1], scalar2=None, op0=ALU.mult),
                                    deps=[t_r, t_wv, t_acc] + prev, sig=True)
                            else:
                                t_acc = S.add('dve', lambda e, sl=sl, c0=c0, n=n, h=h: e.scalar_tensor_tensor(
                                    out=Isc[:, c0:c0 + n], in0=tmpR[sl][:, 0:n], scalar=wv[:, h:h + 1], in1=Isc[:, c0:c0 + n],
                                    op0=ALU.mult, op1=ALU.add), deps=[t_r, t_wv, t_acc], sig=True)
                            tmp_free[sl] = t_acc
                    t_mx = S.add('dve', lambda e, W=W: e.tensor_reduce(out=bs[:, 1:2], in_=Isc[:, 0:W], axis=AX.X, op=ALU.max), deps=[t_acc] + prev, sig=True)
                    t_mn = S.add('dve', lambda e, W=W: e.tensor_reduce(out=bs[:, 0:1], in_=Isc[:, 0:W], axis=AX.X, op=ALU.min), deps=[t_acc] + prev, sig=True)
                    t_b = S.add('dve', lambda e: e.tensor_scalar(out=bs[:, 1:2], in0=bs[:, 1:2], scalar1=1.0, scalar2=None, op0=ALU.add), deps=[t_mx], sig=True)
                    t_b = S.add('dve', lambda e: e.tensor_scalar(out=bs[:, 0:1], in0=bs[:, 0:1], scalar1=-1.0, scalar2=None, op0=ALU.add), deps=[t_mn, t_b], sig=True)
                    for (mi, kb) in ((0, 0), (1, nk - 2), (2, nk - 1)):
                        t_b = S.add('dve', lambda e, mi=mi, kb=kb: e.tensor_tensor(out=Isc[:, kb * 128:(kb + 1) * 128], in0=Isc[:, kb * 128:(kb + 1) * 128],
                                                                                  in1=mka[:, mi, :], op=ALU.add), deps=[t_b, t_mn, t_mx, t_cst], sig=True)
                    W1 = (nk // 2) * 128
                    W2 = W - W1
                    t_b = S.add('dve', lambda e: e.tensor_tensor(out=bs[:, 7:8], in0=bs[:, 1:2], in1=bs[:, 0:1], op=ALU.subtract), deps=[t_b], sig=True)
                    t_b = S.add('dve', lambda e: e.tensor_scalar(out=hwt[:, 0:NIT], in0=pw2[:, 0:NIT], scalar1=bs[:, 7:8], scalar2=None, op0=ALU.mult),
                                deps=[t_b, t_pw2], sig=True)
                    t_b = S.add('dve', lambda e: e.tensor_tensor(out=bs[:, 2:3], in0=bs[:, 0:1], in1=hwt[:, 0:1], op=ALU.add), deps=[t_b], sig=True)
                    yield 'front'
                    for it in range(NIT):
                        t_c1 = S.add('dve', lambda e, W1=W1: e.tensor_scalar(out=Msk[:, 0:W1], in0=Isc[:, 0:W1], scalar1=bs[:, 2:3], scalar2=0.0,
                                                                          op0=ALU.is_ge, op1=ALU.add, accum_out=bs[:, 3:4]), deps=[t_b] + prev, sig=True)
                        t_c2 = S.add('act', lambda e, W1=W1, W=W: e.activation(out=Msk[:, W1:W], in_=Isc[:, W1:W], func=AF.Sign, scale=-1.0,
                                                                             bias=bs[:, 2:3], accum_out=bs[:, 8:9]), deps=[t_b] + prev, sig=True)
                        t_b = S.add('dve', lambda e: e.scalar_tensor_tensor(out=bs[:, 9:10], in0=bs[:, 8:9], scalar=-0.5, in1=bs[:, 3:4],
                                                                            op0=ALU.mult, op1=ALU.add), deps=[t_c1, t_c2], sig=True)
                        t_b = S.add('dve', lambda e, W2=W2: e.tensor_scalar(out=bs[:, 4:5], in0=bs[:, 9:10], scalar1=float(TOPK) - 0.5 - W2 / 2.0,
                                                                          scalar2=None, op0=ALU.is_ge), deps=[t_b], sig=True)
                        t_b = S.add('dve', lambda e, it=it: e.scalar_tensor_tensor(out=bs[:, 0:1], in0=bs[:, 4:5], scalar=hwt[:, it:it + 1], in1=bs[:, 0:1],
                                                                                   op0=ALU.mult, op1=ALU.add), deps=[t_b], sig=True)
                        if it + 1 < NIT:
                            t_b = S.add('dve', lambda e, it=it: e.tensor_tensor(out=bs[:, 2:3], in0=bs[:, 0:1], in1=hwt[:, it + 1:it + 2], op=ALU.add),
                                        deps=[t_b], sig=True)
                        yield 'it'
                    t_msk = S.add('dve', lambda e, W=W: e.tensor_scalar(out=Msk[:, 0:W], in0=Isc[:, 0:W], scalar1=bs[:, 0:1], scalar2=-30000.0,
                                                                        op0=ALU.is_lt, op1=ALU.mult),
                                  deps=[t_b], sig=True)
                    stt[i] = dict(nk=nk, t_q1=t_q1, t_q2=t_q2, t_msk=t_msk)
                    G['prevA'] = [t_msk, t_qiT, t_q2, t_acc]

                def genB(i):
                    nonlocal vcount
                    nk = stt[i]['nk']
                    t_msk = stt[i]['t_msk']
                    Msk = MskL[i % 2]
                    qbd = qbdL[i % 2]
                    qready = [stt[i]['t_q1'], stt[i]['t_q2'], t_msk]
                    t_qk = {}
                    t_exp = {}
                    t_v = {}
                    t_pv = None

                    def emit_qk(kb):
                        for g in range(2):
                            bi = (kb % 2) * 2 + g
                            for j in range(2):
                                a = g * 2 + j
                                S.add('pe', lambda e, bi=bi, j=j, a=a, kb=kb: e.matmul(
                                    pST[bi][:, j * 256:(j + 1) * 256], lhsT=ksT[:, kb, a * 128:(a + 1) * 128], rhs=qbd[:, a, :],
                                    start=(j == 0), stop=False, skip_group_check=True), deps=qready + [pst_free[bi], t_cst])
                            t = S.add('pe', lambda e, bi=bi, kb=kb: e.matmul(
                                pST[bi][:, :], lhsT=Msk[:, kb * 128:(kb + 1) * 128], rhs=ident4[:, :, :].rearrange("p a t -> p (a t)"),
                                start=False, stop=True, skip_group_check=True), deps=[t_msk, t_i4], sig=True)
                            t_qk[(kb, g)] = t

                    def emit_v(kb):
                        nonlocal vcount
                        sl = vcount % 3
                        vcount += 1
                        t_v[kb] = (S.add('sp', lambda e, kb=kb, sl=sl: e.dma_start(out=vb[sl][:, :, :].rearrange("p h d -> p (h d)"), in_=s_vds[kb, :, :]),
                                         deps=[vb_free[sl]], dma=f'vb{sl}'), sl)

                    def emit_exp(kb):
                        for g in range(2):
                            bi = (kb % 2) * 2 + g
                            t = S.add('act', lambda e, bi=bi, kb=kb, g=g: e.activation(
                                out=PT[kb % 2][:, g * 512:(g + 1) * 512], in_=pST[bi][:, :], func=AF.Exp),
                                deps=[t_qk[(kb, g)], PT_free[kb % 2]], sig=True)
                            pst_free[bi] = t
                            t_exp[(kb, g)] = t

                    def emit_pv(kb):
                        nonlocal t_pv
                        tv, sl = t_v[kb]
                        for h in range(8):
                            t_pv = S.add('pe', lambda e, h=h, kb=kb, sl=sl: e.matmul(
                                pO[h // 4][:, h % 4, :], lhsT=PT[kb % 2][:, h * 128:(h + 1) * 128], rhs=vb[sl][:, h, :],
                                start=(kb == 0 and h % 4 == 0), stop=(kb == nk - 1), skip_group_check=True),
                                deps=[t_exp[(kb, h // 4)], tv, G['t_onorm']], sig=(h == 7))
                        PT_free[kb % 2] = t_pv
                        vb_free[sl] = t_pv

                    emit_v(0)
                    emit_qk(0)
                    for kb in range(nk):
                        if kb + 1 < nk:
                            emit_v(kb + 1)
                            emit_qk(kb + 1)
                        emit_exp(kb)
                        emit_pv(kb)
                        yield 'kb'
                    so = i % 2
                    t_rd = S.add('dve', lambda e: e.reciprocal(out=rden[:, 0:4], in_=pO[0][:, :, 64]), deps=[t_pv], sig=True)
                    t_rd2 = S.add('dve', lambda e: e.reciprocal(out=rden[:, 4:8], in_=pO[1][:, :, 64]), deps=[t_pv], sig=True)
                    for g in range(2):
                        t_onorm = S.add('dve', lambda e, g=g, so=so: e.tensor_tensor(
                            out=ost[so][:, g * 256:(g + 1) * 256].rearrange("p (h d) -> p h d", h=4), in0=pO[g][:, :, 0:64],
                            in1=rden[:, g * 4:(g + 1) * 4].unsqueeze(2).broadcast_to([128, 4, 64]), op=ALU.mult),
                            deps=[t_rd, t_rd2, ost_free[so]], sig=True)
                    ost_free[so] = S.add('sp', lambda e, i=i, so=so: e.dma_start(out=s_odsa[i, :, :], in_=ost[so][:, :]), deps=[t_onorm], dma=f'ost{so}')
                    G['t_onorm'] = t_onorm
                    G['att_done'] = [t_pv, t_onorm]

                for i in range(nqb):
                    a = genA(i)
                    for tag in a:
                        if tag == 'front':
                            break
                    if i > 0:
                        b = genB(i - 1)
                        nkp = stt[i - 1]['nk']
                        ita = 0
                        for step in range(nkp):
                            next(b, None)
                            target = ((step + 1) * NIT) // nkp
                            while ita < target:
                                next(a, None)
                                ita += 1
                        for _ in a:
                            pass
                        for _ in b:
                            pass
                    else:
                        for _ in a:
                            pass
                for _ in genB(nqb - 1):
                    pass
                S.final_wait()
                S.run(nc, "C", top)
        if 'D' in phases:
            with ExitStack() as pd_:
                S = Sched()
                xq = din("xq", [nqb * 128, D])
                w_o = din("w_o", [D, D])
                g_ffn = din("g_ffn", [128, 8])
                w_gate = din("w_gate", [D, DFF])
                w_up = din("w_up", [D, DFF])
                w_down = din("w_down", [DFF, D])
                g_fin = din("g_fin", [128, D])
                Wo = sb("WoD", [128, 8, D], BF16, pd_)
                Wg = sb("WgD", [128, 8, DFF], BF16, pd_)
                Wu = sb("WuD", [128, 8, DFF], BF16, pd_)
                Wd = sb("WdD", [128, NFC, D], BF16, pd_)
                gF = sb("gFD", [128, 8], F32, pd_)
                gfin = sb("gfinD", [128, D], F32, pd_)
                stg = [sb(f"stgD{i}", [128, 1024], F32, pd_) for i in range(2)]
                xs = [sb(f"xsD{i}", [128, D], F32, pd_) for i in range(2)]
                ob = [sb("obD0", [128, D], BF16, pd_)] * 2
                oT = [sb(f"oTD{i}", [128, 8, 128], BF16, pd_) for i in range(2)]
                ub = [sb("ubD0", [128, D], BF16, pd_)] * 2
                uT = [sb(f"uTD{i}", [128, 8, 128], BF16, pd_) for i in range(2)]
                junk = sb("junkD", [128, D], BF16, pd_)
                ss = [sb(f"ssD{i}", [128, 8], F32, pd_) for i in range(2)]
                sil = [sb(f"silD{i}", [128, 512], F32, pd_) for i in range(2)]
                actT = [sb(f"actTD{i}", [128, NFC, 128], BF16, pd_) for i in range(2)]
                h2 = [sb(f"h2D{i}", [128, D], F32, pd_) for i in range(2)]
                pT = ps("pTD", [128, 1024], BF16, pd_)
                pA = [ps(f"pAD{i}", [128, 512], F32, pd_) for i in range(2)]
                pG = [ps(f"pGD{i}", [128, 512], F32, pd_) for i in range(2)]
                pU = [ps(f"pUD{i}", [128, 512], F32, pd_) for i in range(2)]
                S.add('sp', lambda e: e.dma_start(out=gF[:, :], in_=g_ffn[:, :]), dma='cst')
                t_cst = S.add('sp', lambda e: e.dma_start(out=gfin[:, :], in_=g_fin[:, :]), dma='cst')
                stg_free = [None, None]
                t_w = []
                jobs = []
                for c in range(8):
                    jobs.append((Wo[:, c, :], w_o[c * 128:(c + 1) * 128, :], None, D))
                n_wo = len(jobs)
                for c in range(8):
                    for (Wt, wsrc) in ((Wg, w_gate), (Wu, w_up)):
                        for c0 in range(0, DFF, 1024):
                            n = min(1024, DFF - c0)
                            jobs.append((Wt[:, c, c0:c0 + n], wsrc[c * 128:(c + 1) * 128, c0:c0 + n], gF[:, c:c + 1], n))
                n_wgu = len(jobs)
                for f in range(NFC):
                    jobs.append((Wd[:, f, :], w_down[f * 128:(f + 1) * 128, :], None, D))
                for j, (dst, src, gs, n) in enumerate(jobs):
                    s = j % 2
                    td = S.add('sp', lambda e, s=s, src=src, n=n: e.dma_start(out=stg[s][:, 0:n], in_=src), deps=[stg_free[s]], dma=f'stg{s}')
                    eng = ('dve', 'pool')[s]
                    if gs is None:
                        stg_free[s] = S.add(eng, lambda e, s=s, dst=dst, n=n: e.tensor_copy(out=dst, in_=stg[s][:, 0:n]), deps=[td], sig=True)
                    else:
                        stg_free[s] = S.add(eng, lambda e, s=s, dst=dst, gs=gs, n=n: e.tensor_scalar(
                            out=dst, in0=stg[s][:, 0:n], scalar1=gs, scalar2=None, op0=ALU.mult), deps=[td, t_cst], sig=True)
                    t_w.append(stg_free[s])
                t_wo = t_w[:n_wo][-2:]
                t_wgu = t_w[:n_wgu][-2:]
                t_wd = t_w[-2:]
                pG_free = [None, None]
                pU_free = [None, None]
                sil_free = [None, None]
                pT_free = [None]
                pA_free = [None, None]
                done = {}
                st = {}

                def s1a(i):
                    b = i % 2
                    pv = done.get(i - 2, [])
                    S.add('sp', lambda e: e.dma_start(out=xs[b][:, :], in_=xq[i * 128:(i + 1) * 128, :]), deps=pv, dma=f'ld{b}')
                    S.add('sp', lambda e: e.dma_start(out=ob[b][:, 0:512], in_=s_omla[i, :, :]), deps=pv + [pT_free[0]], dma=f'ld{b}')
                    t_x = S.add('sp', lambda e: e.dma_start(out=ob[b][:, 512:1024], in_=s_odsa[i, :, :]), deps=pv + [pT_free[0]], dma=f'ld{b}')
                    for c in range(8):
                        t_tr = S.add('pe', lambda e, c=c: e.transpose(out=pT[:, c * 128:(c + 1) * 128], in_=ob[b][:, c * 128:(c + 1) * 128],
                                                                      identity=identB[:, :]), deps=[t_x, pT_free[0]], sig=(c == 7))
                    t_oT = S.add('dve', lambda e: e.tensor_copy(out=oT[b][:, :, :], in_=pT[:, :].rearrange("p (c t) -> p c t", c=8)), deps=[t_tr] + pv, sig=True)
                    pT_free[0] = t_oT
                    t_mm = [None, None]
                    for hf in range(2):
                        for c in range(8):
                            t_mm[hf] = S.add('pe', lambda e, c=c, hf=hf: e.matmul(pA[hf][:, :], lhsT=oT[b][:, c, :], rhs=Wo[:, c, hf * 512:(hf + 1) * 512],
                                                                                  start=(c == 0), stop=(c == 7)), deps=[t_oT, t_wo, pA_free[hf]], sig=(c == 7))
                    for hf in range(2):
                        t_h1 = S.add('dve', lambda e, hf=hf: e.tensor_tensor(out=xs[b][:, hf * 512:(hf + 1) * 512], in0=pA[hf][:, :],
                                                                             in1=xs[b][:, hf * 512:(hf + 1) * 512], op=ALU.add), deps=[t_mm[hf], t_x], sig=True)
                        pA_free[hf] = t_h1
                    t_ss = S.add('act', lambda e: e.activation(out=junk[:, :], in_=xs[b][:, :], func=AF.Square, accum_out=ss[b][:, 0:1]), deps=[t_h1] + pv, sig=True)
                    t_sd = S.add('act', lambda e: e.activation(out=ss[b][:, 1:2], in_=ss[b][:, 0:1], func=AF.Sqrt, scale=1.0 / D, bias=epsT[:, 0:1]), deps=[t_ss], sig=True)
                    t_r = S.add('dve', lambda e: e.reciprocal(out=ss[b][:, 2:3], in_=ss[b][:, 1:2]), deps=[t_sd], sig=True)
                    st[i] = dict(t_ub=S.add('dve', lambda e: e.tensor_scalar(out=ub[b][:, :], in0=xs[b][:, :], scalar1=ss[b][:, 2:3], scalar2=None, op0=ALU.mult),
                                            deps=[t_r, pT_free[0]] + pv, sig=True), t_h1=t_h1)

                def s1b(i):
                    b = i % 2
                    pv = done.get(i - 2, [])
                    for c in range(8):
                        t_tr = S.add('pe', lambda e, c=c: e.transpose(out=pT[:, c * 128:(c + 1) * 128], in_=ub[b][:, c * 128:(c + 1) * 128],
                                                                      identity=identB[:, :]), deps=[st[i]['t_ub'], pT_free[0]], sig=(c == 7))
                    st[i]['t_uT'] = S.add('dve', lambda e: e.tensor_copy(out=uT[b][:, :, :], in_=pT[:, :].rearrange("p (c t) -> p c t", c=8)), deps=[t_tr] + pv, sig=True)
                    pT_free[0] = st[i]['t_uT']

                def s2(i, g0, g1):
                    b = i % 2
                    pv = done.get(i - 2, [])
                    t_uT = st[i]['t_uT']
                    for gi in range(g0, g1):
                        sl = gi % 2
                        nf = min(4, NFC - gi * 4)
                        for (pX, Wt, fr) in ((pG, Wg, pG_free), (pU, Wu, pU_free)):
                            for j in range(nf):
                                f = gi * 4 + j
                                for c in range(8):
                                    t_mm = S.add('pe', lambda e, pX=pX, Wt=Wt, sl=sl, j=j, f=f, c=c: e.matmul(
                                        pX[sl][:, j * 128:(j + 1) * 128], lhsT=Wt[:, c, f * 128:(f + 1) * 128], rhs=uT[b][:, c, :],
                                        start=(c == 0), stop=(c == 7)), deps=[t_uT, t_wgu, fr[sl]], sig=(c == 7 and j == nf - 1))
                            if pX is pG:
                                t_g = t_mm
                            else:
                                t_u = t_mm
                        t_sil = S.add('act', lambda e, sl=sl, nf=nf: e.activation(out=sil[sl][:, 0:nf * 128], in_=pG[sl][:, 0:nf * 128], func=AF.Silu),
                                      deps=[t_g, sil_free[sl]], sig=True)
                        pG_free[sl] = t_sil
                        t_act = S.add('dve', lambda e, sl=sl, nf=nf, gi=gi: e.tensor_tensor(
                            out=actT[b][:, gi * 4:gi * 4 + nf, :], in0=pU[sl][:, 0:nf * 128].rearrange("p (f t) -> p f t", f=nf),
                            in1=sil[sl][:, 0:nf * 128].rearrange("p (f t) -> p f t", f=nf), op=ALU.mult), deps=[t_sil, t_u] + pv, sig=True)
                        pU_free[sl] = t_act
                        sil_free[sl] = t_act
                        st[i]['t_act'] = t_act
                        st[i]['t_u'] = t_u

                def s3(i):
                    b = i % 2
                    t_act = st[i]['t_act']
                    t_mm = [None, None]
                    for hf in range(2):
                        for f in range(NFC):
                            t_mm[hf] = S.add('pe', lambda e, f=f, hf=hf: e.matmul(pA[hf][:, :], lhsT=actT[b][:, f, :], rhs=Wd[:, f, hf * 512:(hf + 1) * 512],
                                                                                  start=(f == 0), stop=(f == NFC - 1)), deps=[t_act, t_wd, pA_free[hf]], sig=(f == NFC - 1))
                    for hf in range(2):
                        t_h2 = S.add('dve', lambda e, hf=hf: e.tensor_tensor(out=h2[b][:, hf * 512:(hf + 1) * 512], in0=pA[hf][:, :],
                                                                             in1=xs[b][:, hf * 512:(hf + 1) * 512], op=ALU.add),
                                     deps=[t_mm[hf]] + done.get(i - 2, []), sig=True)
                        pA_free[hf] = t_h2
                    t_ss = S.add('act', lambda e: e.activation(out=junk[:, :], in_=h2[b][:, :], func=AF.Square, accum_out=ss[b][:, 4:5]), deps=[t_h2], sig=True)
                    t_sd = S.add('act', lambda e: e.activation(out=ss[b][:, 5:6], in_=ss[b][:, 4:5], func=AF.Sqrt, scale=1.0 / D, bias=epsT[:, 0:1]), deps=[t_ss], sig=True)
                    t_r = S.add('dve', lambda e: e.reciprocal(out=ss[b][:, 6:7], in_=ss[b][:, 5:6]), deps=[t_sd], sig=True)
                    t_o = S.add('dve', lambda e: e.scalar_tensor_tensor(out=h2[b][:, :], in0=h2[b][:, :], scalar=ss[b][:, 6:7], in1=gfin[:, :], op0=ALU.mult, op1=ALU.mult),
                                deps=[t_r, t_cst], sig=True)
                    t_st = S.add('sp', lambda e: e.dma_start(out=out[i * 128:(i + 1) * 128, :], in_=h2[b][:, :]), deps=[t_o], dma=f'st{b}')
                    done[i] = [t_st, t_o, t_mm[1], t_sd]

                ngrp = (NFC + 3) // 4
                s1a(0)
                s1b(0)
                for i in range(nqb):
                    s2(i, 0, ngrp // 2)
                    if i + 1 < nqb:
                        s1a(i + 1)
                    s2(i, ngrp // 2, ngrp)
                    if i + 1 < nqb:
                        s1b(i + 1)
                    s3(i)
                S.final_wait()
                S.run(nc, "D", top)
    global LAST_INPUTS
    LAST_INPUTS = list(_din.keys())
    return nc, dbg


def bcast(ap, h):
    n = ap.shape[-1]
    return ap.unsqueeze(1).broadcast_to([ap.shape[0], h, n])


BF = ml_dtypes.bfloat16
def rope_tab(pos):
    pos = pos.astype(np.float32)
    out = np.zeros((pos.shape[0], 192), np.float32)
    for (half, off) in ((32, 0), (16, 128)):
        inv = np.power(np.float32(10000.0), -np.arange(half, dtype=np.float32) / np.float32(half)).astype(np.float32)
        ang = (pos[:, None] * inv[None, :]).astype(np.float32)
        c = np.cos(ang).astype(np.float32); s = np.sin(ang).astype(np.float32)
        d = 2 * half
        out[:, off:off + d] = np.concatenate([c, c], 1)
        out[:, off + d:off + 2 * d] = np.concatenate([-s, s], 1)
    return out
def core_inputs(inp, core):
    f32 = np.float32
    b, par = core // 2, core % 2
    x = np.asarray(inp['x'][b], f32)
    xk = np.zeros((65 * 128, 1024), f32); xk[0:16] = inp['meta_tokens']; xk[128:] = x
    xq = np.ascontiguousarray(x.reshape(64, 128, 1024)[par::2].reshape(32 * 128, 1024))
    w_in = np.asarray(inp['w_in'][0], f32)
    c_q, c_kv, k_r, q_s, k_s, v_s, q_i, k_i, w_i = np.split(w_in, np.cumsum([256,128,32,512,512,512,512,64,8])[:-1], axis=1)
    def pc(g, n):
        return np.ascontiguousarray(np.asarray(g, f32).reshape(n, 128).T)
    w_uq = np.asarray(inp['w_uq'][0], f32).reshape(256, 8, 96)
    w_ukv = np.asarray(inp['w_ukv'][0], f32).reshape(128, 8, 128)
    posK = np.concatenate([np.arange(128), 16 + np.arange(64 * 128)])
    ropeK = rope_tab(posK).reshape(65, 128, 192)
    g_idx = 2 * np.arange(32) + par
    posQ = (16 + 128 * g_idx[:, None] + np.arange(128)[None, :]).reshape(-1)
    ropeQ = rope_tab(posQ).reshape(32, 128, 192)
    tri = (np.arange(128)[:, None] <= np.arange(128)[None, :]).astype(f32)
    ones = np.ones((128, 128), f32); zeros = np.zeros((128, 128), f32)
    mk_mult = np.stack([tri, zeros] if par == 0 else [ones, tri]).astype(BF)
    NEG = np.float32(-1e30)
    meta_add = np.zeros((128, 128), f32); meta_add[:, 16:] = NEG
    triA = np.where(tri.T > 0, 0, NEG).astype(f32)
    allneg = np.full((128, 128), NEG, f32)
    mk_add = np.stack([meta_add, triA, allneg] if par == 0 else [meta_add, zeros, triA]).astype(f32)
    vcol0 = np.zeros((128, 8), f32); vcol0[0:16] = 1
    return {
        'xk': xk, 'xq': xq,
        'wk_in': np.ascontiguousarray(np.concatenate([k_s, v_s, c_kv, k_r, k_i], 1)),
        'wq_in': np.ascontiguousarray(np.concatenate([c_q, q_s, q_i, w_i], 1)),
        'g_attn': pc(inp['attn_norm_g'][0], 8),
        'w_uq_n': np.ascontiguousarray(w_uq[:, :, :64].reshape(256, 512)),
        'w_uq_r': np.ascontiguousarray(w_uq[:, :, 64:].reshape(256, 256)),
        'g_q': pc(inp['mla_q_norm_g'][0], 2),
        'w_ukv_k': np.ascontiguousarray(w_ukv[:, :, :64].reshape(128, 512)),
        'w_ukv_v': np.ascontiguousarray(w_ukv[:, :, 64:].reshape(128, 512)),
        'g_kv': pc(inp['mla_kv_norm_g'][0], 1),
        'w_o': np.asarray(inp['w_o'][0], f32),
        'g_ffn': pc(inp['ffn_norm_g'][0], 8),
        'w_gate': np.asarray(inp['w_gate'][0], f32), 'w_up': np.asarray(inp['w_up'][0], f32),
        'w_down': np.asarray(inp['w_down'][0], f32),
        'g_fin': np.ascontiguousarray(np.broadcast_to(np.asarray(inp['final_norm_g'], f32)[None, :], (128, 1024))),
        'ropeK': ropeK, 'ropeQ': ropeQ,
        'ident_f': np.eye(128, dtype=f32), 'ident_b': np.eye(128, dtype=f32).astype(BF),
        'vcol0': vcol0.astype(BF), 'mk_mult': mk_mult, 'mk_add': mk_add,
    }


def kernel(**inputs):
    inp = {k: np.asarray(v) for k, v in inputs.items()}
    nc, _ = build()
    names = set(LAST_INPUTS)
    in_maps = []
    for core in range(8):
        ci = core_inputs(inp, core)
        in_maps.append({k: np.ascontiguousarray(v) for k, v in ci.items() if k in names})
    res = run_bass_kernel_spmd(nc, in_maps, core_ids=list(range(8)))
    out = np.zeros((4, 64, 128, 1024), np.float32)
    for core in range(8):
        b, par = core // 2, core % 2
        out[b, par::2] = np.asarray(res.results[core]["out"], np.float32).reshape(32, 128, 1024)
    return out.reshape(4, 8192, 1024)
```
